# Optimizing a Trainium2 kernel written in Bass

```python
import math
import jax
import jax.numpy as jnp
from jax import lax
import numpy as np

D_MODEL = 1024
BATCH = 4
SEQ = 8192
DEPTH = 4

CHUNK = 64
GDN_HEADS = 8
GDN_DK = 128
GDN_DV = 128
GDN_CONV = 4
GDN_QK = GDN_HEADS * GDN_DK
GDN_V = GDN_HEADS * GDN_DV
GDN_IN = 2 * GDN_QK + 2 * GDN_V + 2 * GDN_HEADS
GDN_DT_MIN = 1e-3
GDN_DT_MAX = 1e-1
S5_GROUP = 16
S5_GROUPS = D_MODEL // S5_GROUP
S5_STATE = 64
S5_DT_MIN = 1e-3
S5_DT_MAX = 1e-1
D_FF = 11 * D_MODEL // 4
N_EXPERTS = 8
TOP_K = 2
D_FF_EXPERT = D_FF // 2
N_EVEN = (DEPTH + 1) // 2
N_ODD = DEPTH // 2
ALPHA = (2 * DEPTH) ** 0.25
BETA_INIT = (8 * DEPTH) ** -0.25
LN_EPS = 1e-5
NORM_EPS = 1e-6

kernel_name = 'hybrid_gdn_s5_moe_deepnorm'


def layer_norm(x, g, b):
    xf = x.astype(jnp.float32)
    mu = jnp.mean(xf, -1, keepdims=True)
    var = jnp.mean(jnp.square(xf - mu), -1, keepdims=True)
    y = (xf - mu) * lax.rsqrt(var + LN_EPS) * g.astype(jnp.float32) + b.astype(jnp.float32)
    return y.astype(x.dtype)


def l2norm(t):
    return t * lax.rsqrt(jnp.sum(t * t, -1, keepdims=True) + NORM_EPS)


def causal_dwconv(x, w):
    k = w.shape[0]
    return lax.conv_general_dilated(x, w[:, None, :].astype(x.dtype), window_strides=(1,),
                                    padding=[(k - 1, 0)], dimension_numbers=('NWC', 'WIO', 'NWC'),
                                    feature_group_count=x.shape[-1])


def gated_delta_rule_chunked(q, k, v, g, beta):
    bsz, seq, nh, dk = q.shape
    dv = v.shape[-1]
    n = seq // CHUNK

    def to_chunks(t):
        t = t.reshape((bsz, n, CHUNK) + t.shape[2:])
        return jnp.moveaxis(t, 3, 1)

    q, k, v, g, beta = (to_chunks(t) for t in (q, k, v, g, beta))
    g = jnp.cumsum(g, axis=-1)
    idx = jnp.arange(CHUNK)
    causal = idx[:, None] >= idx[None, :]
    strict = idx[:, None] > idx[None, :]
    decay = jnp.exp(jnp.where(causal, g[..., :, None] - g[..., None, :], -jnp.inf))
    k_beta = k * beta[..., None]
    v_beta = v * beta[..., None]
    lmat = jnp.where(strict, jnp.einsum('bhncd,bhnsd->bhncs', k_beta, k) * decay, 0.0)
    eye = jnp.eye(CHUNK, dtype=jnp.float32)
    t_inv = lax.linalg.triangular_solve(eye + lmat, jnp.broadcast_to(eye, lmat.shape),
                                        left_side=True, lower=True, unit_diagonal=True)
    u = jnp.einsum('bhncs,bhnsd->bhncd', t_inv, v_beta)
    w = jnp.einsum('bhncs,bhnsd->bhncd', t_inv, k_beta * jnp.exp(g)[..., None])
    a_intra = jnp.einsum('bhncd,bhnsd->bhncs', q, k) * decay

    def step(state, inp):
        q_c, k_c, u_c, w_c, g_c, a_c = inp
        v_new = u_c - jnp.einsum('bhcd,bhde->bhce', w_c, state)
        o = (jnp.einsum('bhcd,bhde->bhce', q_c * jnp.exp(g_c)[..., None], state)
             + jnp.einsum('bhcs,bhse->bhce', a_c, v_new))
        g_last = g_c[..., -1]
        state = (state * jnp.exp(g_last)[..., None, None]
                 + jnp.einsum('bhcd,bhce->bhde', k_c * jnp.exp(g_last[..., None] - g_c)[..., None], v_new))
        return state, o

    xs = tuple(jnp.moveaxis(t, 2, 0) for t in (q, k, u, w, g, a_intra))
    s0 = jnp.zeros((bsz, nh, dk, dv), jnp.float32)
    _, o = lax.scan(step, s0, xs)
    o = jnp.moveaxis(jnp.moveaxis(o, 0, 2), 1, 3)
    return o.reshape(bsz, seq, nh, dv)


def gdn_mixer(x, w_in, conv_w, a_log, dt_bias, norm_g, w_out):
    bsz, seq, _ = x.shape
    f32 = jnp.float32
    proj = x @ w_in
    qkv, z, b, a = jnp.split(proj, [2 * GDN_QK + GDN_V, 2 * GDN_QK + 2 * GDN_V,
                                    2 * GDN_QK + 2 * GDN_V + GDN_HEADS], axis=-1)
    qkv = jax.nn.silu(causal_dwconv(qkv, conv_w)).astype(f32)
    q, k, v = jnp.split(qkv, [GDN_QK, 2 * GDN_QK], axis=-1)
    q = l2norm(q.reshape(bsz, seq, GDN_HEADS, GDN_DK)) * (GDN_DK ** -0.5)
    k = l2norm(k.reshape(bsz, seq, GDN_HEADS, GDN_DK))
    v = v.reshape(bsz, seq, GDN_HEADS, GDN_DV)
    beta = jax.nn.sigmoid(b.astype(f32))
    g = -jnp.exp(a_log.astype(f32)) * jax.nn.softplus(a.astype(f32) + dt_bias.astype(f32))
    o = gated_delta_rule_chunked(q, k, v, g, beta)
    o = o * lax.rsqrt(jnp.mean(o * o, -1, keepdims=True) + NORM_EPS) * norm_g.astype(f32)
    o = o * jax.nn.silu(z.reshape(bsz, seq, GDN_HEADS, GDN_DV).astype(f32))
    return o.reshape(bsz, seq, GDN_V).astype(x.dtype) @ w_out


def s5_mixer(x, w_in, lam_re, lam_im, log_dt, b_re, b_im, c_re, c_im, d_skip, w_glu):
    bsz, seq, _ = x.shape
    f32 = jnp.float32
    n = seq // CHUNK
    u = (x @ w_in).astype(f32)
    lam = lax.complex(lam_re.astype(f32), lam_im.astype(f32))
    dt = jnp.exp(log_dt.astype(f32))[:, None]
    a_bar = jnp.exp(lam * dt)
    b_bar = ((a_bar - 1.0) / lam)[..., None] * lax.complex(b_re.astype(f32), b_im.astype(f32))
    c = lax.complex(c_re.astype(f32), c_im.astype(f32))
    steps = jnp.arange(1, CHUNK + 1, dtype=f32)[:, None, None]
    a_pow = jnp.exp((lam * dt)[None] * steps)

    def binop(e1, e2):
        a1, b1 = e1
        a2, b2 = e2
        return a2 * a1, a2 * b1 + b2

    def step(h, u_c):
        bu = jnp.einsum('gpc,btgc->btgp', b_bar, u_c.astype(jnp.complex64))
        _, hs = lax.associative_scan(binop, (jnp.broadcast_to(a_bar, bu.shape), bu), axis=1)
        hs = hs + a_pow[None] * h[:, None]
        y = jnp.einsum('gcp,btgp->btgc', c, hs).real
        return hs[:, -1], y

    uc = jnp.moveaxis(u.reshape(bsz, n, CHUNK, S5_GROUPS, S5_GROUP), 1, 0)
    h0 = jnp.zeros((bsz, S5_GROUPS, S5_STATE), jnp.complex64)
    _, ys = lax.scan(step, h0, uc)
    y = jnp.moveaxis(ys, 0, 1).reshape(bsz, seq, D_MODEL) + d_skip.astype(f32) * u
    hid = jax.nn.gelu(y.astype(x.dtype), approximate=False)
    val, gate = jnp.split(hid @ w_glu, 2, axis=-1)
    return val * jax.nn.sigmoid(gate)


def swiglu(x, w1, w3, w2):
    return (jax.nn.silu(x @ w1) * (x @ w3)) @ w2


def moe_swiglu(x, w_router, b_router, w1, w3, w2):
    bsz, seq, d = x.shape
    xt = x.reshape(-1, d)
    logits = (xt @ w_router).astype(jnp.float32) + b_router.astype(jnp.float32)
    top_v, top_i = lax.top_k(logits, TOP_K)
    gates = jax.nn.softmax(top_v, axis=-1)
    dense_gate = jnp.sum(jax.nn.one_hot(top_i, N_EXPERTS, dtype=jnp.float32) * gates[..., None], axis=1)
    y = jnp.zeros_like(xt)
    for e in range(N_EXPERTS):
        y = y + dense_gate[:, e:e + 1].astype(x.dtype) * swiglu(xt, w1[e], w3[e], w2[e])
    return y.reshape(bsz, seq, d)


def setup_inputs(seed: int = 0) -> dict:
    key = jax.random.key(seed)
    ks = jax.random.split(key, 27)
    f32 = jnp.float32

    def nrm(i, shape, scale):
        return jax.random.normal(ks[i], shape, f32) * scale

    x = nrm(0, (BATCH, SEQ, D_MODEL), 1.0)
    gdn_w_in = nrm(1, (N_EVEN, D_MODEL, GDN_IN), D_MODEL ** -0.5)
    gdn_conv_w = nrm(2, (N_EVEN, GDN_CONV, 2 * GDN_QK + GDN_V), GDN_CONV ** -0.5)
    gdn_a_log = jnp.log(jax.random.uniform(ks[3], (N_EVEN, GDN_HEADS), f32, 1.0, 16.0))
    dt = jnp.exp(jax.random.uniform(ks[4], (N_EVEN, GDN_HEADS), f32, math.log(GDN_DT_MIN), math.log(GDN_DT_MAX)))
    gdn_dt_bias = dt + jnp.log(-jnp.expm1(-dt))
    gdn_norm_g = 1.0 + nrm(5, (N_EVEN, GDN_DV), 0.02)
    gdn_w_out = nrm(6, (N_EVEN, GDN_V, D_MODEL), GDN_V ** -0.5 * BETA_INIT)
    ffn_w1 = nrm(7, (N_EVEN, D_MODEL, D_FF), D_MODEL ** -0.5)
    ffn_w3 = nrm(8, (N_EVEN, D_MODEL, D_FF), D_MODEL ** -0.5)
    ffn_w2 = nrm(9, (N_EVEN, D_FF, D_MODEL), D_FF ** -0.5 * BETA_INIT)
    s5_w_in = nrm(10, (N_ODD, D_MODEL, D_MODEL), D_MODEL ** -0.5)
    s5_lam_re = -0.5 + nrm(11, (N_ODD, S5_GROUPS, S5_STATE), 0.01)
    s5_lam_im = math.pi * jnp.arange(S5_STATE, dtype=f32) + nrm(12, (N_ODD, S5_GROUPS, S5_STATE), 0.01)
    s5_log_dt = jax.random.uniform(ks[13], (N_ODD, S5_GROUPS), f32, math.log(S5_DT_MIN), math.log(S5_DT_MAX))
    s5_b_re = nrm(14, (N_ODD, S5_GROUPS, S5_STATE, S5_GROUP), (2 * S5_GROUP) ** -0.5)
    s5_b_im = nrm(15, (N_ODD, S5_GROUPS, S5_STATE, S5_GROUP), (2 * S5_GROUP) ** -0.5)
    s5_c_re = nrm(16, (N_ODD, S5_GROUPS, S5_GROUP, S5_STATE), S5_STATE ** -0.5)
    s5_c_im = nrm(17, (N_ODD, S5_GROUPS, S5_GROUP, S5_STATE), S5_STATE ** -0.5)
    s5_d = nrm(18, (N_ODD, D_MODEL), 1.0)
    s5_w_glu = nrm(19, (N_ODD, D_MODEL, 2 * D_MODEL), D_MODEL ** -0.5 * BETA_INIT)
    moe_w_router = nrm(20, (N_ODD, D_MODEL, N_EXPERTS), D_MODEL ** -0.5)
    moe_b_router = nrm(21, (N_ODD, N_EXPERTS), 0.01)
    moe_w1 = nrm(22, (N_ODD, N_EXPERTS, D_MODEL, D_FF_EXPERT), D_MODEL ** -0.5)
    moe_w3 = nrm(23, (N_ODD, N_EXPERTS, D_MODEL, D_FF_EXPERT), D_MODEL ** -0.5)
    moe_w2 = nrm(24, (N_ODD, N_EXPERTS, D_FF_EXPERT, D_MODEL), D_FF_EXPERT ** -0.5 * BETA_INIT)
    ln_g = 1.0 + nrm(25, (DEPTH, 2, D_MODEL), 0.02)
    ln_b = nrm(26, (DEPTH, 2, D_MODEL), 0.02)
    return {'x': x, 'gdn_w_in': gdn_w_in, 'gdn_conv_w': gdn_conv_w, 'gdn_a_log': gdn_a_log,
            'gdn_dt_bias': gdn_dt_bias, 'gdn_norm_g': gdn_norm_g, 'gdn_w_out': gdn_w_out,
            'ffn_w1': ffn_w1, 'ffn_w3': ffn_w3, 'ffn_w2': ffn_w2,
            's5_w_in': s5_w_in, 's5_lam_re': s5_lam_re, 's5_lam_im': s5_lam_im, 's5_log_dt': s5_log_dt,
            's5_b_re': s5_b_re, 's5_b_im': s5_b_im, 's5_c_re': s5_c_re, 's5_c_im': s5_c_im,
            's5_d': s5_d, 's5_w_glu': s5_w_glu,
            'moe_w_router': moe_w_router, 'moe_b_router': moe_b_router,
            'moe_w1': moe_w1, 'moe_w3': moe_w3, 'moe_w2': moe_w2,
            'ln_g': ln_g, 'ln_b': ln_b}


def reference(x, gdn_w_in, gdn_conv_w, gdn_a_log, gdn_dt_bias, gdn_norm_g, gdn_w_out,
              ffn_w1, ffn_w3, ffn_w2,
              s5_w_in, s5_lam_re, s5_lam_im, s5_log_dt, s5_b_re, s5_b_im, s5_c_re, s5_c_im,
              s5_d, s5_w_glu,
              moe_w_router, moe_b_router, moe_w1, moe_w3, moe_w2,
              ln_g, ln_b):
    for i in range(DEPTH):
        j = i // 2
        if i % 2 == 0:
            h = gdn_mixer(x, gdn_w_in[j], gdn_conv_w[j], gdn_a_log[j], gdn_dt_bias[j],
                          gdn_norm_g[j], gdn_w_out[j])
        else:
            h = s5_mixer(x, s5_w_in[j], s5_lam_re[j], s5_lam_im[j], s5_log_dt[j], s5_b_re[j],
                         s5_b_im[j], s5_c_re[j], s5_c_im[j], s5_d[j], s5_w_glu[j])
        x = layer_norm(ALPHA * x + h, ln_g[i, 0], ln_b[i, 0])
        if i % 2 == 0:
            f = swiglu(x, ffn_w1[j], ffn_w3[j], ffn_w2[j])
        else:
            f = moe_swiglu(x, moe_w_router[j], moe_b_router[j], moe_w1[j], moe_w3[j], moe_w2[j])
        x = layer_norm(ALPHA * x + f, ln_g[i, 1], ln_b[i, 1])
    return x
```

```python
import contextlib
import numpy as np
import concourse.bass as bass
import concourse.mybir as mybir
from concourse.bass_utils import run_bass_kernel_spmd

F32 = mybir.dt.float32
BF16 = mybir.dt.bfloat16
AF = mybir.ActivationFunctionType
ALU = mybir.AluOpType
AX = mybir.AxisListType

D = 1024
DEPTH = 4
ALPHA = (2 * DEPTH) ** 0.25
LN_EPS = 1e-5
NORM_EPS = 1e-6
NEXP = 8
FE = 1408
NFT = FE // 128


class Sched:
    ENGS = ("pe", "dve", "act", "pool", "sp")

    def __init__(self, nc, es):
        self.nc = nc
        self.es = es
        self.q = {e: [] for e in self.ENGS}
        self.sem = {}
        self.cnt = {}
        self.known = {e: {} for e in self.ENGS}
        self.last_w = {}
        self.readers = {}
        self.phase = 0
        self.ename = {}
        for e in ("pe", "dve", "act", "pool"):
            self.ename[e] = e + "0"
            self._mksem(e + "0")
        self.n_ops = 0

    def _mksem(self, name):
        self.sem[name] = self.es.enter_context(self.nc.semaphore("s_" + name))
        self.cnt[name] = 0

    def _deps(self, reads, writes):
        deps = {}

        def add(tok):
            if tok is None:
                return
            s, v = tok
            if deps.get(s, 0) < v:
                deps[s] = v

        for k in reads:
            add(self.last_w.get(k))
        for k in writes:
            add(self.last_w.get(k))
            for s, v in self.readers.get(k, {}).items():
                add((s, v))
        return deps

    def _commit(self, tok, reads, writes):
        for k in writes:
            self.last_w[k] = tok
            self.readers[k] = {}
        for k in reads:
            if k in writes:
                continue
            r = self.readers.setdefault(k, {})
            if r.get(tok[0], 0) < tok[1]:
                r[tok[0]] = tok[1]

    def _waits(self, eng, deps):
        waits = []
        kn = self.known[eng]
        for s, v in deps.items():
            if eng == "pe" and s == self.ename["pe"]:
                continue
            if kn.get(s, 0) < v:
                kn[s] = v
                waits.append((s, v))
        return waits

    def op(self, eng, method, reads=(), writes=(), **kw):
        if eng != "pe":
            ex = [k for k in reads if ".bank" in k and k not in writes]
            if ex:
                writes = list(writes) + ex
        deps = self._deps(reads, writes)
        waits = self._waits(eng, deps)
        en = self.ename[eng]
        self.cnt[en] += 1
        tok = (en, self.cnt[en])
        self.q[eng].append((waits, (method, kw), (en, 1)))
        self._commit(tok, reads, writes)
        self.n_ops += 1

    def dma(self, queue, stream, out, in_, reads=(), writes=(), **kw):
        sname = "d_" + stream.split(".", 1)[-1]
        if sname not in self.sem:
            self._mksem(sname)
        deps = self._deps(reads, writes)
        waits = self._waits(queue, deps)
        self.cnt[sname] += 16
        tok = (sname, self.cnt[sname])
        kw = dict(kw)
        kw["out"] = out
        kw["in_"] = in_
        self.q[queue].append((waits, ("dma_start", kw), (sname, 16)))
        self._commit(tok, reads, writes)
        self.n_ops += 1

    def coll(self, kind, stream, ins, outs, replica_groups, reads=(), writes=()):
        sname = "d_" + stream
        if sname not in self.sem:
            self._mksem(sname)
        deps = self._deps(reads, writes)
        waits = self._waits("pool", deps)
        self.cnt[sname] += 16
        tok = (sname, self.cnt[sname])
        kw = dict(kind=kind, op=ALU.bypass, replica_groups=replica_groups, ins=list(ins), outs=list(outs))
        self.q["pool"].append((waits, ("collective_compute", kw), (sname, 16)))
        self._commit(tok, reads, writes)
        self.n_ops += 1

    def barrier(self):
        for e in self.ENGS:
            waits = []
            for s_, v in self.cnt.items():
                if v > 0 and self.known[e].get(s_, 0) < v:
                    self.known[e][s_] = v
                    waits.append((s_, v))
            if waits:
                self.q[e].append((waits, None, None))

    def finish(self):
        self.barrier()

    def new_phase(self):
        self.phase += 1
        self.last_w = {}
        self.readers = {}
        for e in ("pe", "dve", "act"):
            old_name = self.ename[e]
            for kn in self.known.values():
                kn.pop(old_name, None)
            del self.cnt[old_name]
            nm = f"{e}{self.phase}"
            self.ename[e] = nm
            self._mksem(nm)

    def emit(self):
        nc = self.nc
        S = self

        def replay(name, eng):
            for waits, fn, inc in S.q[name]:
                for s, v in waits:
                    eng.wait_ge(S.sem[s], v)
                if fn is None:
                    continue
                inst = getattr(eng, fn[0])(**fn[1])
                inst.then_inc(S.sem[inc[0]], inc[1])
            S.q[name] = []

        with nc.Block() as block:
            @block.tensor
            def _(e):
                replay("pe", e)

            @block.vector
            def _(e):
                replay("dve", e)

            @block.scalar
            def _(e):
                replay("act", e)

            @block.gpsimd
            def _(e):
                replay("pool", e)

            @block.sync
            def _(e):
                replay("sp", e)


class Ctx:
    _uid = [0]

    def __init__(self, nc, es, S=None):
        self.nc = nc
        self.es = es
        self.S = S if S is not None else Sched(nc, es)
        Ctx._uid[0] += 1
        self.n = Ctx._uid[0] * 1000

    def sb(self, shape, dt=F32, name=None):
        self.n += 1
        return self.es.enter_context(self.nc.sbuf_tensor(f"{name or 'sb'}_{self.n}", list(shape), dt))

    def ps(self, shape, dt=F32, name=None):
        self.n += 1
        return self.es.enter_context(self.nc.psum_tensor(f"{name or 'ps'}_{self.n}", list(shape), dt))


def tp_phase(C, T, glu, moe, featT, wproj, xres, lnp, w1, w3, w2, wr, br, ident_d, xout, xoutT, tb=1024, pfx="tp"):
    S = C.S
    nproj = 2 * D if glu else D
    ne = NEXP if moe else 2
    tb = min(tb, T)
    nblk = T // tb
    ntt = tb // 128
    hw = min(512, tb)
    nhalf = tb // hw
    P = pfx

    def K(name):
        return P + "." + name

    ident = C.sb([128, 128], F32, "ident")
    lnb = C.sb([128, 4, D], F32, "lnb")
    wproj_sb = C.sb([128, 8, nproj], BF16, "wproj")
    wr_sb = C.sb([128, 8, NEXP], F32, "wr")
    br_sb = C.sb([128, NEXP], F32, "br")
    w1_sb = C.sb([128, 8, FE], BF16, "w1")
    w3_sb = C.sb([128, 8, FE], BF16, "w3")
    w2_sb = C.sb([128, NFT, D], BF16, "w2")
    featT_sb = [C.sb([128, 8, 128], BF16, "featT") for _ in range(2)]
    xres_sb = [C.sb([128, D], F32, "xres") for _ in range(2)]
    hglu_sb = C.sb([128, 512], F32, "hglu")
    stats = [C.sb([128, 2, 6], F32, "stats") for _ in range(2)]
    mv = [C.sb([128, 2], F32, "mv") for _ in range(2)]
    rstd = [C.sb([128, 1], F32, "rstd") for _ in range(2)]
    x1 = [C.sb([128, D], F32, "x1") for _ in range(2)]
    x1T32 = [C.sb([128, 8, 128], F32, "x1T32") for _ in range(2)]
    x1T = C.sb([128, 8, tb], BF16, "x1T")
    yacc = C.sb([128, ntt, D], F32, "yacc")
    gates = C.sb([128, ntt, NEXP], F32, "gates")
    rt = [C.sb([128, NEXP], F32, "rt") for _ in range(4)]
    rcol = [C.sb([128, 1], F32, "rcol") for _ in range(4)]
    hT = C.sb([128, NFT, hw], BF16, "hT")
    sg = [C.sb([128, hw], BF16, "sg") for _ in range(2)]
    epsc = C.sb([128, 1], F32, "epsc")
    pg = [C.ps([128, 512], F32, "pg") for _ in range(2)]
    pu = [C.ps([128, 512], F32, "pu") for _ in range(2)]
    py = [C.ps([128, 512], F32, "py") for _ in range(2)]
    ptr = C.ps([128, 512], F32, "ptr")
    prt = C.ps([128, 512], F32, "prt")

    S.op("dve", "memset", ap=epsc[:], constant=LN_EPS, writes=[K("epsc")])
    S.dma("sp", K("c0"), ident[:], ident_d, writes=[K("ident")])
    for i in range(4):
        S.dma("sp", K("c1"), lnb[:, i, :], lnp[i:i + 1, :].partition_broadcast(128), writes=[K("lnb")])
    S.dma("pool", K("c2"), wproj_sb[:], wproj.rearrange("(j p) n -> p j n", p=128), writes=[K("wproj")])
    S.dma("sp", K("c3"), wr_sb[:], wr.rearrange("(j p) n -> p j n", p=128), writes=[K("wr")])
    S.dma("sp", K("c4"), br_sb[:], br.partition_broadcast(128), writes=[K("br")])

    def layer_norm(src, src_key, dst, dst_key, gi, idx):
        st, m, rs = stats[idx], mv[idx], rstd[idx]
        kst, kmv, krs = K(f"st{idx}"), K(f"mv{idx}"), K(f"rstd{idx}")
        for c in range(2):
            S.op("dve", "bn_stats", out=st[:, c, :], in_=src[:, c * 512:(c + 1) * 512],
                 reads=[src_key], writes=[kst + str(c)])
        S.op("dve", "bn_aggr", out=m[:], in_=st[:], reads=[kst + "0", kst + "1"], writes=[kmv])
        S.op("act", "activation", out=rs[:], in_=m[:, 1:2], func=AF.Sqrt, bias=epsc[:, 0:1], scale=1.0,
             reads=[kmv, K("epsc")], writes=[krs])
        S.op("dve", "reciprocal", out=rs[:], in_=rs[:], reads=[krs], writes=[krs])
        S.op("dve", "tensor_scalar", out=dst, in0=src, scalar1=m[:, 0:1], scalar2=rs[:, 0:1],
             op0=ALU.subtract, op1=ALU.mult, reads=[src_key, kmv, krs], writes=[dst_key])
        S.op("pool", "tensor_tensor", out=dst, in0=dst, in1=lnb[:, gi, :], op=ALU.mult,
             reads=[dst_key, K("lnb")], writes=[dst_key])
        S.op("pool", "tensor_tensor", out=dst, in0=dst, in1=lnb[:, gi + 1, :], op=ALU.add,
             reads=[dst_key, K("lnb")], writes=[dst_key])

    cnt = 0
    for blk in range(nblk):
        t0 = blk * tb
        for tt in range(ntt):
            i2 = tt % 2
            tok0 = t0 + tt * 128
            fT, xr, r = featT_sb[i2], xres_sb[i2], xres_sb[i2]
            kfT, kxr, kr, kx1 = K(f"fT{i2}"), K(f"xr{i2}"), K(f"xr{i2}"), K(f"x1_{i2}")
            S.dma("pool", kfT, fT[:], featT[:, tok0:tok0 + 128].rearrange("(j p) t -> p j t", p=128), writes=[kfT])
            S.dma("sp", kxr, xr[:], xres[tok0:tok0 + 128, :], writes=[kxr])
            for nh in range(2):
                c0, c1 = nh * 512, (nh + 1) * 512
                pv, kpv = py[nh], K(f"py{nh}")
                for k in range(8):
                    S.op("pe", "matmul", out=pv[:], lhsT=fT[:, k, :], rhs=wproj_sb[:, k, c0:c1],
                         start=(k == 0), stop=(k == 7), reads=[kfT, K("wproj")], writes=[kpv])
                if glu:
                    pgt, kpg = pg[nh], K(f"pg{nh}")
                    for k in range(8):
                        S.op("pe", "matmul", out=pgt[:], lhsT=fT[:, k, :], rhs=wproj_sb[:, k, D + c0:D + c1],
                             start=(k == 0), stop=(k == 7), reads=[kfT, K("wproj")], writes=[kpg])
                    S.op("act", "activation", out=hglu_sb[:], in_=pgt[:], func=AF.Sigmoid,
                         reads=[kpg], writes=[K("hglu")])
                    S.op("dve", "tensor_tensor", out=hglu_sb[:], in0=hglu_sb[:], in1=pv[:], op=ALU.mult,
                         reads=[K("hglu"), kpv], writes=[K("hglu")])
                    S.op("dve", "scalar_tensor_tensor", out=r[:, c0:c1], in0=xr[:, c0:c1], scalar=ALPHA,
                         in1=hglu_sb[:], op0=ALU.mult, op1=ALU.add, reads=[kxr, K("hglu")], writes=[kr])
                else:
                    S.op("dve", "scalar_tensor_tensor", out=r[:, c0:c1], in0=xr[:, c0:c1], scalar=ALPHA,
                         in1=pv[:], op0=ALU.mult, op1=ALU.add, reads=[kxr, kpv], writes=[kr])
            xx = x1[i2]
            layer_norm(r[:], kr, xx[:], kx1, 0, i2)
            S.op("act", "mul", out=yacc[:, tt, :], in_=xx[:], mul=ALPHA, reads=[kx1], writes=[K(f"yacc{tt}")])
            xt32 = x1T32[i2]
            for kh in range(2):
                pb, kpb = (ptr, K("ptr")) if kh == 0 else (prt, K("prt"))
                for q4 in range(4):
                    k = kh * 4 + q4
                    S.op("pe", "transpose", out=pb[:, q4 * 128:(q4 + 1) * 128], in_=xx[:, k * 128:(k + 1) * 128],
                         identity=ident[:], reads=[kx1, K("ident")], writes=[kpb])
                S.op("act", "copy", out=xt32[:, kh * 4:(kh + 1) * 4, :].rearrange("p k t -> p (k t)"), in_=pb[:],
                     reads=[kpb], writes=[K(f"x1T32_{i2}")])
                S.op("pool", "tensor_copy", out=x1T[:, kh * 4:(kh + 1) * 4, tt * 128:(tt + 1) * 128],
                     in_=xt32[:, kh * 4:(kh + 1) * 4, :], reads=[K(f"x1T32_{i2}")], writes=[K(f"x1T_{tt}")])
            if moe:
                for k in range(8):
                    S.op("pe", "matmul", out=prt[:, 0:NEXP], lhsT=xt32[:, k, :], rhs=wr_sb[:, k, :],
                         start=(k == 0), stop=(k == 7), reads=[K(f"x1T32_{i2}"), K("wr")], writes=[K("prt")])
                lg, mk, l2, ex = rt
                m1, m2, sm, rsm = rcol
                kk = lambda n: K("rt_" + n)
                S.op("dve", "tensor_tensor", out=lg[:], in0=prt[:, 0:NEXP], in1=br_sb[:], op=ALU.add,
                     reads=[K("prt"), K("br")], writes=[kk("lg")])
                S.op("dve", "reduce_max", out=m1[:], in_=lg[:], axis=AX.X, reads=[kk("lg")], writes=[kk("m1")])
                S.op("dve", "tensor_scalar", out=mk[:], in0=lg[:], scalar1=m1[:, 0:1], scalar2=-1e30,
                     op0=ALU.is_ge, op1=ALU.mult, reads=[kk("lg"), kk("m1")], writes=[kk("mk")])
                S.op("dve", "tensor_tensor", out=l2[:], in0=lg[:], in1=mk[:], op=ALU.add,
                     reads=[kk("lg"), kk("mk")], writes=[kk("l2")])
                S.op("dve", "reduce_max", out=m2[:], in_=l2[:], axis=AX.X, reads=[kk("l2")], writes=[kk("m2")])
                S.op("dve", "tensor_scalar", out=ex[:], in0=lg[:], scalar1=m1[:, 0:1], scalar2=None,
                     op0=ALU.subtract, reads=[kk("lg"), kk("m1")], writes=[kk("ex")])
                S.op("act", "activation", out=ex[:], in_=ex[:], func=AF.Exp, reads=[kk("ex")], writes=[kk("ex")])
                S.op("dve", "scalar_tensor_tensor", out=ex[:], in0=lg[:], scalar=m2[:, 0:1], in1=ex[:],
                     op0=ALU.is_ge, op1=ALU.mult, reads=[kk("lg"), kk("m2"), kk("ex")], writes=[kk("ex")])
                S.op("dve", "reduce_sum", out=sm[:], in_=ex[:], axis=AX.X, reads=[kk("ex")], writes=[kk("sm")])
                S.op("dve", "reciprocal", out=rsm[:], in_=sm[:], reads=[kk("sm")], writes=[kk("rsm")])
                S.op("dve", "tensor_scalar", out=gates[:, tt, :], in0=ex[:], scalar1=rsm[:, 0:1], scalar2=None,
                     op0=ALU.mult, reads=[kk("ex"), kk("rsm")], writes=[K(f"gates{tt}")])
        for ex_i in range(ne):
            S.dma("pool", K("w1"), w1_sb[:], w1[ex_i].rearrange("(j p) n -> p j n", p=128), writes=[K("w1")])
            S.dma("pool", K("w3"), w3_sb[:], w3[ex_i].rearrange("(j p) n -> p j n", p=128), writes=[K("w3")])
            S.dma("pool", K("w2"), w2_sb[:], w2[ex_i].rearrange("(j p) n -> p j n", p=128), writes=[K("w2")])
            for hf in range(nhalf):
                ts0, ts1 = hf * hw, (hf + 1) * hw
                tkeys = [K(f"x1T_{tt}") for tt in range(ts0 // 128, ts1 // 128)]
                for f in range(NFT):
                    b = cnt % 2
                    cnt += 1
                    f0, f1 = f * 128, (f + 1) * 128
                    for k in range(8):
                        S.op("pe", "matmul", out=pg[b][:, 0:hw], lhsT=w1_sb[:, k, f0:f1], rhs=x1T[:, k, ts0:ts1],
                             start=(k == 0), stop=(k == 7), reads=[K("w1")] + tkeys, writes=[K(f"pg{b}")])
                    for k in range(8):
                        S.op("pe", "matmul", out=pu[b][:, 0:hw], lhsT=w3_sb[:, k, f0:f1], rhs=x1T[:, k, ts0:ts1],
                             start=(k == 0), stop=(k == 7), reads=[K("w3")] + tkeys, writes=[K(f"pu{b}")])
                    S.op("act", "activation", out=sg[b][:], in_=pg[b][:, 0:hw], func=AF.Silu,
                         reads=[K(f"pg{b}")], writes=[K(f"sg{b}")])
                    S.op("dve", "tensor_tensor", out=hT[:, f, :], in0=sg[b][:], in1=pu[b][:, 0:hw], op=ALU.mult,
                         reads=[K(f"sg{b}"), K(f"pu{b}")], writes=[K(f"hT{f}")])
                hkeys = [K(f"hT{f}") for f in range(NFT)]
                for tl in range(hw // 128):
                    tt = hf * (hw // 128) + tl
                    kya = K(f"yacc{tt}")
                    for nh in range(2):
                        c0, c1 = nh * 512, (nh + 1) * 512
                        kpy = K(f"py{nh}")
                        for f in range(NFT):
                            S.op("pe", "matmul", out=py[nh][:], lhsT=hT[:, f, tl * 128:(tl + 1) * 128],
                                 rhs=w2_sb[:, f, c0:c1], start=(f == 0), stop=(f == NFT - 1),
                                 reads=hkeys + [K("w2")], writes=[kpy])
                        if moe:
                            S.op("dve", "scalar_tensor_tensor", out=yacc[:, tt, c0:c1], in0=py[nh][:],
                                 scalar=gates[:, tt, ex_i:ex_i + 1], in1=yacc[:, tt, c0:c1], op0=ALU.mult,
                                 op1=ALU.add, reads=[kpy, K(f"gates{tt}"), kya], writes=[kya])
                        else:
                            S.op("dve", "tensor_tensor", out=yacc[:, tt, c0:c1], in0=py[nh][:],
                                 in1=yacc[:, tt, c0:c1], op=ALU.add, reads=[kpy, kya], writes=[kya])
        for tt in range(ntt):
            i2 = tt % 2
            tok0 = t0 + tt * 128
            xx, kxo = x1[i2], K(f"x1_{i2}")
            layer_norm(yacc[:, tt, :], K(f"yacc{tt}"), xx[:], kxo, 2, i2)
            S.dma("sp", kxo, xout[tok0:tok0 + 128, :], xx[:], reads=[kxo])
            xt, kxt = x1T32[i2], K(f"x1T32_{i2}")
            for kh in range(2):
                pb, kpb = (ptr, K("ptr")) if kh == 0 else (prt, K("prt"))
                for q4 in range(4):
                    k = kh * 4 + q4
                    S.op("pe", "transpose", out=pb[:, q4 * 128:(q4 + 1) * 128], in_=xx[:, k * 128:(k + 1) * 128],
                         identity=ident[:], reads=[kxo, K("ident")], writes=[kpb])
                S.op("act", "copy", out=xt[:, kh * 4:(kh + 1) * 4, :].rearrange("p k t -> p (k t)"), in_=pb[:],
                     reads=[kpb], writes=[kxt])
            S.dma("sp", kxt, xoutT[:, tok0:tok0 + 128].rearrange("(j p) t -> p j t", p=128), xt[:], reads=[kxt])


def build_tp(T, glu, moe, tb=1024):
    nc = bass.Bass("TRN2", target_bir_lowering=False)
    nproj = 2 * D if glu else D
    ne = NEXP if moe else 2
    dt = lambda n, s, k="ExternalInput": nc.dram_tensor(n, s, F32, kind=k).ap()
    featT = dt("featT", [D, T]); wproj = dt("wproj", [D, nproj]); xres = dt("xres", [T, D])
    lnp = dt("lnp", [4, D]); w1 = dt("w1", [ne, D, FE]); w3 = dt("w3", [ne, D, FE]); w2 = dt("w2", [ne, FE, D])
    wr = dt("wr", [D, NEXP]); br = dt("br", [1, NEXP]); ident_d = dt("ident", [128, 128])
    xout = dt("xout", [T, D], "ExternalOutput"); xoutT = dt("xoutT", [D, T], "ExternalOutput")
    with contextlib.ExitStack() as es:
        C = Ctx(nc, es)
        tp_phase(C, T, glu, moe, featT, wproj, xres, lnp, w1, w3, w2, wr, br, ident_d, xout, xoutT, tb=tb)
        C.S.finish()
        C.S.emit()
    return nc


S5_L = 128
PI = float(np.pi)


def s5_phase(C, T, xT, w_in, lamr_d, lami_d, ldt_d, bpad_re_d, bpad_im_d, cpad_re_d, cpad_im_d, dsk_d, iota_d,
             hidT, pfx="s5"):
    S = C.S
    L = S5_L
    NB = T // 512
    NGP = 16
    P = pfx

    def K(n):
        return P + "." + n

    w_sb = C.sb([128, 8, 512], BF16, "s5w")
    xT_sb = [C.sb([128, 8, 512], BF16, "s5x") for _ in range(2)]
    u_sb = [C.sb([128, 4, 512], F32, "s5u") for _ in range(2)]
    bre = C.sb([128, NGP, 128], F32, "bre")
    bim = C.sb([128, NGP, 128], F32, "bim")
    cre = C.sb([128, NGP, 128], F32, "cre")
    cim = C.sb([128, NGP, 128], F32, "cim")
    dsk = C.sb([128, 4], F32, "dsk")
    iota = C.sb([128, L], F32, "iota")
    prm = {n: C.sb([128, NGP], F32, "p_" + n) for n in
           ("lamr", "lami", "ldt", "dt", "zr", "zi", "r", "cz", "sz", "ar", "ai", "den", "cr", "ci", "ncr",
            "t1", "t2", "zl", "er", "ei", "nei")}
    negpi = C.sb([128, 1], F32, "negpi")
    tre = C.sb([128, NGP, L], F32, "tre")
    tim = C.sb([128, NGP, L], F32, "tim")
    cph = C.sb([128, NGP, L], F32, "cph")
    sph = C.sb([128, NGP, L], F32, "sph")
    rmat = C.sb([128, NGP, L], F32, "rmat")
    ang = C.sb([128, L], F32, "ang")
    ang2 = C.sb([128, L], F32, "ang2")
    hp = C.sb([128, NGP, 2], F32, "hp")
    tmpc = C.sb([128, 2], F32, "tmpc")
    BUr = [C.sb([128, 512], F32, "BUr") for _ in range(2)]
    BUi = [C.sb([128, 512], F32, "BUi") for _ in range(2)]
    mt = [[C.sb([128, 512], F32, "mt") for _ in range(4)] for _ in range(2)]
    btr = [C.sb([128, 512], F32, "btr") for _ in range(2)]
    bti = [C.sb([128, 512], F32, "bti") for _ in range(2)]
    wre = [C.sb([128, 512], F32, "wre") for _ in range(2)]
    wim = [C.sb([128, 512], F32, "wim") for _ in range(2)]
    hre = [[C.sb([128, 512], F32, "hre") for _ in range(4)] for _ in range(2)]
    him = [[C.sb([128, 512], F32, "him") for _ in range(4)] for _ in range(2)]
    yt = [C.sb([128, 512], F32, "yt") for _ in range(2)]
    ho = [C.sb([128, 512], F32, "ho") for _ in range(2)]
    pu_ = [C.ps([128, 512], F32, "s5pu") for _ in range(2)]
    pbr = [C.ps([128, 512], F32, "s5pbr") for _ in range(2)]
    pbi = [C.ps([128, 512], F32, "s5pbi") for _ in range(2)]
    pyy = [C.ps([128, 512], F32, "s5py") for _ in range(2)]

    S.dma("pool", K("w"), w_sb[:], w_in.rearrange("(j p) n -> p j n", p=128), writes=[K("w")])
    for nm, dd, sbt in (("bre", bpad_re_d, bre), ("bim", bpad_im_d, bim), ("cre", cpad_re_d, cre), ("cim", cpad_im_d, cim)):
        S.dma("sp", K(nm), sbt[:], dd.rearrange("g k m -> k g m"), writes=[K(nm)])
    S.dma("sp", K("dsk"), dsk[:], dsk_d, writes=[K("dsk")])
    S.dma("sp", K("iota"), iota[:], iota_d, writes=[K("iota")])
    S.dma("sp", K("lamr"), prm["lamr"][:], lamr_d, writes=[K("lamr")])
    S.dma("sp", K("lami"), prm["lami"][:], lami_d, writes=[K("lami")])
    S.dma("sp", K("ldt"), prm["ldt"][:], ldt_d, writes=[K("ldt")])
    S.op("dve", "memset", ap=negpi[:], constant=-PI, writes=[K("negpi")])
    itmp = C.sb([128, L], mybir.dt.int32, "itmp")
    ftmp = C.sb([128, L], F32, "ftmp")
    S.op("dve", "memset", ap=hp[:], constant=0.0, writes=[K(f"hp{g}") for g in range(NGP)])

    def pp(n):
        return prm[n][:]

    def ew(eng, method, out_n, reads, **kw):
        S.op(eng, method, reads=[K(x) for x in reads], writes=[K(out_n)], **kw)

    def sincos(angle_ap, angle_key, sin_ap, sin_key, cos_ap, cos_key, tmp_ap, tmp_key, width):
        for off, o_ap, o_key in ((0.5, sin_ap, sin_key), (0.75, cos_ap, cos_key)):
            S.op("dve", "tensor_scalar", out=tmp_ap, in0=angle_ap, scalar1=1.0 / (2.0 * PI), scalar2=off,
                 op0=ALU.mult, op1=ALU.add, reads=[angle_key], writes=[tmp_key])
            S.op("dve", "tensor_copy", out=itmp[:, 0:width], in_=tmp_ap, reads=[tmp_key], writes=[K("itmp")])
            S.op("dve", "tensor_copy", out=ftmp[:, 0:width], in_=itmp[:, 0:width], reads=[K("itmp")], writes=[K("ftmp")])
            S.op("dve", "tensor_tensor", out=tmp_ap, in0=tmp_ap, in1=ftmp[:, 0:width], op=ALU.subtract,
                 reads=[tmp_key, K("ftmp")], writes=[tmp_key])
            S.op("dve", "scalar_tensor_tensor", out=tmp_ap, in0=tmp_ap, scalar=0.0, in1=tmp_ap, op0=ALU.is_lt,
                 op1=ALU.add, reads=[tmp_key], writes=[tmp_key])
            S.op("act", "activation", out=o_ap, in_=tmp_ap, func=AF.Sin, bias=negpi[:, 0:1], scale=2.0 * PI,
                 reads=[tmp_key, K("negpi")], writes=[o_key])

    ew("act", "activation", "dt", ["ldt"], out=pp("dt"), in_=pp("ldt"), func=AF.Exp)
    ew("dve", "tensor_tensor", "zr", ["lamr", "dt"], out=pp("zr"), in0=pp("lamr"), in1=pp("dt"), op=ALU.mult)
    ew("dve", "tensor_tensor", "zi", ["lami", "dt"], out=pp("zi"), in0=pp("lami"), in1=pp("dt"), op=ALU.mult)
    ew("act", "activation", "r", ["zr"], out=pp("r"), in_=pp("zr"), func=AF.Exp)
    sincos(pp("zi"), K("zi"), pp("sz"), K("sz"), pp("cz"), K("cz"), pp("t1"), K("t1"), NGP)
    ew("dve", "tensor_tensor", "ar", ["r", "cz"], out=pp("ar"), in0=pp("r"), in1=pp("cz"), op=ALU.mult)
    ew("dve", "tensor_scalar", "ar", ["ar"], out=pp("ar"), in0=pp("ar"), scalar1=-1.0, scalar2=None, op0=ALU.add)
    ew("dve", "tensor_tensor", "ai", ["r", "sz"], out=pp("ai"), in0=pp("r"), in1=pp("sz"), op=ALU.mult)
    ew("dve", "tensor_tensor", "den", ["lamr"], out=pp("den"), in0=pp("lamr"), in1=pp("lamr"), op=ALU.mult)
    ew("dve", "tensor_tensor", "t1", ["lami"], out=pp("t1"), in0=pp("lami"), in1=pp("lami"), op=ALU.mult)
    ew("dve", "tensor_tensor", "den", ["den", "t1"], out=pp("den"), in0=pp("den"), in1=pp("t1"), op=ALU.add)
    ew("dve", "reciprocal", "den", ["den"], out=pp("den"), in_=pp("den"))
    ew("dve", "tensor_tensor", "t1", ["ar", "lamr"], out=pp("t1"), in0=pp("ar"), in1=pp("lamr"), op=ALU.mult)
    ew("dve", "tensor_tensor", "t2", ["ai", "lami"], out=pp("t2"), in0=pp("ai"), in1=pp("lami"), op=ALU.mult)
    ew("dve", "tensor_tensor", "cr", ["t1", "t2"], out=pp("cr"), in0=pp("t1"), in1=pp("t2"), op=ALU.add)
    ew("dve", "tensor_tensor", "cr", ["cr", "den"], out=pp("cr"), in0=pp("cr"), in1=pp("den"), op=ALU.mult)
    ew("dve", "tensor_tensor", "t1", ["ai", "lamr"], out=pp("t1"), in0=pp("ai"), in1=pp("lamr"), op=ALU.mult)
    ew("dve", "tensor_tensor", "t2", ["ar", "lami"], out=pp("t2"), in0=pp("ar"), in1=pp("lami"), op=ALU.mult)
    ew("dve", "tensor_tensor", "ci", ["t1", "t2"], out=pp("ci"), in0=pp("t1"), in1=pp("t2"), op=ALU.subtract)
    ew("dve", "tensor_tensor", "ci", ["ci", "den"], out=pp("ci"), in0=pp("ci"), in1=pp("den"), op=ALU.mult)
    ew("dve", "tensor_scalar", "ncr", ["cr"], out=pp("ncr"), in0=pp("cr"), scalar1=-1.0, scalar2=None, op0=ALU.mult)
    ew("dve", "tensor_scalar", "zl", ["zi"], out=pp("zl"), in0=pp("zi"), scalar1=float(L), scalar2=None, op0=ALU.mult)
    sincos(pp("zl"), K("zl"), pp("ei"), K("ei"), pp("er"), K("er"), pp("t1"), K("t1"), NGP)
    ew("dve", "tensor_scalar", "nei", ["ei"], out=pp("nei"), in0=pp("ei"), scalar1=-1.0, scalar2=None, op0=ALU.mult)
    for g in range(NGP):
        S.op("dve", "tensor_scalar", out=ang[:], in0=iota[:], scalar1=prm["zi"][:, g:g + 1], scalar2=None,
             op0=ALU.mult, reads=[K("iota"), K("zi")], writes=[K("ang")])
        sincos(ang[:], K("ang"), sph[:, g, :], K("sph"), cph[:, g, :], K("cph"), ang2[:], K("ang2"), L)
        S.op("dve", "tensor_scalar", out=ang2[:], in0=cph[:, g, :], scalar1=prm["cr"][:, g:g + 1], scalar2=None,
             op0=ALU.mult, reads=[K("cph"), K("cr")], writes=[K("ang2")])
        S.op("dve", "scalar_tensor_tensor", out=tre[:, g, :], in0=sph[:, g, :], scalar=prm["ci"][:, g:g + 1],
             in1=ang2[:], op0=ALU.mult, op1=ALU.add, reads=[K("sph"), K("ci"), K("ang2")], writes=[K("tre")])
        S.op("dve", "tensor_scalar", out=ang2[:], in0=cph[:, g, :], scalar1=prm["ci"][:, g:g + 1], scalar2=None,
             op0=ALU.mult, reads=[K("cph"), K("ci")], writes=[K("ang2")])
        S.op("dve", "scalar_tensor_tensor", out=tim[:, g, :], in0=sph[:, g, :], scalar=prm["ncr"][:, g:g + 1],
             in1=ang2[:], op0=ALU.mult, op1=ALU.add, reads=[K("sph"), K("ncr"), K("ang2")], writes=[K("tim")])
        S.op("pool", "memset", ap=rmat[:, g, :], constant=1.0, writes=[K("rmat")])
        S.op("dve", "tensor_scalar", out=rmat[:, g, :], in0=rmat[:, g, :], scalar1=prm["r"][:, g:g + 1], scalar2=None,
             op0=ALU.mult, reads=[K("rmat"), K("r")], writes=[K("rmat")])

    NC4 = 512 // L

    def v3(t):
        return t[:].rearrange("p (c l) -> p c l", l=L)

    def tb3(tab, g):
        return tab[:, g:g + 1, :].broadcast_to([128, NC4, L])

    gi = 0
    for blk in range(NB):
        t0 = blk * 512
        bi = blk % 2
        xs, us = xT_sb[bi], u_sb[bi]
        kx, ku = K(f"x{bi}"), K(f"u{bi}")
        S.dma("pool", kx, xs[:], xT[:, t0:t0 + 512].rearrange("(j p) t -> p j t", p=128), writes=[kx])
        for ft in range(4):
            b2 = ft % 2
            for k in range(8):
                S.op("pe", "matmul", out=pu_[b2][:], lhsT=w_sb[:, k, ft * 128:(ft + 1) * 128], rhs=xs[:, k, :],
                     start=(k == 0), stop=(k == 7), reads=[K("w"), kx], writes=[K(f"pu{b2}")])
            S.op("act", "copy", out=us[:, ft, :], in_=pu_[b2][:], reads=[K(f"pu{b2}")], writes=[ku + f"_{ft}"])
        for ft in range(4):
            f2 = ft % 2
            for gl4 in range(4):
                g = ft * 4 + gl4
                b2 = gi % 2
                gi += 1
                kb = lambda n: K(f"{n}{b2}")
                S.op("pe", "matmul", out=pbr[b2][:], lhsT=bre[:, g, :], rhs=us[:, ft, :], start=True, stop=True,
                     reads=[K("bre"), ku + f"_{ft}"], writes=[kb("pbr")])
                S.op("pe", "matmul", out=pbi[b2][:], lhsT=bim[:, g, :], rhs=us[:, ft, :], start=True, stop=True,
                     reads=[K("bim"), ku + f"_{ft}"], writes=[kb("pbi")])
                S.op("act", "copy", out=BUr[b2][:], in_=pbr[b2][:], reads=[kb("pbr")], writes=[kb("BUr")])
                S.op("act", "copy", out=BUi[b2][:], in_=pbi[b2][:], reads=[kb("pbi")], writes=[kb("BUi")])
                m0, m1, m2, m3 = mt[b2]
                S.op("dve", "tensor_tensor", out=v3(m0), in0=v3(BUr[b2]), in1=tb3(tre, g), op=ALU.mult,
                     reads=[kb("BUr"), K("tre")], writes=[kb("m0")])
                S.op("pool", "tensor_tensor", out=v3(m1), in0=v3(BUi[b2]), in1=tb3(tim, g), op=ALU.mult,
                     reads=[kb("BUi"), K("tim")], writes=[kb("m1")])
                S.op("dve", "tensor_tensor", out=btr[b2][:], in0=m0[:], in1=m1[:], op=ALU.subtract,
                     reads=[kb("m0"), kb("m1")], writes=[kb("btr")])
                S.op("pool", "tensor_tensor", out=v3(m2), in0=v3(BUi[b2]), in1=tb3(tre, g), op=ALU.mult,
                     reads=[kb("BUi"), K("tre")], writes=[kb("m2")])
                S.op("dve", "tensor_tensor", out=v3(m3), in0=v3(BUr[b2]), in1=tb3(tim, g), op=ALU.mult,
                     reads=[kb("BUr"), K("tim")], writes=[kb("m3")])
                S.op("pool", "tensor_tensor", out=bti[b2][:], in0=m2[:], in1=m3[:], op=ALU.add,
                     reads=[kb("m2"), kb("m3")], writes=[kb("bti")])
                khp = K(f"hp{g}")
                for c in range(NC4):
                    cs = slice(c * L, (c + 1) * L)
                    S.op("dve", "tensor_tensor_scan", out=wre[b2][:, cs], data0=rmat[:, g, :], data1=btr[b2][:, cs],
                         initial=hp[:, g, 0:1], op0=ALU.mult, op1=ALU.add,
                         reads=[K("rmat"), kb("btr"), khp], writes=[kb("wre")])
                    S.op("dve", "tensor_tensor_scan", out=wim[b2][:, cs], data0=rmat[:, g, :], data1=bti[b2][:, cs],
                         initial=hp[:, g, 1:2], op0=ALU.mult, op1=ALU.add,
                         reads=[K("rmat"), kb("bti"), khp], writes=[kb("wim")])
                    last = (c + 1) * L - 1
                    S.op("dve", "tensor_scalar", out=tmpc[:, 0:1], in0=wre[b2][:, last:last + 1],
                         scalar1=prm["er"][:, g:g + 1], scalar2=None, op0=ALU.mult,
                         reads=[kb("wre"), K("er")], writes=[K("tmpc0")])
                    S.op("dve", "tensor_scalar", out=tmpc[:, 1:2], in0=wre[b2][:, last:last + 1],
                         scalar1=prm["ei"][:, g:g + 1], scalar2=None, op0=ALU.mult,
                         reads=[kb("wre"), K("ei")], writes=[K("tmpc1")])
                    S.op("dve", "scalar_tensor_tensor", out=hp[:, g, 0:1], in0=wim[b2][:, last:last + 1],
                         scalar=prm["nei"][:, g:g + 1], in1=tmpc[:, 0:1], op0=ALU.mult, op1=ALU.add,
                         reads=[kb("wim"), K("nei"), K("tmpc0")], writes=[khp])
                    S.op("dve", "scalar_tensor_tensor", out=hp[:, g, 1:2], in0=wim[b2][:, last:last + 1],
                         scalar=prm["er"][:, g:g + 1], in1=tmpc[:, 1:2], op0=ALU.mult, op1=ALU.add,
                         reads=[kb("wim"), K("er"), K("tmpc1")], writes=[khp])
                hr, hi_ = hre[f2][gl4], him[f2][gl4]
                khr, khi = K(f"hre{f2}{gl4}"), K(f"him{f2}{gl4}")
                S.op("pool", "tensor_tensor", out=v3(m0), in0=v3(wre[b2]), in1=tb3(cph, g), op=ALU.mult,
                     reads=[kb("wre"), K("cph")], writes=[kb("m0")])
                S.op("pool", "tensor_tensor", out=v3(m1), in0=v3(wim[b2]), in1=tb3(sph, g), op=ALU.mult,
                     reads=[kb("wim"), K("sph")], writes=[kb("m1")])
                S.op("pool", "tensor_tensor", out=hr[:], in0=m0[:], in1=m1[:], op=ALU.subtract,
                     reads=[kb("m0"), kb("m1")], writes=[khr])
                S.op("dve", "tensor_tensor", out=v3(m2), in0=v3(wre[b2]), in1=tb3(sph, g), op=ALU.mult,
                     reads=[kb("wre"), K("sph")], writes=[kb("m2")])
                S.op("pool", "tensor_tensor", out=v3(m3), in0=v3(wim[b2]), in1=tb3(cph, g), op=ALU.mult,
                     reads=[kb("wim"), K("cph")], writes=[kb("m3")])
                S.op("dve", "scalar_tensor_tensor", out=hi_[:], in0=m2[:], scalar=-1.0, in1=m3[:], op0=ALU.mult,
                     op1=ALU.subtract, reads=[kb("m2"), kb("m3")], writes=[khi])
            for gl4 in range(4):
                g = ft * 4 + gl4
                S.op("pe", "matmul", out=pyy[f2][:], lhsT=cre[:, g, :], rhs=hre[f2][gl4][:], start=(gl4 == 0), stop=False,
                     reads=[K("cre"), K(f"hre{f2}{gl4}")], writes=[K(f"pyy{f2}")])
                S.op("pe", "matmul", out=pyy[f2][:], lhsT=cim[:, g, :], rhs=him[f2][gl4][:], start=False, stop=(gl4 == 3),
                     reads=[K("cim"), K(f"him{f2}{gl4}")], writes=[K(f"pyy{f2}")])
            S.op("dve", "scalar_tensor_tensor", out=yt[f2][:], in0=us[:, ft, :], scalar=dsk[:, ft:ft + 1], in1=pyy[f2][:],
                 op0=ALU.mult, op1=ALU.add, reads=[ku + f"_{ft}", K("dsk"), K(f"pyy{f2}")], writes=[K(f"yt{f2}")])
            S.op("act", "activation", out=ho[f2][:], in_=yt[f2][:], func=AF.Gelu, reads=[K(f"yt{f2}")],
                 writes=[K(f"ho{f2}")])
            S.dma("sp", K(f"ho{f2}"), hidT[ft * 128:(ft + 1) * 128, t0:t0 + 512], ho[f2][:], reads=[K(f"ho{f2}")])


def build_s5(T):
    nc = bass.Bass("TRN2", target_bir_lowering=False)
    dt = lambda n, s, k="ExternalInput": nc.dram_tensor(n, s, F32, kind=k).ap()
    xT = dt("xT", [D, T]); w_in = dt("w_in", [D, 512])
    lamr = dt("lamr", [128, 16]); lami = dt("lami", [128, 16]); ldt = dt("ldt", [128, 16])
    bre = dt("bpad_re", [16, 128, 128]); bim = dt("bpad_im", [16, 128, 128])
    cre = dt("cpad_re", [16, 128, 128]); cim = dt("cpad_im", [16, 128, 128])
    dsk = dt("dsk", [128, 4]); iota = dt("iota", [128, S5_L])
    hidT = dt("hidT", [512, T], "ExternalOutput")
    with contextlib.ExitStack() as es:
        C = Ctx(nc, es)
        s5_phase(C, T, xT, w_in, lamr, lami, ldt, bre, bim, cre, cim, dsk, iota, hidT)
        C.S.finish()
        C.S.emit()
    return nc


def s5_host_layout(ghalf, s5_lam_re, s5_lam_im, s5_log_dt, s5_b_re, s5_b_im, s5_c_re, s5_c_im, s5_d, s5_w_in):
    g0 = 32 * ghalf
    lamr = np.zeros((128, 16), np.float32); lami = np.zeros((128, 16), np.float32); ldt = np.zeros((128, 16), np.float32)
    bre = np.zeros((16, 128, 128), np.float32); bim = np.zeros((16, 128, 128), np.float32)
    cre = np.zeros((16, 128, 128), np.float32); cim = np.zeros((16, 128, 128), np.float32)
    for gp in range(16):
        for gl in range(2):
            g = g0 + 2 * gp + gl
            lamr[gl * 64:(gl + 1) * 64, gp] = s5_lam_re[g]
            lami[gl * 64:(gl + 1) * 64, gp] = s5_lam_im[g]
            ldt[gl * 64:(gl + 1) * 64, gp] = s5_log_dt[g]
            r0 = 32 * (gp % 4) + 16 * gl
            bre[gp, r0:r0 + 16, gl * 64:(gl + 1) * 64] = s5_b_re[g].T
            bim[gp, r0:r0 + 16, gl * 64:(gl + 1) * 64] = s5_b_im[g].T
            cre[gp, gl * 64:(gl + 1) * 64, r0:r0 + 16] = s5_c_re[g].T
            cim[gp, gl * 64:(gl + 1) * 64, r0:r0 + 16] = s5_c_im[g].T
    dsk = np.ascontiguousarray(s5_d[512 * ghalf:512 * (ghalf + 1)].reshape(4, 128).T)
    iota = np.tile(np.arange(1, S5_L + 1, dtype=np.float32)[None, :], (128, 1))
    w = np.ascontiguousarray(s5_w_in[:, 512 * ghalf:512 * (ghalf + 1)])
    return dict(w_in=w, lamr=lamr, lami=lami, ldt=ldt, bpad_re=bre, bpad_im=bim, cpad_re=cre, cpad_im=cim,
                dsk=dsk, iota=iota)


GDN_LV = 99
GDN_CLAMP = False


def gdn_phase(C, T, xT, wq_d, wk_d, wv_d, wz_d, wba_d, convw_d, hp_d, normg_d, gconst_d, ogT, pfx="gd"):
    S = C.S
    NB = T // 512
    P = pfx
    NH = 4

    def K(n):
        return P + "." + n

    wq = C.sb([128, 8, 512], BF16, "wq"); wk = C.sb([128, 8, 512], BF16, "wk")
    wv = C.sb([128, 8, 512], BF16, "wv"); wz = C.sb([128, 8, 512], BF16, "wz")
    wba = C.sb([128, 8, 8], F32, "wba")
    convw = C.sb([128, 3, NH, 4], F32, "convw")
    hpar = C.sb([128, 8], F32, "hpar")
    nea = C.sb([128, NH], F32, "nea")
    normg = C.sb([128, 128], F32, "normg")
    cst = C.sb([128, 6, 128], F32, "gcst")
    ident, MU, MS, NEGL, NEGUT, ONES = (cst[:, i, :] for i in range(6))
    onec = C.sb([128, 1], F32, "onec"); eps6 = C.sb([128, 1], F32, "eps6")
    xs = [C.sb([128, 8, 512], BF16, "gxs") for _ in range(2)]
    xs32 = C.sb([128, 8, 512], F32, "gxs32")
    xb = [[C.sb([128, 515], F32, "xb") for _ in range(NH)] for _ in range(3)]
    qkvc = [[C.sb([128, 512], F32, "qkvc") for _ in range(NH)] for _ in range(3)]
    cacc = [C.sb([128, 512], F32, "cacc") for _ in range(2)]
    zs = C.sb([128, 512], F32, "zs")
    ba = C.sb([128, 8], F32, "ba")
    gt = {n: C.sb([128, NH], F32, "g_" + n) for n in
          ("beta", "nbeta", "x", "e", "sp", "g", "eg", "kes", "elast", "bks")}
    gc8 = C.sb([128, 8], F32, "gc8")
    Sst = [C.sb([128, 128], F32, "Sst") for _ in range(NH)]
    ogT_sb = C.sb([128, NH, 512], F32, "ogT")

    def mk(n):
        return [C.sb([128, 128], F32, n) for _ in range(2)]

    Kn, KnT, Kbg, Kend, Vb, sq, G1, Dl, DTu, AT, PT, nWT, Vnew, o1, o_, og = (mk(n) for n in (
        "Kn", "KnT", "Kbg", "Kend", "Vb", "sq", "G1", "Dl", "DTu", "AT", "PT", "nWT", "Vnew", "o1", "o", "og"))
    Nn = [mk("Na"), mk("Nb")]
    NTn = [mk("NTa"), mk("NTb")]
    junk = mk("junk")
    Qc = mk("Qc")
    col = {n: [C.sb([128, 1], F32, "c_" + n) for _ in range(2)] for n in
           ("ssqk", "rk", "ssqq", "rq", "rq2", "ssqo", "v1", "fac")}
    bank = [C.ps([128, 512], F32, f"gb{i}") for i in range(8)]
    BK = [K(f"bank{i}") for i in range(8)]

    for nm, dd, sbt in (("wq", wq_d, wq), ("wk", wk_d, wk), ("wv", wv_d, wv), ("wz", wz_d, wz)):
        S.dma("pool", K(nm), sbt[:], dd.rearrange("(j p) n -> p j n", p=128), writes=[K(nm)])
    S.dma("sp", K("wba"), wba[:], wba_d.rearrange("(j p) n -> p j n", p=128), writes=[K("wba")])
    S.dma("sp", K("convw"), convw[:], convw_d, writes=[K("convw")])
    S.dma("sp", K("hpar"), hpar[:], hp_d, writes=[K("hpar")])
    S.dma("sp", K("normg"), normg[:], normg_d, writes=[K("normg")])
    S.dma("sp", K("cst"), cst[:], gconst_d.rearrange("i p f -> p i f"), writes=[K("cst")])
    S.op("dve", "memset", ap=onec[:], constant=1.0, writes=[K("onec")])
    S.op("dve", "memset", ap=eps6[:], constant=NORM_EPS, writes=[K("eps6")])
    for h in range(NH):
        S.op("dve", "memset", ap=Sst[h][:], constant=0.0, writes=[K(f"S{h}")])
        for i in range(3):
            S.op("pool", "memset", ap=xb[i][h][:, 0:3], constant=0.0, writes=[K(f"xb{i}{h}")])
    S.op("act", "activation", out=nea[:], in_=hpar[:, 0:4], func=AF.Exp, reads=[K("hpar")], writes=[K("nea")])
    S.op("dve", "tensor_scalar", out=nea[:], in0=nea[:], scalar1=-1.0, scalar2=None, op0=ALU.mult,
         reads=[K("nea")], writes=[K("nea")])

    def mm(bi, c0, c1, lhsT, rhs, reads, start=True, stop=True, rows=128):
        S.op("pe", "matmul", out=bank[bi][0:rows, c0:c1], lhsT=lhsT, rhs=rhs, start=start, stop=stop,
             reads=reads, writes=[BK[bi]])

    def tr(bi, c0, in_, reads):
        S.op("pe", "transpose", out=bank[bi][:, c0:c0 + 128], in_=in_, identity=ident,
             reads=reads + [K("cst")], writes=[BK[bi]])

    it = 0
    for blk in range(NB):
        t0 = blk * 512
        bi2 = blk % 2
        xsb, kx = xs[bi2], K(f"xs{bi2}")
        S.dma("pool", kx, xsb[:], xT[:, t0:t0 + 512].rearrange("(j p) t -> p j t", p=128), writes=[kx])
        S.dma("sp", K("xs32"), xs32[:], xT[:, t0:t0 + 512].rearrange("(j p) t -> p j t", p=128), writes=[K("xs32")])
        ci = 0
        for i, wsb, wkey in ((0, wq, "wq"), (1, wk, "wk"), (2, wv, "wv")):
            for h in range(NH):
                for k in range(8):
                    mm(0, 0, 512, wsb[:, k, h * 128:(h + 1) * 128], xsb[:, k, :], [K(wkey), kx], start=(k == 0),
                       stop=(k == 7))
                xbt, kxb = xb[i][h], K(f"xb{i}{h}")
                S.op("act", "copy", out=xbt[:, 3:515], in_=bank[0][:], reads=[BK[0]], writes=[kxb])
                ca, kca = cacc[ci % 2], K(f"cacc{ci % 2}")
                ci += 1
                eng = "dve"
                S.op(eng, "tensor_scalar", out=ca[:], in0=xbt[:, 0:512], scalar1=convw[:, i, h, 0:1], scalar2=None,
                     op0=ALU.mult, reads=[kxb, K("convw")], writes=[kca])
                for j in range(1, 4):
                    S.op(eng, "scalar_tensor_tensor", out=ca[:], in0=xbt[:, j:j + 512], scalar=convw[:, i, h, j:j + 1],
                         in1=ca[:], op0=ALU.mult, op1=ALU.add, reads=[kxb, K("convw"), kca], writes=[kca])
                S.op("pool", "tensor_copy", out=xbt[:, 0:3], in_=xbt[:, 512:515], reads=[kxb], writes=[kxb])
                S.op("act", "activation", out=qkvc[i][h][:], in_=ca[:], func=AF.Silu, reads=[kca],
                     writes=[K(f"qkvc{i}{h}")])
        for tl in range(4):
            tsl = slice(tl * 128, (tl + 1) * 128)
            if GDN_LV < 2:
                continue
            for k in range(8):
                mm(1, 0, 512, xsb[:, k, tsl], wz[:, k, :], [kx, K("wz")], start=(k == 0), stop=(k == 7))
            S.op("act", "activation", out=zs[:], in_=bank[1][:], func=AF.Silu, reads=[BK[1]], writes=[K("zs")])
            for k in range(8):
                mm(1, 0, 8, xs32[:, k, tsl], wba[:, k, :], [K("xs32"), K("wba")], start=(k == 0), stop=(k == 7))
            S.op("dve", "tensor_copy", out=ba[:], in_=bank[1][:, 0:8], reads=[BK[1]], writes=[K("ba")])
            G = lambda n: gt[n][:]
            kg = lambda n: K("g_" + n)
            S.op("act", "activation", out=G("beta"), in_=ba[:, 0:4], func=AF.Sigmoid, reads=[K("ba")],
                 writes=[kg("beta")])
            S.op("dve", "tensor_scalar", out=G("nbeta"), in0=G("beta"), scalar1=-1.0, scalar2=None, op0=ALU.mult,
                 reads=[kg("beta")], writes=[kg("nbeta")])
            S.op("dve", "tensor_tensor", out=G("x"), in0=ba[:, 4:8], in1=hpar[:, 4:8], op=ALU.add,
                 reads=[K("ba"), K("hpar")], writes=[kg("x")])
            S.op("act", "activation", out=G("e"), in_=G("x"), func=AF.Exp, reads=[kg("x")], writes=[kg("e")])
            S.op("act", "activation", out=G("sp"), in_=G("e"), func=AF.Ln, bias=onec[:, 0:1], scale=1.0,
                 reads=[kg("e"), K("onec")], writes=[kg("sp")])
            S.op("dve", "tensor_tensor", out=G("g"), in0=G("sp"), in1=nea[:], op=ALU.mult,
                 reads=[kg("sp"), K("nea")], writes=[kg("g")])
            mm(1, 0, 4, MU, G("g"), [K("cst"), kg("g")])
            mm(1, 4, 8, ONES, G("g"), [K("cst"), kg("g")])
            S.op("dve", "tensor_copy", out=gc8[:], in_=bank[1][:, 0:8], reads=[BK[1]], writes=[K("gc8")])
            S.op("act", "activation", out=G("eg"), in_=gc8[:, 0:4], func=AF.Exp, reads=[K("gc8")], writes=[kg("eg")])
            S.op("act", "activation", out=G("elast"), in_=gc8[:, 4:8], func=AF.Exp, reads=[K("gc8")],
                 writes=[kg("elast")])
            S.op("dve", "tensor_tensor", out=G("kes"), in0=gc8[:, 4:8], in1=gc8[:, 0:4], op=ALU.subtract,
                 reads=[K("gc8")], writes=[kg("kes")])
            S.op("act", "activation", out=G("kes"), in_=G("kes"), func=AF.Exp, reads=[kg("kes")], writes=[kg("kes")])
            S.op("dve", "tensor_tensor", out=G("bks"), in0=G("beta"), in1=G("eg"), op=ALU.mult,
                 reads=[kg("beta"), kg("eg")], writes=[kg("bks")])
            if GDN_LV < 2.5:
                continue
            for hpair in range(0, NH, 2):
                recs = []
                for h in (hpair, hpair + 1):
                    rec = []
                    real_op = S.op
                    S.op = (lambda eng, method, reads=(), writes=(), _rec=rec, **kw:
                            _rec.append((eng, method, reads, writes, kw)))
                    try:
                        p2 = it % 2
                        bA, bB, bC = 2 + 3 * p2, 3 + 3 * p2, 4 + 3 * p2
                        it += 1
                        T_ = lambda lst: lst[p2][:]
                        kt = lambda n: K(f"{n}{p2}")
                        cl = lambda n: col[n][p2][:]
                        QT = qkvc[0][h][:, tsl]
                        KTr = qkvc[1][h][:, tsl]
                        VTr = qkvc[2][h][:, tsl]
                        kq, kk_, kv = K(f"qkvc0{h}"), K(f"qkvc1{h}"), K(f"qkvc2{h}")
                        hs = slice(h, h + 1)
                        S.op("pool", "tensor_copy", out=T_(Qc), in_=QT, reads=[kq], writes=[kt("Qc")])
                        tr(bA, 0, KTr, [kk_])
                        S.op("act", "activation", out=T_(junk), in_=bank[bA][:, 0:128], func=AF.Square,
                             reads=[BK[bA]], writes=[kt("junk")])
                        S.op("dve", "reduce_sum", out=cl("ssqk"), in_=T_(junk), axis=AX.X, reads=[kt("junk")],
                             writes=[kt("ssqk")])
                        S.op("act", "activation", out=cl("rk"), in_=cl("ssqk"), func=AF.Sqrt, bias=eps6[:, 0:1], scale=1.0,
                             reads=[kt("ssqk"), K("eps6")], writes=[kt("rk")])
                        S.op("dve", "reciprocal", out=cl("rk"), in_=cl("rk"), reads=[kt("rk")], writes=[kt("rk")])
                        S.op("dve", "tensor_scalar", out=T_(Kn), in0=bank[bA][:, 0:128], scalar1=cl("rk"), scalar2=None,
                             op0=ALU.mult, reads=[BK[bA], kt("rk")], writes=[kt("Kn")])
                        tr(bA, 128, T_(Kn), [kt("Kn")])
                        S.op("act", "copy", out=T_(KnT), in_=bank[bA][:, 128:256], reads=[BK[bA]], writes=[kt("KnT")])
                        S.op("dve", "tensor_scalar", out=T_(Kbg), in0=T_(Kn), scalar1=gt["bks"][:, hs], scalar2=None, op0=ALU.mult,
                             reads=[kt("Kn"), kg("bks")], writes=[kt("Kbg")])
                        S.op("dve", "tensor_scalar", out=T_(Kend), in0=T_(Kn), scalar1=gt["kes"][:, hs], scalar2=None, op0=ALU.mult,
                             reads=[kt("Kn"), kg("kes")], writes=[kt("Kend")])
                        if GDN_LV < 3:
                            continue
                        tr(bA, 256, VTr, [kv])
                        S.op("dve", "tensor_scalar", out=T_(Vb), in0=bank[bA][:, 256:384], scalar1=gt["beta"][:, hs],
                             scalar2=None, op0=ALU.mult, reads=[BK[bA], kg("beta")], writes=[kt("Vb")])
                        if GDN_LV < 3.5:
                            continue
                        tr(bA, 384, QT, [kq])
                        S.op("act", "activation", out=T_(sq), in_=bank[bA][:, 384:512], func=AF.Square, reads=[BK[bA]],
                             writes=[kt("sq")])
                        S.op("dve", "reduce_sum", out=cl("ssqq"), in_=T_(sq), axis=AX.X, reads=[kt("sq")], writes=[kt("ssqq")])
                        S.op("act", "activation", out=cl("rq"), in_=cl("ssqq"), func=AF.Sqrt, bias=eps6[:, 0:1],
                             scale=1.0, reads=[kt("ssqq"), K("eps6")], writes=[kt("rq")])
                        S.op("dve", "reciprocal", out=cl("rq"), in_=cl("rq"), reads=[kt("rq")], writes=[kt("rq")])
                        S.op("dve", "tensor_scalar", out=cl("rq"), in0=cl("rq"), scalar1=128.0 ** -0.5, scalar2=None,
                             op0=ALU.mult, reads=[kt("rq")], writes=[kt("rq")])
                        S.op("dve", "scalar_tensor_tensor", out=cl("rq2"), in0=cl("rq"), scalar=1.0 / 128.0, in1=cl("rq"),
                             op0=ALU.mult, op1=ALU.mult, reads=[kt("rq")], writes=[kt("rq2")])
                        if GDN_LV < 4:
                            continue
                        S.op("dve", "tensor_scalar", out=T_(G1), in0=MU, scalar1=gt["g"][:, hs], scalar2=None, op0=ALU.mult,
                             reads=[K("cst"), kg("g")], writes=[kt("G1")])
                        mm(bB, 0, 128, T_(G1), MS, [kt("G1"), K("cst")], start=True, stop=False)
                        mm(bB, 0, 128, ident, NEGL, [K("cst")], start=False, stop=True)
                        mm(bB, 128, 256, MS, T_(G1), [kt("G1"), K("cst")], start=True, stop=False)
                        mm(bB, 128, 256, ident, NEGUT, [K("cst")], start=False, stop=True)
                        S.op("act", "activation", out=T_(Dl), in_=bank[bB][:, 0:128], func=AF.Exp, reads=[BK[bB]],
                             writes=[kt("Dl")])
                        S.op("act", "activation", out=T_(DTu), in_=bank[bB][:, 128:256], func=AF.Exp, reads=[BK[bB]],
                             writes=[kt("DTu")])
                        if GDN_LV < 5:
                            continue
                        mm(bB, 256, 384, T_(KnT), T_(KnT), [kt("KnT")])
                        mm(bB, 384, 512, T_(KnT), QT, [kt("KnT"), kq])
                        N0, NT0 = Nn[0][p2], NTn[0][p2]
                        S.op("dve", "scalar_tensor_tensor", out=N0[:], in0=bank[bB][:, 256:384], scalar=gt["nbeta"][:, hs],
                             in1=T_(Dl), op0=ALU.mult, op1=ALU.mult, reads=[BK[bB], kg("nbeta"), kt("Dl")], writes=[kt("N0")])
                        S.op("dve", "tensor_tensor", out=T_(AT), in0=bank[bB][:, 384:512], in1=T_(DTu), op=ALU.mult,
                             reads=[BK[bB], kt("DTu")], writes=[kt("AT")])
                        tr(bC, 0, N0[:], [kt("N0")])
                        S.op("act", "copy", out=NT0[:], in_=bank[bC][:, 0:128], reads=[BK[bC]], writes=[kt("NT0")])
                        S.op("pool", "tensor_tensor", out=T_(PT), in0=NT0[:], in1=ident, op=ALU.add,
                             reads=[kt("NT0"), K("cst")], writes=[kt("PT")])
                        if GDN_LV < 6:
                            continue
                        for j in range(1, 7):
                            a, b = (j - 1) % 2, j % 2
                            Na, NTa, Nb, NTb = Nn[a][p2], NTn[a][p2], Nn[b][p2], NTn[b][p2]
                            mm(bC, 128, 256, NTa[:], Na[:], [kt(f"N{a}"), kt(f"NT{a}")])
                            if j < 6:
                                mm(bC, 256, 384, Na[:], NTa[:], [kt(f"N{a}"), kt(f"NT{a}")])
                            S.op("act", "copy", out=Nb[:], in_=bank[bC][:, 128:256], reads=[BK[bC]], writes=[kt(f"N{b}")])
                            if j < 6:
                                S.op("dve", "tensor_copy", out=NTb[:], in_=bank[bC][:, 256:384], reads=[BK[bC]],
                                     writes=[kt(f"NT{b}")])
                            mm(bC, 384, 512, Nb[:], T_(PT), [kt(f"N{b}"), kt("PT")])
                            S.op("dve", "tensor_tensor", out=T_(PT), in0=bank[bC][:, 384:512], in1=T_(PT), op=ALU.add,
                                 reads=[BK[bC], kt("PT")], writes=[kt("PT")])
                        if GDN_LV < 7:
                            continue
                        mm(bC, 0, 128, T_(Kbg), T_(PT), [kt("Kbg"), kt("PT")])
                        S.op("act", "mul", out=T_(nWT), in_=bank[bC][:, 0:128], mul=-1.0, reads=[BK[bC]], writes=[kt("nWT")])
                        if GDN_LV < 8:
                            continue
                        St, kS = Sst[h], K(f"S{h}")
                        mm(bC, 0, 128, T_(PT), T_(Vb), [kt("PT"), kt("Vb")], start=True, stop=False)
                        mm(bC, 0, 128, T_(nWT), St[:], [kt("nWT"), kS], start=False, stop=True)
                        S.op("act", "copy", out=T_(Vnew), in_=bank[bC][:, 0:128], reads=[BK[bC]], writes=[kt("Vnew")])
                        mm(bC, 128, 256, T_(Qc), St[:], [kt("Qc"), kS])
                        mm(bC, 256, 384, T_(AT), T_(Vnew), [kt("AT"), kt("Vnew")])
                        mm(bC, 384, 512, T_(Kend), T_(Vnew), [kt("Kend"), kt("Vnew")])
                        S.op("dve", "tensor_scalar", out=T_(o1), in0=bank[bC][:, 128:256], scalar1=gt["eg"][:, hs], scalar2=None, op0=ALU.mult,
                             reads=[BK[bC], kg("eg")], writes=[kt("o1")])
                        S.op("dve", "tensor_tensor", out=T_(o_), in0=bank[bC][:, 256:384], in1=T_(o1), op=ALU.add,
                             reads=[BK[bC], kt("o1")], writes=[kt("o")])
                        S.op("dve", "scalar_tensor_tensor", out=St[:], in0=St[:], scalar=gt["elast"][:, hs],
                             in1=bank[bC][:, 384:512], op0=ALU.mult, op1=ALU.add, reads=[kS, kg("elast"), BK[bC]], writes=[kS])
                        if GDN_LV < 9:
                            continue
                        S.op("act", "activation", out=T_(junk), in_=T_(o_), func=AF.Square,
                             reads=[kt("o")], writes=[kt("junk")])
                        S.op("dve", "reduce_sum", out=cl("ssqo"), in_=T_(junk), axis=AX.X, reads=[kt("junk")],
                             writes=[kt("ssqo")])
                        S.op("dve", "tensor_tensor", out=cl("v1"), in0=cl("ssqo"), in1=cl("rq2"), op=ALU.mult,
                             reads=[kt("ssqo"), kt("rq2")], writes=[kt("v1")])
                        S.op("act", "activation", out=cl("ssqq"), in_=cl("v1"), func=AF.Sqrt, bias=eps6[:, 0:1], scale=1.0,
                             reads=[kt("v1"), K("eps6")], writes=[kt("ssqq")])
                        S.op("dve", "reciprocal", out=cl("v1"), in_=cl("ssqq"), reads=[kt("ssqq")], writes=[kt("v1")])
                        S.op("dve", "tensor_tensor", out=cl("fac"), in0=cl("v1"), in1=cl("rq"), op=ALU.mult,
                             reads=[kt("v1"), kt("rq")], writes=[kt("fac")])
                        if GDN_LV < 11:
                            continue
                        S.op("dve", "scalar_tensor_tensor", out=T_(og), in0=T_(o_), scalar=cl("fac"), in1=normg[:],
                             op0=ALU.mult, op1=ALU.mult, reads=[kt("o"), kt("fac"), K("normg")], writes=[kt("og")])
                        S.op("dve", "tensor_tensor", out=T_(og), in0=T_(og), in1=zs[:, h * 128:(h + 1) * 128], op=ALU.mult,
                             reads=[kt("og"), K("zs")], writes=[kt("og")])
                        if GDN_LV < 11:
                            continue
                        tr(bA, 0, T_(og), [kt("og")])
                        S.op("act", "copy", out=ogT_sb[:, h, tsl], in_=bank[bA][:, 0:128], reads=[BK[bA]],
                             writes=[K("ogT")])
                    finally:
                        S.op = real_op
                    recs.append(rec)
                na, nb = len(recs[0]), len(recs[1])
                for q in range(max(na, nb)):
                    for rr in recs:
                        if q < len(rr):
                            eng_, method_, reads_, writes_, kw_ = rr[q]
                            S.op(eng_, method_, reads=reads_, writes=writes_, **kw_)

        S.dma("sp", K("ogT"), ogT[:, t0:t0 + 512].rearrange("(h p) t -> p h t", p=128), ogT_sb[:], reads=[K("ogT")])


def build_gdn(T):
    nc = bass.Bass("TRN2", target_bir_lowering=False)
    dt = lambda n, s, k="ExternalInput": nc.dram_tensor(n, s, F32, kind=k).ap()
    xT = dt("xT", [D, T])
    wq = dt("wq", [D, 512]); wk = dt("wk", [D, 512]); wv = dt("wv", [D, 512]); wz = dt("wz", [D, 512])
    wba = dt("wba", [D, 8]); convw = dt("convw", [128, 3, 4, 4]); hp = dt("hpar", [128, 8])
    normg = dt("normg", [128, 128]); gconst = dt("gconst", [6, 128, 128])
    ogT = dt("ogT", [512, T], "ExternalOutput")
    with contextlib.ExitStack() as es:
        C = Ctx(nc, es)
        gdn_phase(C, T, xT, wq, wk, wv, wz, wba, convw, hp, normg, gconst, ogT)
        C.S.finish()
        C.S.emit()
    return nc


def gdn_consts():
    i = np.arange(128)
    ident = np.eye(128, dtype=np.float32)
    MU = (i[:, None] <= i[None, :]).astype(np.float32)
    MS = (i[:, None] > i[None, :]).astype(np.float32)
    NEGL = np.where(i[:, None] > i[None, :], 0.0, -100.0).astype(np.float32)
    NEGUT = np.where(i[None, :] >= i[:, None], 0.0, -100.0).astype(np.float32)
    ONES = np.ones((128, 128), np.float32)
    return np.stack([ident, MU, MS, NEGL, NEGUT, ONES])


def gdn_host_layout(hh, w_in, conv_w, a_log, dt_bias, norm_g):
    QK = 1024
    c0 = 512 * hh
    wq = np.ascontiguousarray(w_in[:, c0:c0 + 512])
    wk = np.ascontiguousarray(w_in[:, QK + c0:QK + c0 + 512])
    wv = np.ascontiguousarray(w_in[:, 2 * QK + c0:2 * QK + c0 + 512])
    wz = np.ascontiguousarray(w_in[:, 3 * QK + c0:3 * QK + c0 + 512])
    wba = np.ascontiguousarray(np.concatenate([w_in[:, 4 * QK + 4 * hh:4 * QK + 4 * hh + 4],
                                               w_in[:, 4 * QK + 8 + 4 * hh:4 * QK + 8 + 4 * hh + 4]], axis=1))
    convw = np.zeros((128, 3, 4, 4), np.float32)
    for i in range(3):
        for h in range(4):
            convw[:, i, h, :] = conv_w[:, i * QK + c0 + h * 128:i * QK + c0 + (h + 1) * 128].T
    hp = np.tile(np.concatenate([a_log[4 * hh:4 * hh + 4], dt_bias[4 * hh:4 * hh + 4]])[None, :], (128, 1)).astype(np.float32)
    normg = np.tile(norm_g[None, :], (128, 1)).astype(np.float32)
    return dict(wq=wq, wk=wk, wv=wv, wz=wz, wba=wba, convw=convw, hpar=hp, normg=normg, gconst=gdn_consts())


GDN_NAMES = ("wq", "wk", "wv", "wz", "wba", "convw", "hpar", "normg")
S5_NAMES = ("w_in", "lamr", "lami", "ldt", "bpad_re", "bpad_im", "cpad_re", "cpad_im", "dsk")
GDN_SHAPES = dict(wq=[D, 512], wk=[D, 512], wv=[D, 512], wz=[D, 512], wba=[D, 8], convw=[128, 3, 4, 4],
                  hpar=[128, 8], normg=[128, 128])
S5_SHAPES = dict(w_in=[D, 512], lamr=[128, 16], lami=[128, 16], ldt=[128, 16], bpad_re=[16, 128, 128],
                 bpad_im=[16, 128, 128], cpad_re=[16, 128, 128], cpad_im=[16, 128, 128], dsk=[128, 4])


def build_fused(T):
    nc = bass.Bass("TRN2", target_bir_lowering=False)
    ext = lambda n, sh: nc.dram_tensor(n, sh, F32, kind="ExternalInput").ap()
    xT0 = ext("xT0", [D, T])
    xtok0 = ext("xtok0", [T, D])
    ident_d = ext("ident", [128, 128])
    gconst = ext("gconst", [6, 128, 128])
    iota = ext("iota", [128, S5_L])
    wr0 = ext("wr_dummy", [D, NEXP])
    br0 = ext("br_dummy", [1, NEXP])
    y = nc.dram_tensor("y", [T, D], F32, kind="ExternalOutput").ap()
    XT = nc.dram_tensor("XT_i", [D, T], F32).ap()
    FEAT = nc.dram_tensor("FEAT_i", [D, T], F32).ap()
    XA = nc.dram_tensor("XA_i", [T, D], F32).ap()
    XB = nc.dram_tensor("XB_i", [T, D], F32).ap()
    TH = T // 2
    tb = min(1024, TH)
    with contextlib.ExitStack() as es:
        S = Sched(nc, es)

        def phase(fn, last=False):
            with contextlib.ExitStack() as pes:
                C = Ctx(nc, pes, S)
                fn(C)
                S.barrier()
                S.emit()
            if not last:
                S.new_phase()

        for i in range(DEPTH):
            xT_src = xT0 if i == 0 else XT
            xres_src = xtok0 if i == 0 else (XA if i % 2 == 1 else XB)
            xout_dst = y if i == DEPTH - 1 else (XA if i % 2 == 0 else XB)
            lnp = ext(f"L{i}_lnp", [4, D])
            if i % 2 == 0:
                for hh in range(2):
                    d = {n: ext(f"L{i}_{hh}_{n}", GDN_SHAPES[n]) for n in GDN_NAMES}
                    phase(lambda C, d=d, hh=hh: gdn_phase(
                        C, T, xT_src, d["wq"], d["wk"], d["wv"], d["wz"], d["wba"], d["convw"], d["hpar"], d["normg"],
                        gconst, FEAT[hh * 512:(hh + 1) * 512, :], pfx=f"g{i}{hh}"))
                glu, moe, ne, nproj = False, False, 2, D
            else:
                for gh in range(2):
                    d = {n: ext(f"L{i}_{gh}_{n}", S5_SHAPES[n]) for n in S5_NAMES}
                    phase(lambda C, d=d, gh=gh: s5_phase(
                        C, T, xT_src, d["w_in"], d["lamr"], d["lami"], d["ldt"], d["bpad_re"], d["bpad_im"],
                        d["cpad_re"], d["cpad_im"], d["dsk"], iota, FEAT[gh * 512:(gh + 1) * 512, :], pfx=f"s{i}{gh}"))
                glu, moe, ne, nproj = True, True, NEXP, 2 * D
            wproj = ext(f"L{i}_wproj", [D, nproj])
            w1 = ext(f"L{i}_w1", [ne, D, FE]); w3 = ext(f"L{i}_w3", [ne, D, FE]); w2 = ext(f"L{i}_w2", [ne, FE, D])
            if moe:
                wr = ext(f"L{i}_wr", [D, NEXP]); br = ext(f"L{i}_br", [1, NEXP])
            else:
                wr, br = wr0, br0
            for hf in range(2):
                sl = slice(hf * TH, (hf + 1) * TH)
                phase(lambda C, sl=sl, hf=hf: tp_phase(
                    C, TH, glu, moe, FEAT[:, sl], wproj, xres_src[sl, :], lnp, w1, w3, w2, wr, br, ident_d,
                    xout_dst[sl, :], XT[:, sl], tb=tb, pfx=f"t{i}{hf}"), last=(i == DEPTH - 1 and hf == 1))
        print("n_sems", len(S.sem), "n_ops", S.n_ops)
        S.finish()
        S.emit()
    return nc


def fused_inputs(xb, P):
    f = lambda a: np.ascontiguousarray(np.asarray(a, dtype=np.float32))
    m = dict(xT0=np.ascontiguousarray(xb.T), xtok0=np.ascontiguousarray(xb), ident=np.eye(128, dtype=np.float32),
             gconst=gdn_consts(), iota=np.tile(np.arange(1, S5_L + 1, dtype=np.float32)[None, :], (128, 1)),
             wr_dummy=np.zeros((D, NEXP), np.float32), br_dummy=np.zeros((1, NEXP), np.float32))
    for i in range(DEPTH):
        j = i // 2
        m[f"L{i}_lnp"] = f(np.stack([P["ln_g"][i, 0], P["ln_b"][i, 0], P["ln_g"][i, 1], P["ln_b"][i, 1]]))
        if i % 2 == 0:
            for hh in range(2):
                lay = gdn_host_layout(hh, f(P["gdn_w_in"][j]), f(P["gdn_conv_w"][j]), f(P["gdn_a_log"][j]),
                                      f(P["gdn_dt_bias"][j]), f(P["gdn_norm_g"][j]))
                for n in GDN_NAMES:
                    m[f"L{i}_{hh}_{n}"] = lay[n]
            m[f"L{i}_wproj"] = f(P["gdn_w_out"][j])
            m[f"L{i}_w1"] = f(np.stack([P["ffn_w1"][j][:, :FE], P["ffn_w1"][j][:, FE:]]))
            m[f"L{i}_w3"] = f(np.stack([P["ffn_w3"][j][:, :FE], P["ffn_w3"][j][:, FE:]]))
            m[f"L{i}_w2"] = f(np.stack([P["ffn_w2"][j][:FE], P["ffn_w2"][j][FE:]]))
        else:
            for gh in range(2):
                lay = s5_host_layout(gh, f(P["s5_lam_re"][j]), f(P["s5_lam_im"][j]), f(P["s5_log_dt"][j]),
                                     f(P["s5_b_re"][j]), f(P["s5_b_im"][j]), f(P["s5_c_re"][j]), f(P["s5_c_im"][j]),
                                     f(P["s5_d"][j]), f(P["s5_w_in"][j]))
                for n in S5_NAMES:
                    m[f"L{i}_{gh}_{n}"] = lay[n]
            m[f"L{i}_wproj"] = f(P["s5_w_glu"][j])
            m[f"L{i}_w1"] = f(P["moe_w1"][j]); m[f"L{i}_w3"] = f(P["moe_w3"][j]); m[f"L{i}_w2"] = f(P["moe_w2"][j])
            m[f"L{i}_wr"] = f(P["moe_w_router"][j]); m[f"L{i}_br"] = f(P["moe_b_router"][j]).reshape(1, NEXP)
    return m


SEQ = 8192
BATCH = 4
_PROGS = {}


def kernel(**P):
    x = np.ascontiguousarray(np.asarray(P["x"], dtype=np.float32))
    T = x.shape[1]
    if T not in _PROGS:
        _PROGS[T] = build_fused(T)
    nc = _PROGS[T]
    shared = None
    in_maps = []
    for c in range(8):
        b = c % BATCH
        m = fused_inputs(x[b], P) if shared is None else dict(shared)
        if shared is None:
            shared = m
        else:
            m["xT0"] = np.ascontiguousarray(x[b].T)
            m["xtok0"] = x[b]
        in_maps.append(m)
    res = run_bass_kernel_spmd(nc, in_maps, core_ids=list(range(8))).results
    return np.stack([res[b]["y"] for b in range(BATCH)]).astype(np.float32)
```

```python
import contextlib
import numpy as np
import concourse.bass as bass
import concourse.mybir as mybir
from concourse.bass_utils import run_bass_kernel_spmd

F32 = mybir.dt.float32
BF16 = mybir.dt.bfloat16
AF = mybir.ActivationFunctionType
ALU = mybir.AluOpType
AX = mybir.AxisListType

D = 1024
DEPTH = 4
ALPHA = (2 * DEPTH) ** 0.25
LN_EPS = 1e-5
NORM_EPS = 1e-6
NEXP = 8
FE = 1408
NFT = FE // 128


class Sched:
    ENGS = ("pe", "dve", "act", "pool", "sp")

    def __init__(self, nc, es):
        self.nc = nc
        self.es = es
        self.q = {e: [] for e in self.ENGS}
        self.sem = {}
        self.cnt = {}
        self.known = {e: {} for e in self.ENGS}
        self.last_w = {}
        self.readers = {}
        self.phase = 0
        self.ename = {}
        for e in ("pe", "dve", "act", "pool"):
            self.ename[e] = e + "0"
            self._mksem(e + "0")
        self.n_ops = 0

    def _mksem(self, name):
        self.sem[name] = self.es.enter_context(self.nc.semaphore("s_" + name))
        self.cnt[name] = 0

    def _deps(self, reads, writes):
        deps = {}

        def add(tok):
            if tok is None:
                return
            s, v = tok
            if deps.get(s, 0) < v:
                deps[s] = v

        for k in reads:
            add(self.last_w.get(k))
        for k in writes:
            add(self.last_w.get(k))
            for s, v in self.readers.get(k, {}).items():
                add((s, v))
        return deps

    def _commit(self, tok, reads, writes):
        for k in writes:
            self.last_w[k] = tok
            self.readers[k] = {}
        for k in reads:
            if k in writes:
                continue
            r = self.readers.setdefault(k, {})
            if r.get(tok[0], 0) < tok[1]:
                r[tok[0]] = tok[1]

    def _waits(self, eng, deps):
        waits = []
        kn = self.known[eng]
        for s, v in deps.items():
            if eng == "pe" and s == self.ename["pe"]:
                continue
            if kn.get(s, 0) < v:
                kn[s] = v
                waits.append((s, v))
        return waits

    def op(self, eng, method, reads=(), writes=(), **kw):
        if eng != "pe":
            ex = [k for k in reads if ".bank" in k and k not in writes]
            if ex:
                writes = list(writes) + ex
        deps = self._deps(reads, writes)
        waits = self._waits(eng, deps)
        en = self.ename[eng]
        self.cnt[en] += 1
        tok = (en, self.cnt[en])
        self.q[eng].append((waits, (method, kw), (en, 1)))
        self._commit(tok, reads, writes)
        self.n_ops += 1

    def dma(self, queue, stream, out, in_, reads=(), writes=(), **kw):
        sname = "d_" + stream.split(".", 1)[-1]
        if sname not in self.sem:
            self._mksem(sname)
        deps = self._deps(reads, writes)
        waits = self._waits(queue, deps)
        self.cnt[sname] += 16
        tok = (sname, self.cnt[sname])
        kw = dict(kw)
        kw["out"] = out
        kw["in_"] = in_
        self.q[queue].append((waits, ("dma_start", kw), (sname, 16)))
        self._commit(tok, reads, writes)
        self.n_ops += 1

    def coll(self, kind, stream, ins, outs, replica_groups, reads=(), writes=()):
        sname = "d_" + stream
        if sname not in self.sem:
            self._mksem(sname)
        deps = self._deps(reads, writes)
        waits = self._waits("pool", deps)
        self.cnt[sname] += 16
        tok = (sname, self.cnt[sname])
        kw = dict(kind=kind, op=ALU.bypass, replica_groups=replica_groups, ins=list(ins), outs=list(outs))
        self.q["pool"].append((waits, ("collective_compute", kw), (sname, 16)))
        self._commit(tok, reads, writes)
        self.n_ops += 1

    def barrier(self):
        for e in self.ENGS:
            waits = []
            for s_, v in self.cnt.items():
                if v > 0 and self.known[e].get(s_, 0) < v:
                    self.known[e][s_] = v
                    waits.append((s_, v))
            if waits:
                self.q[e].append((waits, None, None))

    def finish(self):
        self.barrier()

    def new_phase(self):
        self.phase += 1
        self.last_w = {}
        self.readers = {}
        for e in ("pe", "dve", "act"):
            old_name = self.ename[e]
            for kn in self.known.values():
                kn.pop(old_name, None)
            del self.cnt[old_name]
            nm = f"{e}{self.phase}"
            self.ename[e] = nm
            self._mksem(nm)

    def emit(self):
        nc = self.nc
        S = self

        def replay(name, eng):
            for waits, fn, inc in S.q[name]:
                for s, v in waits:
                    eng.wait_ge(S.sem[s], v)
                if fn is None:
                    continue
                inst = getattr(eng, fn[0])(**fn[1])
                inst.then_inc(S.sem[inc[0]], inc[1])
            S.q[name] = []

        with nc.Block() as block:
            @block.tensor
            def _(e):
                replay("pe", e)

            @block.vector
            def _(e):
                replay("dve", e)

            @block.scalar
            def _(e):
                replay("act", e)

            @block.gpsimd
            def _(e):
                replay("pool", e)

            @block.sync
            def _(e):
                replay("sp", e)


class Ctx:
    _uid = [0]

    def __init__(self, nc, es, S=None):
        self.nc = nc
        self.es = es
        self.S = S if S is not None else Sched(nc, es)
        Ctx._uid[0] += 1
        self.n = Ctx._uid[0] * 1000

    def sb(self, shape, dt=F32, name=None):
        self.n += 1
        return self.es.enter_context(self.nc.sbuf_tensor(f"{name or 'sb'}_{self.n}", list(shape), dt))

    def ps(self, shape, dt=F32, name=None):
        self.n += 1
        return self.es.enter_context(self.nc.psum_tensor(f"{name or 'ps'}_{self.n}", list(shape), dt))


def tp_phase(C, T, glu, moe, featT, wproj, xres, lnp, w1, w3, w2, wr, br, ident_d, xout, xoutT, tb=1024, pfx="tp"):
    S = C.S
    nproj = 2 * D if glu else D
    ne = NEXP if moe else 2
    tb = min(tb, T)
    nblk = T // tb
    ntt = tb // 128
    hw = min(512, tb)
    nhalf = tb // hw
    P = pfx

    def K(name):
        return P + "." + name

    ident = C.sb([128, 128], F32, "ident")
    lnb = C.sb([128, 4, D], F32, "lnb")
    wproj_sb = C.sb([128, 8, nproj], BF16, "wproj")
    wr_sb = C.sb([128, 8, NEXP], F32, "wr")
    br_sb = C.sb([128, NEXP], F32, "br")
    w1_sb = C.sb([128, 8, FE], BF16, "w1")
    w3_sb = C.sb([128, 8, FE], BF16, "w3")
    w2_sb = C.sb([128, NFT, D], BF16, "w2")
    featT_sb = [C.sb([128, 8, 128], BF16, "featT") for _ in range(2)]
    xres_sb = [C.sb([128, D], F32, "xres") for _ in range(2)]
    hglu_sb = C.sb([128, 512], F32, "hglu")
    stats = [C.sb([128, 2, 6], F32, "stats") for _ in range(2)]
    mv = [C.sb([128, 2], F32, "mv") for _ in range(2)]
    rstd = [C.sb([128, 1], F32, "rstd") for _ in range(2)]
    x1 = [C.sb([128, D], F32, "x1") for _ in range(2)]
    x1T32 = [C.sb([128, 8, 128], F32, "x1T32") for _ in range(2)]
    x1T = C.sb([128, 8, tb], BF16, "x1T")
    yacc = C.sb([128, ntt, D], F32, "yacc")
    gates = C.sb([128, ntt, NEXP], F32, "gates")
    rt = [C.sb([128, NEXP], F32, "rt") for _ in range(4)]
    rcol = [C.sb([128, 1], F32, "rcol") for _ in range(4)]
    hT = C.sb([128, NFT, hw], BF16, "hT")
    sg = [C.sb([128, hw], BF16, "sg") for _ in range(2)]
    epsc = C.sb([128, 1], F32, "epsc")
    pg = [C.ps([128, 512], F32, "pg") for _ in range(2)]
    pu = [C.ps([128, 512], F32, "pu") for _ in range(2)]
    py = [C.ps([128, 512], F32, "py") for _ in range(2)]
    ptr = C.ps([128, 512], F32, "ptr")
    prt = C.ps([128, 512], F32, "prt")

    S.op("dve", "memset", ap=epsc[:], constant=LN_EPS, writes=[K("epsc")])
    S.dma("sp", K("c0"), ident[:], ident_d, writes=[K("ident")])
    for i in range(4):
        S.dma("sp", K("c1"), lnb[:, i, :], lnp[i:i + 1, :].partition_broadcast(128), writes=[K("lnb")])
    S.dma("pool", K("c2"), wproj_sb[:], wproj.rearrange("(j p) n -> p j n", p=128), writes=[K("wproj")])
    S.dma("sp", K("c3"), wr_sb[:], wr.rearrange("(j p) n -> p j n", p=128), writes=[K("wr")])
    S.dma("sp", K("c4"), br_sb[:], br.partition_broadcast(128), writes=[K("br")])

    def layer_norm(src, src_key, dst, dst_key, gi, idx):
        st, m, rs = stats[idx], mv[idx], rstd[idx]
        kst, kmv, krs = K(f"st{idx}"), K(f"mv{idx}"), K(f"rstd{idx}")
        for c in range(2):
            S.op("dve", "bn_stats", out=st[:, c, :], in_=src[:, c * 512:(c + 1) * 512],
                 reads=[src_key], writes=[kst + str(c)])
        S.op("dve", "bn_aggr", out=m[:], in_=st[:], reads=[kst + "0", kst + "1"], writes=[kmv])
        S.op("act", "activation", out=rs[:], in_=m[:, 1:2], func=AF.Sqrt, bias=epsc[:, 0:1], scale=1.0,
             reads=[kmv, K("epsc")], writes=[krs])
        S.op("dve", "reciprocal", out=rs[:], in_=rs[:], reads=[krs], writes=[krs])
        S.op("dve", "tensor_scalar", out=dst, in0=src, scalar1=m[:, 0:1], scalar2=rs[:, 0:1],
             op0=ALU.subtract, op1=ALU.mult, reads=[src_key, kmv, krs], writes=[dst_key])
        S.op("pool", "tensor_tensor", out=dst, in0=dst, in1=lnb[:, gi, :], op=ALU.mult,
             reads=[dst_key, K("lnb")], writes=[dst_key])
        S.op("pool", "tensor_tensor", out=dst, in0=dst, in1=lnb[:, gi + 1, :], op=ALU.add,
             reads=[dst_key, K("lnb")], writes=[dst_key])

    cnt = 0
    for blk in range(nblk):
        t0 = blk * tb
        for tt in range(ntt):
            i2 = tt % 2
            tok0 = t0 + tt * 128
            fT, xr, r = featT_sb[i2], xres_sb[i2], xres_sb[i2]
            kfT, kxr, kr, kx1 = K(f"fT{i2}"), K(f"xr{i2}"), K(f"xr{i2}"), K(f"x1_{i2}")
            S.dma("pool", kfT, fT[:], featT[:, tok0:tok0 + 128].rearrange("(j p) t -> p j t", p=128), writes=[kfT])
            S.dma("sp", kxr, xr[:], xres[tok0:tok0 + 128, :], writes=[kxr])
            for nh in range(2):
                c0, c1 = nh * 512, (nh + 1) * 512
                pv, kpv = py[nh], K(f"py{nh}")
                for k in range(8):
                    S.op("pe", "matmul", out=pv[:], lhsT=fT[:, k, :], rhs=wproj_sb[:, k, c0:c1],
                         start=(k == 0), stop=(k == 7), reads=[kfT, K("wproj")], writes=[kpv])
                if glu:
                    pgt, kpg = pg[nh], K(f"pg{nh}")
                    for k in range(8):
                        S.op("pe", "matmul", out=pgt[:], lhsT=fT[:, k, :], rhs=wproj_sb[:, k, D + c0:D + c1],
                             start=(k == 0), stop=(k == 7), reads=[kfT, K("wproj")], writes=[kpg])
                    S.op("act", "activation", out=hglu_sb[:], in_=pgt[:], func=AF.Sigmoid,
                         reads=[kpg], writes=[K("hglu")])
                    S.op("dve", "tensor_tensor", out=hglu_sb[:], in0=hglu_sb[:], in1=pv[:], op=ALU.mult,
                         reads=[K("hglu"), kpv], writes=[K("hglu")])
                    S.op("dve", "scalar_tensor_tensor", out=r[:, c0:c1], in0=xr[:, c0:c1], scalar=ALPHA,
                         in1=hglu_sb[:], op0=ALU.mult, op1=ALU.add, reads=[kxr, K("hglu")], writes=[kr])
                else:
                    S.op("dve", "scalar_tensor_tensor", out=r[:, c0:c1], in0=xr[:, c0:c1], scalar=ALPHA,
                         in1=pv[:], op0=ALU.mult, op1=ALU.add, reads=[kxr, kpv], writes=[kr])
            xx = x1[i2]
            layer_norm(r[:], kr, xx[:], kx1, 0, i2)
            S.op("act", "mul", out=yacc[:, tt, :], in_=xx[:], mul=ALPHA, reads=[kx1], writes=[K(f"yacc{tt}")])
            xt32 = x1T32[i2]
            for kh in range(2):
                pb, kpb = (ptr, K("ptr")) if kh == 0 else (prt, K("prt"))
                for q4 in range(4):
                    k = kh * 4 + q4
                    S.op("pe", "transpose", out=pb[:, q4 * 128:(q4 + 1) * 128], in_=xx[:, k * 128:(k + 1) * 128],
                         identity=ident[:], reads=[kx1, K("ident")], writes=[kpb])
                S.op("act", "copy", out=xt32[:, kh * 4:(kh + 1) * 4, :].rearrange("p k t -> p (k t)"), in_=pb[:],
                     reads=[kpb], writes=[K(f"x1T32_{i2}")])
                S.op("pool", "tensor_copy", out=x1T[:, kh * 4:(kh + 1) * 4, tt * 128:(tt + 1) * 128],
                     in_=xt32[:, kh * 4:(kh + 1) * 4, :], reads=[K(f"x1T32_{i2}")], writes=[K(f"x1T_{tt}")])
            if moe:
                for k in range(8):
                    S.op("pe", "matmul", out=prt[:, 0:NEXP], lhsT=xt32[:, k, :], rhs=wr_sb[:, k, :],
                         start=(k == 0), stop=(k == 7), reads=[K(f"x1T32_{i2}"), K("wr")], writes=[K("prt")])
                lg, mk, l2, ex = rt
                m1, m2, sm, rsm = rcol
                kk = lambda n: K("rt_" + n)
                S.op("dve", "tensor_tensor", out=lg[:], in0=prt[:, 0:NEXP], in1=br_sb[:], op=ALU.add,
                     reads=[K("prt"), K("br")], writes=[kk("lg")])
                S.op("dve", "reduce_max", out=m1[:], in_=lg[:], axis=AX.X, reads=[kk("lg")], writes=[kk("m1")])
                S.op("dve", "tensor_scalar", out=mk[:], in0=lg[:], scalar1=m1[:, 0:1], scalar2=-1e30,
                     op0=ALU.is_ge, op1=ALU.mult, reads=[kk("lg"), kk("m1")], writes=[kk("mk")])
                S.op("dve", "tensor_tensor", out=l2[:], in0=lg[:], in1=mk[:], op=ALU.add,
                     reads=[kk("lg"), kk("mk")], writes=[kk("l2")])
                S.op("dve", "reduce_max", out=m2[:], in_=l2[:], axis=AX.X, reads=[kk("l2")], writes=[kk("m2")])
                S.op("dve", "tensor_scalar", out=ex[:], in0=lg[:], scalar1=m1[:, 0:1], scalar2=None,
                     op0=ALU.subtract, reads=[kk("lg"), kk("m1")], writes=[kk("ex")])
                S.op("act", "activation", out=ex[:], in_=ex[:], func=AF.Exp, reads=[kk("ex")], writes=[kk("ex")])
                S.op("dve", "scalar_tensor_tensor", out=ex[:], in0=lg[:], scalar=m2[:, 0:1], in1=ex[:],
                     op0=ALU.is_ge, op1=ALU.mult, reads=[kk("lg"), kk("m2"), kk("ex")], writes=[kk("ex")])
                S.op("dve", "reduce_sum", out=sm[:], in_=ex[:], axis=AX.X, reads=[kk("ex")], writes=[kk("sm")])
                S.op("dve", "reciprocal", out=rsm[:], in_=sm[:], reads=[kk("sm")], writes=[kk("rsm")])
                S.op("dve", "tensor_scalar", out=gates[:, tt, :], in0=ex[:], scalar1=rsm[:, 0:1], scalar2=None,
                     op0=ALU.mult, reads=[kk("ex"), kk("rsm")], writes=[K(f"gates{tt}")])
        for ex_i in range(ne):
            S.dma("pool", K("w1"), w1_sb[:], w1[ex_i].rearrange("(j p) n -> p j n", p=128), writes=[K("w1")])
            S.dma("pool", K("w3"), w3_sb[:], w3[ex_i].rearrange("(j p) n -> p j n", p=128), writes=[K("w3")])
            S.dma("pool", K("w2"), w2_sb[:], w2[ex_i].rearrange("(j p) n -> p j n", p=128), writes=[K("w2")])
            for hf in range(nhalf):
                ts0, ts1 = hf * hw, (hf + 1) * hw
                tkeys = [K(f"x1T_{tt}") for tt in range(ts0 // 128, ts1 // 128)]
                for f in range(NFT):
                    b = cnt % 2
                    cnt += 1
                    f0, f1 = f * 128, (f + 1) * 128
                    for k in range(8):
                        S.op("pe", "matmul", out=pg[b][:, 0:hw], lhsT=w1_sb[:, k, f0:f1], rhs=x1T[:, k, ts0:ts1],
                             start=(k == 0), stop=(k == 7), reads=[K("w1")] + tkeys, writes=[K(f"pg{b}")])
                    for k in range(8):
                        S.op("pe", "matmul", out=pu[b][:, 0:hw], lhsT=w3_sb[:, k, f0:f1], rhs=x1T[:, k, ts0:ts1],
                             start=(k == 0), stop=(k == 7), reads=[K("w3")] + tkeys, writes=[K(f"pu{b}")])
                    S.op("act", "activation", out=sg[b][:], in_=pg[b][:, 0:hw], func=AF.Silu,
                         reads=[K(f"pg{b}")], writes=[K(f"sg{b}")])
                    S.op("dve", "tensor_tensor", out=hT[:, f, :], in0=sg[b][:], in1=pu[b][:, 0:hw], op=ALU.mult,
                         reads=[K(f"sg{b}"), K(f"pu{b}")], writes=[K(f"hT{f}")])
                hkeys = [K(f"hT{f}") for f in range(NFT)]
                for tl in range(hw // 128):
                    tt = hf * (hw // 128) + tl
                    kya = K(f"yacc{tt}")
                    for nh in range(2):
                        c0, c1 = nh * 512, (nh + 1) * 512
                        kpy = K(f"py{nh}")
                        for f in range(NFT):
                            S.op("pe", "matmul", out=py[nh][:], lhsT=hT[:, f, tl * 128:(tl + 1) * 128],
                                 rhs=w2_sb[:, f, c0:c1], start=(f == 0), stop=(f == NFT - 1),
                                 reads=hkeys + [K("w2")], writes=[kpy])
                        if moe:
                            S.op("dve", "scalar_tensor_tensor", out=yacc[:, tt, c0:c1], in0=py[nh][:],
                                 scalar=gates[:, tt, ex_i:ex_i + 1], in1=yacc[:, tt, c0:c1], op0=ALU.mult,
                                 op1=ALU.add, reads=[kpy, K(f"gates{tt}"), kya], writes=[kya])
                        else:
                            S.op("dve", "tensor_tensor", out=yacc[:, tt, c0:c1], in0=py[nh][:],
                                 in1=yacc[:, tt, c0:c1], op=ALU.add, reads=[kpy, kya], writes=[kya])
        for tt in range(ntt):
            i2 = tt % 2
            tok0 = t0 + tt * 128
            xx, kxo = x1[i2], K(f"x1_{i2}")
            layer_norm(yacc[:, tt, :], K(f"yacc{tt}"), xx[:], kxo, 2, i2)
            S.dma("sp", kxo, xout[tok0:tok0 + 128, :], xx[:], reads=[kxo])
            xt, kxt = x1T32[i2], K(f"x1T32_{i2}")
            for kh in range(2):
                pb, kpb = (ptr, K("ptr")) if kh == 0 else (prt, K("prt"))
                for q4 in range(4):
                    k = kh * 4 + q4
                    S.op("pe", "transpose", out=pb[:, q4 * 128:(q4 + 1) * 128], in_=xx[:, k * 128:(k + 1) * 128],
                         identity=ident[:], reads=[kxo, K("ident")], writes=[kpb])
                S.op("act", "copy", out=xt[:, kh * 4:(kh + 1) * 4, :].rearrange("p k t -> p (k t)"), in_=pb[:],
                     reads=[kpb], writes=[kxt])
            S.dma("sp", kxt, xoutT[:, tok0:tok0 + 128].rearrange("(j p) t -> p j t", p=128), xt[:], reads=[kxt])


def build_tp(T, glu, moe, tb=1024):
    nc = bass.Bass("TRN2", target_bir_lowering=False)
    nproj = 2 * D if glu else D
    ne = NEXP if moe else 2
    dt = lambda n, s, k="ExternalInput": nc.dram_tensor(n, s, F32, kind=k).ap()
    featT = dt("featT", [D, T]); wproj = dt("wproj", [D, nproj]); xres = dt("xres", [T, D])
    lnp = dt("lnp", [4, D]); w1 = dt("w1", [ne, D, FE]); w3 = dt("w3", [ne, D, FE]); w2 = dt("w2", [ne, FE, D])
    wr = dt("wr", [D, NEXP]); br = dt("br", [1, NEXP]); ident_d = dt("ident", [128, 128])
    xout = dt("xout", [T, D], "ExternalOutput"); xoutT = dt("xoutT", [D, T], "ExternalOutput")
    with contextlib.ExitStack() as es:
        C = Ctx(nc, es)
        tp_phase(C, T, glu, moe, featT, wproj, xres, lnp, w1, w3, w2, wr, br, ident_d, xout, xoutT, tb=tb)
        C.S.finish()
        C.S.emit()
    return nc


S5_L = 128
PI = float(np.pi)


def s5_phase(C, T, xT, w_in, lamr_d, lami_d, ldt_d, bpad_re_d, bpad_im_d, cpad_re_d, cpad_im_d, dsk_d, iota_d,
             hidT, pfx="s5"):
    S = C.S
    L = S5_L
    NB = T // 512
    NGP = 16
    P = pfx

    def K(n):
        return P + "." + n

    w_sb = C.sb([128, 8, 512], BF16, "s5w")
    xT_sb = [C.sb([128, 8, 512], BF16, "s5x") for _ in range(2)]
    u_sb = [C.sb([128, 4, 512], F32, "s5u") for _ in range(2)]
    bre = C.sb([128, NGP, 128], F32, "bre")
    bim = C.sb([128, NGP, 128], F32, "bim")
    cre = C.sb([128, NGP, 128], F32, "cre")
    cim = C.sb([128, NGP, 128], F32, "cim")
    dsk = C.sb([128, 4], F32, "dsk")
    iota = C.sb([128, L], F32, "iota")
    prm = {n: C.sb([128, NGP], F32, "p_" + n) for n in
           ("lamr", "lami", "ldt", "dt", "zr", "zi", "r", "cz", "sz", "ar", "ai", "den", "cr", "ci", "ncr",
            "t1", "t2", "zl", "er", "ei", "nei")}
    negpi = C.sb([128, 1], F32, "negpi")
    tre = C.sb([128, NGP, L], F32, "tre")
    tim = C.sb([128, NGP, L], F32, "tim")
    cph = C.sb([128, NGP, L], F32, "cph")
    sph = C.sb([128, NGP, L], F32, "sph")
    rmat = C.sb([128, NGP, L], F32, "rmat")
    ang = C.sb([128, L], F32, "ang")
    ang2 = C.sb([128, L], F32, "ang2")
    hp = C.sb([128, NGP, 2], F32, "hp")
    tmpc2 = [C.sb([128, 2], F32, "tmpc") for _ in range(2)]
    BUr = [C.sb([128, 512], F32, "BUr") for _ in range(2)]
    BUi = [C.sb([128, 512], F32, "BUi") for _ in range(2)]
    mt = [[C.sb([128, 512], F32, "mt") for _ in range(4)] for _ in range(2)]
    btr = [C.sb([128, 512], F32, "btr") for _ in range(2)]
    bti = [C.sb([128, 512], F32, "bti") for _ in range(2)]
    wre = [C.sb([128, 512], F32, "wre") for _ in range(2)]
    wim = [C.sb([128, 512], F32, "wim") for _ in range(2)]
    hre = [[C.sb([128, 512], F32, "hre") for _ in range(4)] for _ in range(2)]
    him = [[C.sb([128, 512], F32, "him") for _ in range(4)] for _ in range(2)]
    yt = [C.sb([128, 512], F32, "yt") for _ in range(2)]
    ho = [C.sb([128, 512], F32, "ho") for _ in range(2)]
    pu_ = [C.ps([128, 512], F32, "s5pu") for _ in range(2)]
    pbr = [C.ps([128, 512], F32, "s5pbr") for _ in range(2)]
    pbi = [C.ps([128, 512], F32, "s5pbi") for _ in range(2)]
    pyy = [C.ps([128, 512], F32, "s5py") for _ in range(2)]

    S.dma("pool", K("w"), w_sb[:], w_in.rearrange("(j p) n -> p j n", p=128), writes=[K("w")])
    for nm, dd, sbt in (("bre", bpad_re_d, bre), ("bim", bpad_im_d, bim), ("cre", cpad_re_d, cre), ("cim", cpad_im_d, cim)):
        S.dma("sp", K(nm), sbt[:], dd.rearrange("g k m -> k g m"), writes=[K(nm)])
    S.dma("sp", K("dsk"), dsk[:], dsk_d, writes=[K("dsk")])
    S.dma("sp", K("iota"), iota[:], iota_d, writes=[K("iota")])
    S.dma("sp", K("lamr"), prm["lamr"][:], lamr_d, writes=[K("lamr")])
    S.dma("sp", K("lami"), prm["lami"][:], lami_d, writes=[K("lami")])
    S.dma("sp", K("ldt"), prm["ldt"][:], ldt_d, writes=[K("ldt")])
    S.op("dve", "memset", ap=negpi[:], constant=-PI, writes=[K("negpi")])
    itmp = C.sb([128, L], mybir.dt.int32, "itmp")
    ftmp = C.sb([128, L], F32, "ftmp")
    S.op("dve", "memset", ap=hp[:], constant=0.0, writes=[K(f"hp{g}") for g in range(NGP)])

    def pp(n):
        return prm[n][:]

    def ew(eng, method, out_n, reads, **kw):
        S.op(eng, method, reads=[K(x) for x in reads], writes=[K(out_n)], **kw)

    def sincos(angle_ap, angle_key, sin_ap, sin_key, cos_ap, cos_key, tmp_ap, tmp_key, width):
        for off, o_ap, o_key in ((0.5, sin_ap, sin_key), (0.75, cos_ap, cos_key)):
            S.op("dve", "tensor_scalar", out=tmp_ap, in0=angle_ap, scalar1=1.0 / (2.0 * PI), scalar2=off,
                 op0=ALU.mult, op1=ALU.add, reads=[angle_key], writes=[tmp_key])
            S.op("dve", "tensor_copy", out=itmp[:, 0:width], in_=tmp_ap, reads=[tmp_key], writes=[K("itmp")])
            S.op("dve", "tensor_copy", out=ftmp[:, 0:width], in_=itmp[:, 0:width], reads=[K("itmp")], writes=[K("ftmp")])
            S.op("dve", "tensor_tensor", out=tmp_ap, in0=tmp_ap, in1=ftmp[:, 0:width], op=ALU.subtract,
                 reads=[tmp_key, K("ftmp")], writes=[tmp_key])
            S.op("dve", "scalar_tensor_tensor", out=tmp_ap, in0=tmp_ap, scalar=0.0, in1=tmp_ap, op0=ALU.is_lt,
                 op1=ALU.add, reads=[tmp_key], writes=[tmp_key])
            S.op("act", "activation", out=o_ap, in_=tmp_ap, func=AF.Sin, bias=negpi[:, 0:1], scale=2.0 * PI,
                 reads=[tmp_key, K("negpi")], writes=[o_key])

    ew("act", "activation", "dt", ["ldt"], out=pp("dt"), in_=pp("ldt"), func=AF.Exp)
    ew("dve", "tensor_tensor", "zr", ["lamr", "dt"], out=pp("zr"), in0=pp("lamr"), in1=pp("dt"), op=ALU.mult)
    ew("dve", "tensor_tensor", "zi", ["lami", "dt"], out=pp("zi"), in0=pp("lami"), in1=pp("dt"), op=ALU.mult)
    ew("act", "activation", "r", ["zr"], out=pp("r"), in_=pp("zr"), func=AF.Exp)
    sincos(pp("zi"), K("zi"), pp("sz"), K("sz"), pp("cz"), K("cz"), pp("t1"), K("t1"), NGP)
    ew("dve", "tensor_tensor", "ar", ["r", "cz"], out=pp("ar"), in0=pp("r"), in1=pp("cz"), op=ALU.mult)
    ew("dve", "tensor_scalar", "ar", ["ar"], out=pp("ar"), in0=pp("ar"), scalar1=-1.0, scalar2=None, op0=ALU.add)
    ew("dve", "tensor_tensor", "ai", ["r", "sz"], out=pp("ai"), in0=pp("r"), in1=pp("sz"), op=ALU.mult)
    ew("dve", "tensor_tensor", "den", ["lamr"], out=pp("den"), in0=pp("lamr"), in1=pp("lamr"), op=ALU.mult)
    ew("dve", "tensor_tensor", "t1", ["lami"], out=pp("t1"), in0=pp("lami"), in1=pp("lami"), op=ALU.mult)
    ew("dve", "tensor_tensor", "den", ["den", "t1"], out=pp("den"), in0=pp("den"), in1=pp("t1"), op=ALU.add)
    ew("dve", "reciprocal", "den", ["den"], out=pp("den"), in_=pp("den"))
    ew("dve", "tensor_tensor", "t1", ["ar", "lamr"], out=pp("t1"), in0=pp("ar"), in1=pp("lamr"), op=ALU.mult)
    ew("dve", "tensor_tensor", "t2", ["ai", "lami"], out=pp("t2"), in0=pp("ai"), in1=pp("lami"), op=ALU.mult)
    ew("dve", "tensor_tensor", "cr", ["t1", "t2"], out=pp("cr"), in0=pp("t1"), in1=pp("t2"), op=ALU.add)
    ew("dve", "tensor_tensor", "cr", ["cr", "den"], out=pp("cr"), in0=pp("cr"), in1=pp("den"), op=ALU.mult)
    ew("dve", "tensor_tensor", "t1", ["ai", "lamr"], out=pp("t1"), in0=pp("ai"), in1=pp("lamr"), op=ALU.mult)
    ew("dve", "tensor_tensor", "t2", ["ar", "lami"], out=pp("t2"), in0=pp("ar"), in1=pp("lami"), op=ALU.mult)
    ew("dve", "tensor_tensor", "ci", ["t1", "t2"], out=pp("ci"), in0=pp("t1"), in1=pp("t2"), op=ALU.subtract)
    ew("dve", "tensor_tensor", "ci", ["ci", "den"], out=pp("ci"), in0=pp("ci"), in1=pp("den"), op=ALU.mult)
    ew("dve", "tensor_scalar", "ncr", ["cr"], out=pp("ncr"), in0=pp("cr"), scalar1=-1.0, scalar2=None, op0=ALU.mult)
    ew("dve", "tensor_scalar", "zl", ["zi"], out=pp("zl"), in0=pp("zi"), scalar1=float(L), scalar2=None, op0=ALU.mult)
    sincos(pp("zl"), K("zl"), pp("ei"), K("ei"), pp("er"), K("er"), pp("t1"), K("t1"), NGP)
    ew("dve", "tensor_scalar", "nei", ["ei"], out=pp("nei"), in0=pp("ei"), scalar1=-1.0, scalar2=None, op0=ALU.mult)
    for g in range(NGP):
        S.op("dve", "tensor_scalar", out=ang[:], in0=iota[:], scalar1=prm["zi"][:, g:g + 1], scalar2=None,
             op0=ALU.mult, reads=[K("iota"), K("zi")], writes=[K("ang")])
        sincos(ang[:], K("ang"), sph[:, g, :], K("sph"), cph[:, g, :], K("cph"), ang2[:], K("ang2"), L)
        S.op("dve", "tensor_scalar", out=ang2[:], in0=cph[:, g, :], scalar1=prm["cr"][:, g:g + 1], scalar2=None,
             op0=ALU.mult, reads=[K("cph"), K("cr")], writes=[K("ang2")])
        S.op("dve", "scalar_tensor_tensor", out=tre[:, g, :], in0=sph[:, g, :], scalar=prm["ci"][:, g:g + 1],
             in1=ang2[:], op0=ALU.mult, op1=ALU.add, reads=[K("sph"), K("ci"), K("ang2")], writes=[K("tre")])
        S.op("dve", "tensor_scalar", out=ang2[:], in0=cph[:, g, :], scalar1=prm["ci"][:, g:g + 1], scalar2=None,
             op0=ALU.mult, reads=[K("cph"), K("ci")], writes=[K("ang2")])
        S.op("dve", "scalar_tensor_tensor", out=tim[:, g, :], in0=sph[:, g, :], scalar=prm["ncr"][:, g:g + 1],
             in1=ang2[:], op0=ALU.mult, op1=ALU.add, reads=[K("sph"), K("ncr"), K("ang2")], writes=[K("tim")])
        S.op("pool", "memset", ap=rmat[:, g, :], constant=1.0, writes=[K("rmat")])
        S.op("dve", "tensor_scalar", out=rmat[:, g, :], in0=rmat[:, g, :], scalar1=prm["r"][:, g:g + 1], scalar2=None,
             op0=ALU.mult, reads=[K("rmat"), K("r")], writes=[K("rmat")])

    NC4 = 512 // L

    def v3(t):
        return t[:].rearrange("p (c l) -> p c l", l=L)

    def tb3(tab, g):
        return tab[:, g:g + 1, :].broadcast_to([128, NC4, L])

    gi = 0
    for blk in range(NB):
        t0 = blk * 512
        bi = blk % 2
        xs, us = xT_sb[bi], u_sb[bi]
        kx, ku = K(f"x{bi}"), K(f"u{bi}")
        S.dma("pool", kx, xs[:], xT[:, t0:t0 + 512].rearrange("(j p) t -> p j t", p=128), writes=[kx])
        for ft in range(4):
            b2 = ft % 2
            for k in range(8):
                S.op("pe", "matmul", out=pu_[b2][:], lhsT=w_sb[:, k, ft * 128:(ft + 1) * 128], rhs=xs[:, k, :],
                     start=(k == 0), stop=(k == 7), reads=[K("w"), kx], writes=[K(f"pu{b2}")])
            S.op("act", "copy", out=us[:, ft, :], in_=pu_[b2][:], reads=[K(f"pu{b2}")], writes=[ku + f"_{ft}"])
        for ft in range(4):
            f2 = ft % 2
            for gpair in range(0, 4, 2):
                recs = []
                for gl4 in (gpair, gpair + 1):
                    rec = []
                    real_op = S.op
                    S.op = (lambda eng, method, reads=(), writes=(), _rec=rec, **kw:
                            _rec.append((eng, method, reads, writes, kw)))
                    try:
                        g = ft * 4 + gl4
                        b2 = gi % 2
                        gi += 1
                        kb = lambda n: K(f"{n}{b2}")
                        tmpc = tmpc2[b2]
                        S.op("pe", "matmul", out=pbr[b2][:], lhsT=bre[:, g, :], rhs=us[:, ft, :], start=True, stop=True,
                             reads=[K("bre"), ku + f"_{ft}"], writes=[kb("pbr")])
                        S.op("pe", "matmul", out=pbi[b2][:], lhsT=bim[:, g, :], rhs=us[:, ft, :], start=True, stop=True,
                             reads=[K("bim"), ku + f"_{ft}"], writes=[kb("pbi")])
                        S.op("act", "copy", out=BUr[b2][:], in_=pbr[b2][:], reads=[kb("pbr")], writes=[kb("BUr")])
                        S.op("act", "copy", out=BUi[b2][:], in_=pbi[b2][:], reads=[kb("pbi")], writes=[kb("BUi")])
                        m0, m1, m2, m3 = mt[b2]
                        S.op("dve", "tensor_tensor", out=v3(m0), in0=v3(BUr[b2]), in1=tb3(tre, g), op=ALU.mult,
                             reads=[kb("BUr"), K("tre")], writes=[kb("m0")])
                        S.op("pool", "tensor_tensor", out=v3(m1), in0=v3(BUi[b2]), in1=tb3(tim, g), op=ALU.mult,
                             reads=[kb("BUi"), K("tim")], writes=[kb("m1")])
                        S.op("dve", "tensor_tensor", out=btr[b2][:], in0=m0[:], in1=m1[:], op=ALU.subtract,
                             reads=[kb("m0"), kb("m1")], writes=[kb("btr")])
                        S.op("pool", "tensor_tensor", out=v3(m2), in0=v3(BUi[b2]), in1=tb3(tre, g), op=ALU.mult,
                             reads=[kb("BUi"), K("tre")], writes=[kb("m2")])
                        S.op("dve", "tensor_tensor", out=v3(m3), in0=v3(BUr[b2]), in1=tb3(tim, g), op=ALU.mult,
                             reads=[kb("BUr"), K("tim")], writes=[kb("m3")])
                        S.op("pool", "tensor_tensor", out=bti[b2][:], in0=m2[:], in1=m3[:], op=ALU.add,
                             reads=[kb("m2"), kb("m3")], writes=[kb("bti")])
                        khp = K(f"hp{g}")
                        for c in range(NC4):
                            cs = slice(c * L, (c + 1) * L)
                            S.op("dve", "tensor_tensor_scan", out=wre[b2][:, cs], data0=rmat[:, g, :], data1=btr[b2][:, cs],
                                 initial=hp[:, g, 0:1], op0=ALU.mult, op1=ALU.add,
                                 reads=[K("rmat"), kb("btr"), khp], writes=[kb("wre")])
                            S.op("dve", "tensor_tensor_scan", out=wim[b2][:, cs], data0=rmat[:, g, :], data1=bti[b2][:, cs],
                                 initial=hp[:, g, 1:2], op0=ALU.mult, op1=ALU.add,
                                 reads=[K("rmat"), kb("bti"), khp], writes=[kb("wim")])
                            last = (c + 1) * L - 1
                            S.op("dve", "tensor_scalar", out=tmpc[:, 0:1], in0=wre[b2][:, last:last + 1],
                                 scalar1=prm["er"][:, g:g + 1], scalar2=None, op0=ALU.mult,
                                 reads=[kb("wre"), K("er")], writes=[kb("tmpc0")])
                            S.op("dve", "tensor_scalar", out=tmpc[:, 1:2], in0=wre[b2][:, last:last + 1],
                                 scalar1=prm["ei"][:, g:g + 1], scalar2=None, op0=ALU.mult,
                                 reads=[kb("wre"), K("ei")], writes=[kb("tmpc1")])
                            S.op("dve", "scalar_tensor_tensor", out=hp[:, g, 0:1], in0=wim[b2][:, last:last + 1],
                                 scalar=prm["nei"][:, g:g + 1], in1=tmpc[:, 0:1], op0=ALU.mult, op1=ALU.add,
                                 reads=[kb("wim"), K("nei"), kb("tmpc0")], writes=[khp])
                            S.op("dve", "scalar_tensor_tensor", out=hp[:, g, 1:2], in0=wim[b2][:, last:last + 1],
                                 scalar=prm["er"][:, g:g + 1], in1=tmpc[:, 1:2], op0=ALU.mult, op1=ALU.add,
                                 reads=[kb("wim"), K("er"), kb("tmpc1")], writes=[khp])
                        hr, hi_ = hre[f2][gl4], him[f2][gl4]
                        khr, khi = K(f"hre{f2}{gl4}"), K(f"him{f2}{gl4}")
                        S.op("pool", "tensor_tensor", out=v3(m0), in0=v3(wre[b2]), in1=tb3(cph, g), op=ALU.mult,
                             reads=[kb("wre"), K("cph")], writes=[kb("m0")])
                        S.op("pool", "tensor_tensor", out=v3(m1), in0=v3(wim[b2]), in1=tb3(sph, g), op=ALU.mult,
                             reads=[kb("wim"), K("sph")], writes=[kb("m1")])
                        S.op("pool", "tensor_tensor", out=hr[:], in0=m0[:], in1=m1[:], op=ALU.subtract,
                             reads=[kb("m0"), kb("m1")], writes=[khr])
                        S.op("dve", "tensor_tensor", out=v3(m2), in0=v3(wre[b2]), in1=tb3(sph, g), op=ALU.mult,
                             reads=[kb("wre"), K("sph")], writes=[kb("m2")])
                        S.op("pool", "tensor_tensor", out=v3(m3), in0=v3(wim[b2]), in1=tb3(cph, g), op=ALU.mult,
                             reads=[kb("wim"), K("cph")], writes=[kb("m3")])
                        S.op("dve", "scalar_tensor_tensor", out=hi_[:], in0=m2[:], scalar=-1.0, in1=m3[:], op0=ALU.mult,
                             op1=ALU.subtract, reads=[kb("m2"), kb("m3")], writes=[khi])
                    finally:
                        S.op = real_op
                    recs.append(rec)
                for q in range(max(len(recs[0]), len(recs[1]))):
                    for rr in recs:
                        if q < len(rr):
                            eng_, method_, reads_, writes_, kw_ = rr[q]
                            S.op(eng_, method_, reads=reads_, writes=writes_, **kw_)
            for gl4 in range(4):
                g = ft * 4 + gl4
                S.op("pe", "matmul", out=pyy[f2][:], lhsT=cre[:, g, :], rhs=hre[f2][gl4][:], start=(gl4 == 0), stop=False,
                     reads=[K("cre"), K(f"hre{f2}{gl4}")], writes=[K(f"pyy{f2}")])
                S.op("pe", "matmul", out=pyy[f2][:], lhsT=cim[:, g, :], rhs=him[f2][gl4][:], start=False, stop=(gl4 == 3),
                     reads=[K("cim"), K(f"him{f2}{gl4}")], writes=[K(f"pyy{f2}")])
            S.op("dve", "scalar_tensor_tensor", out=yt[f2][:], in0=us[:, ft, :], scalar=dsk[:, ft:ft + 1], in1=pyy[f2][:],
                 op0=ALU.mult, op1=ALU.add, reads=[ku + f"_{ft}", K("dsk"), K(f"pyy{f2}")], writes=[K(f"yt{f2}")])
            S.op("act", "activation", out=ho[f2][:], in_=yt[f2][:], func=AF.Gelu, reads=[K(f"yt{f2}")],
                 writes=[K(f"ho{f2}")])
            S.dma("sp", K(f"ho{f2}"), hidT[ft * 128:(ft + 1) * 128, t0:t0 + 512], ho[f2][:], reads=[K(f"ho{f2}")])


def build_s5(T):
    nc = bass.Bass("TRN2", target_bir_lowering=False)
    dt = lambda n, s, k="ExternalInput": nc.dram_tensor(n, s, F32, kind=k).ap()
    xT = dt("xT", [D, T]); w_in = dt("w_in", [D, 512])
    lamr = dt("lamr", [128, 16]); lami = dt("lami", [128, 16]); ldt = dt("ldt", [128, 16])
    bre = dt("bpad_re", [16, 128, 128]); bim = dt("bpad_im", [16, 128, 128])
    cre = dt("cpad_re", [16, 128, 128]); cim = dt("cpad_im", [16, 128, 128])
    dsk = dt("dsk", [128, 4]); iota = dt("iota", [128, S5_L])
    hidT = dt("hidT", [512, T], "ExternalOutput")
    with contextlib.ExitStack() as es:
        C = Ctx(nc, es)
        s5_phase(C, T, xT, w_in, lamr, lami, ldt, bre, bim, cre, cim, dsk, iota, hidT)
        C.S.finish()
        C.S.emit()
    return nc


def s5_host_layout(ghalf, s5_lam_re, s5_lam_im, s5_log_dt, s5_b_re, s5_b_im, s5_c_re, s5_c_im, s5_d, s5_w_in):
    g0 = 32 * ghalf
    lamr = np.zeros((128, 16), np.float32); lami = np.zeros((128, 16), np.float32); ldt = np.zeros((128, 16), np.float32)
    bre = np.zeros((16, 128, 128), np.float32); bim = np.zeros((16, 128, 128), np.float32)
    cre = np.zeros((16, 128, 128), np.float32); cim = np.zeros((16, 128, 128), np.float32)
    for gp in range(16):
        for gl in range(2):
            g = g0 + 2 * gp + gl
            lamr[gl * 64:(gl + 1) * 64, gp] = s5_lam_re[g]
            lami[gl * 64:(gl + 1) * 64, gp] = s5_lam_im[g]
            ldt[gl * 64:(gl + 1) * 64, gp] = s5_log_dt[g]
            r0 = 32 * (gp % 4) + 16 * gl
            bre[gp, r0:r0 + 16, gl * 64:(gl + 1) * 64] = s5_b_re[g].T
            bim[gp, r0:r0 + 16, gl * 64:(gl + 1) * 64] = s5_b_im[g].T
            cre[gp, gl * 64:(gl + 1) * 64, r0:r0 + 16] = s5_c_re[g].T
            cim[gp, gl * 64:(gl + 1) * 64, r0:r0 + 16] = s5_c_im[g].T
    dsk = np.ascontiguousarray(s5_d[512 * ghalf:512 * (ghalf + 1)].reshape(4, 128).T)
    iota = np.tile(np.arange(1, S5_L + 1, dtype=np.float32)[None, :], (128, 1))
    w = np.ascontiguousarray(s5_w_in[:, 512 * ghalf:512 * (ghalf + 1)])
    return dict(w_in=w, lamr=lamr, lami=lami, ldt=ldt, bpad_re=bre, bpad_im=bim, cpad_re=cre, cpad_im=cim,
                dsk=dsk, iota=iota)


GDN_LV = 99
GDN_CLAMP = False


def gdn_phase(C, T, xT, wq_d, wk_d, wv_d, wz_d, wba_d, convw_d, hp_d, normg_d, gconst_d, ogT, pfx="gd"):
    S = C.S
    NB = T // 512
    P = pfx
    NH = 4

    def K(n):
        return P + "." + n

    wq = C.sb([128, 8, 512], BF16, "wq"); wk = C.sb([128, 8, 512], BF16, "wk")
    wv = C.sb([128, 8, 512], BF16, "wv"); wz = C.sb([128, 8, 512], BF16, "wz")
    wba = C.sb([128, 8, 8], F32, "wba")
    convw = C.sb([128, 3, NH, 4], F32, "convw")
    hpar = C.sb([128, 8], F32, "hpar")
    nea = C.sb([128, NH], F32, "nea")
    normg = C.sb([128, 128], F32, "normg")
    cst = C.sb([128, 6, 128], F32, "gcst")
    ident, MU, MS, NEGL, NEGUT, ONES = (cst[:, i, :] for i in range(6))
    onec = C.sb([128, 1], F32, "onec"); eps6 = C.sb([128, 1], F32, "eps6")
    xs = [C.sb([128, 8, 512], BF16, "gxs") for _ in range(2)]
    xs32 = C.sb([128, 8, 512], F32, "gxs32")
    xb = [[C.sb([128, 515], F32, "xb") for _ in range(NH)] for _ in range(3)]
    qkvc = [[C.sb([128, 512], F32, "qkvc") for _ in range(NH)] for _ in range(3)]
    cacc = [C.sb([128, 512], F32, "cacc") for _ in range(2)]
    zs = C.sb([128, 512], F32, "zs")
    ba = C.sb([128, 8], F32, "ba")
    gt = {n: C.sb([128, NH], F32, "g_" + n) for n in
          ("beta", "nbeta", "x", "e", "sp", "g", "eg", "kes", "elast", "bks")}
    gc8 = C.sb([128, 8], F32, "gc8")
    Sst = [C.sb([128, 128], F32, "Sst") for _ in range(NH)]
    ogT_sb = C.sb([128, NH, 512], F32, "ogT")

    def mk(n):
        return [C.sb([128, 128], F32, n) for _ in range(2)]

    Kn, KnT, Kbg, Kend, Vb, sq, G1, Dl, DTu, AT, PT, nWT, Vnew, o1, o_, og = (mk(n) for n in (
        "Kn", "KnT", "Kbg", "Kend", "Vb", "sq", "G1", "Dl", "DTu", "AT", "PT", "nWT", "Vnew", "o1", "o", "og"))
    Nn = [mk("Na"), mk("Nb")]
    NTn = [mk("NTa"), mk("NTb")]
    junk = mk("junk")
    Qc = mk("Qc")
    col = {n: [C.sb([128, 1], F32, "c_" + n) for _ in range(2)] for n in
           ("ssqk", "rk", "ssqq", "rq", "rq2", "ssqo", "v1", "fac")}
    bank = [C.ps([128, 512], F32, f"gb{i}") for i in range(8)]
    BK = [K(f"bank{i}") for i in range(8)]

    for nm, dd, sbt in (("wq", wq_d, wq), ("wk", wk_d, wk), ("wv", wv_d, wv), ("wz", wz_d, wz)):
        S.dma("pool", K(nm), sbt[:], dd.rearrange("(j p) n -> p j n", p=128), writes=[K(nm)])
    S.dma("sp", K("wba"), wba[:], wba_d.rearrange("(j p) n -> p j n", p=128), writes=[K("wba")])
    S.dma("sp", K("convw"), convw[:], convw_d, writes=[K("convw")])
    S.dma("sp", K("hpar"), hpar[:], hp_d, writes=[K("hpar")])
    S.dma("sp", K("normg"), normg[:], normg_d, writes=[K("normg")])
    S.dma("sp", K("cst"), cst[:], gconst_d.rearrange("i p f -> p i f"), writes=[K("cst")])
    S.op("dve", "memset", ap=onec[:], constant=1.0, writes=[K("onec")])
    S.op("dve", "memset", ap=eps6[:], constant=NORM_EPS, writes=[K("eps6")])
    for h in range(NH):
        S.op("dve", "memset", ap=Sst[h][:], constant=0.0, writes=[K(f"S{h}")])
        for i in range(3):
            S.op("pool", "memset", ap=xb[i][h][:, 0:3], constant=0.0, writes=[K(f"xb{i}{h}")])
    S.op("act", "activation", out=nea[:], in_=hpar[:, 0:4], func=AF.Exp, reads=[K("hpar")], writes=[K("nea")])
    S.op("dve", "tensor_scalar", out=nea[:], in0=nea[:], scalar1=-1.0, scalar2=None, op0=ALU.mult,
         reads=[K("nea")], writes=[K("nea")])

    def mm(bi, c0, c1, lhsT, rhs, reads, start=True, stop=True, rows=128):
        S.op("pe", "matmul", out=bank[bi][0:rows, c0:c1], lhsT=lhsT, rhs=rhs, start=start, stop=stop,
             reads=reads, writes=[BK[bi]])

    def tr(bi, c0, in_, reads):
        S.op("pe", "transpose", out=bank[bi][:, c0:c0 + 128], in_=in_, identity=ident,
             reads=reads + [K("cst")], writes=[BK[bi]])

    it = 0
    for blk in range(NB):
        t0 = blk * 512
        bi2 = blk % 2
        xsb, kx = xs[bi2], K(f"xs{bi2}")
        S.dma("pool", kx, xsb[:], xT[:, t0:t0 + 512].rearrange("(j p) t -> p j t", p=128), writes=[kx])
        S.dma("sp", K("xs32"), xs32[:], xT[:, t0:t0 + 512].rearrange("(j p) t -> p j t", p=128), writes=[K("xs32")])
        ci = 0
        for i, wsb, wkey in ((0, wq, "wq"), (1, wk, "wk"), (2, wv, "wv")):
            for h in range(NH):
                for k in range(8):
                    mm(0, 0, 512, wsb[:, k, h * 128:(h + 1) * 128], xsb[:, k, :], [K(wkey), kx], start=(k == 0),
                       stop=(k == 7))
                xbt, kxb = xb[i][h], K(f"xb{i}{h}")
                S.op("act", "copy", out=xbt[:, 3:515], in_=bank[0][:], reads=[BK[0]], writes=[kxb])
                ca, kca = cacc[ci % 2], K(f"cacc{ci % 2}")
                ci += 1
                eng = "dve"
                S.op(eng, "tensor_scalar", out=ca[:], in0=xbt[:, 0:512], scalar1=convw[:, i, h, 0:1], scalar2=None,
                     op0=ALU.mult, reads=[kxb, K("convw")], writes=[kca])
                for j in range(1, 4):
                    S.op(eng, "scalar_tensor_tensor", out=ca[:], in0=xbt[:, j:j + 512], scalar=convw[:, i, h, j:j + 1],
                         in1=ca[:], op0=ALU.mult, op1=ALU.add, reads=[kxb, K("convw"), kca], writes=[kca])
                S.op("pool", "tensor_copy", out=xbt[:, 0:3], in_=xbt[:, 512:515], reads=[kxb], writes=[kxb])
                S.op("act", "activation", out=qkvc[i][h][:], in_=ca[:], func=AF.Silu, reads=[kca],
                     writes=[K(f"qkvc{i}{h}")])
        for tl in range(4):
            tsl = slice(tl * 128, (tl + 1) * 128)
            if GDN_LV < 2:
                continue
            for k in range(8):
                mm(1, 0, 512, xsb[:, k, tsl], wz[:, k, :], [kx, K("wz")], start=(k == 0), stop=(k == 7))
            S.op("act", "activation", out=zs[:], in_=bank[1][:], func=AF.Silu, reads=[BK[1]], writes=[K("zs")])
            for k in range(8):
                mm(1, 0, 8, xs32[:, k, tsl], wba[:, k, :], [K("xs32"), K("wba")], start=(k == 0), stop=(k == 7))
            S.op("dve", "tensor_copy", out=ba[:], in_=bank[1][:, 0:8], reads=[BK[1]], writes=[K("ba")])
            G = lambda n: gt[n][:]
            kg = lambda n: K("g_" + n)
            S.op("act", "activation", out=G("beta"), in_=ba[:, 0:4], func=AF.Sigmoid, reads=[K("ba")],
                 writes=[kg("beta")])
            S.op("dve", "tensor_scalar", out=G("nbeta"), in0=G("beta"), scalar1=-1.0, scalar2=None, op0=ALU.mult,
                 reads=[kg("beta")], writes=[kg("nbeta")])
            S.op("dve", "tensor_tensor", out=G("x"), in0=ba[:, 4:8], in1=hpar[:, 4:8], op=ALU.add,
                 reads=[K("ba"), K("hpar")], writes=[kg("x")])
            S.op("act", "activation", out=G("e"), in_=G("x"), func=AF.Exp, reads=[kg("x")], writes=[kg("e")])
            S.op("act", "activation", out=G("sp"), in_=G("e"), func=AF.Ln, bias=onec[:, 0:1], scale=1.0,
                 reads=[kg("e"), K("onec")], writes=[kg("sp")])
            S.op("dve", "tensor_tensor", out=G("g"), in0=G("sp"), in1=nea[:], op=ALU.mult,
                 reads=[kg("sp"), K("nea")], writes=[kg("g")])
            mm(1, 0, 4, MU, G("g"), [K("cst"), kg("g")])
            mm(1, 4, 8, ONES, G("g"), [K("cst"), kg("g")])
            S.op("dve", "tensor_copy", out=gc8[:], in_=bank[1][:, 0:8], reads=[BK[1]], writes=[K("gc8")])
            S.op("act", "activation", out=G("eg"), in_=gc8[:, 0:4], func=AF.Exp, reads=[K("gc8")], writes=[kg("eg")])
            S.op("act", "activation", out=G("elast"), in_=gc8[:, 4:8], func=AF.Exp, reads=[K("gc8")],
                 writes=[kg("elast")])
            S.op("dve", "tensor_tensor", out=G("kes"), in0=gc8[:, 4:8], in1=gc8[:, 0:4], op=ALU.subtract,
                 reads=[K("gc8")], writes=[kg("kes")])
            S.op("act", "activation", out=G("kes"), in_=G("kes"), func=AF.Exp, reads=[kg("kes")], writes=[kg("kes")])
            S.op("dve", "tensor_tensor", out=G("bks"), in0=G("beta"), in1=G("eg"), op=ALU.mult,
                 reads=[kg("beta"), kg("eg")], writes=[kg("bks")])
            if GDN_LV < 2.5:
                continue
            for hpair in range(0, NH, 2):
                recs = []
                for h in (hpair, hpair + 1):
                    rec = []
                    real_op = S.op
                    S.op = (lambda eng, method, reads=(), writes=(), _rec=rec, **kw:
                            _rec.append((eng, method, reads, writes, kw)))
                    try:
                        p2 = it % 2
                        bA, bB, bC = 2 + 3 * p2, 3 + 3 * p2, 4 + 3 * p2
                        it += 1
                        T_ = lambda lst: lst[p2][:]
                        kt = lambda n: K(f"{n}{p2}")
                        cl = lambda n: col[n][p2][:]
                        QT = qkvc[0][h][:, tsl]
                        KTr = qkvc[1][h][:, tsl]
                        VTr = qkvc[2][h][:, tsl]
                        kq, kk_, kv = K(f"qkvc0{h}"), K(f"qkvc1{h}"), K(f"qkvc2{h}")
                        hs = slice(h, h + 1)
                        S.op("pool", "tensor_copy", out=T_(Qc), in_=QT, reads=[kq], writes=[kt("Qc")])
                        tr(bA, 0, KTr, [kk_])
                        S.op("act", "activation", out=T_(junk), in_=bank[bA][:, 0:128], func=AF.Square,
                             reads=[BK[bA]], writes=[kt("junk")])
                        S.op("dve", "reduce_sum", out=cl("ssqk"), in_=T_(junk), axis=AX.X, reads=[kt("junk")],
                             writes=[kt("ssqk")])
                        S.op("act", "activation", out=cl("rk"), in_=cl("ssqk"), func=AF.Sqrt, bias=eps6[:, 0:1], scale=1.0,
                             reads=[kt("ssqk"), K("eps6")], writes=[kt("rk")])
                        S.op("dve", "reciprocal", out=cl("rk"), in_=cl("rk"), reads=[kt("rk")], writes=[kt("rk")])
                        S.op("dve", "tensor_scalar", out=T_(Kn), in0=bank[bA][:, 0:128], scalar1=cl("rk"), scalar2=None,
                             op0=ALU.mult, reads=[BK[bA], kt("rk")], writes=[kt("Kn")])
                        tr(bA, 128, T_(Kn), [kt("Kn")])
                        S.op("act", "copy", out=T_(KnT), in_=bank[bA][:, 128:256], reads=[BK[bA]], writes=[kt("KnT")])
                        S.op("dve", "tensor_scalar", out=T_(Kbg), in0=T_(Kn), scalar1=gt["bks"][:, hs], scalar2=None, op0=ALU.mult,
                             reads=[kt("Kn"), kg("bks")], writes=[kt("Kbg")])
                        S.op("dve", "tensor_scalar", out=T_(Kend), in0=T_(Kn), scalar1=gt["kes"][:, hs], scalar2=None, op0=ALU.mult,
                             reads=[kt("Kn"), kg("kes")], writes=[kt("Kend")])
                        if GDN_LV < 3:
                            continue
                        tr(bA, 256, VTr, [kv])
                        S.op("dve", "tensor_scalar", out=T_(Vb), in0=bank[bA][:, 256:384], scalar1=gt["beta"][:, hs],
                             scalar2=None, op0=ALU.mult, reads=[BK[bA], kg("beta")], writes=[kt("Vb")])
                        if GDN_LV < 3.5:
                            continue
                        tr(bA, 384, QT, [kq])
                        S.op("act", "activation", out=T_(sq), in_=bank[bA][:, 384:512], func=AF.Square, reads=[BK[bA]],
                             writes=[kt("sq")])
                        S.op("dve", "reduce_sum", out=cl("ssqq"), in_=T_(sq), axis=AX.X, reads=[kt("sq")], writes=[kt("ssqq")])
                        S.op("act", "activation", out=cl("rq"), in_=cl("ssqq"), func=AF.Sqrt, bias=eps6[:, 0:1],
                             scale=1.0, reads=[kt("ssqq"), K("eps6")], writes=[kt("rq")])
                        S.op("dve", "reciprocal", out=cl("rq"), in_=cl("rq"), reads=[kt("rq")], writes=[kt("rq")])
                        S.op("dve", "tensor_scalar", out=cl("rq"), in0=cl("rq"), scalar1=128.0 ** -0.5, scalar2=None,
                             op0=ALU.mult, reads=[kt("rq")], writes=[kt("rq")])
                        S.op("dve", "scalar_tensor_tensor", out=cl("rq2"), in0=cl("rq"), scalar=1.0 / 128.0, in1=cl("rq"),
                             op0=ALU.mult, op1=ALU.mult, reads=[kt("rq")], writes=[kt("rq2")])
                        if GDN_LV < 4:
                            continue
                        S.op("dve", "tensor_scalar", out=T_(G1), in0=MU, scalar1=gt["g"][:, hs], scalar2=None, op0=ALU.mult,
                             reads=[K("cst"), kg("g")], writes=[kt("G1")])
                        mm(bB, 0, 128, T_(G1), MS, [kt("G1"), K("cst")], start=True, stop=False)
                        mm(bB, 0, 128, ident, NEGL, [K("cst")], start=False, stop=True)
                        mm(bB, 128, 256, MS, T_(G1), [kt("G1"), K("cst")], start=True, stop=False)
                        mm(bB, 128, 256, ident, NEGUT, [K("cst")], start=False, stop=True)
                        S.op("act", "activation", out=T_(Dl), in_=bank[bB][:, 0:128], func=AF.Exp, reads=[BK[bB]],
                             writes=[kt("Dl")])
                        S.op("act", "activation", out=T_(DTu), in_=bank[bB][:, 128:256], func=AF.Exp, reads=[BK[bB]],
                             writes=[kt("DTu")])
                        if GDN_LV < 5:
                            continue
                        mm(bB, 256, 384, T_(KnT), T_(KnT), [kt("KnT")])
                        mm(bB, 384, 512, T_(KnT), QT, [kt("KnT"), kq])
                        N0, NT0 = Nn[0][p2], NTn[0][p2]
                        S.op("dve", "scalar_tensor_tensor", out=N0[:], in0=bank[bB][:, 256:384], scalar=gt["nbeta"][:, hs],
                             in1=T_(Dl), op0=ALU.mult, op1=ALU.mult, reads=[BK[bB], kg("nbeta"), kt("Dl")], writes=[kt("N0")])
                        S.op("dve", "tensor_tensor", out=T_(AT), in0=bank[bB][:, 384:512], in1=T_(DTu), op=ALU.mult,
                             reads=[BK[bB], kt("DTu")], writes=[kt("AT")])
                        tr(bC, 0, N0[:], [kt("N0")])
                        S.op("act", "copy", out=NT0[:], in_=bank[bC][:, 0:128], reads=[BK[bC]], writes=[kt("NT0")])
                        S.op("pool", "tensor_tensor", out=T_(PT), in0=NT0[:], in1=ident, op=ALU.add,
                             reads=[kt("NT0"), K("cst")], writes=[kt("PT")])
                        if GDN_LV < 6:
                            continue
                        for j in range(1, 7):
                            a, b = (j - 1) % 2, j % 2
                            Na, NTa, Nb, NTb = Nn[a][p2], NTn[a][p2], Nn[b][p2], NTn[b][p2]
                            mm(bC, 128, 256, NTa[:], Na[:], [kt(f"N{a}"), kt(f"NT{a}")])
                            if j < 6:
                                mm(bC, 256, 384, Na[:], NTa[:], [kt(f"N{a}"), kt(f"NT{a}")])
                            S.op("act", "copy", out=Nb[:], in_=bank[bC][:, 128:256], reads=[BK[bC]], writes=[kt(f"N{b}")])
                            if j < 6:
                                S.op("dve", "tensor_copy", out=NTb[:], in_=bank[bC][:, 256:384], reads=[BK[bC]],
                                     writes=[kt(f"NT{b}")])
                            mm(bC, 384, 512, Nb[:], T_(PT), [kt(f"N{b}"), kt("PT")])
                            S.op("dve", "tensor_tensor", out=T_(PT), in0=bank[bC][:, 384:512], in1=T_(PT), op=ALU.add,
                                 reads=[BK[bC], kt("PT")], writes=[kt("PT")])
                        if GDN_LV < 7:
                            continue
                        mm(bC, 0, 128, T_(Kbg), T_(PT), [kt("Kbg"), kt("PT")])
                        S.op("act", "mul", out=T_(nWT), in_=bank[bC][:, 0:128], mul=-1.0, reads=[BK[bC]], writes=[kt("nWT")])
                        if GDN_LV < 8:
                            continue
                        St, kS = Sst[h], K(f"S{h}")
                        mm(bC, 0, 128, T_(PT), T_(Vb), [kt("PT"), kt("Vb")], start=True, stop=False)
                        mm(bC, 0, 128, T_(nWT), St[:], [kt("nWT"), kS], start=False, stop=True)
                        S.op("act", "copy", out=T_(Vnew), in_=bank[bC][:, 0:128], reads=[BK[bC]], writes=[kt("Vnew")])
                        mm(bC, 128, 256, T_(Qc), St[:], [kt("Qc"), kS])
                        mm(bC, 256, 384, T_(AT), T_(Vnew), [kt("AT"), kt("Vnew")])
                        mm(bC, 384, 512, T_(Kend), T_(Vnew), [kt("Kend"), kt("Vnew")])
                        S.op("dve", "tensor_scalar", out=T_(o1), in0=bank[bC][:, 128:256], scalar1=gt["eg"][:, hs], scalar2=None, op0=ALU.mult,
                             reads=[BK[bC], kg("eg")], writes=[kt("o1")])
                        S.op("dve", "tensor_tensor", out=T_(o_), in0=bank[bC][:, 256:384], in1=T_(o1), op=ALU.add,
                             reads=[BK[bC], kt("o1")], writes=[kt("o")])
                        S.op("dve", "scalar_tensor_tensor", out=St[:], in0=St[:], scalar=gt["elast"][:, hs],
                             in1=bank[bC][:, 384:512], op0=ALU.mult, op1=ALU.add, reads=[kS, kg("elast"), BK[bC]], writes=[kS])
                        if GDN_LV < 9:
                            continue
                        S.op("act", "activation", out=T_(junk), in_=T_(o_), func=AF.Square,
                             reads=[kt("o")], writes=[kt("junk")])
                        S.op("dve", "reduce_sum", out=cl("ssqo"), in_=T_(junk), axis=AX.X, reads=[kt("junk")],
                             writes=[kt("ssqo")])
                        S.op("dve", "tensor_tensor", out=cl("v1"), in0=cl("ssqo"), in1=cl("rq2"), op=ALU.mult,
                             reads=[kt("ssqo"), kt("rq2")], writes=[kt("v1")])
                        S.op("act", "activation", out=cl("ssqq"), in_=cl("v1"), func=AF.Sqrt, bias=eps6[:, 0:1], scale=1.0,
                             reads=[kt("v1"), K("eps6")], writes=[kt("ssqq")])
                        S.op("dve", "reciprocal", out=cl("v1"), in_=cl("ssqq"), reads=[kt("ssqq")], writes=[kt("v1")])
                        S.op("dve", "tensor_tensor", out=cl("fac"), in0=cl("v1"), in1=cl("rq"), op=ALU.mult,
                             reads=[kt("v1"), kt("rq")], writes=[kt("fac")])
                        if GDN_LV < 11:
                            continue
                        S.op("dve", "scalar_tensor_tensor", out=T_(og), in0=T_(o_), scalar=cl("fac"), in1=normg[:],
                             op0=ALU.mult, op1=ALU.mult, reads=[kt("o"), kt("fac"), K("normg")], writes=[kt("og")])
                        S.op("dve", "tensor_tensor", out=T_(og), in0=T_(og), in1=zs[:, h * 128:(h + 1) * 128], op=ALU.mult,
                             reads=[kt("og"), K("zs")], writes=[kt("og")])
                        if GDN_LV < 11:
                            continue
                        tr(bA, 0, T_(og), [kt("og")])
                        S.op("act", "copy", out=ogT_sb[:, h, tsl], in_=bank[bA][:, 0:128], reads=[BK[bA]],
                             writes=[K("ogT")])
                    finally:
                        S.op = real_op
                    recs.append(rec)
                na, nb = len(recs[0]), len(recs[1])
                for q in range(max(na, nb)):
                    for rr in recs:
                        if q < len(rr):
                            eng_, method_, reads_, writes_, kw_ = rr[q]
                            S.op(eng_, method_, reads=reads_, writes=writes_, **kw_)

        S.dma("sp", K("ogT"), ogT[:, t0:t0 + 512].rearrange("(h p) t -> p h t", p=128), ogT_sb[:], reads=[K("ogT")])


def build_gdn(T):
    nc = bass.Bass("TRN2", target_bir_lowering=False)
    dt = lambda n, s, k="ExternalInput": nc.dram_tensor(n, s, F32, kind=k).ap()
    xT = dt("xT", [D, T])
    wq = dt("wq", [D, 512]); wk = dt("wk", [D, 512]); wv = dt("wv", [D, 512]); wz = dt("wz", [D, 512])
    wba = dt("wba", [D, 8]); convw = dt("convw", [128, 3, 4, 4]); hp = dt("hpar", [128, 8])
    normg = dt("normg", [128, 128]); gconst = dt("gconst", [6, 128, 128])
    ogT = dt("ogT", [512, T], "ExternalOutput")
    with contextlib.ExitStack() as es:
        C = Ctx(nc, es)
        gdn_phase(C, T, xT, wq, wk, wv, wz, wba, convw, hp, normg, gconst, ogT)
        C.S.finish()
        C.S.emit()
    return nc


def gdn_consts():
    i = np.arange(128)
    ident = np.eye(128, dtype=np.float32)
    MU = (i[:, None] <= i[None, :]).astype(np.float32)
    MS = (i[:, None] > i[None, :]).astype(np.float32)
    NEGL = np.where(i[:, None] > i[None, :], 0.0, -100.0).astype(np.float32)
    NEGUT = np.where(i[None, :] >= i[:, None], 0.0, -100.0).astype(np.float32)
    ONES = np.ones((128, 128), np.float32)
    return np.stack([ident, MU, MS, NEGL, NEGUT, ONES])


def gdn_host_layout(hh, w_in, conv_w, a_log, dt_bias, norm_g):
    QK = 1024
    c0 = 512 * hh
    wq = np.ascontiguousarray(w_in[:, c0:c0 + 512])
    wk = np.ascontiguousarray(w_in[:, QK + c0:QK + c0 + 512])
    wv = np.ascontiguousarray(w_in[:, 2 * QK + c0:2 * QK + c0 + 512])
    wz = np.ascontiguousarray(w_in[:, 3 * QK + c0:3 * QK + c0 + 512])
    wba = np.ascontiguousarray(np.concatenate([w_in[:, 4 * QK + 4 * hh:4 * QK + 4 * hh + 4],
                                               w_in[:, 4 * QK + 8 + 4 * hh:4 * QK + 8 + 4 * hh + 4]], axis=1))
    convw = np.zeros((128, 3, 4, 4), np.float32)
    for i in range(3):
        for h in range(4):
            convw[:, i, h, :] = conv_w[:, i * QK + c0 + h * 128:i * QK + c0 + (h + 1) * 128].T
    hp = np.tile(np.concatenate([a_log[4 * hh:4 * hh + 4], dt_bias[4 * hh:4 * hh + 4]])[None, :], (128, 1)).astype(np.float32)
    normg = np.tile(norm_g[None, :], (128, 1)).astype(np.float32)
    return dict(wq=wq, wk=wk, wv=wv, wz=wz, wba=wba, convw=convw, hpar=hp, normg=normg, gconst=gdn_consts())


GDN_NAMES = ("wq", "wk", "wv", "wz", "wba", "convw", "hpar", "normg")
S5_NAMES = ("w_in", "lamr", "lami", "ldt", "bpad_re", "bpad_im", "cpad_re", "cpad_im", "dsk")
GDN_SHAPES = dict(wq=[D, 512], wk=[D, 512], wv=[D, 512], wz=[D, 512], wba=[D, 8], convw=[128, 3, 4, 4],
                  hpar=[128, 8], normg=[128, 128])
S5_SHAPES = dict(w_in=[D, 512], lamr=[128, 16], lami=[128, 16], ldt=[128, 16], bpad_re=[16, 128, 128],
                 bpad_im=[16, 128, 128], cpad_re=[16, 128, 128], cpad_im=[16, 128, 128], dsk=[128, 4])


def build_fused(T):
    nc = bass.Bass("TRN2", target_bir_lowering=False)
    ext = lambda n, sh: nc.dram_tensor(n, sh, F32, kind="ExternalInput").ap()
    xT0 = ext("xT0", [D, T])
    xtok0 = ext("xtok0", [T, D])
    ident_d = ext("ident", [128, 128])
    gconst = ext("gconst", [6, 128, 128])
    iota = ext("iota", [128, S5_L])
    wr0 = ext("wr_dummy", [D, NEXP])
    br0 = ext("br_dummy", [1, NEXP])
    y = nc.dram_tensor("y", [T, D], F32, kind="ExternalOutput").ap()
    XT = nc.dram_tensor("XT_i", [D, T], F32).ap()
    FEAT = nc.dram_tensor("FEAT_i", [D, T], F32).ap()
    XA = nc.dram_tensor("XA_i", [T, D], F32).ap()
    XB = nc.dram_tensor("XB_i", [T, D], F32).ap()
    TH = T // 2
    tb = min(1024, TH)
    with contextlib.ExitStack() as es:
        S = Sched(nc, es)

        def phase(fn, last=False):
            with contextlib.ExitStack() as pes:
                C = Ctx(nc, pes, S)
                fn(C)
                S.barrier()
                S.emit()
            if not last:
                S.new_phase()

        for i in range(DEPTH):
            xT_src = xT0 if i == 0 else XT
            xres_src = xtok0 if i == 0 else (XA if i % 2 == 1 else XB)
            xout_dst = y if i == DEPTH - 1 else (XA if i % 2 == 0 else XB)
            lnp = ext(f"L{i}_lnp", [4, D])
            if i % 2 == 0:
                for hh in range(2):
                    d = {n: ext(f"L{i}_{hh}_{n}", GDN_SHAPES[n]) for n in GDN_NAMES}
                    phase(lambda C, d=d, hh=hh: gdn_phase(
                        C, T, xT_src, d["wq"], d["wk"], d["wv"], d["wz"], d["wba"], d["convw"], d["hpar"], d["normg"],
                        gconst, FEAT[hh * 512:(hh + 1) * 512, :], pfx=f"g{i}{hh}"))
                glu, moe, ne, nproj = False, False, 2, D
            else:
                for gh in range(2):
                    d = {n: ext(f"L{i}_{gh}_{n}", S5_SHAPES[n]) for n in S5_NAMES}
                    phase(lambda C, d=d, gh=gh: s5_phase(
                        C, T, xT_src, d["w_in"], d["lamr"], d["lami"], d["ldt"], d["bpad_re"], d["bpad_im"],
                        d["cpad_re"], d["cpad_im"], d["dsk"], iota, FEAT[gh * 512:(gh + 1) * 512, :], pfx=f"s{i}{gh}"))
                glu, moe, ne, nproj = True, True, NEXP, 2 * D
            wproj = ext(f"L{i}_wproj", [D, nproj])
            w1 = ext(f"L{i}_w1", [ne, D, FE]); w3 = ext(f"L{i}_w3", [ne, D, FE]); w2 = ext(f"L{i}_w2", [ne, FE, D])
            if moe:
                wr = ext(f"L{i}_wr", [D, NEXP]); br = ext(f"L{i}_br", [1, NEXP])
            else:
                wr, br = wr0, br0
            for hf in range(2):
                sl = slice(hf * TH, (hf + 1) * TH)
                phase(lambda C, sl=sl, hf=hf: tp_phase(
                    C, TH, glu, moe, FEAT[:, sl], wproj, xres_src[sl, :], lnp, w1, w3, w2, wr, br, ident_d,
                    xout_dst[sl, :], XT[:, sl], tb=tb, pfx=f"t{i}{hf}"), last=(i == DEPTH - 1 and hf == 1))
        print("n_sems", len(S.sem), "n_ops", S.n_ops)
        S.finish()
        S.emit()
    return nc


def fused_inputs(xb, P):
    f = lambda a: np.ascontiguousarray(np.asarray(a, dtype=np.float32))
    m = dict(xT0=np.ascontiguousarray(xb.T), xtok0=np.ascontiguousarray(xb), ident=np.eye(128, dtype=np.float32),
             gconst=gdn_consts(), iota=np.tile(np.arange(1, S5_L + 1, dtype=np.float32)[None, :], (128, 1)),
             wr_dummy=np.zeros((D, NEXP), np.float32), br_dummy=np.zeros((1, NEXP), np.float32))
    for i in range(DEPTH):
        j = i // 2
        m[f"L{i}_lnp"] = f(np.stack([P["ln_g"][i, 0], P["ln_b"][i, 0], P["ln_g"][i, 1], P["ln_b"][i, 1]]))
        if i % 2 == 0:
            for hh in range(2):
                lay = gdn_host_layout(hh, f(P["gdn_w_in"][j]), f(P["gdn_conv_w"][j]), f(P["gdn_a_log"][j]),
                                      f(P["gdn_dt_bias"][j]), f(P["gdn_norm_g"][j]))
                for n in GDN_NAMES:
                    m[f"L{i}_{hh}_{n}"] = lay[n]
            m[f"L{i}_wproj"] = f(P["gdn_w_out"][j])
            m[f"L{i}_w1"] = f(np.stack([P["ffn_w1"][j][:, :FE], P["ffn_w1"][j][:, FE:]]))
            m[f"L{i}_w3"] = f(np.stack([P["ffn_w3"][j][:, :FE], P["ffn_w3"][j][:, FE:]]))
            m[f"L{i}_w2"] = f(np.stack([P["ffn_w2"][j][:FE], P["ffn_w2"][j][FE:]]))
        else:
            for gh in range(2):
                lay = s5_host_layout(gh, f(P["s5_lam_re"][j]), f(P["s5_lam_im"][j]), f(P["s5_log_dt"][j]),
                                     f(P["s5_b_re"][j]), f(P["s5_b_im"][j]), f(P["s5_c_re"][j]), f(P["s5_c_im"][j]),
                                     f(P["s5_d"][j]), f(P["s5_w_in"][j]))
                for n in S5_NAMES:
                    m[f"L{i}_{gh}_{n}"] = lay[n]
            m[f"L{i}_wproj"] = f(P["s5_w_glu"][j])
            m[f"L{i}_w1"] = f(P["moe_w1"][j]); m[f"L{i}_w3"] = f(P["moe_w3"][j]); m[f"L{i}_w2"] = f(P["moe_w2"][j])
            m[f"L{i}_wr"] = f(P["moe_w_router"][j]); m[f"L{i}_br"] = f(P["moe_b_router"][j]).reshape(1, NEXP)
    return m


SEQ = 8192
BATCH = 4
_PROGS = {}


def kernel(**P):
    x = np.ascontiguousarray(np.asarray(P["x"], dtype=np.float32))
    T = x.shape[1]
    if T not in _PROGS:
        _PROGS[T] = build_fused(T)
    nc = _PROGS[T]
    shared = None
    in_maps = []
    for c in range(8):
        b = c % BATCH
        m = fused_inputs(x[b], P) if shared is None else dict(shared)
        if shared is None:
            shared = m
        else:
            m["xT0"] = np.ascontiguousarray(x[b].T)
            m["xtok0"] = x[b]
        in_maps.append(m)
    res = run_bass_kernel_spmd(nc, in_maps, core_ids=list(range(8))).results
    return np.stack([res[b]["y"] for b in range(BATCH)]).astype(np.float32)
```

```python
import contextlib
import numpy as np
import concourse.bass as bass
import concourse.mybir as mybir
from concourse.bass_utils import run_bass_kernel_spmd

F32 = mybir.dt.float32
BF16 = mybir.dt.bfloat16
AF = mybir.ActivationFunctionType
ALU = mybir.AluOpType
AX = mybir.AxisListType

D = 1024
DEPTH = 4
ALPHA = (2 * DEPTH) ** 0.25
LN_EPS = 1e-5
NORM_EPS = 1e-6
NEXP = 8
FE = 1408
NFT = FE // 128


class Sched:
    ENGS = ("pe", "dve", "act", "pool", "sp")

    def __init__(self, nc, es):
        self.nc = nc
        self.es = es
        self.q = {e: [] for e in self.ENGS}
        self.sem = {}
        self.cnt = {}
        self.known = {e: {} for e in self.ENGS}
        self.last_w = {}
        self.readers = {}
        self.phase = 0
        self.ename = {}
        for e in ("pe", "dve", "act", "pool"):
            self.ename[e] = e + "0"
            self._mksem(e + "0")
        self.n_ops = 0

    def _mksem(self, name):
        self.sem[name] = self.es.enter_context(self.nc.semaphore("s_" + name))
        self.cnt[name] = 0

    def _deps(self, reads, writes):
        deps = {}

        def add(tok):
            if tok is None:
                return
            s, v = tok
            if deps.get(s, 0) < v:
                deps[s] = v

        for k in reads:
            add(self.last_w.get(k))
        for k in writes:
            add(self.last_w.get(k))
            for s, v in self.readers.get(k, {}).items():
                add((s, v))
        return deps

    def _commit(self, tok, reads, writes):
        for k in writes:
            self.last_w[k] = tok
            self.readers[k] = {}
        for k in reads:
            if k in writes:
                continue
            r = self.readers.setdefault(k, {})
            if r.get(tok[0], 0) < tok[1]:
                r[tok[0]] = tok[1]

    def _waits(self, eng, deps):
        waits = []
        kn = self.known[eng]
        for s, v in deps.items():
            if eng == "pe" and s == self.ename["pe"]:
                continue
            if kn.get(s, 0) < v:
                kn[s] = v
                waits.append((s, v))
        return waits

    def op(self, eng, method, reads=(), writes=(), **kw):
        if eng != "pe":
            ex = [k for k in reads if ".bank" in k and k not in writes]
            if ex:
                writes = list(writes) + ex
        deps = self._deps(reads, writes)
        waits = self._waits(eng, deps)
        en = self.ename[eng]
        self.cnt[en] += 1
        tok = (en, self.cnt[en])
        self.q[eng].append((waits, (method, kw), (en, 1)))
        self._commit(tok, reads, writes)
        self.n_ops += 1

    def dma(self, queue, stream, out, in_, reads=(), writes=(), **kw):
        sname = "d_" + stream.split(".", 1)[-1]
        if sname not in self.sem:
            self._mksem(sname)
        deps = self._deps(reads, writes)
        waits = self._waits(queue, deps)
        self.cnt[sname] += 16
        tok = (sname, self.cnt[sname])
        kw = dict(kw)
        kw["out"] = out
        kw["in_"] = in_
        self.q[queue].append((waits, ("dma_start", kw), (sname, 16)))
        self._commit(tok, reads, writes)
        self.n_ops += 1

    def coll(self, kind, stream, ins, outs, replica_groups, reads=(), writes=()):
        sname = "d_" + stream
        if sname not in self.sem:
            self._mksem(sname)
        deps = self._deps(reads, writes)
        waits = self._waits("pool", deps)
        self.cnt[sname] += 16
        tok = (sname, self.cnt[sname])
        kw = dict(kind=kind, op=ALU.bypass, replica_groups=replica_groups, ins=list(ins), outs=list(outs))
        self.q["pool"].append((waits, ("collective_compute", kw), (sname, 16)))
        self._commit(tok, reads, writes)
        self.n_ops += 1

    def barrier(self):
        for e in self.ENGS:
            waits = []
            for s_, v in self.cnt.items():
                if v > 0 and self.known[e].get(s_, 0) < v:
                    self.known[e][s_] = v
                    waits.append((s_, v))
            if waits:
                self.q[e].append((waits, None, None))

    def finish(self):
        self.barrier()

    def new_phase(self):
        self.phase += 1
        self.last_w = {}
        self.readers = {}
        for e in ("pe", "dve", "act"):
            old_name = self.ename[e]
            for kn in self.known.values():
                kn.pop(old_name, None)
            del self.cnt[old_name]
            nm = f"{e}{self.phase}"
            self.ename[e] = nm
            self._mksem(nm)

    def emit(self):
        nc = self.nc
        S = self

        def replay(name, eng):
            for waits, fn, inc in S.q[name]:
                for s, v in waits:
                    eng.wait_ge(S.sem[s], v)
                if fn is None:
                    continue
                inst = getattr(eng, fn[0])(**fn[1])
                inst.then_inc(S.sem[inc[0]], inc[1])
            S.q[name] = []

        with nc.Block() as block:
            @block.tensor
            def _(e):
                replay("pe", e)

            @block.vector
            def _(e):
                replay("dve", e)

            @block.scalar
            def _(e):
                replay("act", e)

            @block.gpsimd
            def _(e):
                replay("pool", e)

            @block.sync
            def _(e):
                replay("sp", e)


class Ctx:
    _uid = [0]

    def __init__(self, nc, es, S=None):
        self.nc = nc
        self.es = es
        self.S = S if S is not None else Sched(nc, es)
        Ctx._uid[0] += 1
        self.n = Ctx._uid[0] * 1000

    def sb(self, shape, dt=F32, name=None):
        self.n += 1
        return self.es.enter_context(self.nc.sbuf_tensor(f"{name or 'sb'}_{self.n}", list(shape), dt))

    def ps(self, shape, dt=F32, name=None):
        self.n += 1
        return self.es.enter_context(self.nc.psum_tensor(f"{name or 'ps'}_{self.n}", list(shape), dt))


def tp_phase(C, T, glu, moe, featT, wproj, xres, lnp, w1, w3, w2, wr, br, ident_d, xout, xoutT, tb=1024, pfx="tp"):
    S = C.S
    nproj = 2 * D if glu else D
    ne = NEXP if moe else 2
    tb = min(tb, T)
    nblk = T // tb
    ntt = tb // 128
    hw = min(512, tb)
    nhalf = tb // hw
    P = pfx

    def K(name):
        return P + "." + name

    ident = C.sb([128, 128], F32, "ident")
    lnb = C.sb([128, 4, D], F32, "lnb")
    wproj_sb = C.sb([128, 8, nproj], BF16, "wproj")
    wr_sb = C.sb([128, 8, NEXP], F32, "wr")
    br_sb = C.sb([128, NEXP], F32, "br")
    w1_sb = C.sb([128, 8, FE], BF16, "w1")
    w3_sb = C.sb([128, 8, FE], BF16, "w3")
    w2_sb = C.sb([128, NFT, D], BF16, "w2")
    featT_sb = [C.sb([128, 8, 128], BF16, "featT") for _ in range(2)]
    _xres1 = C.sb([128, D], F32, "xres")
    xres_sb = [_xres1, _xres1]
    hglu_sb = C.sb([128, 512], F32, "hglu")
    stats = [C.sb([128, 2, 6], F32, "stats") for _ in range(2)]
    mv = [C.sb([128, 2], F32, "mv") for _ in range(2)]
    rstd = [C.sb([128, 1], F32, "rstd") for _ in range(2)]
    x1 = [C.sb([128, D], F32, "x1") for _ in range(2)]
    _x1T321 = C.sb([128, 8, 128], F32, "x1T32")
    x1T32 = [_x1T321, _x1T321]
    x1T = C.sb([128, 8, tb], BF16, "x1T")
    yacc = C.sb([128, ntt, D], F32, "yacc")
    gates = C.sb([128, ntt, NEXP], F32, "gates")
    rt = [C.sb([128, NEXP], F32, "rt") for _ in range(4)]
    rcol = [C.sb([128, 1], F32, "rcol") for _ in range(4)]
    hT = C.sb([128, NFT, tb], BF16, "hT")
    epsc = C.sb([128, 1], F32, "epsc")
    pg = [C.ps([128, 512], F32, "pg") for _ in range(2)]
    pu = [C.ps([128, 512], F32, "pu") for _ in range(2)]
    py = [C.ps([128, 512], F32, "py") for _ in range(2)]
    ptr = C.ps([128, 512], F32, "ptr")
    prt = C.ps([128, 512], F32, "prt")

    S.op("dve", "memset", ap=epsc[:], constant=LN_EPS, writes=[K("epsc")])
    S.dma("sp", K("c0"), ident[:], ident_d, writes=[K("ident")])
    for i in range(4):
        S.dma("sp", K("c1"), lnb[:, i, :], lnp[i:i + 1, :].partition_broadcast(128), writes=[K("lnb")])
    S.dma("pool", K("c2"), wproj_sb[:], wproj.rearrange("(j p) n -> p j n", p=128), writes=[K("wproj")])
    S.dma("sp", K("c3"), wr_sb[:], wr.rearrange("(j p) n -> p j n", p=128), writes=[K("wr")])
    S.dma("sp", K("c4"), br_sb[:], br.partition_broadcast(128), writes=[K("br")])

    def layer_norm(src, src_key, dst, dst_key, gi, idx):
        st, m, rs = stats[idx], mv[idx], rstd[idx]
        kst, kmv, krs = K(f"st{idx}"), K(f"mv{idx}"), K(f"rstd{idx}")
        for c in range(2):
            S.op("dve", "bn_stats", out=st[:, c, :], in_=src[:, c * 512:(c + 1) * 512],
                 reads=[src_key], writes=[kst + str(c)])
        S.op("dve", "bn_aggr", out=m[:], in_=st[:], reads=[kst + "0", kst + "1"], writes=[kmv])
        S.op("act", "activation", out=rs[:], in_=m[:, 1:2], func=AF.Sqrt, bias=epsc[:, 0:1], scale=1.0,
             reads=[kmv, K("epsc")], writes=[krs])
        S.op("dve", "reciprocal", out=rs[:], in_=rs[:], reads=[krs], writes=[krs])
        S.op("dve", "tensor_scalar", out=dst, in0=src, scalar1=m[:, 0:1], scalar2=rs[:, 0:1],
             op0=ALU.subtract, op1=ALU.mult, reads=[src_key, kmv, krs], writes=[dst_key])
        S.op("pool", "tensor_tensor", out=dst, in0=dst, in1=lnb[:, gi, :], op=ALU.mult,
             reads=[dst_key, K("lnb")], writes=[dst_key])
        S.op("pool", "tensor_tensor", out=dst, in0=dst, in1=lnb[:, gi + 1, :], op=ALU.add,
             reads=[dst_key, K("lnb")], writes=[dst_key])

    cnt = 0
    for blk in range(nblk):
        t0 = blk * tb
        for tt in range(ntt):
            i2 = tt % 2
            tok0 = t0 + tt * 128
            fT, xr, r = featT_sb[i2], xres_sb[i2], xres_sb[i2]
            kfT, kxr, kr, kx1 = K(f"fT{i2}"), K("xr0"), K("xr0"), K(f"x1_{i2}")
            S.dma("pool", kfT, fT[:], featT[:, tok0:tok0 + 128].rearrange("(j p) t -> p j t", p=128), writes=[kfT])
            S.dma("sp", kxr, xr[:], xres[tok0:tok0 + 128, :], writes=[kxr])
            for nh in range(2):
                c0, c1 = nh * 512, (nh + 1) * 512
                pv, kpv = py[nh], K(f"py{nh}")
                for k in range(8):
                    S.op("pe", "matmul", out=pv[:], lhsT=fT[:, k, :], rhs=wproj_sb[:, k, c0:c1],
                         start=(k == 0), stop=(k == 7), reads=[kfT, K("wproj")], writes=[kpv])
                if glu:
                    pgt, kpg = pg[nh], K(f"pg{nh}")
                    for k in range(8):
                        S.op("pe", "matmul", out=pgt[:], lhsT=fT[:, k, :], rhs=wproj_sb[:, k, D + c0:D + c1],
                             start=(k == 0), stop=(k == 7), reads=[kfT, K("wproj")], writes=[kpg])
                    S.op("act", "activation", out=hglu_sb[:], in_=pgt[:], func=AF.Sigmoid,
                         reads=[kpg], writes=[K("hglu")])
                    S.op("dve", "tensor_tensor", out=hglu_sb[:], in0=hglu_sb[:], in1=pv[:], op=ALU.mult,
                         reads=[K("hglu"), kpv], writes=[K("hglu")])
                    S.op("dve", "scalar_tensor_tensor", out=r[:, c0:c1], in0=xr[:, c0:c1], scalar=ALPHA,
                         in1=hglu_sb[:], op0=ALU.mult, op1=ALU.add, reads=[kxr, K("hglu")], writes=[kr])
                else:
                    S.op("dve", "scalar_tensor_tensor", out=r[:, c0:c1], in0=xr[:, c0:c1], scalar=ALPHA,
                         in1=pv[:], op0=ALU.mult, op1=ALU.add, reads=[kxr, kpv], writes=[kr])
            xx = x1[i2]
            layer_norm(r[:], kr, xx[:], kx1, 0, i2)
            S.op("act", "mul", out=yacc[:, tt, :], in_=xx[:], mul=ALPHA, reads=[kx1], writes=[K(f"yacc{tt}")])
            xt32 = x1T32[i2]
            for kh in range(2):
                pb, kpb = (ptr, K("ptr")) if kh == 0 else (prt, K("prt"))
                for q4 in range(4):
                    k = kh * 4 + q4
                    S.op("pe", "transpose", out=pb[:, q4 * 128:(q4 + 1) * 128], in_=xx[:, k * 128:(k + 1) * 128],
                         identity=ident[:], reads=[kx1, K("ident")], writes=[kpb])
                S.op("act", "copy", out=xt32[:, kh * 4:(kh + 1) * 4, :].rearrange("p k t -> p (k t)"), in_=pb[:],
                     reads=[kpb], writes=[K("x1T32_0")])
                S.op("pool", "tensor_copy", out=x1T[:, kh * 4:(kh + 1) * 4, tt * 128:(tt + 1) * 128],
                     in_=xt32[:, kh * 4:(kh + 1) * 4, :], reads=[K("x1T32_0")], writes=[K(f"x1T_{tt}")])
            if moe:
                for k in range(8):
                    S.op("pe", "matmul", out=prt[:, 0:NEXP], lhsT=xt32[:, k, :], rhs=wr_sb[:, k, :],
                         start=(k == 0), stop=(k == 7), reads=[K("x1T32_0"), K("wr")], writes=[K("prt")])
                lg, mk, l2, ex = rt
                m1, m2, sm, rsm = rcol
                kk = lambda n: K("rt_" + n)
                S.op("dve", "tensor_tensor", out=lg[:], in0=prt[:, 0:NEXP], in1=br_sb[:], op=ALU.add,
                     reads=[K("prt"), K("br")], writes=[kk("lg")])
                S.op("dve", "reduce_max", out=m1[:], in_=lg[:], axis=AX.X, reads=[kk("lg")], writes=[kk("m1")])
                S.op("dve", "tensor_scalar", out=mk[:], in0=lg[:], scalar1=m1[:, 0:1], scalar2=-1e30,
                     op0=ALU.is_ge, op1=ALU.mult, reads=[kk("lg"), kk("m1")], writes=[kk("mk")])
                S.op("dve", "tensor_tensor", out=l2[:], in0=lg[:], in1=mk[:], op=ALU.add,
                     reads=[kk("lg"), kk("mk")], writes=[kk("l2")])
                S.op("dve", "reduce_max", out=m2[:], in_=l2[:], axis=AX.X, reads=[kk("l2")], writes=[kk("m2")])
                S.op("dve", "tensor_scalar", out=ex[:], in0=lg[:], scalar1=m1[:, 0:1], scalar2=None,
                     op0=ALU.subtract, reads=[kk("lg"), kk("m1")], writes=[kk("ex")])
                S.op("act", "activation", out=ex[:], in_=ex[:], func=AF.Exp, reads=[kk("ex")], writes=[kk("ex")])
                S.op("dve", "scalar_tensor_tensor", out=ex[:], in0=lg[:], scalar=m2[:, 0:1], in1=ex[:],
                     op0=ALU.is_ge, op1=ALU.mult, reads=[kk("lg"), kk("m2"), kk("ex")], writes=[kk("ex")])
                S.op("dve", "reduce_sum", out=sm[:], in_=ex[:], axis=AX.X, reads=[kk("ex")], writes=[kk("sm")])
                S.op("dve", "reciprocal", out=rsm[:], in_=sm[:], reads=[kk("sm")], writes=[kk("rsm")])
                S.op("dve", "tensor_scalar", out=gates[:, tt, :], in0=ex[:], scalar1=rsm[:, 0:1], scalar2=None,
                     op0=ALU.mult, reads=[kk("ex"), kk("rsm")], writes=[K(f"gates{tt}")])
        for ex_i in range(ne):
            S.dma("pool", K("w1"), w1_sb[:], w1[ex_i].rearrange("(j p) n -> p j n", p=128), writes=[K("w1")])
            S.dma("pool", K("w3"), w3_sb[:], w3[ex_i].rearrange("(j p) n -> p j n", p=128), writes=[K("w3")])
            S.dma("pool", K("w2"), w2_sb[:], w2[ex_i].rearrange("(j p) n -> p j n", p=128), writes=[K("w2")])
            for hf in range(nhalf):
                ts0, ts1 = hf * hw, (hf + 1) * hw
                tkeys = [K(f"x1T_{tt}") for tt in range(ts0 // 128, ts1 // 128)]
                for f in range(NFT):
                    b = cnt % 2
                    cnt += 1
                    f0, f1 = f * 128, (f + 1) * 128
                    for k in range(8):
                        S.op("pe", "matmul", out=pg[b][:, 0:hw], lhsT=w1_sb[:, k, f0:f1], rhs=x1T[:, k, ts0:ts1],
                             start=(k == 0), stop=(k == 7), reads=[K("w1")] + tkeys, writes=[K(f"pg{b}")])
                    for k in range(8):
                        S.op("pe", "matmul", out=pu[b][:, 0:hw], lhsT=w3_sb[:, k, f0:f1], rhs=x1T[:, k, ts0:ts1],
                             start=(k == 0), stop=(k == 7), reads=[K("w3")] + tkeys, writes=[K(f"pu{b}")])
                    S.op("act", "activation", out=hT[:, f, ts0:ts1], in_=pg[b][:, 0:hw], func=AF.Silu,
                         reads=[K(f"pg{b}")], writes=[K(f"hT{f}_{hf}")])
                    S.op("dve", "tensor_tensor", out=hT[:, f, ts0:ts1], in0=hT[:, f, ts0:ts1], in1=pu[b][:, 0:hw],
                         op=ALU.mult, reads=[K(f"hT{f}_{hf}"), K(f"pu{b}")], writes=[K(f"hT{f}_{hf}")])
            for hf in range(nhalf):
                hkeys = [K(f"hT{f}_{hf}") for f in range(NFT)]
                for tl in range(hw // 128):
                    tt = hf * (hw // 128) + tl
                    kya = K(f"yacc{tt}")
                    for nh in range(2):
                        c0, c1 = nh * 512, (nh + 1) * 512
                        kpy = K(f"py{nh}")
                        for f in range(NFT):
                            S.op("pe", "matmul", out=py[nh][:], lhsT=hT[:, f, tt * 128:(tt + 1) * 128],
                                 rhs=w2_sb[:, f, c0:c1], start=(f == 0), stop=(f == NFT - 1),
                                 reads=hkeys + [K("w2")], writes=[kpy])
                        if moe:
                            S.op("dve", "scalar_tensor_tensor", out=yacc[:, tt, c0:c1], in0=py[nh][:],
                                 scalar=gates[:, tt, ex_i:ex_i + 1], in1=yacc[:, tt, c0:c1], op0=ALU.mult,
                                 op1=ALU.add, reads=[kpy, K(f"gates{tt}"), kya], writes=[kya])
                        else:
                            S.op("dve", "tensor_tensor", out=yacc[:, tt, c0:c1], in0=py[nh][:],
                                 in1=yacc[:, tt, c0:c1], op=ALU.add, reads=[kpy, kya], writes=[kya])
        for tt in range(ntt):
            i2 = tt % 2
            tok0 = t0 + tt * 128
            xx, kxo = x1[i2], K(f"x1_{i2}")
            layer_norm(yacc[:, tt, :], K(f"yacc{tt}"), xx[:], kxo, 2, i2)
            S.dma("sp", kxo, xout[tok0:tok0 + 128, :], xx[:], reads=[kxo])
            xt, kxt = x1T32[i2], K("x1T32_0")
            for kh in range(2):
                pb, kpb = (ptr, K("ptr")) if kh == 0 else (prt, K("prt"))
                for q4 in range(4):
                    k = kh * 4 + q4
                    S.op("pe", "transpose", out=pb[:, q4 * 128:(q4 + 1) * 128], in_=xx[:, k * 128:(k + 1) * 128],
                         identity=ident[:], reads=[kxo, K("ident")], writes=[kpb])
                S.op("act", "copy", out=xt[:, kh * 4:(kh + 1) * 4, :].rearrange("p k t -> p (k t)"), in_=pb[:],
                     reads=[kpb], writes=[kxt])
            S.dma("sp", kxt, xoutT[:, tok0:tok0 + 128].rearrange("(j p) t -> p j t", p=128), xt[:], reads=[kxt])


def build_tp(T, glu, moe, tb=1024):
    nc = bass.Bass("TRN2", target_bir_lowering=False)
    nproj = 2 * D if glu else D
    ne = NEXP if moe else 2
    dt = lambda n, s, k="ExternalInput": nc.dram_tensor(n, s, F32, kind=k).ap()
    featT = dt("featT", [D, T]); wproj = dt("wproj", [D, nproj]); xres = dt("xres", [T, D])
    lnp = dt("lnp", [4, D]); w1 = dt("w1", [ne, D, FE]); w3 = dt("w3", [ne, D, FE]); w2 = dt("w2", [ne, FE, D])
    wr = dt("wr", [D, NEXP]); br = dt("br", [1, NEXP]); ident_d = dt("ident", [128, 128])
    xout = dt("xout", [T, D], "ExternalOutput"); xoutT = dt("xoutT", [D, T], "ExternalOutput")
    with contextlib.ExitStack() as es:
        C = Ctx(nc, es)
        tp_phase(C, T, glu, moe, featT, wproj, xres, lnp, w1, w3, w2, wr, br, ident_d, xout, xoutT, tb=tb)
        C.S.finish()
        C.S.emit()
    return nc


S5_L = 128
PI = float(np.pi)


def s5_phase(C, T, xT, w_in, lamr_d, lami_d, ldt_d, bpad_re_d, bpad_im_d, cpad_re_d, cpad_im_d, dsk_d, iota_d,
             hidT, pfx="s5"):
    S = C.S
    L = S5_L
    NB = T // 512
    NGP = 16
    P = pfx

    def K(n):
        return P + "." + n

    w_sb = C.sb([128, 8, 512], BF16, "s5w")
    xT_sb = [C.sb([128, 8, 512], BF16, "s5x") for _ in range(2)]
    u_sb = [C.sb([128, 4, 512], F32, "s5u") for _ in range(2)]
    bre = C.sb([128, NGP, 128], F32, "bre")
    bim = C.sb([128, NGP, 128], F32, "bim")
    cre = C.sb([128, NGP, 128], F32, "cre")
    cim = C.sb([128, NGP, 128], F32, "cim")
    dsk = C.sb([128, 4], F32, "dsk")
    iota = C.sb([128, L], F32, "iota")
    prm = {n: C.sb([128, NGP], F32, "p_" + n) for n in
           ("lamr", "lami", "ldt", "dt", "zr", "zi", "r", "cz", "sz", "ar", "ai", "den", "cr", "ci", "ncr",
            "t1", "t2", "zl", "er", "ei", "nei")}
    negpi = C.sb([128, 1], F32, "negpi")
    tre = C.sb([128, NGP, L], F32, "tre")
    tim = C.sb([128, NGP, L], F32, "tim")
    cph = C.sb([128, NGP, L], F32, "cph")
    sph = C.sb([128, NGP, L], F32, "sph")
    rmat = C.sb([128, NGP, L], F32, "rmat")
    ang = C.sb([128, L], F32, "ang")
    ang2 = C.sb([128, L], F32, "ang2")
    hp = C.sb([128, NGP, 2], F32, "hp")
    tmpc2 = [C.sb([128, 2], F32, "tmpc") for _ in range(2)]
    BUr = [C.sb([128, 512], F32, "BUr") for _ in range(2)]
    BUi = [C.sb([128, 512], F32, "BUi") for _ in range(2)]
    mt = [[C.sb([128, 512], F32, "mt") for _ in range(4)] for _ in range(2)]
    btr = [C.sb([128, 512], F32, "btr") for _ in range(2)]
    bti = [C.sb([128, 512], F32, "bti") for _ in range(2)]
    wre = [C.sb([128, 512], F32, "wre") for _ in range(2)]
    wim = [C.sb([128, 512], F32, "wim") for _ in range(2)]
    hre = [[C.sb([128, 512], F32, "hre") for _ in range(4)] for _ in range(2)]
    him = [[C.sb([128, 512], F32, "him") for _ in range(4)] for _ in range(2)]
    yt = [C.sb([128, 512], F32, "yt") for _ in range(2)]
    ho = [C.sb([128, 512], F32, "ho") for _ in range(2)]
    pu_ = [C.ps([128, 512], F32, "s5pu") for _ in range(2)]
    pbr = [C.ps([128, 512], F32, "s5pbr") for _ in range(2)]
    pbi = [C.ps([128, 512], F32, "s5pbi") for _ in range(2)]
    pyy = [C.ps([128, 512], F32, "s5py") for _ in range(2)]

    S.dma("pool", K("w"), w_sb[:], w_in.rearrange("(j p) n -> p j n", p=128), writes=[K("w")])
    for nm, dd, sbt in (("bre", bpad_re_d, bre), ("bim", bpad_im_d, bim), ("cre", cpad_re_d, cre), ("cim", cpad_im_d, cim)):
        S.dma("sp", K(nm), sbt[:], dd.rearrange("g k m -> k g m"), writes=[K(nm)])
    S.dma("sp", K("dsk"), dsk[:], dsk_d, writes=[K("dsk")])
    S.dma("sp", K("iota"), iota[:], iota_d, writes=[K("iota")])
    S.dma("sp", K("lamr"), prm["lamr"][:], lamr_d, writes=[K("lamr")])
    S.dma("sp", K("lami"), prm["lami"][:], lami_d, writes=[K("lami")])
    S.dma("sp", K("ldt"), prm["ldt"][:], ldt_d, writes=[K("ldt")])
    S.op("dve", "memset", ap=negpi[:], constant=-PI, writes=[K("negpi")])
    itmp = C.sb([128, L], mybir.dt.int32, "itmp")
    ftmp = C.sb([128, L], F32, "ftmp")
    S.op("dve", "memset", ap=hp[:], constant=0.0, writes=[K(f"hp{g}") for g in range(NGP)])

    def pp(n):
        return prm[n][:]

    def ew(eng, method, out_n, reads, **kw):
        S.op(eng, method, reads=[K(x) for x in reads], writes=[K(out_n)], **kw)

    def sincos(angle_ap, angle_key, sin_ap, sin_key, cos_ap, cos_key, tmp_ap, tmp_key, width):
        for off, o_ap, o_key in ((0.5, sin_ap, sin_key), (0.75, cos_ap, cos_key)):
            S.op("dve", "tensor_scalar", out=tmp_ap, in0=angle_ap, scalar1=1.0 / (2.0 * PI), scalar2=off,
                 op0=ALU.mult, op1=ALU.add, reads=[angle_key], writes=[tmp_key])
            S.op("dve", "tensor_copy", out=itmp[:, 0:width], in_=tmp_ap, reads=[tmp_key], writes=[K("itmp")])
            S.op("dve", "tensor_copy", out=ftmp[:, 0:width], in_=itmp[:, 0:width], reads=[K("itmp")], writes=[K("ftmp")])
            S.op("dve", "tensor_tensor", out=tmp_ap, in0=tmp_ap, in1=ftmp[:, 0:width], op=ALU.subtract,
                 reads=[tmp_key, K("ftmp")], writes=[tmp_key])
            S.op("dve", "scalar_tensor_tensor", out=tmp_ap, in0=tmp_ap, scalar=0.0, in1=tmp_ap, op0=ALU.is_lt,
                 op1=ALU.add, reads=[tmp_key], writes=[tmp_key])
            S.op("act", "activation", out=o_ap, in_=tmp_ap, func=AF.Sin, bias=negpi[:, 0:1], scale=2.0 * PI,
                 reads=[tmp_key, K("negpi")], writes=[o_key])

    ew("act", "activation", "dt", ["ldt"], out=pp("dt"), in_=pp("ldt"), func=AF.Exp)
    ew("dve", "tensor_tensor", "zr", ["lamr", "dt"], out=pp("zr"), in0=pp("lamr"), in1=pp("dt"), op=ALU.mult)
    ew("dve", "tensor_tensor", "zi", ["lami", "dt"], out=pp("zi"), in0=pp("lami"), in1=pp("dt"), op=ALU.mult)
    ew("act", "activation", "r", ["zr"], out=pp("r"), in_=pp("zr"), func=AF.Exp)
    sincos(pp("zi"), K("zi"), pp("sz"), K("sz"), pp("cz"), K("cz"), pp("t1"), K("t1"), NGP)
    ew("dve", "tensor_tensor", "ar", ["r", "cz"], out=pp("ar"), in0=pp("r"), in1=pp("cz"), op=ALU.mult)
    ew("dve", "tensor_scalar", "ar", ["ar"], out=pp("ar"), in0=pp("ar"), scalar1=-1.0, scalar2=None, op0=ALU.add)
    ew("dve", "tensor_tensor", "ai", ["r", "sz"], out=pp("ai"), in0=pp("r"), in1=pp("sz"), op=ALU.mult)
    ew("dve", "tensor_tensor", "den", ["lamr"], out=pp("den"), in0=pp("lamr"), in1=pp("lamr"), op=ALU.mult)
    ew("dve", "tensor_tensor", "t1", ["lami"], out=pp("t1"), in0=pp("lami"), in1=pp("lami"), op=ALU.mult)
    ew("dve", "tensor_tensor", "den", ["den", "t1"], out=pp("den"), in0=pp("den"), in1=pp("t1"), op=ALU.add)
    ew("dve", "reciprocal", "den", ["den"], out=pp("den"), in_=pp("den"))
    ew("dve", "tensor_tensor", "t1", ["ar", "lamr"], out=pp("t1"), in0=pp("ar"), in1=pp("lamr"), op=ALU.mult)
    ew("dve", "tensor_tensor", "t2", ["ai", "lami"], out=pp("t2"), in0=pp("ai"), in1=pp("lami"), op=ALU.mult)
    ew("dve", "tensor_tensor", "cr", ["t1", "t2"], out=pp("cr"), in0=pp("t1"), in1=pp("t2"), op=ALU.add)
    ew("dve", "tensor_tensor", "cr", ["cr", "den"], out=pp("cr"), in0=pp("cr"), in1=pp("den"), op=ALU.mult)
    ew("dve", "tensor_tensor", "t1", ["ai", "lamr"], out=pp("t1"), in0=pp("ai"), in1=pp("lamr"), op=ALU.mult)
    ew("dve", "tensor_tensor", "t2", ["ar", "lami"], out=pp("t2"), in0=pp("ar"), in1=pp("lami"), op=ALU.mult)
    ew("dve", "tensor_tensor", "ci", ["t1", "t2"], out=pp("ci"), in0=pp("t1"), in1=pp("t2"), op=ALU.subtract)
    ew("dve", "tensor_tensor", "ci", ["ci", "den"], out=pp("ci"), in0=pp("ci"), in1=pp("den"), op=ALU.mult)
    ew("dve", "tensor_scalar", "ncr", ["cr"], out=pp("ncr"), in0=pp("cr"), scalar1=-1.0, scalar2=None, op0=ALU.mult)
    ew("dve", "tensor_scalar", "zl", ["zi"], out=pp("zl"), in0=pp("zi"), scalar1=float(L), scalar2=None, op0=ALU.mult)
    sincos(pp("zl"), K("zl"), pp("ei"), K("ei"), pp("er"), K("er"), pp("t1"), K("t1"), NGP)
    ew("dve", "tensor_scalar", "nei", ["ei"], out=pp("nei"), in0=pp("ei"), scalar1=-1.0, scalar2=None, op0=ALU.mult)
    for g in range(NGP):
        S.op("dve", "tensor_scalar", out=ang[:], in0=iota[:], scalar1=prm["zi"][:, g:g + 1], scalar2=None,
             op0=ALU.mult, reads=[K("iota"), K("zi")], writes=[K("ang")])
        sincos(ang[:], K("ang"), sph[:, g, :], K("sph"), cph[:, g, :], K("cph"), ang2[:], K("ang2"), L)
        S.op("dve", "tensor_scalar", out=ang2[:], in0=cph[:, g, :], scalar1=prm["cr"][:, g:g + 1], scalar2=None,
             op0=ALU.mult, reads=[K("cph"), K("cr")], writes=[K("ang2")])
        S.op("dve", "scalar_tensor_tensor", out=tre[:, g, :], in0=sph[:, g, :], scalar=prm["ci"][:, g:g + 1],
             in1=ang2[:], op0=ALU.mult, op1=ALU.add, reads=[K("sph"), K("ci"), K("ang2")], writes=[K("tre")])
        S.op("dve", "tensor_scalar", out=ang2[:], in0=cph[:, g, :], scalar1=prm["ci"][:, g:g + 1], scalar2=None,
             op0=ALU.mult, reads=[K("cph"), K("ci")], writes=[K("ang2")])
        S.op("dve", "scalar_tensor_tensor", out=tim[:, g, :], in0=sph[:, g, :], scalar=prm["ncr"][:, g:g + 1],
             in1=ang2[:], op0=ALU.mult, op1=ALU.add, reads=[K("sph"), K("ncr"), K("ang2")], writes=[K("tim")])
        S.op("pool", "memset", ap=rmat[:, g, :], constant=1.0, writes=[K("rmat")])
        S.op("dve", "tensor_scalar", out=rmat[:, g, :], in0=rmat[:, g, :], scalar1=prm["r"][:, g:g + 1], scalar2=None,
             op0=ALU.mult, reads=[K("rmat"), K("r")], writes=[K("rmat")])

    NC4 = 512 // L

    def v3(t):
        return t[:].rearrange("p (c l) -> p c l", l=L)

    def tb3(tab, g):
        return tab[:, g:g + 1, :].broadcast_to([128, NC4, L])

    gi = 0
    for blk in range(NB):
        t0 = blk * 512
        bi = blk % 2
        xs, us = xT_sb[bi], u_sb[bi]
        kx, ku = K(f"x{bi}"), K(f"u{bi}")
        S.dma("pool", kx, xs[:], xT[:, t0:t0 + 512].rearrange("(j p) t -> p j t", p=128), writes=[kx])
        for ft in range(4):
            b2 = ft % 2
            for k in range(8):
                S.op("pe", "matmul", out=pu_[b2][:], lhsT=w_sb[:, k, ft * 128:(ft + 1) * 128], rhs=xs[:, k, :],
                     start=(k == 0), stop=(k == 7), reads=[K("w"), kx], writes=[K(f"pu{b2}")])
            S.op("act", "copy", out=us[:, ft, :], in_=pu_[b2][:], reads=[K(f"pu{b2}")], writes=[ku + f"_{ft}"])
        for ft in range(4):
            f2 = ft % 2
            for gpair in range(0, 4, 2):
                recs = []
                for gl4 in (gpair, gpair + 1):
                    rec = []
                    real_op = S.op
                    S.op = (lambda eng, method, reads=(), writes=(), _rec=rec, **kw:
                            _rec.append((eng, method, reads, writes, kw)))
                    try:
                        g = ft * 4 + gl4
                        b2 = gi % 2
                        gi += 1
                        kb = lambda n: K(f"{n}{b2}")
                        tmpc = tmpc2[b2]
                        S.op("pe", "matmul", out=pbr[b2][:], lhsT=bre[:, g, :], rhs=us[:, ft, :], start=True, stop=True,
                             reads=[K("bre"), ku + f"_{ft}"], writes=[kb("pbr")])
                        S.op("pe", "matmul", out=pbi[b2][:], lhsT=bim[:, g, :], rhs=us[:, ft, :], start=True, stop=True,
                             reads=[K("bim"), ku + f"_{ft}"], writes=[kb("pbi")])
                        S.op("act", "copy", out=BUr[b2][:], in_=pbr[b2][:], reads=[kb("pbr")], writes=[kb("BUr")])
                        S.op("act", "copy", out=BUi[b2][:], in_=pbi[b2][:], reads=[kb("pbi")], writes=[kb("BUi")])
                        m0, m1, m2, m3 = mt[b2]
                        S.op("dve", "tensor_tensor", out=v3(m0), in0=v3(BUr[b2]), in1=tb3(tre, g), op=ALU.mult,
                             reads=[kb("BUr"), K("tre")], writes=[kb("m0")])
                        S.op("pool", "tensor_tensor", out=v3(m1), in0=v3(BUi[b2]), in1=tb3(tim, g), op=ALU.mult,
                             reads=[kb("BUi"), K("tim")], writes=[kb("m1")])
                        S.op("dve", "tensor_tensor", out=btr[b2][:], in0=m0[:], in1=m1[:], op=ALU.subtract,
                             reads=[kb("m0"), kb("m1")], writes=[kb("btr")])
                        S.op("pool", "tensor_tensor", out=v3(m2), in0=v3(BUi[b2]), in1=tb3(tre, g), op=ALU.mult,
                             reads=[kb("BUi"), K("tre")], writes=[kb("m2")])
                        S.op("dve", "tensor_tensor", out=v3(m3), in0=v3(BUr[b2]), in1=tb3(tim, g), op=ALU.mult,
                             reads=[kb("BUr"), K("tim")], writes=[kb("m3")])
                        S.op("pool", "tensor_tensor", out=bti[b2][:], in0=m2[:], in1=m3[:], op=ALU.add,
                             reads=[kb("m2"), kb("m3")], writes=[kb("bti")])
                        khp = K(f"hp{g}")
                        for c in range(NC4):
                            cs = slice(c * L, (c + 1) * L)
                            S.op("dve", "tensor_tensor_scan", out=wre[b2][:, cs], data0=rmat[:, g, :], data1=btr[b2][:, cs],
                                 initial=hp[:, g, 0:1], op0=ALU.mult, op1=ALU.add,
                                 reads=[K("rmat"), kb("btr"), khp], writes=[kb("wre")])
                            S.op("dve", "tensor_tensor_scan", out=wim[b2][:, cs], data0=rmat[:, g, :], data1=bti[b2][:, cs],
                                 initial=hp[:, g, 1:2], op0=ALU.mult, op1=ALU.add,
                                 reads=[K("rmat"), kb("bti"), khp], writes=[kb("wim")])
                            last = (c + 1) * L - 1
                            S.op("dve", "tensor_scalar", out=tmpc[:, 0:1], in0=wre[b2][:, last:last + 1],
                                 scalar1=prm["er"][:, g:g + 1], scalar2=None, op0=ALU.mult,
                                 reads=[kb("wre"), K("er")], writes=[kb("tmpc0")])
                            S.op("dve", "tensor_scalar", out=tmpc[:, 1:2], in0=wre[b2][:, last:last + 1],
                                 scalar1=prm["ei"][:, g:g + 1], scalar2=None, op0=ALU.mult,
                                 reads=[kb("wre"), K("ei")], writes=[kb("tmpc1")])
                            S.op("dve", "scalar_tensor_tensor", out=hp[:, g, 0:1], in0=wim[b2][:, last:last + 1],
                                 scalar=prm["nei"][:, g:g + 1], in1=tmpc[:, 0:1], op0=ALU.mult, op1=ALU.add,
                                 reads=[kb("wim"), K("nei"), kb("tmpc0")], writes=[khp])
                            S.op("dve", "scalar_tensor_tensor", out=hp[:, g, 1:2], in0=wim[b2][:, last:last + 1],
                                 scalar=prm["er"][:, g:g + 1], in1=tmpc[:, 1:2], op0=ALU.mult, op1=ALU.add,
                                 reads=[kb("wim"), K("er"), kb("tmpc1")], writes=[khp])
                        hr, hi_ = hre[f2][gl4], him[f2][gl4]
                        khr, khi = K(f"hre{f2}{gl4}"), K(f"him{f2}{gl4}")
                        S.op("pool", "tensor_tensor", out=v3(m0), in0=v3(wre[b2]), in1=tb3(cph, g), op=ALU.mult,
                             reads=[kb("wre"), K("cph")], writes=[kb("m0")])
                        S.op("pool", "tensor_tensor", out=v3(m1), in0=v3(wim[b2]), in1=tb3(sph, g), op=ALU.mult,
                             reads=[kb("wim"), K("sph")], writes=[kb("m1")])
                        S.op("pool", "tensor_tensor", out=hr[:], in0=m0[:], in1=m1[:], op=ALU.subtract,
                             reads=[kb("m0"), kb("m1")], writes=[khr])
                        S.op("dve", "tensor_tensor", out=v3(m2), in0=v3(wre[b2]), in1=tb3(sph, g), op=ALU.mult,
                             reads=[kb("wre"), K("sph")], writes=[kb("m2")])
                        S.op("pool", "tensor_tensor", out=v3(m3), in0=v3(wim[b2]), in1=tb3(cph, g), op=ALU.mult,
                             reads=[kb("wim"), K("cph")], writes=[kb("m3")])
                        S.op("dve", "scalar_tensor_tensor", out=hi_[:], in0=m2[:], scalar=-1.0, in1=m3[:], op0=ALU.mult,
                             op1=ALU.subtract, reads=[kb("m2"), kb("m3")], writes=[khi])
                    finally:
                        S.op = real_op
                    recs.append(rec)
                for q in range(max(len(recs[0]), len(recs[1]))):
                    for rr in recs:
                        if q < len(rr):
                            eng_, method_, reads_, writes_, kw_ = rr[q]
                            S.op(eng_, method_, reads=reads_, writes=writes_, **kw_)
            for gl4 in range(4):
                g = ft * 4 + gl4
                S.op("pe", "matmul", out=pyy[f2][:], lhsT=cre[:, g, :], rhs=hre[f2][gl4][:], start=(gl4 == 0), stop=False,
                     reads=[K("cre"), K(f"hre{f2}{gl4}")], writes=[K(f"pyy{f2}")])
                S.op("pe", "matmul", out=pyy[f2][:], lhsT=cim[:, g, :], rhs=him[f2][gl4][:], start=False, stop=(gl4 == 3),
                     reads=[K("cim"), K(f"him{f2}{gl4}")], writes=[K(f"pyy{f2}")])
            S.op("dve", "scalar_tensor_tensor", out=yt[f2][:], in0=us[:, ft, :], scalar=dsk[:, ft:ft + 1], in1=pyy[f2][:],
                 op0=ALU.mult, op1=ALU.add, reads=[ku + f"_{ft}", K("dsk"), K(f"pyy{f2}")], writes=[K(f"yt{f2}")])
            S.op("act", "activation", out=ho[f2][:], in_=yt[f2][:], func=AF.Gelu, reads=[K(f"yt{f2}")],
                 writes=[K(f"ho{f2}")])
            S.dma("sp", K(f"ho{f2}"), hidT[ft * 128:(ft + 1) * 128, t0:t0 + 512], ho[f2][:], reads=[K(f"ho{f2}")])


def build_s5(T):
    nc = bass.Bass("TRN2", target_bir_lowering=False)
    dt = lambda n, s, k="ExternalInput": nc.dram_tensor(n, s, F32, kind=k).ap()
    xT = dt("xT", [D, T]); w_in = dt("w_in", [D, 512])
    lamr = dt("lamr", [128, 16]); lami = dt("lami", [128, 16]); ldt = dt("ldt", [128, 16])
    bre = dt("bpad_re", [16, 128, 128]); bim = dt("bpad_im", [16, 128, 128])
    cre = dt("cpad_re", [16, 128, 128]); cim = dt("cpad_im", [16, 128, 128])
    dsk = dt("dsk", [128, 4]); iota = dt("iota", [128, S5_L])
    hidT = dt("hidT", [512, T], "ExternalOutput")
    with contextlib.ExitStack() as es:
        C = Ctx(nc, es)
        s5_phase(C, T, xT, w_in, lamr, lami, ldt, bre, bim, cre, cim, dsk, iota, hidT)
        C.S.finish()
        C.S.emit()
    return nc


def s5_host_layout(ghalf, s5_lam_re, s5_lam_im, s5_log_dt, s5_b_re, s5_b_im, s5_c_re, s5_c_im, s5_d, s5_w_in):
    g0 = 32 * ghalf
    lamr = np.zeros((128, 16), np.float32); lami = np.zeros((128, 16), np.float32); ldt = np.zeros((128, 16), np.float32)
    bre = np.zeros((16, 128, 128), np.float32); bim = np.zeros((16, 128, 128), np.float32)
    cre = np.zeros((16, 128, 128), np.float32); cim = np.zeros((16, 128, 128), np.float32)
    for gp in range(16):
        for gl in range(2):
            g = g0 + 2 * gp + gl
            lamr[gl * 64:(gl + 1) * 64, gp] = s5_lam_re[g]
            lami[gl * 64:(gl + 1) * 64, gp] = s5_lam_im[g]
            ldt[gl * 64:(gl + 1) * 64, gp] = s5_log_dt[g]
            r0 = 32 * (gp % 4) + 16 * gl
            bre[gp, r0:r0 + 16, gl * 64:(gl + 1) * 64] = s5_b_re[g].T
            bim[gp, r0:r0 + 16, gl * 64:(gl + 1) * 64] = s5_b_im[g].T
            cre[gp, gl * 64:(gl + 1) * 64, r0:r0 + 16] = s5_c_re[g].T
            cim[gp, gl * 64:(gl + 1) * 64, r0:r0 + 16] = s5_c_im[g].T
    dsk = np.ascontiguousarray(s5_d[512 * ghalf:512 * (ghalf + 1)].reshape(4, 128).T)
    iota = np.tile(np.arange(1, S5_L + 1, dtype=np.float32)[None, :], (128, 1))
    w = np.ascontiguousarray(s5_w_in[:, 512 * ghalf:512 * (ghalf + 1)])
    return dict(w_in=w, lamr=lamr, lami=lami, ldt=ldt, bpad_re=bre, bpad_im=bim, cpad_re=cre, cpad_im=cim,
                dsk=dsk, iota=iota)


GDN_LV = 99
GDN_CLAMP = False


def gdn_phase(C, T, xT, wq_d, wk_d, wv_d, wz_d, wba_d, convw_d, hp_d, normg_d, gconst_d, ogT, pfx="gd"):
    S = C.S
    NB = T // 512
    P = pfx
    NH = 4

    def K(n):
        return P + "." + n

    wq = C.sb([128, 8, 512], BF16, "wq"); wk = C.sb([128, 8, 512], BF16, "wk")
    wv = C.sb([128, 8, 512], BF16, "wv"); wz = C.sb([128, 8, 512], BF16, "wz")
    wba = C.sb([128, 8, 8], F32, "wba")
    convw = C.sb([128, 3, NH, 4], F32, "convw")
    hpar = C.sb([128, 8], F32, "hpar")
    nea = C.sb([128, NH], F32, "nea")
    normg = C.sb([128, 128], F32, "normg")
    cst = C.sb([128, 6, 128], F32, "gcst")
    ident, MU, MS, NEGL, NEGUT, ONES = (cst[:, i, :] for i in range(6))
    onec = C.sb([128, 1], F32, "onec"); eps6 = C.sb([128, 1], F32, "eps6")
    xs = [C.sb([128, 8, 512], BF16, "gxs") for _ in range(2)]
    xs32 = C.sb([128, 8, 512], F32, "gxs32")
    xb = [[C.sb([128, 515], F32, "xb") for _ in range(NH)] for _ in range(3)]
    qkvc = [[C.sb([128, 512], F32, "qkvc") for _ in range(NH)] for _ in range(3)]
    cacc = [C.sb([128, 512], F32, "cacc") for _ in range(2)]
    zs = C.sb([128, 512], F32, "zs")
    ba = C.sb([128, 8], F32, "ba")
    gt = {n: C.sb([128, NH], F32, "g_" + n) for n in
          ("beta", "nbeta", "x", "e", "sp", "g", "eg", "kes", "elast", "bks")}
    gc8 = C.sb([128, 8], F32, "gc8")
    Sst = [C.sb([128, 128], F32, "Sst") for _ in range(NH)]
    ogT_sb = C.sb([128, NH, 512], F32, "ogT")

    def mk(n):
        return [C.sb([128, 128], F32, n) for _ in range(2)]

    Kn, KnT, Kbg, Kend, Vb, sq, G1, Dl, DTu, AT, PT, nWT, Vnew, o1, o_, og = (mk(n) for n in (
        "Kn", "KnT", "Kbg", "Kend", "Vb", "sq", "G1", "Dl", "DTu", "AT", "PT", "nWT", "Vnew", "o1", "o", "og"))
    Nn = [mk("Na"), mk("Nb")]
    NTn = [mk("NTa"), mk("NTb")]
    junk = mk("junk")
    Qc = mk("Qc")
    col = {n: [C.sb([128, 1], F32, "c_" + n) for _ in range(2)] for n in
           ("ssqk", "rk", "ssqq", "rq", "rq2", "ssqo", "v1", "fac")}
    bank = [C.ps([128, 512], F32, f"gb{i}") for i in range(8)]
    BK = [K(f"bank{i}") for i in range(8)]

    for nm, dd, sbt in (("wq", wq_d, wq), ("wk", wk_d, wk), ("wv", wv_d, wv), ("wz", wz_d, wz)):
        S.dma("pool", K(nm), sbt[:], dd.rearrange("(j p) n -> p j n", p=128), writes=[K(nm)])
    S.dma("sp", K("wba"), wba[:], wba_d.rearrange("(j p) n -> p j n", p=128), writes=[K("wba")])
    S.dma("sp", K("convw"), convw[:], convw_d, writes=[K("convw")])
    S.dma("sp", K("hpar"), hpar[:], hp_d, writes=[K("hpar")])
    S.dma("sp", K("normg"), normg[:], normg_d, writes=[K("normg")])
    S.dma("sp", K("cst"), cst[:], gconst_d.rearrange("i p f -> p i f"), writes=[K("cst")])
    S.op("dve", "memset", ap=onec[:], constant=1.0, writes=[K("onec")])
    S.op("dve", "memset", ap=eps6[:], constant=NORM_EPS, writes=[K("eps6")])
    for h in range(NH):
        S.op("dve", "memset", ap=Sst[h][:], constant=0.0, writes=[K(f"S{h}")])
        for i in range(3):
            S.op("pool", "memset", ap=xb[i][h][:, 0:3], constant=0.0, writes=[K(f"xb{i}{h}")])
    S.op("act", "activation", out=nea[:], in_=hpar[:, 0:4], func=AF.Exp, reads=[K("hpar")], writes=[K("nea")])
    S.op("dve", "tensor_scalar", out=nea[:], in0=nea[:], scalar1=-1.0, scalar2=None, op0=ALU.mult,
         reads=[K("nea")], writes=[K("nea")])

    def mm(bi, c0, c1, lhsT, rhs, reads, start=True, stop=True, rows=128):
        S.op("pe", "matmul", out=bank[bi][0:rows, c0:c1], lhsT=lhsT, rhs=rhs, start=start, stop=stop,
             reads=reads, writes=[BK[bi]])

    def tr(bi, c0, in_, reads):
        S.op("pe", "transpose", out=bank[bi][:, c0:c0 + 128], in_=in_, identity=ident,
             reads=reads + [K("cst")], writes=[BK[bi]])

    it = 0
    for blk in range(NB):
        t0 = blk * 512
        bi2 = blk % 2
        xsb, kx = xs[bi2], K(f"xs{bi2}")
        S.dma("pool", kx, xsb[:], xT[:, t0:t0 + 512].rearrange("(j p) t -> p j t", p=128), writes=[kx])
        S.dma("sp", K("xs32"), xs32[:], xT[:, t0:t0 + 512].rearrange("(j p) t -> p j t", p=128), writes=[K("xs32")])
        ci = 0
        for i, wsb, wkey in ((0, wq, "wq"), (1, wk, "wk"), (2, wv, "wv")):
            for h in range(NH):
                for k in range(8):
                    mm(0, 0, 512, wsb[:, k, h * 128:(h + 1) * 128], xsb[:, k, :], [K(wkey), kx], start=(k == 0),
                       stop=(k == 7))
                xbt, kxb = xb[i][h], K(f"xb{i}{h}")
                S.op("act", "copy", out=xbt[:, 3:515], in_=bank[0][:], reads=[BK[0]], writes=[kxb])
                ca, kca = cacc[ci % 2], K(f"cacc{ci % 2}")
                ci += 1
                eng = "dve"
                S.op(eng, "tensor_scalar", out=ca[:], in0=xbt[:, 0:512], scalar1=convw[:, i, h, 0:1], scalar2=None,
                     op0=ALU.mult, reads=[kxb, K("convw")], writes=[kca])
                for j in range(1, 4):
                    S.op(eng, "scalar_tensor_tensor", out=ca[:], in0=xbt[:, j:j + 512], scalar=convw[:, i, h, j:j + 1],
                         in1=ca[:], op0=ALU.mult, op1=ALU.add, reads=[kxb, K("convw"), kca], writes=[kca])
                S.op("pool", "tensor_copy", out=xbt[:, 0:3], in_=xbt[:, 512:515], reads=[kxb], writes=[kxb])
                S.op("act", "activation", out=qkvc[i][h][:], in_=ca[:], func=AF.Silu, reads=[kca],
                     writes=[K(f"qkvc{i}{h}")])
        for tl in range(4):
            tsl = slice(tl * 128, (tl + 1) * 128)
            if GDN_LV < 2:
                continue
            for k in range(8):
                mm(1, 0, 512, xsb[:, k, tsl], wz[:, k, :], [kx, K("wz")], start=(k == 0), stop=(k == 7))
            S.op("act", "activation", out=zs[:], in_=bank[1][:], func=AF.Silu, reads=[BK[1]], writes=[K("zs")])
            for k in range(8):
                mm(1, 0, 8, xs32[:, k, tsl], wba[:, k, :], [K("xs32"), K("wba")], start=(k == 0), stop=(k == 7))
            S.op("dve", "tensor_copy", out=ba[:], in_=bank[1][:, 0:8], reads=[BK[1]], writes=[K("ba")])
            G = lambda n: gt[n][:]
            kg = lambda n: K("g_" + n)
            S.op("act", "activation", out=G("beta"), in_=ba[:, 0:4], func=AF.Sigmoid, reads=[K("ba")],
                 writes=[kg("beta")])
            S.op("dve", "tensor_scalar", out=G("nbeta"), in0=G("beta"), scalar1=-1.0, scalar2=None, op0=ALU.mult,
                 reads=[kg("beta")], writes=[kg("nbeta")])
            S.op("dve", "tensor_tensor", out=G("x"), in0=ba[:, 4:8], in1=hpar[:, 4:8], op=ALU.add,
                 reads=[K("ba"), K("hpar")], writes=[kg("x")])
            S.op("act", "activation", out=G("e"), in_=G("x"), func=AF.Exp, reads=[kg("x")], writes=[kg("e")])
            S.op("act", "activation", out=G("sp"), in_=G("e"), func=AF.Ln, bias=onec[:, 0:1], scale=1.0,
                 reads=[kg("e"), K("onec")], writes=[kg("sp")])
            S.op("dve", "tensor_tensor", out=G("g"), in0=G("sp"), in1=nea[:], op=ALU.mult,
                 reads=[kg("sp"), K("nea")], writes=[kg("g")])
            mm(1, 0, 4, MU, G("g"), [K("cst"), kg("g")])
            mm(1, 4, 8, ONES, G("g"), [K("cst"), kg("g")])
            S.op("dve", "tensor_copy", out=gc8[:], in_=bank[1][:, 0:8], reads=[BK[1]], writes=[K("gc8")])
            S.op("act", "activation", out=G("eg"), in_=gc8[:, 0:4], func=AF.Exp, reads=[K("gc8")], writes=[kg("eg")])
            S.op("act", "activation", out=G("elast"), in_=gc8[:, 4:8], func=AF.Exp, reads=[K("gc8")],
                 writes=[kg("elast")])
            S.op("dve", "tensor_tensor", out=G("kes"), in0=gc8[:, 4:8], in1=gc8[:, 0:4], op=ALU.subtract,
                 reads=[K("gc8")], writes=[kg("kes")])
            S.op("act", "activation", out=G("kes"), in_=G("kes"), func=AF.Exp, reads=[kg("kes")], writes=[kg("kes")])
            S.op("dve", "tensor_tensor", out=G("bks"), in0=G("beta"), in1=G("eg"), op=ALU.mult,
                 reads=[kg("beta"), kg("eg")], writes=[kg("bks")])
            if GDN_LV < 2.5:
                continue
            for hpair in range(0, NH, 2):
                recs = []
                for h in (hpair, hpair + 1):
                    rec = []
                    real_op = S.op
                    S.op = (lambda eng, method, reads=(), writes=(), _rec=rec, **kw:
                            _rec.append((eng, method, reads, writes, kw)))
                    try:
                        p2 = it % 2
                        bA, bB, bC = 2 + 3 * p2, 3 + 3 * p2, 4 + 3 * p2
                        it += 1
                        T_ = lambda lst: lst[p2][:]
                        kt = lambda n: K(f"{n}{p2}")
                        cl = lambda n: col[n][p2][:]
                        QT = qkvc[0][h][:, tsl]
                        KTr = qkvc[1][h][:, tsl]
                        VTr = qkvc[2][h][:, tsl]
                        kq, kk_, kv = K(f"qkvc0{h}"), K(f"qkvc1{h}"), K(f"qkvc2{h}")
                        hs = slice(h, h + 1)
                        S.op("pool", "tensor_copy", out=T_(Qc), in_=QT, reads=[kq], writes=[kt("Qc")])
                        tr(bA, 0, KTr, [kk_])
                        S.op("act", "activation", out=T_(junk), in_=bank[bA][:, 0:128], func=AF.Square,
                             reads=[BK[bA]], writes=[kt("junk")])
                        S.op("dve", "reduce_sum", out=cl("ssqk"), in_=T_(junk), axis=AX.X, reads=[kt("junk")],
                             writes=[kt("ssqk")])
                        S.op("act", "activation", out=cl("rk"), in_=cl("ssqk"), func=AF.Sqrt, bias=eps6[:, 0:1], scale=1.0,
                             reads=[kt("ssqk"), K("eps6")], writes=[kt("rk")])
                        S.op("dve", "reciprocal", out=cl("rk"), in_=cl("rk"), reads=[kt("rk")], writes=[kt("rk")])
                        S.op("dve", "tensor_scalar", out=T_(Kn), in0=bank[bA][:, 0:128], scalar1=cl("rk"), scalar2=None,
                             op0=ALU.mult, reads=[BK[bA], kt("rk")], writes=[kt("Kn")])
                        tr(bA, 128, T_(Kn), [kt("Kn")])
                        S.op("act", "copy", out=T_(KnT), in_=bank[bA][:, 128:256], reads=[BK[bA]], writes=[kt("KnT")])
                        S.op("dve", "tensor_scalar", out=T_(Kbg), in0=T_(Kn), scalar1=gt["bks"][:, hs], scalar2=None, op0=ALU.mult,
                             reads=[kt("Kn"), kg("bks")], writes=[kt("Kbg")])
                        S.op("dve", "tensor_scalar", out=T_(Kend), in0=T_(Kn), scalar1=gt["kes"][:, hs], scalar2=None, op0=ALU.mult,
                             reads=[kt("Kn"), kg("kes")], writes=[kt("Kend")])
                        if GDN_LV < 3:
                            continue
                        tr(bA, 256, VTr, [kv])
                        S.op("dve", "tensor_scalar", out=T_(Vb), in0=bank[bA][:, 256:384], scalar1=gt["beta"][:, hs],
                             scalar2=None, op0=ALU.mult, reads=[BK[bA], kg("beta")], writes=[kt("Vb")])
                        if GDN_LV < 3.5:
                            continue
                        tr(bA, 384, QT, [kq])
                        S.op("act", "activation", out=T_(sq), in_=bank[bA][:, 384:512], func=AF.Square, reads=[BK[bA]],
                             writes=[kt("sq")])
                        S.op("dve", "reduce_sum", out=cl("ssqq"), in_=T_(sq), axis=AX.X, reads=[kt("sq")], writes=[kt("ssqq")])
                        S.op("act", "activation", out=cl("rq"), in_=cl("ssqq"), func=AF.Sqrt, bias=eps6[:, 0:1],
                             scale=1.0, reads=[kt("ssqq"), K("eps6")], writes=[kt("rq")])
                        S.op("dve", "reciprocal", out=cl("rq"), in_=cl("rq"), reads=[kt("rq")], writes=[kt("rq")])
                        S.op("dve", "tensor_scalar", out=cl("rq"), in0=cl("rq"), scalar1=128.0 ** -0.5, scalar2=None,
                             op0=ALU.mult, reads=[kt("rq")], writes=[kt("rq")])
                        S.op("dve", "scalar_tensor_tensor", out=cl("rq2"), in0=cl("rq"), scalar=1.0 / 128.0, in1=cl("rq"),
                             op0=ALU.mult, op1=ALU.mult, reads=[kt("rq")], writes=[kt("rq2")])
                        if GDN_LV < 4:
                            continue
                        S.op("dve", "tensor_scalar", out=T_(G1), in0=MU, scalar1=gt["g"][:, hs], scalar2=None, op0=ALU.mult,
                             reads=[K("cst"), kg("g")], writes=[kt("G1")])
                        mm(bB, 0, 128, T_(G1), MS, [kt("G1"), K("cst")], start=True, stop=False)
                        mm(bB, 0, 128, ident, NEGL, [K("cst")], start=False, stop=True)
                        mm(bB, 128, 256, MS, T_(G1), [kt("G1"), K("cst")], start=True, stop=False)
                        mm(bB, 128, 256, ident, NEGUT, [K("cst")], start=False, stop=True)
                        S.op("act", "activation", out=T_(Dl), in_=bank[bB][:, 0:128], func=AF.Exp, reads=[BK[bB]],
                             writes=[kt("Dl")])
                        S.op("act", "activation", out=T_(DTu), in_=bank[bB][:, 128:256], func=AF.Exp, reads=[BK[bB]],
                             writes=[kt("DTu")])
                        if GDN_LV < 5:
                            continue
                        mm(bB, 256, 384, T_(KnT), T_(KnT), [kt("KnT")])
                        mm(bB, 384, 512, T_(KnT), QT, [kt("KnT"), kq])
                        N0, NT0 = Nn[0][p2], NTn[0][p2]
                        S.op("dve", "scalar_tensor_tensor", out=N0[:], in0=bank[bB][:, 256:384], scalar=gt["nbeta"][:, hs],
                             in1=T_(Dl), op0=ALU.mult, op1=ALU.mult, reads=[BK[bB], kg("nbeta"), kt("Dl")], writes=[kt("N0")])
                        S.op("dve", "tensor_tensor", out=T_(AT), in0=bank[bB][:, 384:512], in1=T_(DTu), op=ALU.mult,
                             reads=[BK[bB], kt("DTu")], writes=[kt("AT")])
                        tr(bC, 0, N0[:], [kt("N0")])
                        S.op("act", "copy", out=NT0[:], in_=bank[bC][:, 0:128], reads=[BK[bC]], writes=[kt("NT0")])
                        S.op("pool", "tensor_tensor", out=T_(PT), in0=NT0[:], in1=ident, op=ALU.add,
                             reads=[kt("NT0"), K("cst")], writes=[kt("PT")])
                        if GDN_LV < 6:
                            continue
                        for j in range(1, 7):
                            a, b = (j - 1) % 2, j % 2
                            Na, NTa, Nb, NTb = Nn[a][p2], NTn[a][p2], Nn[b][p2], NTn[b][p2]
                            mm(bC, 128, 256, NTa[:], Na[:], [kt(f"N{a}"), kt(f"NT{a}")])
                            if j < 6:
                                mm(bC, 256, 384, Na[:], NTa[:], [kt(f"N{a}"), kt(f"NT{a}")])
                            S.op("act", "copy", out=Nb[:], in_=bank[bC][:, 128:256], reads=[BK[bC]], writes=[kt(f"N{b}")])
                            if j < 6:
                                S.op("dve", "tensor_copy", out=NTb[:], in_=bank[bC][:, 256:384], reads=[BK[bC]],
                                     writes=[kt(f"NT{b}")])
                            mm(bC, 384, 512, Nb[:], T_(PT), [kt(f"N{b}"), kt("PT")])
                            S.op("dve", "tensor_tensor", out=T_(PT), in0=bank[bC][:, 384:512], in1=T_(PT), op=ALU.add,
                                 reads=[BK[bC], kt("PT")], writes=[kt("PT")])
                        if GDN_LV < 7:
                            continue
                        mm(bC, 0, 128, T_(Kbg), T_(PT), [kt("Kbg"), kt("PT")])
                        S.op("act", "mul", out=T_(nWT), in_=bank[bC][:, 0:128], mul=-1.0, reads=[BK[bC]], writes=[kt("nWT")])
                        if GDN_LV < 8:
                            continue
                        St, kS = Sst[h], K(f"S{h}")
                        mm(bC, 0, 128, T_(PT), T_(Vb), [kt("PT"), kt("Vb")], start=True, stop=False)
                        mm(bC, 0, 128, T_(nWT), St[:], [kt("nWT"), kS], start=False, stop=True)
                        S.op("act", "copy", out=T_(Vnew), in_=bank[bC][:, 0:128], reads=[BK[bC]], writes=[kt("Vnew")])
                        mm(bC, 128, 256, T_(Qc), St[:], [kt("Qc"), kS])
                        mm(bC, 256, 384, T_(AT), T_(Vnew), [kt("AT"), kt("Vnew")])
                        mm(bC, 384, 512, T_(Kend), T_(Vnew), [kt("Kend"), kt("Vnew")])
                        S.op("dve", "tensor_scalar", out=T_(o1), in0=bank[bC][:, 128:256], scalar1=gt["eg"][:, hs], scalar2=None, op0=ALU.mult,
                             reads=[BK[bC], kg("eg")], writes=[kt("o1")])
                        S.op("dve", "tensor_tensor", out=T_(o_), in0=bank[bC][:, 256:384], in1=T_(o1), op=ALU.add,
                             reads=[BK[bC], kt("o1")], writes=[kt("o")])
                        S.op("dve", "scalar_tensor_tensor", out=St[:], in0=St[:], scalar=gt["elast"][:, hs],
                             in1=bank[bC][:, 384:512], op0=ALU.mult, op1=ALU.add, reads=[kS, kg("elast"), BK[bC]], writes=[kS])
                        if GDN_LV < 9:
                            continue
                        S.op("act", "activation", out=T_(junk), in_=T_(o_), func=AF.Square,
                             reads=[kt("o")], writes=[kt("junk")])
                        S.op("dve", "reduce_sum", out=cl("ssqo"), in_=T_(junk), axis=AX.X, reads=[kt("junk")],
                             writes=[kt("ssqo")])
                        S.op("dve", "tensor_tensor", out=cl("v1"), in0=cl("ssqo"), in1=cl("rq2"), op=ALU.mult,
                             reads=[kt("ssqo"), kt("rq2")], writes=[kt("v1")])
                        S.op("act", "activation", out=cl("ssqq"), in_=cl("v1"), func=AF.Sqrt, bias=eps6[:, 0:1], scale=1.0,
                             reads=[kt("v1"), K("eps6")], writes=[kt("ssqq")])
                        S.op("dve", "reciprocal", out=cl("v1"), in_=cl("ssqq"), reads=[kt("ssqq")], writes=[kt("v1")])
                        S.op("dve", "tensor_tensor", out=cl("fac"), in0=cl("v1"), in1=cl("rq"), op=ALU.mult,
                             reads=[kt("v1"), kt("rq")], writes=[kt("fac")])
                        if GDN_LV < 11:
                            continue
                        S.op("dve", "scalar_tensor_tensor", out=T_(og), in0=T_(o_), scalar=cl("fac"), in1=normg[:],
                             op0=ALU.mult, op1=ALU.mult, reads=[kt("o"), kt("fac"), K("normg")], writes=[kt("og")])
                        S.op("dve", "tensor_tensor", out=T_(og), in0=T_(og), in1=zs[:, h * 128:(h + 1) * 128], op=ALU.mult,
                             reads=[kt("og"), K("zs")], writes=[kt("og")])
                        if GDN_LV < 11:
                            continue
                        tr(bA, 0, T_(og), [kt("og")])
                        S.op("act", "copy", out=ogT_sb[:, h, tsl], in_=bank[bA][:, 0:128], reads=[BK[bA]],
                             writes=[K("ogT")])
                    finally:
                        S.op = real_op
                    recs.append(rec)
                na, nb = len(recs[0]), len(recs[1])
                for q in range(max(na, nb)):
                    for rr in recs:
                        if q < len(rr):
                            eng_, method_, reads_, writes_, kw_ = rr[q]
                            S.op(eng_, method_, reads=reads_, writes=writes_, **kw_)

        S.dma("sp", K("ogT"), ogT[:, t0:t0 + 512].rearrange("(h p) t -> p h t", p=128), ogT_sb[:], reads=[K("ogT")])


def build_gdn(T):
    nc = bass.Bass("TRN2", target_bir_lowering=False)
    dt = lambda n, s, k="ExternalInput": nc.dram_tensor(n, s, F32, kind=k).ap()
    xT = dt("xT", [D, T])
    wq = dt("wq", [D, 512]); wk = dt("wk", [D, 512]); wv = dt("wv", [D, 512]); wz = dt("wz", [D, 512])
    wba = dt("wba", [D, 8]); convw = dt("convw", [128, 3, 4, 4]); hp = dt("hpar", [128, 8])
    normg = dt("normg", [128, 128]); gconst = dt("gconst", [6, 128, 128])
    ogT = dt("ogT", [512, T], "ExternalOutput")
    with contextlib.ExitStack() as es:
        C = Ctx(nc, es)
        gdn_phase(C, T, xT, wq, wk, wv, wz, wba, convw, hp, normg, gconst, ogT)
        C.S.finish()
        C.S.emit()
    return nc


def gdn_consts():
    i = np.arange(128)
    ident = np.eye(128, dtype=np.float32)
    MU = (i[:, None] <= i[None, :]).astype(np.float32)
    MS = (i[:, None] > i[None, :]).astype(np.float32)
    NEGL = np.where(i[:, None] > i[None, :], 0.0, -100.0).astype(np.float32)
    NEGUT = np.where(i[None, :] >= i[:, None], 0.0, -100.0).astype(np.float32)
    ONES = np.ones((128, 128), np.float32)
    return np.stack([ident, MU, MS, NEGL, NEGUT, ONES])


def gdn_host_layout(hh, w_in, conv_w, a_log, dt_bias, norm_g):
    QK = 1024
    c0 = 512 * hh
    wq = np.ascontiguousarray(w_in[:, c0:c0 + 512])
    wk = np.ascontiguousarray(w_in[:, QK + c0:QK + c0 + 512])
    wv = np.ascontiguousarray(w_in[:, 2 * QK + c0:2 * QK + c0 + 512])
    wz = np.ascontiguousarray(w_in[:, 3 * QK + c0:3 * QK + c0 + 512])
    wba = np.ascontiguousarray(np.concatenate([w_in[:, 4 * QK + 4 * hh:4 * QK + 4 * hh + 4],
                                               w_in[:, 4 * QK + 8 + 4 * hh:4 * QK + 8 + 4 * hh + 4]], axis=1))
    convw = np.zeros((128, 3, 4, 4), np.float32)
    for i in range(3):
        for h in range(4):
            convw[:, i, h, :] = conv_w[:, i * QK + c0 + h * 128:i * QK + c0 + (h + 1) * 128].T
    hp = np.tile(np.concatenate([a_log[4 * hh:4 * hh + 4], dt_bias[4 * hh:4 * hh + 4]])[None, :], (128, 1)).astype(np.float32)
    normg = np.tile(norm_g[None, :], (128, 1)).astype(np.float32)
    return dict(wq=wq, wk=wk, wv=wv, wz=wz, wba=wba, convw=convw, hpar=hp, normg=normg, gconst=gdn_consts())


GDN_NAMES = ("wq", "wk", "wv", "wz", "wba", "convw", "hpar", "normg")
S5_NAMES = ("w_in", "lamr", "lami", "ldt", "bpad_re", "bpad_im", "cpad_re", "cpad_im", "dsk")
GDN_SHAPES = dict(wq=[D, 512], wk=[D, 512], wv=[D, 512], wz=[D, 512], wba=[D, 8], convw=[128, 3, 4, 4],
                  hpar=[128, 8], normg=[128, 128])
S5_SHAPES = dict(w_in=[D, 512], lamr=[128, 16], lami=[128, 16], ldt=[128, 16], bpad_re=[16, 128, 128],
                 bpad_im=[16, 128, 128], cpad_re=[16, 128, 128], cpad_im=[16, 128, 128], dsk=[128, 4])


def build_fused(T):
    nc = bass.Bass("TRN2", target_bir_lowering=False)
    ext = lambda n, sh: nc.dram_tensor(n, sh, F32, kind="ExternalInput").ap()
    xT0 = ext("xT0", [D, T])
    xtok0 = ext("xtok0", [T, D])
    ident_d = ext("ident", [128, 128])
    gconst = ext("gconst", [6, 128, 128])
    iota = ext("iota", [128, S5_L])
    wr0 = ext("wr_dummy", [D, NEXP])
    br0 = ext("br_dummy", [1, NEXP])
    y = nc.dram_tensor("y", [T, D], F32, kind="ExternalOutput").ap()
    XT = nc.dram_tensor("XT_i", [D, T], F32).ap()
    FEAT = nc.dram_tensor("FEAT_i", [D, T], F32).ap()
    XA = nc.dram_tensor("XA_i", [T, D], F32).ap()
    XB = nc.dram_tensor("XB_i", [T, D], F32).ap()
    TH = T // 2
    tb = min(1024, TH)
    with contextlib.ExitStack() as es:
        S = Sched(nc, es)

        def phase(fn, last=False):
            with contextlib.ExitStack() as pes:
                C = Ctx(nc, pes, S)
                fn(C)
                S.barrier()
                S.emit()
            if not last:
                S.new_phase()

        for i in range(DEPTH):
            xT_src = xT0 if i == 0 else XT
            xres_src = xtok0 if i == 0 else (XA if i % 2 == 1 else XB)
            xout_dst = y if i == DEPTH - 1 else (XA if i % 2 == 0 else XB)
            lnp = ext(f"L{i}_lnp", [4, D])
            if i % 2 == 0:
                for hh in range(2):
                    d = {n: ext(f"L{i}_{hh}_{n}", GDN_SHAPES[n]) for n in GDN_NAMES}
                    phase(lambda C, d=d, hh=hh: gdn_phase(
                        C, T, xT_src, d["wq"], d["wk"], d["wv"], d["wz"], d["wba"], d["convw"], d["hpar"], d["normg"],
                        gconst, FEAT[hh * 512:(hh + 1) * 512, :], pfx=f"g{i}{hh}"))
                glu, moe, ne, nproj = False, False, 2, D
            else:
                for gh in range(2):
                    d = {n: ext(f"L{i}_{gh}_{n}", S5_SHAPES[n]) for n in S5_NAMES}
                    phase(lambda C, d=d, gh=gh: s5_phase(
                        C, T, xT_src, d["w_in"], d["lamr"], d["lami"], d["ldt"], d["bpad_re"], d["bpad_im"],
                        d["cpad_re"], d["cpad_im"], d["dsk"], iota, FEAT[gh * 512:(gh + 1) * 512, :], pfx=f"s{i}{gh}"))
                glu, moe, ne, nproj = True, True, NEXP, 2 * D
            wproj = ext(f"L{i}_wproj", [D, nproj])
            w1 = ext(f"L{i}_w1", [ne, D, FE]); w3 = ext(f"L{i}_w3", [ne, D, FE]); w2 = ext(f"L{i}_w2", [ne, FE, D])
            if moe:
                wr = ext(f"L{i}_wr", [D, NEXP]); br = ext(f"L{i}_br", [1, NEXP])
            else:
                wr, br = wr0, br0
            for hf in range(2):
                sl = slice(hf * TH, (hf + 1) * TH)
                phase(lambda C, sl=sl, hf=hf: tp_phase(
                    C, TH, glu, moe, FEAT[:, sl], wproj, xres_src[sl, :], lnp, w1, w3, w2, wr, br, ident_d,
                    xout_dst[sl, :], XT[:, sl], tb=tb, pfx=f"t{i}{hf}"), last=(i == DEPTH - 1 and hf == 1))
        print("n_sems", len(S.sem), "n_ops", S.n_ops)
        S.finish()
        S.emit()
    return nc


def fused_inputs(xb, P):
    f = lambda a: np.ascontiguousarray(np.asarray(a, dtype=np.float32))
    m = dict(xT0=np.ascontiguousarray(xb.T), xtok0=np.ascontiguousarray(xb), ident=np.eye(128, dtype=np.float32),
             gconst=gdn_consts(), iota=np.tile(np.arange(1, S5_L + 1, dtype=np.float32)[None, :], (128, 1)),
             wr_dummy=np.zeros((D, NEXP), np.float32), br_dummy=np.zeros((1, NEXP), np.float32))
    for i in range(DEPTH):
        j = i // 2
        m[f"L{i}_lnp"] = f(np.stack([P["ln_g"][i, 0], P["ln_b"][i, 0], P["ln_g"][i, 1], P["ln_b"][i, 1]]))
        if i % 2 == 0:
            for hh in range(2):
                lay = gdn_host_layout(hh, f(P["gdn_w_in"][j]), f(P["gdn_conv_w"][j]), f(P["gdn_a_log"][j]),
                                      f(P["gdn_dt_bias"][j]), f(P["gdn_norm_g"][j]))
                for n in GDN_NAMES:
                    m[f"L{i}_{hh}_{n}"] = lay[n]
            m[f"L{i}_wproj"] = f(P["gdn_w_out"][j])
            m[f"L{i}_w1"] = f(np.stack([P["ffn_w1"][j][:, :FE], P["ffn_w1"][j][:, FE:]]))
            m[f"L{i}_w3"] = f(np.stack([P["ffn_w3"][j][:, :FE], P["ffn_w3"][j][:, FE:]]))
            m[f"L{i}_w2"] = f(np.stack([P["ffn_w2"][j][:FE], P["ffn_w2"][j][FE:]]))
        else:
            for gh in range(2):
                lay = s5_host_layout(gh, f(P["s5_lam_re"][j]), f(P["s5_lam_im"][j]), f(P["s5_log_dt"][j]),
                                     f(P["s5_b_re"][j]), f(P["s5_b_im"][j]), f(P["s5_c_re"][j]), f(P["s5_c_im"][j]),
                                     f(P["s5_d"][j]), f(P["s5_w_in"][j]))
                for n in S5_NAMES:
                    m[f"L{i}_{gh}_{n}"] = lay[n]
            m[f"L{i}_wproj"] = f(P["s5_w_glu"][j])
            m[f"L{i}_w1"] = f(P["moe_w1"][j]); m[f"L{i}_w3"] = f(P["moe_w3"][j]); m[f"L{i}_w2"] = f(P["moe_w2"][j])
            m[f"L{i}_wr"] = f(P["moe_w_router"][j]); m[f"L{i}_br"] = f(P["moe_b_router"][j]).reshape(1, NEXP)
    return m


SEQ = 8192
BATCH = 4
_PROGS = {}


def kernel(**P):
    x = np.ascontiguousarray(np.asarray(P["x"], dtype=np.float32))
    T = x.shape[1]
    if T not in _PROGS:
        _PROGS[T] = build_fused(T)
    nc = _PROGS[T]
    shared = None
    in_maps = []
    for c in range(8):
        b = c % BATCH
        m = fused_inputs(x[b], P) if shared is None else dict(shared)
        if shared is None:
            shared = m
        else:
            m["xT0"] = np.ascontiguousarray(x[b].T)
            m["xtok0"] = x[b]
        in_maps.append(m)
    res = run_bass_kernel_spmd(nc, in_maps, core_ids=list(range(8))).results
    return np.stack([res[b]["y"] for b in range(BATCH)]).astype(np.float32)
```

```python
import contextlib
import numpy as np
import concourse.bass as bass
import concourse.mybir as mybir
from concourse.bass_utils import run_bass_kernel_spmd

F32 = mybir.dt.float32
BF16 = mybir.dt.bfloat16
AF = mybir.ActivationFunctionType
ALU = mybir.AluOpType
AX = mybir.AxisListType

D = 1024
DEPTH = 4
ALPHA = (2 * DEPTH) ** 0.25
LN_EPS = 1e-5
NORM_EPS = 1e-6
NEXP = 8
FE = 1408
NFT = FE // 128


class Sched:
    ENGS = ("pe", "dve", "act", "pool", "sp")

    def __init__(self, nc, es):
        self.nc = nc
        self.es = es
        self.q = {e: [] for e in self.ENGS}
        self.sem = {}
        self.cnt = {}
        self.known = {e: {} for e in self.ENGS}
        self.last_w = {}
        self.readers = {}
        self.phase = 0
        self.ename = {}
        for e in ("pe", "dve", "act", "pool"):
            self.ename[e] = e + "0"
            self._mksem(e + "0")
        self.n_ops = 0

    def _mksem(self, name):
        self.sem[name] = self.es.enter_context(self.nc.semaphore("s_" + name))
        self.cnt[name] = 0

    def _deps(self, reads, writes):
        deps = {}

        def add(tok):
            if tok is None:
                return
            s, v = tok
            if deps.get(s, 0) < v:
                deps[s] = v

        for k in reads:
            add(self.last_w.get(k))
        for k in writes:
            add(self.last_w.get(k))
            for s, v in self.readers.get(k, {}).items():
                add((s, v))
        return deps

    def _commit(self, tok, reads, writes):
        for k in writes:
            self.last_w[k] = tok
            self.readers[k] = {}
        for k in reads:
            if k in writes:
                continue
            r = self.readers.setdefault(k, {})
            if r.get(tok[0], 0) < tok[1]:
                r[tok[0]] = tok[1]

    def _waits(self, eng, deps):
        waits = []
        kn = self.known[eng]
        for s, v in deps.items():
            if eng == "pe" and s == self.ename["pe"]:
                continue
            if kn.get(s, 0) < v:
                kn[s] = v
                waits.append((s, v))
        return waits

    def op(self, eng, method, reads=(), writes=(), **kw):
        if eng != "pe":
            ex = [k for k in reads if ".bank" in k and k not in writes]
            if ex:
                writes = list(writes) + ex
        deps = self._deps(reads, writes)
        waits = self._waits(eng, deps)
        en = self.ename[eng]
        self.cnt[en] += 1
        tok = (en, self.cnt[en])
        self.q[eng].append((waits, (method, kw), (en, 1)))
        self._commit(tok, reads, writes)
        self.n_ops += 1

    def dma(self, queue, stream, out, in_, reads=(), writes=(), **kw):
        sname = "d_" + stream.split(".", 1)[-1]
        if sname not in self.sem:
            self._mksem(sname)
        deps = self._deps(reads, writes)
        waits = self._waits(queue, deps)
        self.cnt[sname] += 16
        tok = (sname, self.cnt[sname])
        kw = dict(kw)
        kw["out"] = out
        kw["in_"] = in_
        self.q[queue].append((waits, ("dma_start", kw), (sname, 16)))
        self._commit(tok, reads, writes)
        self.n_ops += 1

    def coll(self, kind, stream, ins, outs, replica_groups, reads=(), writes=()):
        sname = "d_" + stream
        if sname not in self.sem:
            self._mksem(sname)
        deps = self._deps(reads, writes)
        waits = self._waits("pool", deps)
        self.cnt[sname] += 16
        tok = (sname, self.cnt[sname])
        kw = dict(kind=kind, op=ALU.bypass, replica_groups=replica_groups, ins=list(ins), outs=list(outs))
        self.q["pool"].append((waits, ("collective_compute", kw), (sname, 16)))
        self._commit(tok, reads, writes)
        self.n_ops += 1

    def barrier(self):
        for e in self.ENGS:
            waits = []
            for s_, v in self.cnt.items():
                if v > 0 and self.known[e].get(s_, 0) < v:
                    self.known[e][s_] = v
                    waits.append((s_, v))
            if waits:
                self.q[e].append((waits, None, None))

    def finish(self):
        self.barrier()

    def new_phase(self):
        self.phase += 1
        self.last_w = {}
        self.readers = {}
        for e in ("pe", "dve", "act"):
            old_name = self.ename[e]
            for kn in self.known.values():
                kn.pop(old_name, None)
            del self.cnt[old_name]
            nm = f"{e}{self.phase}"
            self.ename[e] = nm
            self._mksem(nm)

    def emit(self):
        nc = self.nc
        S = self

        def replay(name, eng):
            for waits, fn, inc in S.q[name]:
                for s, v in waits:
                    eng.wait_ge(S.sem[s], v)
                if fn is None:
                    continue
                inst = getattr(eng, fn[0])(**fn[1])
                inst.then_inc(S.sem[inc[0]], inc[1])
            S.q[name] = []

        with nc.Block() as block:
            @block.tensor
            def _(e):
                replay("pe", e)

            @block.vector
            def _(e):
                replay("dve", e)

            @block.scalar
            def _(e):
                replay("act", e)

            @block.gpsimd
            def _(e):
                replay("pool", e)

            @block.sync
            def _(e):
                replay("sp", e)


class Ctx:
    _uid = [0]

    def __init__(self, nc, es, S=None):
        self.nc = nc
        self.es = es
        self.S = S if S is not None else Sched(nc, es)
        Ctx._uid[0] += 1
        self.n = Ctx._uid[0] * 1000

    def sb(self, shape, dt=F32, name=None):
        self.n += 1
        return self.es.enter_context(self.nc.sbuf_tensor(f"{name or 'sb'}_{self.n}", list(shape), dt))

    def ps(self, shape, dt=F32, name=None):
        self.n += 1
        return self.es.enter_context(self.nc.psum_tensor(f"{name or 'ps'}_{self.n}", list(shape), dt))


def tp_phase(C, T, glu, moe, featT, wproj, xres, lnp, w1, w3, w2, wr, br, ident_d, xout, xoutT, tb=1024, pfx="tp"):
    S = C.S
    nproj = 2 * D if glu else D
    ne = NEXP if moe else 2
    tb = min(tb, T)
    nblk = T // tb
    ntt = tb // 128
    hw = min(512, tb)
    nhalf = tb // hw
    P = pfx

    def K(name):
        return P + "." + name

    ident = C.sb([128, 128], F32, "ident")
    lnb = C.sb([128, 4, D], F32, "lnb")
    wproj_sb = C.sb([128, 8, nproj], BF16, "wproj")
    wr_sb = C.sb([128, 8, NEXP], F32, "wr")
    br_sb = C.sb([128, NEXP], F32, "br")
    w1_sb = C.sb([128, 8, FE], BF16, "w1")
    w3_sb = C.sb([128, 8, FE], BF16, "w3")
    w2_sb = C.sb([128, NFT, D], BF16, "w2")
    featT_sb = [C.sb([128, 8, 128], BF16, "featT") for _ in range(2)]
    _xres1 = C.sb([128, D], F32, "xres")
    xres_sb = [_xres1, _xres1]
    hglu_sb = C.sb([128, 512], F32, "hglu")
    stats = [C.sb([128, 2, 6], F32, "stats") for _ in range(2)]
    mv = [C.sb([128, 2], F32, "mv") for _ in range(2)]
    rstd = [C.sb([128, 1], F32, "rstd") for _ in range(2)]
    x1 = [C.sb([128, D], F32, "x1") for _ in range(2)]
    _x1T321 = C.sb([128, 8, 128], F32, "x1T32")
    x1T32 = [_x1T321, _x1T321]
    x1T = C.sb([128, 8, tb], BF16, "x1T")
    yacc = C.sb([128, ntt, D], F32, "yacc")
    gates = C.sb([128, ntt, NEXP], F32, "gates")
    rt = [C.sb([128, NEXP], F32, "rt") for _ in range(4)]
    rcol = [C.sb([128, 1], F32, "rcol") for _ in range(4)]
    hT = C.sb([128, NFT, tb], BF16, "hT")
    epsc = C.sb([128, 1], F32, "epsc")
    pg = [C.ps([128, 512], F32, "pg") for _ in range(2)]
    pu = [C.ps([128, 512], F32, "pu") for _ in range(2)]
    py = [C.ps([128, 512], F32, "py") for _ in range(2)]
    ptr = C.ps([128, 512], F32, "ptr")
    prt = C.ps([128, 512], F32, "prt")

    S.op("dve", "memset", ap=epsc[:], constant=LN_EPS, writes=[K("epsc")])
    S.dma("sp", K("c0"), ident[:], ident_d, writes=[K("ident")])
    for i in range(4):
        S.dma("sp", K("c1"), lnb[:, i, :], lnp[i:i + 1, :].partition_broadcast(128), writes=[K("lnb")])
    S.dma("pool", K("c2"), wproj_sb[:], wproj.rearrange("(j p) n -> p j n", p=128), writes=[K("wproj")])
    S.dma("sp", K("c3"), wr_sb[:], wr.rearrange("(j p) n -> p j n", p=128), writes=[K("wr")])
    S.dma("sp", K("c4"), br_sb[:], br.partition_broadcast(128), writes=[K("br")])

    def layer_norm(src, src_key, dst, dst_key, gi, idx):
        st, m, rs = stats[idx], mv[idx], rstd[idx]
        kst, kmv, krs = K(f"st{idx}"), K(f"mv{idx}"), K(f"rstd{idx}")
        for c in range(2):
            S.op("dve", "bn_stats", out=st[:, c, :], in_=src[:, c * 512:(c + 1) * 512],
                 reads=[src_key], writes=[kst + str(c)])
        S.op("dve", "bn_aggr", out=m[:], in_=st[:], reads=[kst + "0", kst + "1"], writes=[kmv])
        S.op("act", "activation", out=rs[:], in_=m[:, 1:2], func=AF.Sqrt, bias=epsc[:, 0:1], scale=1.0,
             reads=[kmv, K("epsc")], writes=[krs])
        S.op("dve", "reciprocal", out=rs[:], in_=rs[:], reads=[krs], writes=[krs])
        S.op("dve", "tensor_scalar", out=dst, in0=src, scalar1=m[:, 0:1], scalar2=rs[:, 0:1],
             op0=ALU.subtract, op1=ALU.mult, reads=[src_key, kmv, krs], writes=[dst_key])
        S.op("pool", "tensor_tensor", out=dst, in0=dst, in1=lnb[:, gi, :], op=ALU.mult,
             reads=[dst_key, K("lnb")], writes=[dst_key])
        S.op("pool", "tensor_tensor", out=dst, in0=dst, in1=lnb[:, gi + 1, :], op=ALU.add,
             reads=[dst_key, K("lnb")], writes=[dst_key])

    cnt = 0
    for blk in range(nblk):
        t0 = blk * tb
        for tt in range(ntt):
            i2 = tt % 2
            tok0 = t0 + tt * 128
            fT, xr, r = featT_sb[i2], xres_sb[i2], xres_sb[i2]
            kfT, kxr, kr, kx1 = K(f"fT{i2}"), K("xr0"), K("xr0"), K(f"x1_{i2}")
            S.dma("pool", kfT, fT[:], featT[:, tok0:tok0 + 128].rearrange("(j p) t -> p j t", p=128), writes=[kfT])
            S.dma("sp", kxr, xr[:], xres[tok0:tok0 + 128, :], writes=[kxr])
            for nh in range(2):
                c0, c1 = nh * 512, (nh + 1) * 512
                pv, kpv = py[nh], K(f"py{nh}")
                for k in range(8):
                    S.op("pe", "matmul", out=pv[:], lhsT=fT[:, k, :], rhs=wproj_sb[:, k, c0:c1],
                         start=(k == 0), stop=(k == 7), reads=[kfT, K("wproj")], writes=[kpv])
                if glu:
                    pgt, kpg = pg[nh], K(f"pg{nh}")
                    for k in range(8):
                        S.op("pe", "matmul", out=pgt[:], lhsT=fT[:, k, :], rhs=wproj_sb[:, k, D + c0:D + c1],
                             start=(k == 0), stop=(k == 7), reads=[kfT, K("wproj")], writes=[kpg])
                    S.op("act", "activation", out=hglu_sb[:], in_=pgt[:], func=AF.Sigmoid,
                         reads=[kpg], writes=[K("hglu")])
                    S.op("dve", "tensor_tensor", out=hglu_sb[:], in0=hglu_sb[:], in1=pv[:], op=ALU.mult,
                         reads=[K("hglu"), kpv], writes=[K("hglu")])
                    S.op("dve", "scalar_tensor_tensor", out=r[:, c0:c1], in0=xr[:, c0:c1], scalar=ALPHA,
                         in1=hglu_sb[:], op0=ALU.mult, op1=ALU.add, reads=[kxr, K("hglu")], writes=[kr])
                else:
                    S.op("dve", "scalar_tensor_tensor", out=r[:, c0:c1], in0=xr[:, c0:c1], scalar=ALPHA,
                         in1=pv[:], op0=ALU.mult, op1=ALU.add, reads=[kxr, kpv], writes=[kr])
            xx = x1[i2]
            layer_norm(r[:], kr, xx[:], kx1, 0, i2)
            S.op("act", "mul", out=yacc[:, tt, :], in_=xx[:], mul=ALPHA, reads=[kx1], writes=[K(f"yacc{tt}")])
            xt32 = x1T32[i2]
            for kh in range(2):
                pb, kpb = (ptr, K("ptr")) if kh == 0 else (prt, K("prt"))
                for q4 in range(4):
                    k = kh * 4 + q4
                    S.op("pe", "transpose", out=pb[:, q4 * 128:(q4 + 1) * 128], in_=xx[:, k * 128:(k + 1) * 128],
                         identity=ident[:], reads=[kx1, K("ident")], writes=[kpb])
                S.op("act", "copy", out=xt32[:, kh * 4:(kh + 1) * 4, :].rearrange("p k t -> p (k t)"), in_=pb[:],
                     reads=[kpb], writes=[K("x1T32_0")])
                S.op("pool", "tensor_copy", out=x1T[:, kh * 4:(kh + 1) * 4, tt * 128:(tt + 1) * 128],
                     in_=xt32[:, kh * 4:(kh + 1) * 4, :], reads=[K("x1T32_0")], writes=[K(f"x1T_{tt}")])
            if moe:
                for k in range(8):
                    S.op("pe", "matmul", out=prt[:, 0:NEXP], lhsT=xt32[:, k, :], rhs=wr_sb[:, k, :],
                         start=(k == 0), stop=(k == 7), reads=[K("x1T32_0"), K("wr")], writes=[K("prt")])
                lg, mk, l2, ex = rt
                m1, m2, sm, rsm = rcol
                kk = lambda n: K("rt_" + n)
                S.op("dve", "tensor_tensor", out=lg[:], in0=prt[:, 0:NEXP], in1=br_sb[:], op=ALU.add,
                     reads=[K("prt"), K("br")], writes=[kk("lg")])
                S.op("dve", "reduce_max", out=m1[:], in_=lg[:], axis=AX.X, reads=[kk("lg")], writes=[kk("m1")])
                S.op("dve", "tensor_scalar", out=mk[:], in0=lg[:], scalar1=m1[:, 0:1], scalar2=-1e30,
                     op0=ALU.is_ge, op1=ALU.mult, reads=[kk("lg"), kk("m1")], writes=[kk("mk")])
                S.op("dve", "tensor_tensor", out=l2[:], in0=lg[:], in1=mk[:], op=ALU.add,
                     reads=[kk("lg"), kk("mk")], writes=[kk("l2")])
                S.op("dve", "reduce_max", out=m2[:], in_=l2[:], axis=AX.X, reads=[kk("l2")], writes=[kk("m2")])
                S.op("dve", "tensor_scalar", out=ex[:], in0=lg[:], scalar1=m1[:, 0:1], scalar2=None,
                     op0=ALU.subtract, reads=[kk("lg"), kk("m1")], writes=[kk("ex")])
                S.op("act", "activation", out=ex[:], in_=ex[:], func=AF.Exp, reads=[kk("ex")], writes=[kk("ex")])
                S.op("dve", "scalar_tensor_tensor", out=ex[:], in0=lg[:], scalar=m2[:, 0:1], in1=ex[:],
                     op0=ALU.is_ge, op1=ALU.mult, reads=[kk("lg"), kk("m2"), kk("ex")], writes=[kk("ex")])
                S.op("dve", "reduce_sum", out=sm[:], in_=ex[:], axis=AX.X, reads=[kk("ex")], writes=[kk("sm")])
                S.op("dve", "reciprocal", out=rsm[:], in_=sm[:], reads=[kk("sm")], writes=[kk("rsm")])
                S.op("dve", "tensor_scalar", out=gates[:, tt, :], in0=ex[:], scalar1=rsm[:, 0:1], scalar2=None,
                     op0=ALU.mult, reads=[kk("ex"), kk("rsm")], writes=[K(f"gates{tt}")])
        for ex_i in range(ne):
            S.dma("pool", K("w1"), w1_sb[:], w1[ex_i].rearrange("(j p) n -> p j n", p=128), writes=[K("w1")])
            S.dma("pool", K("w3"), w3_sb[:], w3[ex_i].rearrange("(j p) n -> p j n", p=128), writes=[K("w3")])
            S.dma("pool", K("w2"), w2_sb[:], w2[ex_i].rearrange("(j p) n -> p j n", p=128), writes=[K("w2")])
            for hf in range(nhalf):
                ts0, ts1 = hf * hw, (hf + 1) * hw
                tkeys = [K(f"x1T_{tt}") for tt in range(ts0 // 128, ts1 // 128)]
                for f in range(NFT):
                    b = cnt % 2
                    cnt += 1
                    f0, f1 = f * 128, (f + 1) * 128
                    for k in range(8):
                        S.op("pe", "matmul", out=pg[b][:, 0:hw], lhsT=w1_sb[:, k, f0:f1], rhs=x1T[:, k, ts0:ts1],
                             start=(k == 0), stop=(k == 7), reads=[K("w1")] + tkeys, writes=[K(f"pg{b}")])
                    for k in range(8):
                        S.op("pe", "matmul", out=pu[b][:, 0:hw], lhsT=w3_sb[:, k, f0:f1], rhs=x1T[:, k, ts0:ts1],
                             start=(k == 0), stop=(k == 7), reads=[K("w3")] + tkeys, writes=[K(f"pu{b}")])
                    S.op("act", "activation", out=hT[:, f, ts0:ts1], in_=pg[b][:, 0:hw], func=AF.Silu,
                         reads=[K(f"pg{b}")], writes=[K(f"hT{f}_{hf}")])
                    S.op("dve", "tensor_tensor", out=hT[:, f, ts0:ts1], in0=hT[:, f, ts0:ts1], in1=pu[b][:, 0:hw],
                         op=ALU.mult, reads=[K(f"hT{f}_{hf}"), K(f"pu{b}")], writes=[K(f"hT{f}_{hf}")])
            for hf in range(nhalf):
                hkeys = [K(f"hT{f}_{hf}") for f in range(NFT)]
                for tl in range(hw // 128):
                    tt = hf * (hw // 128) + tl
                    kya = K(f"yacc{tt}")
                    for nh in range(2):
                        c0, c1 = nh * 512, (nh + 1) * 512
                        kpy = K(f"py{nh}")
                        for f in range(NFT):
                            S.op("pe", "matmul", out=py[nh][:], lhsT=hT[:, f, tt * 128:(tt + 1) * 128],
                                 rhs=w2_sb[:, f, c0:c1], start=(f == 0), stop=(f == NFT - 1),
                                 reads=hkeys + [K("w2")], writes=[kpy])
                        if moe:
                            S.op("dve", "scalar_tensor_tensor", out=yacc[:, tt, c0:c1], in0=py[nh][:],
                                 scalar=gates[:, tt, ex_i:ex_i + 1], in1=yacc[:, tt, c0:c1], op0=ALU.mult,
                                 op1=ALU.add, reads=[kpy, K(f"gates{tt}"), kya], writes=[kya])
                        else:
                            S.op("dve", "tensor_tensor", out=yacc[:, tt, c0:c1], in0=py[nh][:],
                                 in1=yacc[:, tt, c0:c1], op=ALU.add, reads=[kpy, kya], writes=[kya])
        for tt in range(ntt):
            i2 = tt % 2
            tok0 = t0 + tt * 128
            xx, kxo = x1[i2], K(f"x1_{i2}")
            layer_norm(yacc[:, tt, :], K(f"yacc{tt}"), xx[:], kxo, 2, i2)
            S.dma("sp", kxo, xout[tok0:tok0 + 128, :], xx[:], reads=[kxo])
            xt, kxt = x1T32[i2], K("x1T32_0")
            for kh in range(2):
                pb, kpb = (ptr, K("ptr")) if kh == 0 else (prt, K("prt"))
                for q4 in range(4):
                    k = kh * 4 + q4
                    S.op("pe", "transpose", out=pb[:, q4 * 128:(q4 + 1) * 128], in_=xx[:, k * 128:(k + 1) * 128],
                         identity=ident[:], reads=[kxo, K("ident")], writes=[kpb])
                S.op("act", "copy", out=xt[:, kh * 4:(kh + 1) * 4, :].rearrange("p k t -> p (k t)"), in_=pb[:],
                     reads=[kpb], writes=[kxt])
            S.dma("sp", kxt, xoutT[:, tok0:tok0 + 128].rearrange("(j p) t -> p j t", p=128), xt[:], reads=[kxt])


def build_tp(T, glu, moe, tb=1024):
    nc = bass.Bass("TRN2", target_bir_lowering=False)
    nproj = 2 * D if glu else D
    ne = NEXP if moe else 2
    dt = lambda n, s, k="ExternalInput": nc.dram_tensor(n, s, F32, kind=k).ap()
    featT = dt("featT", [D, T]); wproj = dt("wproj", [D, nproj]); xres = dt("xres", [T, D])
    lnp = dt("lnp", [4, D]); w1 = dt("w1", [ne, D, FE]); w3 = dt("w3", [ne, D, FE]); w2 = dt("w2", [ne, FE, D])
    wr = dt("wr", [D, NEXP]); br = dt("br", [1, NEXP]); ident_d = dt("ident", [128, 128])
    xout = dt("xout", [T, D], "ExternalOutput"); xoutT = dt("xoutT", [D, T], "ExternalOutput")
    with contextlib.ExitStack() as es:
        C = Ctx(nc, es)
        tp_phase(C, T, glu, moe, featT, wproj, xres, lnp, w1, w3, w2, wr, br, ident_d, xout, xoutT, tb=tb)
        C.S.finish()
        C.S.emit()
    return nc


S5_L = 128
PI = float(np.pi)


def s5_phase(C, T, xT, w_in, lamr_d, lami_d, ldt_d, bpad_re_d, bpad_im_d, cpad_re_d, cpad_im_d, dsk_d, iota_d,
             hidT, pfx="s5"):
    S = C.S
    L = S5_L
    NB = T // 512
    NGP = 16
    P = pfx

    def K(n):
        return P + "." + n

    w_sb = C.sb([128, 8, 512], BF16, "s5w")
    xT_sb = [C.sb([128, 8, 512], BF16, "s5x") for _ in range(2)]
    u_sb = [C.sb([128, 4, 512], F32, "s5u") for _ in range(2)]
    bre = C.sb([128, NGP, 128], F32, "bre")
    bim = C.sb([128, NGP, 128], F32, "bim")
    cre = C.sb([128, NGP, 128], F32, "cre")
    cim = C.sb([128, NGP, 128], F32, "cim")
    dsk = C.sb([128, 4], F32, "dsk")
    iota = C.sb([128, L], F32, "iota")
    prm = {n: C.sb([128, NGP], F32, "p_" + n) for n in
           ("lamr", "lami", "ldt", "dt", "zr", "zi", "r", "cz", "sz", "ar", "ai", "den", "cr", "ci", "ncr",
            "t1", "t2", "zl", "er", "ei", "nei")}
    negpi = C.sb([128, 1], F32, "negpi")
    tre = C.sb([128, NGP, L], F32, "tre")
    tim = C.sb([128, NGP, L], F32, "tim")
    cph = C.sb([128, NGP, L], F32, "cph")
    sph = C.sb([128, NGP, L], F32, "sph")
    rmat = C.sb([128, NGP, L], F32, "rmat")
    ang = C.sb([128, L], F32, "ang")
    ang2 = C.sb([128, L], F32, "ang2")
    hp = C.sb([128, NGP, 2], F32, "hp")
    tmpc2 = [C.sb([128, 2], F32, "tmpc") for _ in range(2)]
    BUr = [C.sb([128, 512], F32, "BUr") for _ in range(2)]
    BUi = [C.sb([128, 512], F32, "BUi") for _ in range(2)]
    mt = [[C.sb([128, 512], F32, "mt") for _ in range(4)] for _ in range(2)]
    btr = [C.sb([128, 512], F32, "btr") for _ in range(2)]
    bti = [C.sb([128, 512], F32, "bti") for _ in range(2)]
    wre = [C.sb([128, 512], F32, "wre") for _ in range(2)]
    wim = [C.sb([128, 512], F32, "wim") for _ in range(2)]
    hre = [[C.sb([128, 512], F32, "hre") for _ in range(4)] for _ in range(2)]
    him = [[C.sb([128, 512], F32, "him") for _ in range(4)] for _ in range(2)]
    yt = [C.sb([128, 512], F32, "yt") for _ in range(2)]
    ho = [C.sb([128, 512], F32, "ho") for _ in range(2)]
    pu_ = [C.ps([128, 512], F32, "s5pu") for _ in range(2)]
    pbr = [C.ps([128, 512], F32, "s5pbr") for _ in range(2)]
    pbi = [C.ps([128, 512], F32, "s5pbi") for _ in range(2)]
    pyy = [C.ps([128, 512], F32, "s5py") for _ in range(2)]

    S.dma("pool", K("w"), w_sb[:], w_in.rearrange("(j p) n -> p j n", p=128), writes=[K("w")])
    for nm, dd, sbt in (("bre", bpad_re_d, bre), ("bim", bpad_im_d, bim), ("cre", cpad_re_d, cre), ("cim", cpad_im_d, cim)):
        S.dma("sp", K(nm), sbt[:], dd.rearrange("g k m -> k g m"), writes=[K(nm)])
    S.dma("sp", K("dsk"), dsk[:], dsk_d, writes=[K("dsk")])
    S.dma("sp", K("iota"), iota[:], iota_d, writes=[K("iota")])
    S.dma("sp", K("lamr"), prm["lamr"][:], lamr_d, writes=[K("lamr")])
    S.dma("sp", K("lami"), prm["lami"][:], lami_d, writes=[K("lami")])
    S.dma("sp", K("ldt"), prm["ldt"][:], ldt_d, writes=[K("ldt")])
    S.op("dve", "memset", ap=negpi[:], constant=-PI, writes=[K("negpi")])
    itmp = C.sb([128, L], mybir.dt.int32, "itmp")
    ftmp = C.sb([128, L], F32, "ftmp")
    S.op("dve", "memset", ap=hp[:], constant=0.0, writes=[K(f"hp{g}") for g in range(NGP)])

    def pp(n):
        return prm[n][:]

    def ew(eng, method, out_n, reads, **kw):
        S.op(eng, method, reads=[K(x) for x in reads], writes=[K(out_n)], **kw)

    def sincos(angle_ap, angle_key, sin_ap, sin_key, cos_ap, cos_key, tmp_ap, tmp_key, width):
        for off, o_ap, o_key in ((0.5, sin_ap, sin_key), (0.75, cos_ap, cos_key)):
            S.op("dve", "tensor_scalar", out=tmp_ap, in0=angle_ap, scalar1=1.0 / (2.0 * PI), scalar2=off,
                 op0=ALU.mult, op1=ALU.add, reads=[angle_key], writes=[tmp_key])
            S.op("dve", "tensor_copy", out=itmp[:, 0:width], in_=tmp_ap, reads=[tmp_key], writes=[K("itmp")])
            S.op("dve", "tensor_copy", out=ftmp[:, 0:width], in_=itmp[:, 0:width], reads=[K("itmp")], writes=[K("ftmp")])
            S.op("dve", "tensor_tensor", out=tmp_ap, in0=tmp_ap, in1=ftmp[:, 0:width], op=ALU.subtract,
                 reads=[tmp_key, K("ftmp")], writes=[tmp_key])
            S.op("dve", "scalar_tensor_tensor", out=tmp_ap, in0=tmp_ap, scalar=0.0, in1=tmp_ap, op0=ALU.is_lt,
                 op1=ALU.add, reads=[tmp_key], writes=[tmp_key])
            S.op("act", "activation", out=o_ap, in_=tmp_ap, func=AF.Sin, bias=negpi[:, 0:1], scale=2.0 * PI,
                 reads=[tmp_key, K("negpi")], writes=[o_key])

    ew("act", "activation", "dt", ["ldt"], out=pp("dt"), in_=pp("ldt"), func=AF.Exp)
    ew("dve", "tensor_tensor", "zr", ["lamr", "dt"], out=pp("zr"), in0=pp("lamr"), in1=pp("dt"), op=ALU.mult)
    ew("dve", "tensor_tensor", "zi", ["lami", "dt"], out=pp("zi"), in0=pp("lami"), in1=pp("dt"), op=ALU.mult)
    ew("act", "activation", "r", ["zr"], out=pp("r"), in_=pp("zr"), func=AF.Exp)
    sincos(pp("zi"), K("zi"), pp("sz"), K("sz"), pp("cz"), K("cz"), pp("t1"), K("t1"), NGP)
    ew("dve", "tensor_tensor", "ar", ["r", "cz"], out=pp("ar"), in0=pp("r"), in1=pp("cz"), op=ALU.mult)
    ew("dve", "tensor_scalar", "ar", ["ar"], out=pp("ar"), in0=pp("ar"), scalar1=-1.0, scalar2=None, op0=ALU.add)
    ew("dve", "tensor_tensor", "ai", ["r", "sz"], out=pp("ai"), in0=pp("r"), in1=pp("sz"), op=ALU.mult)
    ew("dve", "tensor_tensor", "den", ["lamr"], out=pp("den"), in0=pp("lamr"), in1=pp("lamr"), op=ALU.mult)
    ew("dve", "tensor_tensor", "t1", ["lami"], out=pp("t1"), in0=pp("lami"), in1=pp("lami"), op=ALU.mult)
    ew("dve", "tensor_tensor", "den", ["den", "t1"], out=pp("den"), in0=pp("den"), in1=pp("t1"), op=ALU.add)
    ew("dve", "reciprocal", "den", ["den"], out=pp("den"), in_=pp("den"))
    ew("dve", "tensor_tensor", "t1", ["ar", "lamr"], out=pp("t1"), in0=pp("ar"), in1=pp("lamr"), op=ALU.mult)
    ew("dve", "tensor_tensor", "t2", ["ai", "lami"], out=pp("t2"), in0=pp("ai"), in1=pp("lami"), op=ALU.mult)
    ew("dve", "tensor_tensor", "cr", ["t1", "t2"], out=pp("cr"), in0=pp("t1"), in1=pp("t2"), op=ALU.add)
    ew("dve", "tensor_tensor", "cr", ["cr", "den"], out=pp("cr"), in0=pp("cr"), in1=pp("den"), op=ALU.mult)
    ew("dve", "tensor_tensor", "t1", ["ai", "lamr"], out=pp("t1"), in0=pp("ai"), in1=pp("lamr"), op=ALU.mult)
    ew("dve", "tensor_tensor", "t2", ["ar", "lami"], out=pp("t2"), in0=pp("ar"), in1=pp("lami"), op=ALU.mult)
    ew("dve", "tensor_tensor", "ci", ["t1", "t2"], out=pp("ci"), in0=pp("t1"), in1=pp("t2"), op=ALU.subtract)
    ew("dve", "tensor_tensor", "ci", ["ci", "den"], out=pp("ci"), in0=pp("ci"), in1=pp("den"), op=ALU.mult)
    ew("dve", "tensor_scalar", "ncr", ["cr"], out=pp("ncr"), in0=pp("cr"), scalar1=-1.0, scalar2=None, op0=ALU.mult)
    ew("dve", "tensor_scalar", "zl", ["zi"], out=pp("zl"), in0=pp("zi"), scalar1=float(L), scalar2=None, op0=ALU.mult)
    sincos(pp("zl"), K("zl"), pp("ei"), K("ei"), pp("er"), K("er"), pp("t1"), K("t1"), NGP)
    ew("dve", "tensor_scalar", "nei", ["ei"], out=pp("nei"), in0=pp("ei"), scalar1=-1.0, scalar2=None, op0=ALU.mult)
    for g in range(NGP):
        S.op("dve", "tensor_scalar", out=ang[:], in0=iota[:], scalar1=prm["zi"][:, g:g + 1], scalar2=None,
             op0=ALU.mult, reads=[K("iota"), K("zi")], writes=[K("ang")])
        sincos(ang[:], K("ang"), sph[:, g, :], K("sph"), cph[:, g, :], K("cph"), ang2[:], K("ang2"), L)
        S.op("dve", "tensor_scalar", out=ang2[:], in0=cph[:, g, :], scalar1=prm["cr"][:, g:g + 1], scalar2=None,
             op0=ALU.mult, reads=[K("cph"), K("cr")], writes=[K("ang2")])
        S.op("dve", "scalar_tensor_tensor", out=tre[:, g, :], in0=sph[:, g, :], scalar=prm["ci"][:, g:g + 1],
             in1=ang2[:], op0=ALU.mult, op1=ALU.add, reads=[K("sph"), K("ci"), K("ang2")], writes=[K("tre")])
        S.op("dve", "tensor_scalar", out=ang2[:], in0=cph[:, g, :], scalar1=prm["ci"][:, g:g + 1], scalar2=None,
             op0=ALU.mult, reads=[K("cph"), K("ci")], writes=[K("ang2")])
        S.op("dve", "scalar_tensor_tensor", out=tim[:, g, :], in0=sph[:, g, :], scalar=prm["ncr"][:, g:g + 1],
             in1=ang2[:], op0=ALU.mult, op1=ALU.add, reads=[K("sph"), K("ncr"), K("ang2")], writes=[K("tim")])
        S.op("pool", "memset", ap=rmat[:, g, :], constant=1.0, writes=[K("rmat")])
        S.op("dve", "tensor_scalar", out=rmat[:, g, :], in0=rmat[:, g, :], scalar1=prm["r"][:, g:g + 1], scalar2=None,
             op0=ALU.mult, reads=[K("rmat"), K("r")], writes=[K("rmat")])

    NC4 = 512 // L

    def v3(t):
        return t[:].rearrange("p (c l) -> p c l", l=L)

    def tb3(tab, g):
        return tab[:, g:g + 1, :].broadcast_to([128, NC4, L])

    gi = 0
    for blk in range(NB):
        t0 = blk * 512
        bi = blk % 2
        xs, us = xT_sb[bi], u_sb[bi]
        kx, ku = K(f"x{bi}"), K(f"u{bi}")
        S.dma("pool", kx, xs[:], xT[:, t0:t0 + 512].rearrange("(j p) t -> p j t", p=128), writes=[kx])
        for ft in range(4):
            b2 = ft % 2
            for k in range(8):
                S.op("pe", "matmul", out=pu_[b2][:], lhsT=w_sb[:, k, ft * 128:(ft + 1) * 128], rhs=xs[:, k, :],
                     start=(k == 0), stop=(k == 7), reads=[K("w"), kx], writes=[K(f"pu{b2}")])
            S.op("act", "copy", out=us[:, ft, :], in_=pu_[b2][:], reads=[K(f"pu{b2}")], writes=[ku + f"_{ft}"])
        for ft in range(4):
            f2 = ft % 2
            for gpair in range(0, 4, 2):
                recs = []
                for gl4 in (gpair, gpair + 1):
                    rec = []
                    real_op = S.op
                    S.op = (lambda eng, method, reads=(), writes=(), _rec=rec, **kw:
                            _rec.append((eng, method, reads, writes, kw)))
                    try:
                        g = ft * 4 + gl4
                        b2 = gi % 2
                        gi += 1
                        kb = lambda n: K(f"{n}{b2}")
                        tmpc = tmpc2[b2]
                        S.op("pe", "matmul", out=pbr[b2][:], lhsT=bre[:, g, :], rhs=us[:, ft, :], start=True, stop=True,
                             reads=[K("bre"), ku + f"_{ft}"], writes=[kb("pbr")])
                        S.op("pe", "matmul", out=pbi[b2][:], lhsT=bim[:, g, :], rhs=us[:, ft, :], start=True, stop=True,
                             reads=[K("bim"), ku + f"_{ft}"], writes=[kb("pbi")])
                        S.op("act", "copy", out=BUr[b2][:], in_=pbr[b2][:], reads=[kb("pbr")], writes=[kb("BUr")])
                        S.op("act", "copy", out=BUi[b2][:], in_=pbi[b2][:], reads=[kb("pbi")], writes=[kb("BUi")])
                        m0, m1, m2, m3 = mt[b2]
                        S.op("dve", "tensor_tensor", out=v3(m0), in0=v3(BUr[b2]), in1=tb3(tre, g), op=ALU.mult,
                             reads=[kb("BUr"), K("tre")], writes=[kb("m0")])
                        S.op("pool", "tensor_tensor", out=v3(m1), in0=v3(BUi[b2]), in1=tb3(tim, g), op=ALU.mult,
                             reads=[kb("BUi"), K("tim")], writes=[kb("m1")])
                        S.op("dve", "tensor_tensor", out=btr[b2][:], in0=m0[:], in1=m1[:], op=ALU.subtract,
                             reads=[kb("m0"), kb("m1")], writes=[kb("btr")])
                        S.op("pool", "tensor_tensor", out=v3(m2), in0=v3(BUi[b2]), in1=tb3(tre, g), op=ALU.mult,
                             reads=[kb("BUi"), K("tre")], writes=[kb("m2")])
                        S.op("dve", "tensor_tensor", out=v3(m3), in0=v3(BUr[b2]), in1=tb3(tim, g), op=ALU.mult,
                             reads=[kb("BUr"), K("tim")], writes=[kb("m3")])
                        S.op("pool", "tensor_tensor", out=bti[b2][:], in0=m2[:], in1=m3[:], op=ALU.add,
                             reads=[kb("m2"), kb("m3")], writes=[kb("bti")])
                        khp = K(f"hp{g}")
                        for c in range(NC4):
                            cs = slice(c * L, (c + 1) * L)
                            S.op("dve", "tensor_tensor_scan", out=wre[b2][:, cs], data0=rmat[:, g, :], data1=btr[b2][:, cs],
                                 initial=hp[:, g, 0:1], op0=ALU.mult, op1=ALU.add,
                                 reads=[K("rmat"), kb("btr"), khp], writes=[kb("wre")])
                            S.op("dve", "tensor_tensor_scan", out=wim[b2][:, cs], data0=rmat[:, g, :], data1=bti[b2][:, cs],
                                 initial=hp[:, g, 1:2], op0=ALU.mult, op1=ALU.add,
                                 reads=[K("rmat"), kb("bti"), khp], writes=[kb("wim")])
                            last = (c + 1) * L - 1
                            S.op("dve", "tensor_scalar", out=tmpc[:, 0:1], in0=wre[b2][:, last:last + 1],
                                 scalar1=prm["er"][:, g:g + 1], scalar2=None, op0=ALU.mult,
                                 reads=[kb("wre"), K("er")], writes=[kb("tmpc0")])
                            S.op("dve", "tensor_scalar", out=tmpc[:, 1:2], in0=wre[b2][:, last:last + 1],
                                 scalar1=prm["ei"][:, g:g + 1], scalar2=None, op0=ALU.mult,
                                 reads=[kb("wre"), K("ei")], writes=[kb("tmpc1")])
                            S.op("dve", "scalar_tensor_tensor", out=hp[:, g, 0:1], in0=wim[b2][:, last:last + 1],
                                 scalar=prm["nei"][:, g:g + 1], in1=tmpc[:, 0:1], op0=ALU.mult, op1=ALU.add,
                                 reads=[kb("wim"), K("nei"), kb("tmpc0")], writes=[khp])
                            S.op("dve", "scalar_tensor_tensor", out=hp[:, g, 1:2], in0=wim[b2][:, last:last + 1],
                                 scalar=prm["er"][:, g:g + 1], in1=tmpc[:, 1:2], op0=ALU.mult, op1=ALU.add,
                                 reads=[kb("wim"), K("er"), kb("tmpc1")], writes=[khp])
                        hr, hi_ = hre[f2][gl4], him[f2][gl4]
                        khr, khi = K(f"hre{f2}{gl4}"), K(f"him{f2}{gl4}")
                        S.op("pool", "tensor_tensor", out=v3(m0), in0=v3(wre[b2]), in1=tb3(cph, g), op=ALU.mult,
                             reads=[kb("wre"), K("cph")], writes=[kb("m0")])
                        S.op("pool", "tensor_tensor", out=v3(m1), in0=v3(wim[b2]), in1=tb3(sph, g), op=ALU.mult,
                             reads=[kb("wim"), K("sph")], writes=[kb("m1")])
                        S.op("pool", "tensor_tensor", out=hr[:], in0=m0[:], in1=m1[:], op=ALU.subtract,
                             reads=[kb("m0"), kb("m1")], writes=[khr])
                        S.op("dve", "tensor_tensor", out=v3(m2), in0=v3(wre[b2]), in1=tb3(sph, g), op=ALU.mult,
                             reads=[kb("wre"), K("sph")], writes=[kb("m2")])
                        S.op("pool", "tensor_tensor", out=v3(m3), in0=v3(wim[b2]), in1=tb3(cph, g), op=ALU.mult,
                             reads=[kb("wim"), K("cph")], writes=[kb("m3")])
                        S.op("dve", "scalar_tensor_tensor", out=hi_[:], in0=m2[:], scalar=-1.0, in1=m3[:], op0=ALU.mult,
                             op1=ALU.subtract, reads=[kb("m2"), kb("m3")], writes=[khi])
                    finally:
                        S.op = real_op
                    recs.append(rec)
                for q in range(max(len(recs[0]), len(recs[1]))):
                    for rr in recs:
                        if q < len(rr):
                            eng_, method_, reads_, writes_, kw_ = rr[q]
                            S.op(eng_, method_, reads=reads_, writes=writes_, **kw_)
            for gl4 in range(4):
                g = ft * 4 + gl4
                S.op("pe", "matmul", out=pyy[f2][:], lhsT=cre[:, g, :], rhs=hre[f2][gl4][:], start=(gl4 == 0), stop=False,
                     reads=[K("cre"), K(f"hre{f2}{gl4}")], writes=[K(f"pyy{f2}")])
                S.op("pe", "matmul", out=pyy[f2][:], lhsT=cim[:, g, :], rhs=him[f2][gl4][:], start=False, stop=(gl4 == 3),
                     reads=[K("cim"), K(f"him{f2}{gl4}")], writes=[K(f"pyy{f2}")])
            S.op("dve", "scalar_tensor_tensor", out=yt[f2][:], in0=us[:, ft, :], scalar=dsk[:, ft:ft + 1], in1=pyy[f2][:],
                 op0=ALU.mult, op1=ALU.add, reads=[ku + f"_{ft}", K("dsk"), K(f"pyy{f2}")], writes=[K(f"yt{f2}")])
            S.op("act", "activation", out=ho[f2][:], in_=yt[f2][:], func=AF.Gelu, reads=[K(f"yt{f2}")],
                 writes=[K(f"ho{f2}")])
            S.dma("sp", K(f"ho{f2}"), hidT[ft * 128:(ft + 1) * 128, t0:t0 + 512], ho[f2][:], reads=[K(f"ho{f2}")])


def build_s5(T):
    nc = bass.Bass("TRN2", target_bir_lowering=False)
    dt = lambda n, s, k="ExternalInput": nc.dram_tensor(n, s, F32, kind=k).ap()
    xT = dt("xT", [D, T]); w_in = dt("w_in", [D, 512])
    lamr = dt("lamr", [128, 16]); lami = dt("lami", [128, 16]); ldt = dt("ldt", [128, 16])
    bre = dt("bpad_re", [16, 128, 128]); bim = dt("bpad_im", [16, 128, 128])
    cre = dt("cpad_re", [16, 128, 128]); cim = dt("cpad_im", [16, 128, 128])
    dsk = dt("dsk", [128, 4]); iota = dt("iota", [128, S5_L])
    hidT = dt("hidT", [512, T], "ExternalOutput")
    with contextlib.ExitStack() as es:
        C = Ctx(nc, es)
        s5_phase(C, T, xT, w_in, lamr, lami, ldt, bre, bim, cre, cim, dsk, iota, hidT)
        C.S.finish()
        C.S.emit()
    return nc


def s5_host_layout(ghalf, s5_lam_re, s5_lam_im, s5_log_dt, s5_b_re, s5_b_im, s5_c_re, s5_c_im, s5_d, s5_w_in):
    g0 = 32 * ghalf
    lamr = np.zeros((128, 16), np.float32); lami = np.zeros((128, 16), np.float32); ldt = np.zeros((128, 16), np.float32)
    bre = np.zeros((16, 128, 128), np.float32); bim = np.zeros((16, 128, 128), np.float32)
    cre = np.zeros((16, 128, 128), np.float32); cim = np.zeros((16, 128, 128), np.float32)
    for gp in range(16):
        for gl in range(2):
            g = g0 + 2 * gp + gl
            lamr[gl * 64:(gl + 1) * 64, gp] = s5_lam_re[g]
            lami[gl * 64:(gl + 1) * 64, gp] = s5_lam_im[g]
            ldt[gl * 64:(gl + 1) * 64, gp] = s5_log_dt[g]
            r0 = 32 * (gp % 4) + 16 * gl
            bre[gp, r0:r0 + 16, gl * 64:(gl + 1) * 64] = s5_b_re[g].T
            bim[gp, r0:r0 + 16, gl * 64:(gl + 1) * 64] = s5_b_im[g].T
            cre[gp, gl * 64:(gl + 1) * 64, r0:r0 + 16] = s5_c_re[g].T
            cim[gp, gl * 64:(gl + 1) * 64, r0:r0 + 16] = s5_c_im[g].T
    dsk = np.ascontiguousarray(s5_d[512 * ghalf:512 * (ghalf + 1)].reshape(4, 128).T)
    iota = np.tile(np.arange(1, S5_L + 1, dtype=np.float32)[None, :], (128, 1))
    w = np.ascontiguousarray(s5_w_in[:, 512 * ghalf:512 * (ghalf + 1)])
    return dict(w_in=w, lamr=lamr, lami=lami, ldt=ldt, bpad_re=bre, bpad_im=bim, cpad_re=cre, cpad_im=cim,
                dsk=dsk, iota=iota)


GDN_LV = 99
GDN_CLAMP = False


def gdn_phase(C, T, xT, wq_d, wk_d, wv_d, wz_d, wba_d, convw_d, hp_d, normg_d, gconst_d, ogT, pfx="gd"):
    S = C.S
    NB = T // 512
    P = pfx
    NH = 4

    def K(n):
        return P + "." + n

    wq = C.sb([128, 8, 512], BF16, "wq"); wk = C.sb([128, 8, 512], BF16, "wk")
    wv = C.sb([128, 8, 512], BF16, "wv"); wz = C.sb([128, 8, 512], BF16, "wz")
    wba = C.sb([128, 8, 8], F32, "wba")
    convw = C.sb([128, 3, NH, 4], F32, "convw")
    hpar = C.sb([128, 8], F32, "hpar")
    nea = C.sb([128, NH], F32, "nea")
    normg = C.sb([128, 128], F32, "normg")
    cst = C.sb([128, 6, 128], F32, "gcst")
    ident, MU, MS, NEGL, NEGUT, ONES = (cst[:, i, :] for i in range(6))
    onec = C.sb([128, 1], F32, "onec"); eps6 = C.sb([128, 1], F32, "eps6")
    xs = [C.sb([128, 8, 512], BF16, "gxs") for _ in range(2)]
    xs32 = C.sb([128, 8, 512], F32, "gxs32")
    xb = [[C.sb([128, 515], F32, "xb") for _ in range(NH)] for _ in range(3)]
    qkvc = [[C.sb([128, 512], F32, "qkvc") for _ in range(NH)] for _ in range(3)]
    cacc = [C.sb([128, 512], F32, "cacc") for _ in range(2)]
    zs = C.sb([128, 512], F32, "zs")
    ba = C.sb([128, 8], F32, "ba")
    gt = {n: C.sb([128, NH], F32, "g_" + n) for n in
          ("beta", "nbeta", "x", "e", "sp", "g", "eg", "kes", "elast", "bks")}
    gc8 = C.sb([128, 8], F32, "gc8")
    Sst = [C.sb([128, 128], F32, "Sst") for _ in range(NH)]
    ogT_sb = C.sb([128, NH, 512], F32, "ogT")

    def mk(n):
        return [C.sb([128, 128], F32, n) for _ in range(2)]

    Kn, KnT, Kbg, Kend, Vb, sq, G1, Dl, DTu, AT, PT, nWT, Vnew, o1, o_, og = (mk(n) for n in (
        "Kn", "KnT", "Kbg", "Kend", "Vb", "sq", "G1", "Dl", "DTu", "AT", "PT", "nWT", "Vnew", "o1", "o", "og"))
    Nn = [mk("Na"), mk("Nb")]
    NTn = [mk("NTa"), mk("NTb")]
    junk = mk("junk")
    Qc = mk("Qc")
    col = {n: [C.sb([128, 1], F32, "c_" + n) for _ in range(2)] for n in
           ("ssqk", "rk", "ssqq", "rq", "rq2", "ssqo", "v1", "fac")}
    bank = [C.ps([128, 512], F32, f"gb{i}") for i in range(8)]
    BK = [K(f"bank{i}") for i in range(8)]

    for nm, dd, sbt in (("wq", wq_d, wq), ("wk", wk_d, wk), ("wv", wv_d, wv), ("wz", wz_d, wz)):
        S.dma("pool", K(nm), sbt[:], dd.rearrange("(j p) n -> p j n", p=128), writes=[K(nm)])
    S.dma("sp", K("wba"), wba[:], wba_d.rearrange("(j p) n -> p j n", p=128), writes=[K("wba")])
    S.dma("sp", K("convw"), convw[:], convw_d, writes=[K("convw")])
    S.dma("sp", K("hpar"), hpar[:], hp_d, writes=[K("hpar")])
    S.dma("sp", K("normg"), normg[:], normg_d, writes=[K("normg")])
    S.dma("sp", K("cst"), cst[:], gconst_d.rearrange("i p f -> p i f"), writes=[K("cst")])
    S.op("dve", "memset", ap=onec[:], constant=1.0, writes=[K("onec")])
    S.op("dve", "memset", ap=eps6[:], constant=NORM_EPS, writes=[K("eps6")])
    for h in range(NH):
        S.op("dve", "memset", ap=Sst[h][:], constant=0.0, writes=[K(f"S{h}")])
        for i in range(3):
            S.op("pool", "memset", ap=xb[i][h][:, 0:3], constant=0.0, writes=[K(f"xb{i}{h}")])
    S.op("act", "activation", out=nea[:], in_=hpar[:, 0:4], func=AF.Exp, reads=[K("hpar")], writes=[K("nea")])
    S.op("dve", "tensor_scalar", out=nea[:], in0=nea[:], scalar1=-1.0, scalar2=None, op0=ALU.mult,
         reads=[K("nea")], writes=[K("nea")])

    def mm(bi, c0, c1, lhsT, rhs, reads, start=True, stop=True, rows=128):
        S.op("pe", "matmul", out=bank[bi][0:rows, c0:c1], lhsT=lhsT, rhs=rhs, start=start, stop=stop,
             reads=reads, writes=[BK[bi]])

    def tr(bi, c0, in_, reads):
        S.op("pe", "transpose", out=bank[bi][:, c0:c0 + 128], in_=in_, identity=ident,
             reads=reads + [K("cst")], writes=[BK[bi]])

    it = 0
    for blk in range(NB):
        t0 = blk * 512
        bi2 = blk % 2
        xsb, kx = xs[bi2], K(f"xs{bi2}")
        S.dma("pool", kx, xsb[:], xT[:, t0:t0 + 512].rearrange("(j p) t -> p j t", p=128), writes=[kx])
        S.dma("sp", K("xs32"), xs32[:], xT[:, t0:t0 + 512].rearrange("(j p) t -> p j t", p=128), writes=[K("xs32")])
        ci = 0
        for i, wsb, wkey in ((0, wq, "wq"), (1, wk, "wk"), (2, wv, "wv")):
            for h in range(NH):
                for k in range(8):
                    mm(0, 0, 512, wsb[:, k, h * 128:(h + 1) * 128], xsb[:, k, :], [K(wkey), kx], start=(k == 0),
                       stop=(k == 7))
                xbt, kxb = xb[i][h], K(f"xb{i}{h}")
                S.op("act", "copy", out=xbt[:, 3:515], in_=bank[0][:], reads=[BK[0]], writes=[kxb])
                ca, kca = cacc[ci % 2], K(f"cacc{ci % 2}")
                ci += 1
                eng = "dve"
                S.op(eng, "tensor_scalar", out=ca[:], in0=xbt[:, 0:512], scalar1=convw[:, i, h, 0:1], scalar2=None,
                     op0=ALU.mult, reads=[kxb, K("convw")], writes=[kca])
                for j in range(1, 4):
                    S.op(eng, "scalar_tensor_tensor", out=ca[:], in0=xbt[:, j:j + 512], scalar=convw[:, i, h, j:j + 1],
                         in1=ca[:], op0=ALU.mult, op1=ALU.add, reads=[kxb, K("convw"), kca], writes=[kca])
                S.op("pool", "tensor_copy", out=xbt[:, 0:3], in_=xbt[:, 512:515], reads=[kxb], writes=[kxb])
                S.op("act", "activation", out=qkvc[i][h][:], in_=ca[:], func=AF.Silu, reads=[kca],
                     writes=[K(f"qkvc{i}{h}")])
        for tl in range(4):
            tsl = slice(tl * 128, (tl + 1) * 128)
            if GDN_LV < 2:
                continue
            for k in range(8):
                mm(1, 0, 512, xsb[:, k, tsl], wz[:, k, :], [kx, K("wz")], start=(k == 0), stop=(k == 7))
            S.op("act", "activation", out=zs[:], in_=bank[1][:], func=AF.Silu, reads=[BK[1]], writes=[K("zs")])
            for k in range(8):
                mm(1, 0, 8, xs32[:, k, tsl], wba[:, k, :], [K("xs32"), K("wba")], start=(k == 0), stop=(k == 7))
            S.op("dve", "tensor_copy", out=ba[:], in_=bank[1][:, 0:8], reads=[BK[1]], writes=[K("ba")])
            G = lambda n: gt[n][:]
            kg = lambda n: K("g_" + n)
            S.op("act", "activation", out=G("beta"), in_=ba[:, 0:4], func=AF.Sigmoid, reads=[K("ba")],
                 writes=[kg("beta")])
            S.op("dve", "tensor_scalar", out=G("nbeta"), in0=G("beta"), scalar1=-1.0, scalar2=None, op0=ALU.mult,
                 reads=[kg("beta")], writes=[kg("nbeta")])
            S.op("dve", "tensor_tensor", out=G("x"), in0=ba[:, 4:8], in1=hpar[:, 4:8], op=ALU.add,
                 reads=[K("ba"), K("hpar")], writes=[kg("x")])
            S.op("act", "activation", out=G("e"), in_=G("x"), func=AF.Exp, reads=[kg("x")], writes=[kg("e")])
            S.op("act", "activation", out=G("sp"), in_=G("e"), func=AF.Ln, bias=onec[:, 0:1], scale=1.0,
                 reads=[kg("e"), K("onec")], writes=[kg("sp")])
            S.op("dve", "tensor_tensor", out=G("g"), in0=G("sp"), in1=nea[:], op=ALU.mult,
                 reads=[kg("sp"), K("nea")], writes=[kg("g")])
            mm(1, 0, 4, MU, G("g"), [K("cst"), kg("g")])
            mm(1, 4, 8, ONES, G("g"), [K("cst"), kg("g")])
            S.op("dve", "tensor_copy", out=gc8[:], in_=bank[1][:, 0:8], reads=[BK[1]], writes=[K("gc8")])
            S.op("act", "activation", out=G("eg"), in_=gc8[:, 0:4], func=AF.Exp, reads=[K("gc8")], writes=[kg("eg")])
            S.op("act", "activation", out=G("elast"), in_=gc8[:, 4:8], func=AF.Exp, reads=[K("gc8")],
                 writes=[kg("elast")])
            S.op("dve", "tensor_tensor", out=G("kes"), in0=gc8[:, 4:8], in1=gc8[:, 0:4], op=ALU.subtract,
                 reads=[K("gc8")], writes=[kg("kes")])
            S.op("act", "activation", out=G("kes"), in_=G("kes"), func=AF.Exp, reads=[kg("kes")], writes=[kg("kes")])
            S.op("dve", "tensor_tensor", out=G("bks"), in0=G("beta"), in1=G("eg"), op=ALU.mult,
                 reads=[kg("beta"), kg("eg")], writes=[kg("bks")])
            if GDN_LV < 2.5:
                continue
            for hpair in range(0, NH, 2):
                recs = []
                for h in (hpair, hpair + 1):
                    rec = []
                    real_op = S.op
                    S.op = (lambda eng, method, reads=(), writes=(), _rec=rec, **kw:
                            _rec.append((eng, method, reads, writes, kw)))
                    try:
                        p2 = it % 2
                        bA, bB, bC = 2 + 3 * p2, 3 + 3 * p2, 4 + 3 * p2
                        it += 1
                        T_ = lambda lst: lst[p2][:]
                        kt = lambda n: K(f"{n}{p2}")
                        cl = lambda n: col[n][p2][:]
                        QT = qkvc[0][h][:, tsl]
                        KTr = qkvc[1][h][:, tsl]
                        VTr = qkvc[2][h][:, tsl]
                        kq, kk_, kv = K(f"qkvc0{h}"), K(f"qkvc1{h}"), K(f"qkvc2{h}")
                        hs = slice(h, h + 1)
                        S.op("pool", "tensor_copy", out=T_(Qc), in_=QT, reads=[kq], writes=[kt("Qc")])
                        tr(bA, 0, KTr, [kk_])
                        S.op("act", "activation", out=T_(junk), in_=bank[bA][:, 0:128], func=AF.Square,
                             reads=[BK[bA]], writes=[kt("junk")])
                        S.op("dve", "reduce_sum", out=cl("ssqk"), in_=T_(junk), axis=AX.X, reads=[kt("junk")],
                             writes=[kt("ssqk")])
                        S.op("act", "activation", out=cl("rk"), in_=cl("ssqk"), func=AF.Ln, bias=eps6[:, 0:1], scale=1.0,
                             reads=[kt("ssqk"), K("eps6")], writes=[kt("rk")])
                        S.op("act", "activation", out=cl("rk"), in_=cl("rk"), func=AF.Exp, scale=-0.5,
                             reads=[kt("rk")], writes=[kt("rk")])
                        S.op("dve", "tensor_scalar", out=T_(Kn), in0=bank[bA][:, 0:128], scalar1=cl("rk"), scalar2=None,
                             op0=ALU.mult, reads=[BK[bA], kt("rk")], writes=[kt("Kn")])
                        tr(bA, 128, T_(Kn), [kt("Kn")])
                        S.op("act", "copy", out=T_(KnT), in_=bank[bA][:, 128:256], reads=[BK[bA]], writes=[kt("KnT")])
                        S.op("dve", "tensor_scalar", out=T_(Kbg), in0=T_(Kn), scalar1=gt["bks"][:, hs], scalar2=None, op0=ALU.mult,
                             reads=[kt("Kn"), kg("bks")], writes=[kt("Kbg")])
                        S.op("dve", "tensor_scalar", out=T_(Kend), in0=T_(Kn), scalar1=gt["kes"][:, hs], scalar2=None, op0=ALU.mult,
                             reads=[kt("Kn"), kg("kes")], writes=[kt("Kend")])
                        if GDN_LV < 3:
                            continue
                        tr(bA, 256, VTr, [kv])
                        S.op("dve", "tensor_scalar", out=T_(Vb), in0=bank[bA][:, 256:384], scalar1=gt["beta"][:, hs],
                             scalar2=None, op0=ALU.mult, reads=[BK[bA], kg("beta")], writes=[kt("Vb")])
                        if GDN_LV < 3.5:
                            continue
                        tr(bA, 384, QT, [kq])
                        S.op("act", "activation", out=T_(sq), in_=bank[bA][:, 384:512], func=AF.Square, reads=[BK[bA]],
                             writes=[kt("sq")])
                        S.op("dve", "reduce_sum", out=cl("ssqq"), in_=T_(sq), axis=AX.X, reads=[kt("sq")], writes=[kt("ssqq")])
                        S.op("act", "activation", out=cl("rq"), in_=cl("ssqq"), func=AF.Ln, bias=eps6[:, 0:1],
                             scale=1.0, reads=[kt("ssqq"), K("eps6")], writes=[kt("rq")])
                        S.op("act", "activation", out=cl("rq"), in_=cl("rq"), func=AF.Exp, scale=-0.5,
                             reads=[kt("rq")], writes=[kt("rq")])
                        S.op("dve", "tensor_scalar", out=cl("rq"), in0=cl("rq"), scalar1=128.0 ** -0.5, scalar2=None,
                             op0=ALU.mult, reads=[kt("rq")], writes=[kt("rq")])
                        S.op("dve", "scalar_tensor_tensor", out=cl("rq2"), in0=cl("rq"), scalar=1.0 / 128.0, in1=cl("rq"),
                             op0=ALU.mult, op1=ALU.mult, reads=[kt("rq")], writes=[kt("rq2")])
                        if GDN_LV < 4:
                            continue
                        S.op("dve", "tensor_scalar", out=T_(G1), in0=MU, scalar1=gt["g"][:, hs], scalar2=None, op0=ALU.mult,
                             reads=[K("cst"), kg("g")], writes=[kt("G1")])
                        mm(bB, 0, 128, T_(G1), MS, [kt("G1"), K("cst")], start=True, stop=False)
                        mm(bB, 0, 128, ident, NEGL, [K("cst")], start=False, stop=True)
                        mm(bB, 128, 256, MS, T_(G1), [kt("G1"), K("cst")], start=True, stop=False)
                        mm(bB, 128, 256, ident, NEGUT, [K("cst")], start=False, stop=True)
                        S.op("act", "activation", out=T_(Dl), in_=bank[bB][:, 0:128], func=AF.Exp, reads=[BK[bB]],
                             writes=[kt("Dl")])
                        S.op("act", "activation", out=T_(DTu), in_=bank[bB][:, 128:256], func=AF.Exp, reads=[BK[bB]],
                             writes=[kt("DTu")])
                        if GDN_LV < 5:
                            continue
                        mm(bB, 256, 384, T_(KnT), T_(KnT), [kt("KnT")])
                        mm(bB, 384, 512, T_(KnT), QT, [kt("KnT"), kq])
                        N0, NT0 = Nn[0][p2], NTn[0][p2]
                        S.op("dve", "scalar_tensor_tensor", out=N0[:], in0=bank[bB][:, 256:384], scalar=gt["nbeta"][:, hs],
                             in1=T_(Dl), op0=ALU.mult, op1=ALU.mult, reads=[BK[bB], kg("nbeta"), kt("Dl")], writes=[kt("N0")])
                        S.op("dve", "tensor_tensor", out=T_(AT), in0=bank[bB][:, 384:512], in1=T_(DTu), op=ALU.mult,
                             reads=[BK[bB], kt("DTu")], writes=[kt("AT")])
                        tr(bC, 0, N0[:], [kt("N0")])
                        S.op("act", "copy", out=NT0[:], in_=bank[bC][:, 0:128], reads=[BK[bC]], writes=[kt("NT0")])
                        S.op("pool", "tensor_tensor", out=T_(PT), in0=NT0[:], in1=ident, op=ALU.add,
                             reads=[kt("NT0"), K("cst")], writes=[kt("PT")])
                        if GDN_LV < 6:
                            continue
                        for j in range(1, 7):
                            a, b = (j - 1) % 2, j % 2
                            Na, NTa, Nb, NTb = Nn[a][p2], NTn[a][p2], Nn[b][p2], NTn[b][p2]
                            mm(bC, 128, 256, NTa[:], Na[:], [kt(f"N{a}"), kt(f"NT{a}")])
                            if j < 6:
                                mm(bC, 256, 384, Na[:], NTa[:], [kt(f"N{a}"), kt(f"NT{a}")])
                            S.op("act", "copy", out=Nb[:], in_=bank[bC][:, 128:256], reads=[BK[bC]], writes=[kt(f"N{b}")])
                            if j < 6:
                                S.op("dve", "tensor_copy", out=NTb[:], in_=bank[bC][:, 256:384], reads=[BK[bC]],
                                     writes=[kt(f"NT{b}")])
                            mm(bC, 384, 512, Nb[:], T_(PT), [kt(f"N{b}"), kt("PT")])
                            S.op("dve", "tensor_tensor", out=T_(PT), in0=bank[bC][:, 384:512], in1=T_(PT), op=ALU.add,
                                 reads=[BK[bC], kt("PT")], writes=[kt("PT")])
                        if GDN_LV < 7:
                            continue
                        mm(bC, 0, 128, T_(Kbg), T_(PT), [kt("Kbg"), kt("PT")])
                        S.op("act", "mul", out=T_(nWT), in_=bank[bC][:, 0:128], mul=-1.0, reads=[BK[bC]], writes=[kt("nWT")])
                        if GDN_LV < 8:
                            continue
                        St, kS = Sst[h], K(f"S{h}")
                        mm(bC, 0, 128, T_(PT), T_(Vb), [kt("PT"), kt("Vb")], start=True, stop=False)
                        mm(bC, 0, 128, T_(nWT), St[:], [kt("nWT"), kS], start=False, stop=True)
                        S.op("act", "copy", out=T_(Vnew), in_=bank[bC][:, 0:128], reads=[BK[bC]], writes=[kt("Vnew")])
                        mm(bC, 128, 256, T_(Qc), St[:], [kt("Qc"), kS])
                        mm(bC, 256, 384, T_(AT), T_(Vnew), [kt("AT"), kt("Vnew")])
                        mm(bC, 384, 512, T_(Kend), T_(Vnew), [kt("Kend"), kt("Vnew")])
                        S.op("dve", "tensor_scalar", out=T_(o1), in0=bank[bC][:, 128:256], scalar1=gt["eg"][:, hs], scalar2=None, op0=ALU.mult,
                             reads=[BK[bC], kg("eg")], writes=[kt("o1")])
                        S.op("dve", "tensor_tensor", out=T_(o_), in0=bank[bC][:, 256:384], in1=T_(o1), op=ALU.add,
                             reads=[BK[bC], kt("o1")], writes=[kt("o")])
                        S.op("dve", "scalar_tensor_tensor", out=St[:], in0=St[:], scalar=gt["elast"][:, hs],
                             in1=bank[bC][:, 384:512], op0=ALU.mult, op1=ALU.add, reads=[kS, kg("elast"), BK[bC]], writes=[kS])
                        if GDN_LV < 9:
                            continue
                        S.op("act", "activation", out=T_(junk), in_=T_(o_), func=AF.Square,
                             reads=[kt("o")], writes=[kt("junk")])
                        S.op("dve", "reduce_sum", out=cl("ssqo"), in_=T_(junk), axis=AX.X, reads=[kt("junk")],
                             writes=[kt("ssqo")])
                        S.op("dve", "tensor_tensor", out=cl("v1"), in0=cl("ssqo"), in1=cl("rq2"), op=ALU.mult,
                             reads=[kt("ssqo"), kt("rq2")], writes=[kt("v1")])
                        S.op("act", "activation", out=cl("ssqq"), in_=cl("v1"), func=AF.Ln, bias=eps6[:, 0:1], scale=1.0,
                             reads=[kt("v1"), K("eps6")], writes=[kt("ssqq")])
                        S.op("act", "activation", out=cl("v1"), in_=cl("ssqq"), func=AF.Exp, scale=-0.5,
                             reads=[kt("ssqq")], writes=[kt("v1")])
                        S.op("dve", "tensor_tensor", out=cl("fac"), in0=cl("v1"), in1=cl("rq"), op=ALU.mult,
                             reads=[kt("v1"), kt("rq")], writes=[kt("fac")])
                        if GDN_LV < 11:
                            continue
                        S.op("dve", "scalar_tensor_tensor", out=T_(og), in0=T_(o_), scalar=cl("fac"), in1=normg[:],
                             op0=ALU.mult, op1=ALU.mult, reads=[kt("o"), kt("fac"), K("normg")], writes=[kt("og")])
                        S.op("dve", "tensor_tensor", out=T_(og), in0=T_(og), in1=zs[:, h * 128:(h + 1) * 128], op=ALU.mult,
                             reads=[kt("og"), K("zs")], writes=[kt("og")])
                        if GDN_LV < 11:
                            continue
                        tr(bA, 0, T_(og), [kt("og")])
                        S.op("act", "copy", out=ogT_sb[:, h, tsl], in_=bank[bA][:, 0:128], reads=[BK[bA]],
                             writes=[K("ogT")])
                    finally:
                        S.op = real_op
                    recs.append(rec)
                na, nb = len(recs[0]), len(recs[1])
                for q in range(max(na, nb)):
                    for rr in recs:
                        if q < len(rr):
                            eng_, method_, reads_, writes_, kw_ = rr[q]
                            S.op(eng_, method_, reads=reads_, writes=writes_, **kw_)

        S.dma("sp", K("ogT"), ogT[:, t0:t0 + 512].rearrange("(h p) t -> p h t", p=128), ogT_sb[:], reads=[K("ogT")])


def build_gdn(T):
    nc = bass.Bass("TRN2", target_bir_lowering=False)
    dt = lambda n, s, k="ExternalInput": nc.dram_tensor(n, s, F32, kind=k).ap()
    xT = dt("xT", [D, T])
    wq = dt("wq", [D, 512]); wk = dt("wk", [D, 512]); wv = dt("wv", [D, 512]); wz = dt("wz", [D, 512])
    wba = dt("wba", [D, 8]); convw = dt("convw", [128, 3, 4, 4]); hp = dt("hpar", [128, 8])
    normg = dt("normg", [128, 128]); gconst = dt("gconst", [6, 128, 128])
    ogT = dt("ogT", [512, T], "ExternalOutput")
    with contextlib.ExitStack() as es:
        C = Ctx(nc, es)
        gdn_phase(C, T, xT, wq, wk, wv, wz, wba, convw, hp, normg, gconst, ogT)
        C.S.finish()
        C.S.emit()
    return nc


def gdn_consts():
    i = np.arange(128)
    ident = np.eye(128, dtype=np.float32)
    MU = (i[:, None] <= i[None, :]).astype(np.float32)
    MS = (i[:, None] > i[None, :]).astype(np.float32)
    NEGL = np.where(i[:, None] > i[None, :], 0.0, -100.0).astype(np.float32)
    NEGUT = np.where(i[None, :] >= i[:, None], 0.0, -100.0).astype(np.float32)
    ONES = np.ones((128, 128), np.float32)
    return np.stack([ident, MU, MS, NEGL, NEGUT, ONES])


def gdn_host_layout(hh, w_in, conv_w, a_log, dt_bias, norm_g):
    QK = 1024
    c0 = 512 * hh
    wq = np.ascontiguousarray(w_in[:, c0:c0 + 512])
    wk = np.ascontiguousarray(w_in[:, QK + c0:QK + c0 + 512])
    wv = np.ascontiguousarray(w_in[:, 2 * QK + c0:2 * QK + c0 + 512])
    wz = np.ascontiguousarray(w_in[:, 3 * QK + c0:3 * QK + c0 + 512])
    wba = np.ascontiguousarray(np.concatenate([w_in[:, 4 * QK + 4 * hh:4 * QK + 4 * hh + 4],
                                               w_in[:, 4 * QK + 8 + 4 * hh:4 * QK + 8 + 4 * hh + 4]], axis=1))
    convw = np.zeros((128, 3, 4, 4), np.float32)
    for i in range(3):
        for h in range(4):
            convw[:, i, h, :] = conv_w[:, i * QK + c0 + h * 128:i * QK + c0 + (h + 1) * 128].T
    hp = np.tile(np.concatenate([a_log[4 * hh:4 * hh + 4], dt_bias[4 * hh:4 * hh + 4]])[None, :], (128, 1)).astype(np.float32)
    normg = np.tile(norm_g[None, :], (128, 1)).astype(np.float32)
    return dict(wq=wq, wk=wk, wv=wv, wz=wz, wba=wba, convw=convw, hpar=hp, normg=normg, gconst=gdn_consts())


GDN_NAMES = ("wq", "wk", "wv", "wz", "wba", "convw", "hpar", "normg")
S5_NAMES = ("w_in", "lamr", "lami", "ldt", "bpad_re", "bpad_im", "cpad_re", "cpad_im", "dsk")
GDN_SHAPES = dict(wq=[D, 512], wk=[D, 512], wv=[D, 512], wz=[D, 512], wba=[D, 8], convw=[128, 3, 4, 4],
                  hpar=[128, 8], normg=[128, 128])
S5_SHAPES = dict(w_in=[D, 512], lamr=[128, 16], lami=[128, 16], ldt=[128, 16], bpad_re=[16, 128, 128],
                 bpad_im=[16, 128, 128], cpad_re=[16, 128, 128], cpad_im=[16, 128, 128], dsk=[128, 4])


def build_fused(T):
    nc = bass.Bass("TRN2", target_bir_lowering=False)
    ext = lambda n, sh: nc.dram_tensor(n, sh, F32, kind="ExternalInput").ap()
    xT0 = ext("xT0", [D, T])
    xtok0 = ext("xtok0", [T, D])
    ident_d = ext("ident", [128, 128])
    gconst = ext("gconst", [6, 128, 128])
    iota = ext("iota", [128, S5_L])
    wr0 = ext("wr_dummy", [D, NEXP])
    br0 = ext("br_dummy", [1, NEXP])
    y = nc.dram_tensor("y", [T, D], F32, kind="ExternalOutput").ap()
    XT = nc.dram_tensor("XT_i", [D, T], F32).ap()
    FEAT = nc.dram_tensor("FEAT_i", [D, T], F32).ap()
    XA = nc.dram_tensor("XA_i", [T, D], F32).ap()
    XB = nc.dram_tensor("XB_i", [T, D], F32).ap()
    TH = T // 2
    tb = min(1024, TH)
    with contextlib.ExitStack() as es:
        S = Sched(nc, es)

        def phase(fn, last=False):
            with contextlib.ExitStack() as pes:
                C = Ctx(nc, pes, S)
                fn(C)
                S.barrier()
                S.emit()
            if not last:
                S.new_phase()

        for i in range(DEPTH):
            xT_src = xT0 if i == 0 else XT
            xres_src = xtok0 if i == 0 else (XA if i % 2 == 1 else XB)
            xout_dst = y if i == DEPTH - 1 else (XA if i % 2 == 0 else XB)
            lnp = ext(f"L{i}_lnp", [4, D])
            if i % 2 == 0:
                for hh in range(2):
                    d = {n: ext(f"L{i}_{hh}_{n}", GDN_SHAPES[n]) for n in GDN_NAMES}
                    phase(lambda C, d=d, hh=hh: gdn_phase(
                        C, T, xT_src, d["wq"], d["wk"], d["wv"], d["wz"], d["wba"], d["convw"], d["hpar"], d["normg"],
                        gconst, FEAT[hh * 512:(hh + 1) * 512, :], pfx=f"g{i}{hh}"))
                glu, moe, ne, nproj = False, False, 2, D
            else:
                for gh in range(2):
                    d = {n: ext(f"L{i}_{gh}_{n}", S5_SHAPES[n]) for n in S5_NAMES}
                    phase(lambda C, d=d, gh=gh: s5_phase(
                        C, T, xT_src, d["w_in"], d["lamr"], d["lami"], d["ldt"], d["bpad_re"], d["bpad_im"],
                        d["cpad_re"], d["cpad_im"], d["dsk"], iota, FEAT[gh * 512:(gh + 1) * 512, :], pfx=f"s{i}{gh}"))
                glu, moe, ne, nproj = True, True, NEXP, 2 * D
            wproj = ext(f"L{i}_wproj", [D, nproj])
            w1 = ext(f"L{i}_w1", [ne, D, FE]); w3 = ext(f"L{i}_w3", [ne, D, FE]); w2 = ext(f"L{i}_w2", [ne, FE, D])
            if moe:
                wr = ext(f"L{i}_wr", [D, NEXP]); br = ext(f"L{i}_br", [1, NEXP])
            else:
                wr, br = wr0, br0
            for hf in range(2):
                sl = slice(hf * TH, (hf + 1) * TH)
                phase(lambda C, sl=sl, hf=hf: tp_phase(
                    C, TH, glu, moe, FEAT[:, sl], wproj, xres_src[sl, :], lnp, w1, w3, w2, wr, br, ident_d,
                    xout_dst[sl, :], XT[:, sl], tb=tb, pfx=f"t{i}{hf}"), last=(i == DEPTH - 1 and hf == 1))
        print("n_sems", len(S.sem), "n_ops", S.n_ops)
        S.finish()
        S.emit()
    return nc


def fused_inputs(xb, P):
    f = lambda a: np.ascontiguousarray(np.asarray(a, dtype=np.float32))
    m = dict(xT0=np.ascontiguousarray(xb.T), xtok0=np.ascontiguousarray(xb), ident=np.eye(128, dtype=np.float32),
             gconst=gdn_consts(), iota=np.tile(np.arange(1, S5_L + 1, dtype=np.float32)[None, :], (128, 1)),
             wr_dummy=np.zeros((D, NEXP), np.float32), br_dummy=np.zeros((1, NEXP), np.float32))
    for i in range(DEPTH):
        j = i // 2
        m[f"L{i}_lnp"] = f(np.stack([P["ln_g"][i, 0], P["ln_b"][i, 0], P["ln_g"][i, 1], P["ln_b"][i, 1]]))
        if i % 2 == 0:
            for hh in range(2):
                lay = gdn_host_layout(hh, f(P["gdn_w_in"][j]), f(P["gdn_conv_w"][j]), f(P["gdn_a_log"][j]),
                                      f(P["gdn_dt_bias"][j]), f(P["gdn_norm_g"][j]))
                for n in GDN_NAMES:
                    m[f"L{i}_{hh}_{n}"] = lay[n]
            m[f"L{i}_wproj"] = f(P["gdn_w_out"][j])
            m[f"L{i}_w1"] = f(np.stack([P["ffn_w1"][j][:, :FE], P["ffn_w1"][j][:, FE:]]))
            m[f"L{i}_w3"] = f(np.stack([P["ffn_w3"][j][:, :FE], P["ffn_w3"][j][:, FE:]]))
            m[f"L{i}_w2"] = f(np.stack([P["ffn_w2"][j][:FE], P["ffn_w2"][j][FE:]]))
        else:
            for gh in range(2):
                lay = s5_host_layout(gh, f(P["s5_lam_re"][j]), f(P["s5_lam_im"][j]), f(P["s5_log_dt"][j]),
                                     f(P["s5_b_re"][j]), f(P["s5_b_im"][j]), f(P["s5_c_re"][j]), f(P["s5_c_im"][j]),
                                     f(P["s5_d"][j]), f(P["s5_w_in"][j]))
                for n in S5_NAMES:
                    m[f"L{i}_{gh}_{n}"] = lay[n]
            m[f"L{i}_wproj"] = f(P["s5_w_glu"][j])
            m[f"L{i}_w1"] = f(P["moe_w1"][j]); m[f"L{i}_w3"] = f(P["moe_w3"][j]); m[f"L{i}_w2"] = f(P["moe_w2"][j])
            m[f"L{i}_wr"] = f(P["moe_w_router"][j]); m[f"L{i}_br"] = f(P["moe_b_router"][j]).reshape(1, NEXP)
    return m


SEQ = 8192
BATCH = 4
_PROGS = {}


def kernel(**P):
    x = np.ascontiguousarray(np.asarray(P["x"], dtype=np.float32))
    T = x.shape[1]
    if T not in _PROGS:
        _PROGS[T] = build_fused(T)
    nc = _PROGS[T]
    shared = None
    in_maps = []
    for c in range(8):
        b = c % BATCH
        m = fused_inputs(x[b], P) if shared is None else dict(shared)
        if shared is None:
            shared = m
        else:
            m["xT0"] = np.ascontiguousarray(x[b].T)
            m["xtok0"] = x[b]
        in_maps.append(m)
    res = run_bass_kernel_spmd(nc, in_maps, core_ids=list(range(8))).results
    return np.stack([res[b]["y"] for b in range(BATCH)]).astype(np.float32)
```

```python
import contextlib
import numpy as np
import concourse.bass as bass
import concourse.mybir as mybir
from concourse.bass_utils import run_bass_kernel_spmd

F32 = mybir.dt.float32
BF16 = mybir.dt.bfloat16
AF = mybir.ActivationFunctionType
ALU = mybir.AluOpType
AX = mybir.AxisListType

D = 1024
DEPTH = 4
ALPHA = (2 * DEPTH) ** 0.25
LN_EPS = 1e-5
NORM_EPS = 1e-6
NEXP = 8
FE = 1408
NFT = FE // 128


class Sched:
    ENGS = ("pe", "dve", "act", "pool", "sp")

    def __init__(self, nc, es):
        self.nc = nc
        self.es = es
        self.q = {e: [] for e in self.ENGS}
        self.sem = {}
        self.cnt = {}
        self.known = {e: {} for e in self.ENGS}
        self.last_w = {}
        self.readers = {}
        self.phase = 0
        self.ename = {}
        for e in ("pe", "dve", "act", "pool"):
            self.ename[e] = e + "0"
            self._mksem(e + "0")
        self.n_ops = 0

    def _mksem(self, name):
        self.sem[name] = self.es.enter_context(self.nc.semaphore("s_" + name))
        self.cnt[name] = 0

    def _deps(self, reads, writes):
        deps = {}

        def add(tok):
            if tok is None:
                return
            s, v = tok
            if deps.get(s, 0) < v:
                deps[s] = v

        for k in reads:
            add(self.last_w.get(k))
        for k in writes:
            add(self.last_w.get(k))
            for s, v in self.readers.get(k, {}).items():
                add((s, v))
        return deps

    def _commit(self, tok, reads, writes):
        for k in writes:
            self.last_w[k] = tok
            self.readers[k] = {}
        for k in reads:
            if k in writes:
                continue
            r = self.readers.setdefault(k, {})
            if r.get(tok[0], 0) < tok[1]:
                r[tok[0]] = tok[1]

    def _waits(self, eng, deps):
        waits = []
        kn = self.known[eng]
        for s, v in deps.items():
            if eng == "pe" and s == self.ename["pe"]:
                continue
            if kn.get(s, 0) < v:
                kn[s] = v
                waits.append((s, v))
        return waits

    def op(self, eng, method, reads=(), writes=(), **kw):
        if eng != "pe":
            ex = [k for k in reads if ".bank" in k and k not in writes]
            if ex:
                writes = list(writes) + ex
        deps = self._deps(reads, writes)
        waits = self._waits(eng, deps)
        en = self.ename[eng]
        self.cnt[en] += 1
        tok = (en, self.cnt[en])
        self.q[eng].append((waits, (method, kw), (en, 1)))
        self._commit(tok, reads, writes)
        self.n_ops += 1

    def dma(self, queue, stream, out, in_, reads=(), writes=(), **kw):
        sname = "d_" + stream.split(".", 1)[-1]
        if sname not in self.sem:
            self._mksem(sname)
        deps = self._deps(reads, writes)
        waits = self._waits(queue, deps)
        self.cnt[sname] += 16
        tok = (sname, self.cnt[sname])
        kw = dict(kw)
        kw["out"] = out
        kw["in_"] = in_
        self.q[queue].append((waits, ("dma_start", kw), (sname, 16)))
        self._commit(tok, reads, writes)
        self.n_ops += 1

    def coll(self, kind, stream, ins, outs, replica_groups, reads=(), writes=()):
        sname = "d_" + stream
        if sname not in self.sem:
            self._mksem(sname)
        deps = self._deps(reads, writes)
        waits = self._waits("pool", deps)
        self.cnt[sname] += 16
        tok = (sname, self.cnt[sname])
        kw = dict(kind=kind, op=ALU.bypass, replica_groups=replica_groups, ins=list(ins), outs=list(outs))
        self.q["pool"].append((waits, ("collective_compute", kw), (sname, 16)))
        self._commit(tok, reads, writes)
        self.n_ops += 1

    def barrier(self):
        for e in self.ENGS:
            waits = []
            for s_, v in self.cnt.items():
                if v > 0 and self.known[e].get(s_, 0) < v:
                    self.known[e][s_] = v
                    waits.append((s_, v))
            if waits:
                self.q[e].append((waits, None, None))

    def finish(self):
        self.barrier()

    def new_phase(self):
        self.phase += 1
        self.last_w = {}
        self.readers = {}
        for e in ("pe", "dve", "act"):
            old_name = self.ename[e]
            for kn in self.known.values():
                kn.pop(old_name, None)
            del self.cnt[old_name]
            nm = f"{e}{self.phase}"
            self.ename[e] = nm
            self._mksem(nm)

    def emit(self):
        nc = self.nc
        S = self

        def replay(name, eng):
            for waits, fn, inc in S.q[name]:
                for s, v in waits:
                    eng.wait_ge(S.sem[s], v)
                if fn is None:
                    continue
                inst = getattr(eng, fn[0])(**fn[1])
                inst.then_inc(S.sem[inc[0]], inc[1])
            S.q[name] = []

        with nc.Block() as block:
            @block.tensor
            def _(e):
                replay("pe", e)

            @block.vector
            def _(e):
                replay("dve", e)

            @block.scalar
            def _(e):
                replay("act", e)

            @block.gpsimd
            def _(e):
                replay("pool", e)

            @block.sync
            def _(e):
                replay("sp", e)


class Ctx:
    _uid = [0]

    def __init__(self, nc, es, S=None):
        self.nc = nc
        self.es = es
        self.S = S if S is not None else Sched(nc, es)
        Ctx._uid[0] += 1
        self.n = Ctx._uid[0] * 1000

    def sb(self, shape, dt=F32, name=None):
        self.n += 1
        return self.es.enter_context(self.nc.sbuf_tensor(f"{name or 'sb'}_{self.n}", list(shape), dt))

    def ps(self, shape, dt=F32, name=None):
        self.n += 1
        return self.es.enter_context(self.nc.psum_tensor(f"{name or 'ps'}_{self.n}", list(shape), dt))


def tp_phase(C, T, glu, moe, featT, wproj, xres, lnp, w1, w3, w2, wr, br, ident_d, xout, xoutT, tb=1024, pfx="tp"):
    S = C.S
    nproj = 2 * D if glu else D
    ne = NEXP if moe else 2
    tb = min(tb, T)
    nblk = T // tb
    ntt = tb // 128
    hw = min(512, tb)
    nhalf = tb // hw
    P = pfx

    def K(name):
        return P + "." + name

    ident = C.sb([128, 128], F32, "ident")
    lnb = C.sb([128, 4, D], F32, "lnb")
    wproj_sb = C.sb([128, 8, nproj], BF16, "wproj")
    wr_sb = C.sb([128, 8, NEXP], F32, "wr")
    br_sb = C.sb([128, NEXP], F32, "br")
    w1_sb = C.sb([128, 8, FE], BF16, "w1")
    w3_sb = C.sb([128, 8, FE], BF16, "w3")
    w2_sb = C.sb([128, NFT, D], BF16, "w2")
    featT_sb = [C.sb([128, 8, 128], BF16, "featT") for _ in range(2)]
    _xres1 = C.sb([128, D], F32, "xres")
    xres_sb = [_xres1, _xres1]
    hglu_sb = C.sb([128, 512], F32, "hglu")
    stats = [C.sb([128, 2, 6], F32, "stats") for _ in range(2)]
    mv = [C.sb([128, 2], F32, "mv") for _ in range(2)]
    rstd = [C.sb([128, 1], F32, "rstd") for _ in range(2)]
    x1 = [C.sb([128, D], F32, "x1") for _ in range(2)]
    _x1T321 = C.sb([128, 8, 128], F32, "x1T32")
    x1T32 = [_x1T321, _x1T321]
    x1T = C.sb([128, 8, tb], BF16, "x1T")
    yacc = C.sb([128, ntt, D], F32, "yacc")
    gates = C.sb([128, ntt, NEXP], F32, "gates")
    rt = [C.sb([128, NEXP], F32, "rt") for _ in range(4)]
    rcol = [C.sb([128, 1], F32, "rcol") for _ in range(4)]
    hT = C.sb([128, NFT, tb], BF16, "hT")
    epsc = C.sb([128, 1], F32, "epsc")
    pg = [C.ps([128, 512], F32, "pg") for _ in range(2)]
    pu = [C.ps([128, 512], F32, "pu") for _ in range(2)]
    py = [C.ps([128, 512], F32, "py") for _ in range(2)]
    ptr = C.ps([128, 512], F32, "ptr")
    prt = C.ps([128, 512], F32, "prt")

    S.op("dve", "memset", ap=epsc[:], constant=LN_EPS, writes=[K("epsc")])
    S.dma("sp", K("c0"), ident[:], ident_d, writes=[K("ident")])
    for i in range(4):
        S.dma("sp", K("c1"), lnb[:, i, :], lnp[i:i + 1, :].partition_broadcast(128), writes=[K("lnb")])
    S.dma("pool", K("c2"), wproj_sb[:], wproj.rearrange("(j p) n -> p j n", p=128), writes=[K("wproj")])
    S.dma("sp", K("c3"), wr_sb[:], wr.rearrange("(j p) n -> p j n", p=128), writes=[K("wr")])
    S.dma("sp", K("c4"), br_sb[:], br.partition_broadcast(128), writes=[K("br")])

    def layer_norm(src, src_key, dst, dst_key, gi, idx):
        st, m, rs = stats[idx], mv[idx], rstd[idx]
        kst, kmv, krs = K(f"st{idx}"), K(f"mv{idx}"), K(f"rstd{idx}")
        for c in range(2):
            S.op("dve", "bn_stats", out=st[:, c, :], in_=src[:, c * 512:(c + 1) * 512],
                 reads=[src_key], writes=[kst + str(c)])
        S.op("dve", "bn_aggr", out=m[:], in_=st[:], reads=[kst + "0", kst + "1"], writes=[kmv])
        S.op("act", "activation", out=rs[:], in_=m[:, 1:2], func=AF.Ln, bias=epsc[:, 0:1], scale=1.0,
             reads=[kmv, K("epsc")], writes=[krs])
        S.op("act", "activation", out=rs[:], in_=rs[:], func=AF.Exp, scale=-0.5, reads=[krs], writes=[krs])
        S.op("dve", "tensor_scalar", out=dst, in0=src, scalar1=m[:, 0:1], scalar2=rs[:, 0:1],
             op0=ALU.subtract, op1=ALU.mult, reads=[src_key, kmv, krs], writes=[dst_key])
        S.op("pool", "tensor_tensor", out=dst, in0=dst, in1=lnb[:, gi, :], op=ALU.mult,
             reads=[dst_key, K("lnb")], writes=[dst_key])
        S.op("pool", "tensor_tensor", out=dst, in0=dst, in1=lnb[:, gi + 1, :], op=ALU.add,
             reads=[dst_key, K("lnb")], writes=[dst_key])

    cnt = 0
    for blk in range(nblk):
        t0 = blk * tb
        for tt in range(ntt):
            i2 = tt % 2
            tok0 = t0 + tt * 128
            fT, xr, r = featT_sb[i2], xres_sb[i2], xres_sb[i2]
            kfT, kxr, kr, kx1 = K(f"fT{i2}"), K("xr0"), K("xr0"), K(f"x1_{i2}")
            S.dma("pool", kfT, fT[:], featT[:, tok0:tok0 + 128].rearrange("(j p) t -> p j t", p=128), writes=[kfT])
            S.dma("sp", kxr, xr[:], xres[tok0:tok0 + 128, :], writes=[kxr])
            for nh in range(2):
                c0, c1 = nh * 512, (nh + 1) * 512
                pv, kpv = py[nh], K(f"py{nh}")
                for k in range(8):
                    S.op("pe", "matmul", out=pv[:], lhsT=fT[:, k, :], rhs=wproj_sb[:, k, c0:c1],
                         start=(k == 0), stop=(k == 7), reads=[kfT, K("wproj")], writes=[kpv])
                if glu:
                    pgt, kpg = pg[nh], K(f"pg{nh}")
                    for k in range(8):
                        S.op("pe", "matmul", out=pgt[:], lhsT=fT[:, k, :], rhs=wproj_sb[:, k, D + c0:D + c1],
                             start=(k == 0), stop=(k == 7), reads=[kfT, K("wproj")], writes=[kpg])
                    S.op("act", "activation", out=hglu_sb[:], in_=pgt[:], func=AF.Sigmoid,
                         reads=[kpg], writes=[K("hglu")])
                    S.op("dve", "tensor_tensor", out=hglu_sb[:], in0=hglu_sb[:], in1=pv[:], op=ALU.mult,
                         reads=[K("hglu"), kpv], writes=[K("hglu")])
                    S.op("dve", "scalar_tensor_tensor", out=r[:, c0:c1], in0=xr[:, c0:c1], scalar=ALPHA,
                         in1=hglu_sb[:], op0=ALU.mult, op1=ALU.add, reads=[kxr, K("hglu")], writes=[kr])
                else:
                    S.op("dve", "scalar_tensor_tensor", out=r[:, c0:c1], in0=xr[:, c0:c1], scalar=ALPHA,
                         in1=pv[:], op0=ALU.mult, op1=ALU.add, reads=[kxr, kpv], writes=[kr])
            xx = x1[i2]
            layer_norm(r[:], kr, xx[:], kx1, 0, i2)
            S.op("act", "mul", out=yacc[:, tt, :], in_=xx[:], mul=ALPHA, reads=[kx1], writes=[K(f"yacc{tt}")])
            xt32 = x1T32[i2]
            for kh in range(2):
                pb, kpb = (ptr, K("ptr")) if kh == 0 else (prt, K("prt"))
                for q4 in range(4):
                    k = kh * 4 + q4
                    S.op("pe", "transpose", out=pb[:, q4 * 128:(q4 + 1) * 128], in_=xx[:, k * 128:(k + 1) * 128],
                         identity=ident[:], reads=[kx1, K("ident")], writes=[kpb])
                S.op("act", "copy", out=xt32[:, kh * 4:(kh + 1) * 4, :].rearrange("p k t -> p (k t)"), in_=pb[:],
                     reads=[kpb], writes=[K("x1T32_0")])
                S.op("pool", "tensor_copy", out=x1T[:, kh * 4:(kh + 1) * 4, tt * 128:(tt + 1) * 128],
                     in_=xt32[:, kh * 4:(kh + 1) * 4, :], reads=[K("x1T32_0")], writes=[K(f"x1T_{tt}")])
            if moe:
                for k in range(8):
                    S.op("pe", "matmul", out=prt[:, 0:NEXP], lhsT=xt32[:, k, :], rhs=wr_sb[:, k, :],
                         start=(k == 0), stop=(k == 7), reads=[K("x1T32_0"), K("wr")], writes=[K("prt")])
                lg, mk, l2, ex = rt
                m1, m2, sm, rsm = rcol
                kk = lambda n: K("rt_" + n)
                S.op("dve", "tensor_tensor", out=lg[:], in0=prt[:, 0:NEXP], in1=br_sb[:], op=ALU.add,
                     reads=[K("prt"), K("br")], writes=[kk("lg")])
                S.op("dve", "reduce_max", out=m1[:], in_=lg[:], axis=AX.X, reads=[kk("lg")], writes=[kk("m1")])
                S.op("dve", "tensor_scalar", out=mk[:], in0=lg[:], scalar1=m1[:, 0:1], scalar2=-1e30,
                     op0=ALU.is_ge, op1=ALU.mult, reads=[kk("lg"), kk("m1")], writes=[kk("mk")])
                S.op("dve", "tensor_tensor", out=l2[:], in0=lg[:], in1=mk[:], op=ALU.add,
                     reads=[kk("lg"), kk("mk")], writes=[kk("l2")])
                S.op("dve", "reduce_max", out=m2[:], in_=l2[:], axis=AX.X, reads=[kk("l2")], writes=[kk("m2")])
                S.op("dve", "tensor_scalar", out=ex[:], in0=lg[:], scalar1=m1[:, 0:1], scalar2=None,
                     op0=ALU.subtract, reads=[kk("lg"), kk("m1")], writes=[kk("ex")])
                S.op("act", "activation", out=ex[:], in_=ex[:], func=AF.Exp, reads=[kk("ex")], writes=[kk("ex")])
                S.op("dve", "scalar_tensor_tensor", out=ex[:], in0=lg[:], scalar=m2[:, 0:1], in1=ex[:],
                     op0=ALU.is_ge, op1=ALU.mult, reads=[kk("lg"), kk("m2"), kk("ex")], writes=[kk("ex")])
                S.op("dve", "reduce_sum", out=sm[:], in_=ex[:], axis=AX.X, reads=[kk("ex")], writes=[kk("sm")])
                S.op("dve", "reciprocal", out=rsm[:], in_=sm[:], reads=[kk("sm")], writes=[kk("rsm")])
                S.op("dve", "tensor_scalar", out=gates[:, tt, :], in0=ex[:], scalar1=rsm[:, 0:1], scalar2=None,
                     op0=ALU.mult, reads=[kk("ex"), kk("rsm")], writes=[K(f"gates{tt}")])
        for ex_i in range(ne):
            S.dma("pool", K("w1"), w1_sb[:], w1[ex_i].rearrange("(j p) n -> p j n", p=128), writes=[K("w1")])
            S.dma("pool", K("w3"), w3_sb[:], w3[ex_i].rearrange("(j p) n -> p j n", p=128), writes=[K("w3")])
            S.dma("pool", K("w2"), w2_sb[:], w2[ex_i].rearrange("(j p) n -> p j n", p=128), writes=[K("w2")])
            for hf in range(nhalf):
                ts0, ts1 = hf * hw, (hf + 1) * hw
                tkeys = [K(f"x1T_{tt}") for tt in range(ts0 // 128, ts1 // 128)]
                for f in range(NFT):
                    b = cnt % 2
                    cnt += 1
                    f0, f1 = f * 128, (f + 1) * 128
                    for k in range(8):
                        S.op("pe", "matmul", out=pg[b][:, 0:hw], lhsT=w1_sb[:, k, f0:f1], rhs=x1T[:, k, ts0:ts1],
                             start=(k == 0), stop=(k == 7), reads=[K("w1")] + tkeys, writes=[K(f"pg{b}")])
                    for k in range(8):
                        S.op("pe", "matmul", out=pu[b][:, 0:hw], lhsT=w3_sb[:, k, f0:f1], rhs=x1T[:, k, ts0:ts1],
                             start=(k == 0), stop=(k == 7), reads=[K("w3")] + tkeys, writes=[K(f"pu{b}")])
                    S.op("act", "activation", out=hT[:, f, ts0:ts1], in_=pg[b][:, 0:hw], func=AF.Silu,
                         reads=[K(f"pg{b}")], writes=[K(f"hT{f}_{hf}")])
                    S.op("dve", "tensor_tensor", out=hT[:, f, ts0:ts1], in0=hT[:, f, ts0:ts1], in1=pu[b][:, 0:hw],
                         op=ALU.mult, reads=[K(f"hT{f}_{hf}"), K(f"pu{b}")], writes=[K(f"hT{f}_{hf}")])
            for hf in range(nhalf):
                hkeys = [K(f"hT{f}_{hf}") for f in range(NFT)]
                for tl in range(hw // 128):
                    tt = hf * (hw // 128) + tl
                    kya = K(f"yacc{tt}")
                    for nh in range(2):
                        c0, c1 = nh * 512, (nh + 1) * 512
                        kpy = K(f"py{nh}")
                        for f in range(NFT):
                            S.op("pe", "matmul", out=py[nh][:], lhsT=hT[:, f, tt * 128:(tt + 1) * 128],
                                 rhs=w2_sb[:, f, c0:c1], start=(f == 0), stop=(f == NFT - 1),
                                 reads=hkeys + [K("w2")], writes=[kpy])
                        if moe:
                            S.op("dve", "scalar_tensor_tensor", out=yacc[:, tt, c0:c1], in0=py[nh][:],
                                 scalar=gates[:, tt, ex_i:ex_i + 1], in1=yacc[:, tt, c0:c1], op0=ALU.mult,
                                 op1=ALU.add, reads=[kpy, K(f"gates{tt}"), kya], writes=[kya])
                        else:
                            S.op("dve", "tensor_tensor", out=yacc[:, tt, c0:c1], in0=py[nh][:],
                                 in1=yacc[:, tt, c0:c1], op=ALU.add, reads=[kpy, kya], writes=[kya])
        for tt in range(ntt):
            i2 = tt % 2
            tok0 = t0 + tt * 128
            xx, kxo = x1[i2], K(f"x1_{i2}")
            layer_norm(yacc[:, tt, :], K(f"yacc{tt}"), xx[:], kxo, 2, i2)
            S.dma("sp", kxo, xout[tok0:tok0 + 128, :], xx[:], reads=[kxo])
            xt, kxt = x1T32[i2], K("x1T32_0")
            for kh in range(2):
                pb, kpb = (ptr, K("ptr")) if kh == 0 else (prt, K("prt"))
                for q4 in range(4):
                    k = kh * 4 + q4
                    S.op("pe", "transpose", out=pb[:, q4 * 128:(q4 + 1) * 128], in_=xx[:, k * 128:(k + 1) * 128],
                         identity=ident[:], reads=[kxo, K("ident")], writes=[kpb])
                S.op("act", "copy", out=xt[:, kh * 4:(kh + 1) * 4, :].rearrange("p k t -> p (k t)"), in_=pb[:],
                     reads=[kpb], writes=[kxt])
            S.dma("sp", kxt, xoutT[:, tok0:tok0 + 128].rearrange("(j p) t -> p j t", p=128), xt[:], reads=[kxt])


def build_tp(T, glu, moe, tb=1024):
    nc = bass.Bass("TRN2", target_bir_lowering=False)
    nproj = 2 * D if glu else D
    ne = NEXP if moe else 2
    dt = lambda n, s, k="ExternalInput": nc.dram_tensor(n, s, F32, kind=k).ap()
    featT = dt("featT", [D, T]); wproj = dt("wproj", [D, nproj]); xres = dt("xres", [T, D])
    lnp = dt("lnp", [4, D]); w1 = dt("w1", [ne, D, FE]); w3 = dt("w3", [ne, D, FE]); w2 = dt("w2", [ne, FE, D])
    wr = dt("wr", [D, NEXP]); br = dt("br", [1, NEXP]); ident_d = dt("ident", [128, 128])
    xout = dt("xout", [T, D], "ExternalOutput"); xoutT = dt("xoutT", [D, T], "ExternalOutput")
    with contextlib.ExitStack() as es:
        C = Ctx(nc, es)
        tp_phase(C, T, glu, moe, featT, wproj, xres, lnp, w1, w3, w2, wr, br, ident_d, xout, xoutT, tb=tb)
        C.S.finish()
        C.S.emit()
    return nc


S5_L = 128
PI = float(np.pi)


def s5_phase(C, T, xT, w_in, lamr_d, lami_d, ldt_d, bpad_re_d, bpad_im_d, cpad_re_d, cpad_im_d, dsk_d, iota_d,
             hidT, pfx="s5"):
    S = C.S
    L = S5_L
    NB = T // 512
    NGP = 16
    P = pfx

    def K(n):
        return P + "." + n

    w_sb = C.sb([128, 8, 512], BF16, "s5w")
    xT_sb = [C.sb([128, 8, 512], BF16, "s5x") for _ in range(2)]
    u_sb = [C.sb([128, 4, 512], F32, "s5u") for _ in range(2)]
    bre = C.sb([128, NGP, 128], F32, "bre")
    bim = C.sb([128, NGP, 128], F32, "bim")
    cre = C.sb([128, NGP, 128], F32, "cre")
    cim = C.sb([128, NGP, 128], F32, "cim")
    dsk = C.sb([128, 4], F32, "dsk")
    iota = C.sb([128, L], F32, "iota")
    prm = {n: C.sb([128, NGP], F32, "p_" + n) for n in
           ("lamr", "lami", "ldt", "dt", "zr", "zi", "r", "cz", "sz", "ar", "ai", "den", "cr", "ci", "ncr",
            "t1", "t2", "zl", "er", "ei", "nei")}
    negpi = C.sb([128, 1], F32, "negpi")
    tre = C.sb([128, NGP, L], F32, "tre")
    tim = C.sb([128, NGP, L], F32, "tim")
    cph = C.sb([128, NGP, L], F32, "cph")
    sph = C.sb([128, NGP, L], F32, "sph")
    rmat = C.sb([128, NGP, L], F32, "rmat")
    ang = C.sb([128, L], F32, "ang")
    ang2 = C.sb([128, L], F32, "ang2")
    hp = C.sb([128, NGP, 2], F32, "hp")
    tmpc2 = [C.sb([128, 2], F32, "tmpc") for _ in range(2)]
    BUr = [C.sb([128, 512], F32, "BUr") for _ in range(2)]
    BUi = [C.sb([128, 512], F32, "BUi") for _ in range(2)]
    mt = [[C.sb([128, 512], F32, "mt") for _ in range(4)] for _ in range(2)]
    btr = [C.sb([128, 512], F32, "btr") for _ in range(2)]
    bti = [C.sb([128, 512], F32, "bti") for _ in range(2)]
    wre = [C.sb([128, 512], F32, "wre") for _ in range(2)]
    wim = [C.sb([128, 512], F32, "wim") for _ in range(2)]
    hre = [[C.sb([128, 512], F32, "hre") for _ in range(4)] for _ in range(2)]
    him = [[C.sb([128, 512], F32, "him") for _ in range(4)] for _ in range(2)]
    yt = [C.sb([128, 512], F32, "yt") for _ in range(2)]
    ho = [C.sb([128, 512], F32, "ho") for _ in range(2)]
    pu_ = [C.ps([128, 512], F32, "s5pu") for _ in range(2)]
    pbr = [C.ps([128, 512], F32, "s5pbr") for _ in range(2)]
    pbi = [C.ps([128, 512], F32, "s5pbi") for _ in range(2)]
    pyy = [C.ps([128, 512], F32, "s5py") for _ in range(2)]

    S.dma("pool", K("w"), w_sb[:], w_in.rearrange("(j p) n -> p j n", p=128), writes=[K("w")])
    for nm, dd, sbt in (("bre", bpad_re_d, bre), ("bim", bpad_im_d, bim), ("cre", cpad_re_d, cre), ("cim", cpad_im_d, cim)):
        S.dma("sp", K(nm), sbt[:], dd.rearrange("g k m -> k g m"), writes=[K(nm)])
    S.dma("sp", K("dsk"), dsk[:], dsk_d, writes=[K("dsk")])
    S.dma("sp", K("iota"), iota[:], iota_d, writes=[K("iota")])
    S.dma("sp", K("lamr"), prm["lamr"][:], lamr_d, writes=[K("lamr")])
    S.dma("sp", K("lami"), prm["lami"][:], lami_d, writes=[K("lami")])
    S.dma("sp", K("ldt"), prm["ldt"][:], ldt_d, writes=[K("ldt")])
    S.op("dve", "memset", ap=negpi[:], constant=-PI, writes=[K("negpi")])
    itmp = C.sb([128, L], mybir.dt.int32, "itmp")
    ftmp = C.sb([128, L], F32, "ftmp")
    S.op("dve", "memset", ap=hp[:], constant=0.0, writes=[K(f"hp{g}") for g in range(NGP)])

    def pp(n):
        return prm[n][:]

    def ew(eng, method, out_n, reads, **kw):
        S.op(eng, method, reads=[K(x) for x in reads], writes=[K(out_n)], **kw)

    def sincos(angle_ap, angle_key, sin_ap, sin_key, cos_ap, cos_key, tmp_ap, tmp_key, width):
        for off, o_ap, o_key in ((0.5, sin_ap, sin_key), (0.75, cos_ap, cos_key)):
            S.op("dve", "tensor_scalar", out=tmp_ap, in0=angle_ap, scalar1=1.0 / (2.0 * PI), scalar2=off,
                 op0=ALU.mult, op1=ALU.add, reads=[angle_key], writes=[tmp_key])
            S.op("dve", "tensor_copy", out=itmp[:, 0:width], in_=tmp_ap, reads=[tmp_key], writes=[K("itmp")])
            S.op("dve", "tensor_copy", out=ftmp[:, 0:width], in_=itmp[:, 0:width], reads=[K("itmp")], writes=[K("ftmp")])
            S.op("dve", "tensor_tensor", out=tmp_ap, in0=tmp_ap, in1=ftmp[:, 0:width], op=ALU.subtract,
                 reads=[tmp_key, K("ftmp")], writes=[tmp_key])
            S.op("dve", "scalar_tensor_tensor", out=tmp_ap, in0=tmp_ap, scalar=0.0, in1=tmp_ap, op0=ALU.is_lt,
                 op1=ALU.add, reads=[tmp_key], writes=[tmp_key])
            S.op("act", "activation", out=o_ap, in_=tmp_ap, func=AF.Sin, bias=negpi[:, 0:1], scale=2.0 * PI,
                 reads=[tmp_key, K("negpi")], writes=[o_key])

    ew("act", "activation", "dt", ["ldt"], out=pp("dt"), in_=pp("ldt"), func=AF.Exp)
    ew("dve", "tensor_tensor", "zr", ["lamr", "dt"], out=pp("zr"), in0=pp("lamr"), in1=pp("dt"), op=ALU.mult)
    ew("dve", "tensor_tensor", "zi", ["lami", "dt"], out=pp("zi"), in0=pp("lami"), in1=pp("dt"), op=ALU.mult)
    ew("act", "activation", "r", ["zr"], out=pp("r"), in_=pp("zr"), func=AF.Exp)
    sincos(pp("zi"), K("zi"), pp("sz"), K("sz"), pp("cz"), K("cz"), pp("t1"), K("t1"), NGP)
    ew("dve", "tensor_tensor", "ar", ["r", "cz"], out=pp("ar"), in0=pp("r"), in1=pp("cz"), op=ALU.mult)
    ew("dve", "tensor_scalar", "ar", ["ar"], out=pp("ar"), in0=pp("ar"), scalar1=-1.0, scalar2=None, op0=ALU.add)
    ew("dve", "tensor_tensor", "ai", ["r", "sz"], out=pp("ai"), in0=pp("r"), in1=pp("sz"), op=ALU.mult)
    ew("dve", "tensor_tensor", "den", ["lamr"], out=pp("den"), in0=pp("lamr"), in1=pp("lamr"), op=ALU.mult)
    ew("dve", "tensor_tensor", "t1", ["lami"], out=pp("t1"), in0=pp("lami"), in1=pp("lami"), op=ALU.mult)
    ew("dve", "tensor_tensor", "den", ["den", "t1"], out=pp("den"), in0=pp("den"), in1=pp("t1"), op=ALU.add)
    ew("dve", "reciprocal", "den", ["den"], out=pp("den"), in_=pp("den"))
    ew("dve", "tensor_tensor", "t1", ["ar", "lamr"], out=pp("t1"), in0=pp("ar"), in1=pp("lamr"), op=ALU.mult)
    ew("dve", "tensor_tensor", "t2", ["ai", "lami"], out=pp("t2"), in0=pp("ai"), in1=pp("lami"), op=ALU.mult)
    ew("dve", "tensor_tensor", "cr", ["t1", "t2"], out=pp("cr"), in0=pp("t1"), in1=pp("t2"), op=ALU.add)
    ew("dve", "tensor_tensor", "cr", ["cr", "den"], out=pp("cr"), in0=pp("cr"), in1=pp("den"), op=ALU.mult)
    ew("dve", "tensor_tensor", "t1", ["ai", "lamr"], out=pp("t1"), in0=pp("ai"), in1=pp("lamr"), op=ALU.mult)
    ew("dve", "tensor_tensor", "t2", ["ar", "lami"], out=pp("t2"), in0=pp("ar"), in1=pp("lami"), op=ALU.mult)
    ew("dve", "tensor_tensor", "ci", ["t1", "t2"], out=pp("ci"), in0=pp("t1"), in1=pp("t2"), op=ALU.subtract)
    ew("dve", "tensor_tensor", "ci", ["ci", "den"], out=pp("ci"), in0=pp("ci"), in1=pp("den"), op=ALU.mult)
    ew("dve", "tensor_scalar", "ncr", ["cr"], out=pp("ncr"), in0=pp("cr"), scalar1=-1.0, scalar2=None, op0=ALU.mult)
    ew("dve", "tensor_scalar", "zl", ["zi"], out=pp("zl"), in0=pp("zi"), scalar1=float(L), scalar2=None, op0=ALU.mult)
    sincos(pp("zl"), K("zl"), pp("ei"), K("ei"), pp("er"), K("er"), pp("t1"), K("t1"), NGP)
    ew("dve", "tensor_scalar", "nei", ["ei"], out=pp("nei"), in0=pp("ei"), scalar1=-1.0, scalar2=None, op0=ALU.mult)
    for g in range(NGP):
        S.op("dve", "tensor_scalar", out=ang[:], in0=iota[:], scalar1=prm["zi"][:, g:g + 1], scalar2=None,
             op0=ALU.mult, reads=[K("iota"), K("zi")], writes=[K("ang")])
        sincos(ang[:], K("ang"), sph[:, g, :], K("sph"), cph[:, g, :], K("cph"), ang2[:], K("ang2"), L)
        S.op("dve", "tensor_scalar", out=ang2[:], in0=cph[:, g, :], scalar1=prm["cr"][:, g:g + 1], scalar2=None,
             op0=ALU.mult, reads=[K("cph"), K("cr")], writes=[K("ang2")])
        S.op("dve", "scalar_tensor_tensor", out=tre[:, g, :], in0=sph[:, g, :], scalar=prm["ci"][:, g:g + 1],
             in1=ang2[:], op0=ALU.mult, op1=ALU.add, reads=[K("sph"), K("ci"), K("ang2")], writes=[K("tre")])
        S.op("dve", "tensor_scalar", out=ang2[:], in0=cph[:, g, :], scalar1=prm["ci"][:, g:g + 1], scalar2=None,
             op0=ALU.mult, reads=[K("cph"), K("ci")], writes=[K("ang2")])
        S.op("dve", "scalar_tensor_tensor", out=tim[:, g, :], in0=sph[:, g, :], scalar=prm["ncr"][:, g:g + 1],
             in1=ang2[:], op0=ALU.mult, op1=ALU.add, reads=[K("sph"), K("ncr"), K("ang2")], writes=[K("tim")])
        S.op("pool", "memset", ap=rmat[:, g, :], constant=1.0, writes=[K("rmat")])
        S.op("dve", "tensor_scalar", out=rmat[:, g, :], in0=rmat[:, g, :], scalar1=prm["r"][:, g:g + 1], scalar2=None,
             op0=ALU.mult, reads=[K("rmat"), K("r")], writes=[K("rmat")])

    NC4 = 512 // L

    def v3(t):
        return t[:].rearrange("p (c l) -> p c l", l=L)

    def tb3(tab, g):
        return tab[:, g:g + 1, :].broadcast_to([128, NC4, L])

    gi = 0
    for blk in range(NB):
        t0 = blk * 512
        bi = blk % 2
        xs, us = xT_sb[bi], u_sb[bi]
        kx, ku = K(f"x{bi}"), K(f"u{bi}")
        S.dma("pool", kx, xs[:], xT[:, t0:t0 + 512].rearrange("(j p) t -> p j t", p=128), writes=[kx])
        for ft in range(4):
            b2 = ft % 2
            for k in range(8):
                S.op("pe", "matmul", out=pu_[b2][:], lhsT=w_sb[:, k, ft * 128:(ft + 1) * 128], rhs=xs[:, k, :],
                     start=(k == 0), stop=(k == 7), reads=[K("w"), kx], writes=[K(f"pu{b2}")])
            S.op("act", "copy", out=us[:, ft, :], in_=pu_[b2][:], reads=[K(f"pu{b2}")], writes=[ku + f"_{ft}"])
        for ft in range(4):
            f2 = ft % 2
            for gpair in range(0, 4, 2):
                recs = []
                for gl4 in (gpair, gpair + 1):
                    rec = []
                    real_op = S.op
                    S.op = (lambda eng, method, reads=(), writes=(), _rec=rec, **kw:
                            _rec.append((eng, method, reads, writes, kw)))
                    try:
                        g = ft * 4 + gl4
                        b2 = gi % 2
                        gi += 1
                        kb = lambda n: K(f"{n}{b2}")
                        tmpc = tmpc2[b2]
                        S.op("pe", "matmul", out=pbr[b2][:], lhsT=bre[:, g, :], rhs=us[:, ft, :], start=True, stop=True,
                             reads=[K("bre"), ku + f"_{ft}"], writes=[kb("pbr")])
                        S.op("pe", "matmul", out=pbi[b2][:], lhsT=bim[:, g, :], rhs=us[:, ft, :], start=True, stop=True,
                             reads=[K("bim"), ku + f"_{ft}"], writes=[kb("pbi")])
                        S.op("act", "copy", out=BUr[b2][:], in_=pbr[b2][:], reads=[kb("pbr")], writes=[kb("BUr")])
                        S.op("act", "copy", out=BUi[b2][:], in_=pbi[b2][:], reads=[kb("pbi")], writes=[kb("BUi")])
                        m0, m1, m2, m3 = mt[b2]
                        S.op("dve", "tensor_tensor", out=v3(m0), in0=v3(BUr[b2]), in1=tb3(tre, g), op=ALU.mult,
                             reads=[kb("BUr"), K("tre")], writes=[kb("m0")])
                        S.op("pool", "tensor_tensor", out=v3(m1), in0=v3(BUi[b2]), in1=tb3(tim, g), op=ALU.mult,
                             reads=[kb("BUi"), K("tim")], writes=[kb("m1")])
                        S.op("dve", "tensor_tensor", out=btr[b2][:], in0=m0[:], in1=m1[:], op=ALU.subtract,
                             reads=[kb("m0"), kb("m1")], writes=[kb("btr")])
                        S.op("pool", "tensor_tensor", out=v3(m2), in0=v3(BUi[b2]), in1=tb3(tre, g), op=ALU.mult,
                             reads=[kb("BUi"), K("tre")], writes=[kb("m2")])
                        S.op("dve", "tensor_tensor", out=v3(m3), in0=v3(BUr[b2]), in1=tb3(tim, g), op=ALU.mult,
                             reads=[kb("BUr"), K("tim")], writes=[kb("m3")])
                        S.op("pool", "tensor_tensor", out=bti[b2][:], in0=m2[:], in1=m3[:], op=ALU.add,
                             reads=[kb("m2"), kb("m3")], writes=[kb("bti")])
                        khp = K(f"hp{g}")
                        for c in range(NC4):
                            cs = slice(c * L, (c + 1) * L)
                            S.op("dve", "tensor_tensor_scan", out=wre[b2][:, cs], data0=rmat[:, g, :], data1=btr[b2][:, cs],
                                 initial=hp[:, g, 0:1], op0=ALU.mult, op1=ALU.add,
                                 reads=[K("rmat"), kb("btr"), khp], writes=[kb("wre")])
                            S.op("dve", "tensor_tensor_scan", out=wim[b2][:, cs], data0=rmat[:, g, :], data1=bti[b2][:, cs],
                                 initial=hp[:, g, 1:2], op0=ALU.mult, op1=ALU.add,
                                 reads=[K("rmat"), kb("bti"), khp], writes=[kb("wim")])
                            last = (c + 1) * L - 1
                            S.op("dve", "tensor_scalar", out=tmpc[:, 0:1], in0=wre[b2][:, last:last + 1],
                                 scalar1=prm["er"][:, g:g + 1], scalar2=None, op0=ALU.mult,
                                 reads=[kb("wre"), K("er")], writes=[kb("tmpc0")])
                            S.op("dve", "tensor_scalar", out=tmpc[:, 1:2], in0=wre[b2][:, last:last + 1],
                                 scalar1=prm["ei"][:, g:g + 1], scalar2=None, op0=ALU.mult,
                                 reads=[kb("wre"), K("ei")], writes=[kb("tmpc1")])
                            S.op("dve", "scalar_tensor_tensor", out=hp[:, g, 0:1], in0=wim[b2][:, last:last + 1],
                                 scalar=prm["nei"][:, g:g + 1], in1=tmpc[:, 0:1], op0=ALU.mult, op1=ALU.add,
                                 reads=[kb("wim"), K("nei"), kb("tmpc0")], writes=[khp])
                            S.op("dve", "scalar_tensor_tensor", out=hp[:, g, 1:2], in0=wim[b2][:, last:last + 1],
                                 scalar=prm["er"][:, g:g + 1], in1=tmpc[:, 1:2], op0=ALU.mult, op1=ALU.add,
                                 reads=[kb("wim"), K("er"), kb("tmpc1")], writes=[khp])
                        hr, hi_ = hre[f2][gl4], him[f2][gl4]
                        khr, khi = K(f"hre{f2}{gl4}"), K(f"him{f2}{gl4}")
                        S.op("pool", "tensor_tensor", out=v3(m0), in0=v3(wre[b2]), in1=tb3(cph, g), op=ALU.mult,
                             reads=[kb("wre"), K("cph")], writes=[kb("m0")])
                        S.op("pool", "tensor_tensor", out=v3(m1), in0=v3(wim[b2]), in1=tb3(sph, g), op=ALU.mult,
                             reads=[kb("wim"), K("sph")], writes=[kb("m1")])
                        S.op("pool", "tensor_tensor", out=hr[:], in0=m0[:], in1=m1[:], op=ALU.subtract,
                             reads=[kb("m0"), kb("m1")], writes=[khr])
                        S.op("dve", "tensor_tensor", out=v3(m2), in0=v3(wre[b2]), in1=tb3(sph, g), op=ALU.mult,
                             reads=[kb("wre"), K("sph")], writes=[kb("m2")])
                        S.op("pool", "tensor_tensor", out=v3(m3), in0=v3(wim[b2]), in1=tb3(cph, g), op=ALU.mult,
                             reads=[kb("wim"), K("cph")], writes=[kb("m3")])
                        S.op("dve", "scalar_tensor_tensor", out=hi_[:], in0=m2[:], scalar=-1.0, in1=m3[:], op0=ALU.mult,
                             op1=ALU.subtract, reads=[kb("m2"), kb("m3")], writes=[khi])
                    finally:
                        S.op = real_op
                    recs.append(rec)
                for q in range(max(len(recs[0]), len(recs[1]))):
                    for rr in recs:
                        if q < len(rr):
                            eng_, method_, reads_, writes_, kw_ = rr[q]
                            S.op(eng_, method_, reads=reads_, writes=writes_, **kw_)
            for gl4 in range(4):
                g = ft * 4 + gl4
                S.op("pe", "matmul", out=pyy[f2][:], lhsT=cre[:, g, :], rhs=hre[f2][gl4][:], start=(gl4 == 0), stop=False,
                     reads=[K("cre"), K(f"hre{f2}{gl4}")], writes=[K(f"pyy{f2}")])
                S.op("pe", "matmul", out=pyy[f2][:], lhsT=cim[:, g, :], rhs=him[f2][gl4][:], start=False, stop=(gl4 == 3),
                     reads=[K("cim"), K(f"him{f2}{gl4}")], writes=[K(f"pyy{f2}")])
            S.op("dve", "scalar_tensor_tensor", out=yt[f2][:], in0=us[:, ft, :], scalar=dsk[:, ft:ft + 1], in1=pyy[f2][:],
                 op0=ALU.mult, op1=ALU.add, reads=[ku + f"_{ft}", K("dsk"), K(f"pyy{f2}")], writes=[K(f"yt{f2}")])
            S.op("act", "activation", out=ho[f2][:], in_=yt[f2][:], func=AF.Gelu, reads=[K(f"yt{f2}")],
                 writes=[K(f"ho{f2}")])
            S.dma("sp", K(f"ho{f2}"), hidT[ft * 128:(ft + 1) * 128, t0:t0 + 512], ho[f2][:], reads=[K(f"ho{f2}")])


def build_s5(T):
    nc = bass.Bass("TRN2", target_bir_lowering=False)
    dt = lambda n, s, k="ExternalInput": nc.dram_tensor(n, s, F32, kind=k).ap()
    xT = dt("xT", [D, T]); w_in = dt("w_in", [D, 512])
    lamr = dt("lamr", [128, 16]); lami = dt("lami", [128, 16]); ldt = dt("ldt", [128, 16])
    bre = dt("bpad_re", [16, 128, 128]); bim = dt("bpad_im", [16, 128, 128])
    cre = dt("cpad_re", [16, 128, 128]); cim = dt("cpad_im", [16, 128, 128])
    dsk = dt("dsk", [128, 4]); iota = dt("iota", [128, S5_L])
    hidT = dt("hidT", [512, T], "ExternalOutput")
    with contextlib.ExitStack() as es:
        C = Ctx(nc, es)
        s5_phase(C, T, xT, w_in, lamr, lami, ldt, bre, bim, cre, cim, dsk, iota, hidT)
        C.S.finish()
        C.S.emit()
    return nc


def s5_host_layout(ghalf, s5_lam_re, s5_lam_im, s5_log_dt, s5_b_re, s5_b_im, s5_c_re, s5_c_im, s5_d, s5_w_in):
    g0 = 32 * ghalf
    lamr = np.zeros((128, 16), np.float32); lami = np.zeros((128, 16), np.float32); ldt = np.zeros((128, 16), np.float32)
    bre = np.zeros((16, 128, 128), np.float32); bim = np.zeros((16, 128, 128), np.float32)
    cre = np.zeros((16, 128, 128), np.float32); cim = np.zeros((16, 128, 128), np.float32)
    for gp in range(16):
        for gl in range(2):
            g = g0 + 2 * gp + gl
            lamr[gl * 64:(gl + 1) * 64, gp] = s5_lam_re[g]
            lami[gl * 64:(gl + 1) * 64, gp] = s5_lam_im[g]
            ldt[gl * 64:(gl + 1) * 64, gp] = s5_log_dt[g]
            r0 = 32 * (gp % 4) + 16 * gl
            bre[gp, r0:r0 + 16, gl * 64:(gl + 1) * 64] = s5_b_re[g].T
            bim[gp, r0:r0 + 16, gl * 64:(gl + 1) * 64] = s5_b_im[g].T
            cre[gp, gl * 64:(gl + 1) * 64, r0:r0 + 16] = s5_c_re[g].T
            cim[gp, gl * 64:(gl + 1) * 64, r0:r0 + 16] = s5_c_im[g].T
    dsk = np.ascontiguousarray(s5_d[512 * ghalf:512 * (ghalf + 1)].reshape(4, 128).T)
    iota = np.tile(np.arange(1, S5_L + 1, dtype=np.float32)[None, :], (128, 1))
    w = np.ascontiguousarray(s5_w_in[:, 512 * ghalf:512 * (ghalf + 1)])
    return dict(w_in=w, lamr=lamr, lami=lami, ldt=ldt, bpad_re=bre, bpad_im=bim, cpad_re=cre, cpad_im=cim,
                dsk=dsk, iota=iota)


GDN_LV = 99
GDN_CLAMP = False


def gdn_phase(C, T, xT, wq_d, wk_d, wv_d, wz_d, wba_d, convw_d, hp_d, normg_d, gconst_d, ogT, pfx="gd"):
    S = C.S
    NB = T // 512
    P = pfx
    NH = 4

    def K(n):
        return P + "." + n

    wq = C.sb([128, 8, 512], BF16, "wq"); wk = C.sb([128, 8, 512], BF16, "wk")
    wv = C.sb([128, 8, 512], BF16, "wv"); wz = C.sb([128, 8, 512], BF16, "wz")
    wba = C.sb([128, 8, 8], F32, "wba")
    convw = C.sb([128, 3, NH, 4], F32, "convw")
    hpar = C.sb([128, 8], F32, "hpar")
    nea = C.sb([128, NH], F32, "nea")
    normg = C.sb([128, 128], F32, "normg")
    cst = C.sb([128, 6, 128], F32, "gcst")
    ident, MU, MS, NEGL, NEGUT, ONES = (cst[:, i, :] for i in range(6))
    onec = C.sb([128, 1], F32, "onec"); eps6 = C.sb([128, 1], F32, "eps6")
    xs = [C.sb([128, 8, 512], BF16, "gxs") for _ in range(2)]
    xs32 = C.sb([128, 8, 512], F32, "gxs32")
    xb = [[C.sb([128, 515], F32, "xb") for _ in range(NH)] for _ in range(3)]
    qkvc = [[C.sb([128, 512], F32, "qkvc") for _ in range(NH)] for _ in range(3)]
    cacc = [C.sb([128, 512], F32, "cacc") for _ in range(2)]
    zs = C.sb([128, 512], F32, "zs")
    ba = C.sb([128, 8], F32, "ba")
    gt = {n: C.sb([128, NH], F32, "g_" + n) for n in
          ("beta", "nbeta", "x", "e", "sp", "g", "eg", "kes", "elast", "bks")}
    gc8 = C.sb([128, 8], F32, "gc8")
    Sst = [C.sb([128, 128], F32, "Sst") for _ in range(NH)]
    ogT_sb = C.sb([128, NH, 512], F32, "ogT")

    def mk(n):
        return [C.sb([128, 128], F32, n) for _ in range(2)]

    Kn, KnT, Kbg, Kend, Vb, sq, G1, Dl, DTu, AT, PT, nWT, Vnew, o1, o_, og = (mk(n) for n in (
        "Kn", "KnT", "Kbg", "Kend", "Vb", "sq", "G1", "Dl", "DTu", "AT", "PT", "nWT", "Vnew", "o1", "o", "og"))
    Nn = [mk("Na"), mk("Nb")]
    NTn = [mk("NTa"), mk("NTb")]
    junk = mk("junk")
    Qc = mk("Qc")
    col = {n: [C.sb([128, 1], F32, "c_" + n) for _ in range(2)] for n in
           ("ssqk", "rk", "ssqq", "rq", "rq2", "ssqo", "v1", "fac")}
    bank = [C.ps([128, 512], F32, f"gb{i}") for i in range(8)]
    BK = [K(f"bank{i}") for i in range(8)]

    for nm, dd, sbt in (("wq", wq_d, wq), ("wk", wk_d, wk), ("wv", wv_d, wv), ("wz", wz_d, wz)):
        S.dma("pool", K(nm), sbt[:], dd.rearrange("(j p) n -> p j n", p=128), writes=[K(nm)])
    S.dma("sp", K("wba"), wba[:], wba_d.rearrange("(j p) n -> p j n", p=128), writes=[K("wba")])
    S.dma("sp", K("convw"), convw[:], convw_d, writes=[K("convw")])
    S.dma("sp", K("hpar"), hpar[:], hp_d, writes=[K("hpar")])
    S.dma("sp", K("normg"), normg[:], normg_d, writes=[K("normg")])
    S.dma("sp", K("cst"), cst[:], gconst_d.rearrange("i p f -> p i f"), writes=[K("cst")])
    S.op("dve", "memset", ap=onec[:], constant=1.0, writes=[K("onec")])
    S.op("dve", "memset", ap=eps6[:], constant=NORM_EPS, writes=[K("eps6")])
    for h in range(NH):
        S.op("dve", "memset", ap=Sst[h][:], constant=0.0, writes=[K(f"S{h}")])
        for i in range(3):
            S.op("pool", "memset", ap=xb[i][h][:, 0:3], constant=0.0, writes=[K(f"xb{i}{h}")])
    S.op("act", "activation", out=nea[:], in_=hpar[:, 0:4], func=AF.Exp, reads=[K("hpar")], writes=[K("nea")])
    S.op("dve", "tensor_scalar", out=nea[:], in0=nea[:], scalar1=-1.0, scalar2=None, op0=ALU.mult,
         reads=[K("nea")], writes=[K("nea")])

    def mm(bi, c0, c1, lhsT, rhs, reads, start=True, stop=True, rows=128):
        S.op("pe", "matmul", out=bank[bi][0:rows, c0:c1], lhsT=lhsT, rhs=rhs, start=start, stop=stop,
             reads=reads, writes=[BK[bi]])

    def tr(bi, c0, in_, reads):
        S.op("pe", "transpose", out=bank[bi][:, c0:c0 + 128], in_=in_, identity=ident,
             reads=reads + [K("cst")], writes=[BK[bi]])

    it = 0
    for blk in range(NB):
        t0 = blk * 512
        bi2 = blk % 2
        xsb, kx = xs[bi2], K(f"xs{bi2}")
        S.dma("pool", kx, xsb[:], xT[:, t0:t0 + 512].rearrange("(j p) t -> p j t", p=128), writes=[kx])
        S.dma("sp", K("xs32"), xs32[:], xT[:, t0:t0 + 512].rearrange("(j p) t -> p j t", p=128), writes=[K("xs32")])
        ci = 0
        for i, wsb, wkey in ((0, wq, "wq"), (1, wk, "wk"), (2, wv, "wv")):
            for h in range(NH):
                for k in range(8):
                    mm(0, 0, 512, wsb[:, k, h * 128:(h + 1) * 128], xsb[:, k, :], [K(wkey), kx], start=(k == 0),
                       stop=(k == 7))
                xbt, kxb = xb[i][h], K(f"xb{i}{h}")
                S.op("act", "copy", out=xbt[:, 3:515], in_=bank[0][:], reads=[BK[0]], writes=[kxb])
                ca, kca = cacc[ci % 2], K(f"cacc{ci % 2}")
                ci += 1
                eng = "dve"
                S.op(eng, "tensor_scalar", out=ca[:], in0=xbt[:, 0:512], scalar1=convw[:, i, h, 0:1], scalar2=None,
                     op0=ALU.mult, reads=[kxb, K("convw")], writes=[kca])
                for j in range(1, 4):
                    S.op(eng, "scalar_tensor_tensor", out=ca[:], in0=xbt[:, j:j + 512], scalar=convw[:, i, h, j:j + 1],
                         in1=ca[:], op0=ALU.mult, op1=ALU.add, reads=[kxb, K("convw"), kca], writes=[kca])
                S.op("pool", "tensor_copy", out=xbt[:, 0:3], in_=xbt[:, 512:515], reads=[kxb], writes=[kxb])
                S.op("act", "activation", out=qkvc[i][h][:], in_=ca[:], func=AF.Silu, reads=[kca],
                     writes=[K(f"qkvc{i}{h}")])
        for tl in range(4):
            tsl = slice(tl * 128, (tl + 1) * 128)
            if GDN_LV < 2:
                continue
            for k in range(8):
                mm(1, 0, 512, xsb[:, k, tsl], wz[:, k, :], [kx, K("wz")], start=(k == 0), stop=(k == 7))
            S.op("act", "activation", out=zs[:], in_=bank[1][:], func=AF.Silu, reads=[BK[1]], writes=[K("zs")])
            for k in range(8):
                mm(1, 0, 8, xs32[:, k, tsl], wba[:, k, :], [K("xs32"), K("wba")], start=(k == 0), stop=(k == 7))
            S.op("dve", "tensor_copy", out=ba[:], in_=bank[1][:, 0:8], reads=[BK[1]], writes=[K("ba")])
            G = lambda n: gt[n][:]
            kg = lambda n: K("g_" + n)
            S.op("act", "activation", out=G("beta"), in_=ba[:, 0:4], func=AF.Exp, scale=-1.0, reads=[K("ba")],
                 writes=[kg("beta")])
            S.op("dve", "tensor_scalar", out=G("beta"), in0=G("beta"), scalar1=1.0, scalar2=None, op0=ALU.add,
                 reads=[kg("beta")], writes=[kg("beta")])
            S.op("dve", "reciprocal", out=G("beta"), in_=G("beta"), reads=[kg("beta")], writes=[kg("beta")])
            S.op("dve", "tensor_scalar", out=G("nbeta"), in0=G("beta"), scalar1=-1.0, scalar2=None, op0=ALU.mult,
                 reads=[kg("beta")], writes=[kg("nbeta")])
            S.op("dve", "tensor_tensor", out=G("x"), in0=ba[:, 4:8], in1=hpar[:, 4:8], op=ALU.add,
                 reads=[K("ba"), K("hpar")], writes=[kg("x")])
            S.op("act", "activation", out=G("e"), in_=G("x"), func=AF.Exp, reads=[kg("x")], writes=[kg("e")])
            S.op("act", "activation", out=G("sp"), in_=G("e"), func=AF.Ln, bias=onec[:, 0:1], scale=1.0,
                 reads=[kg("e"), K("onec")], writes=[kg("sp")])
            S.op("dve", "tensor_tensor", out=G("g"), in0=G("sp"), in1=nea[:], op=ALU.mult,
                 reads=[kg("sp"), K("nea")], writes=[kg("g")])
            mm(1, 0, 4, MU, G("g"), [K("cst"), kg("g")])
            mm(1, 4, 8, ONES, G("g"), [K("cst"), kg("g")])
            S.op("dve", "tensor_copy", out=gc8[:], in_=bank[1][:, 0:8], reads=[BK[1]], writes=[K("gc8")])
            S.op("act", "activation", out=G("eg"), in_=gc8[:, 0:4], func=AF.Exp, reads=[K("gc8")], writes=[kg("eg")])
            S.op("act", "activation", out=G("elast"), in_=gc8[:, 4:8], func=AF.Exp, reads=[K("gc8")],
                 writes=[kg("elast")])
            S.op("dve", "tensor_tensor", out=G("kes"), in0=gc8[:, 4:8], in1=gc8[:, 0:4], op=ALU.subtract,
                 reads=[K("gc8")], writes=[kg("kes")])
            S.op("act", "activation", out=G("kes"), in_=G("kes"), func=AF.Exp, reads=[kg("kes")], writes=[kg("kes")])
            S.op("dve", "tensor_tensor", out=G("bks"), in0=G("beta"), in1=G("eg"), op=ALU.mult,
                 reads=[kg("beta"), kg("eg")], writes=[kg("bks")])
            if GDN_LV < 2.5:
                continue
            for hpair in range(0, NH, 2):
                recs = []
                for h in (hpair, hpair + 1):
                    rec = []
                    real_op = S.op
                    S.op = (lambda eng, method, reads=(), writes=(), _rec=rec, **kw:
                            _rec.append((eng, method, reads, writes, kw)))
                    try:
                        p2 = it % 2
                        bA, bB, bC = 2 + 3 * p2, 3 + 3 * p2, 4 + 3 * p2
                        it += 1
                        T_ = lambda lst: lst[p2][:]
                        kt = lambda n: K(f"{n}{p2}")
                        cl = lambda n: col[n][p2][:]
                        QT = qkvc[0][h][:, tsl]
                        KTr = qkvc[1][h][:, tsl]
                        VTr = qkvc[2][h][:, tsl]
                        kq, kk_, kv = K(f"qkvc0{h}"), K(f"qkvc1{h}"), K(f"qkvc2{h}")
                        hs = slice(h, h + 1)
                        S.op("pool", "tensor_copy", out=T_(Qc), in_=QT, reads=[kq], writes=[kt("Qc")])
                        tr(bA, 0, KTr, [kk_])
                        S.op("act", "activation", out=T_(junk), in_=bank[bA][:, 0:128], func=AF.Square,
                             reads=[BK[bA]], writes=[kt("junk")])
                        S.op("dve", "reduce_sum", out=cl("ssqk"), in_=T_(junk), axis=AX.X, reads=[kt("junk")],
                             writes=[kt("ssqk")])
                        S.op("act", "activation", out=cl("rk"), in_=cl("ssqk"), func=AF.Ln, bias=eps6[:, 0:1], scale=1.0,
                             reads=[kt("ssqk"), K("eps6")], writes=[kt("rk")])
                        S.op("act", "activation", out=cl("rk"), in_=cl("rk"), func=AF.Exp, scale=-0.5,
                             reads=[kt("rk")], writes=[kt("rk")])
                        S.op("dve", "tensor_scalar", out=T_(Kn), in0=bank[bA][:, 0:128], scalar1=cl("rk"), scalar2=None,
                             op0=ALU.mult, reads=[BK[bA], kt("rk")], writes=[kt("Kn")])
                        tr(bA, 128, T_(Kn), [kt("Kn")])
                        S.op("act", "copy", out=T_(KnT), in_=bank[bA][:, 128:256], reads=[BK[bA]], writes=[kt("KnT")])
                        S.op("dve", "tensor_scalar", out=T_(Kbg), in0=T_(Kn), scalar1=gt["bks"][:, hs], scalar2=None, op0=ALU.mult,
                             reads=[kt("Kn"), kg("bks")], writes=[kt("Kbg")])
                        S.op("dve", "tensor_scalar", out=T_(Kend), in0=T_(Kn), scalar1=gt["kes"][:, hs], scalar2=None, op0=ALU.mult,
                             reads=[kt("Kn"), kg("kes")], writes=[kt("Kend")])
                        if GDN_LV < 3:
                            continue
                        tr(bA, 256, VTr, [kv])
                        S.op("dve", "tensor_scalar", out=T_(Vb), in0=bank[bA][:, 256:384], scalar1=gt["beta"][:, hs],
                             scalar2=None, op0=ALU.mult, reads=[BK[bA], kg("beta")], writes=[kt("Vb")])
                        if GDN_LV < 3.5:
                            continue
                        tr(bA, 384, QT, [kq])
                        S.op("act", "activation", out=T_(sq), in_=bank[bA][:, 384:512], func=AF.Square, reads=[BK[bA]],
                             writes=[kt("sq")])
                        S.op("dve", "reduce_sum", out=cl("ssqq"), in_=T_(sq), axis=AX.X, reads=[kt("sq")], writes=[kt("ssqq")])
                        S.op("act", "activation", out=cl("rq"), in_=cl("ssqq"), func=AF.Ln, bias=eps6[:, 0:1],
                             scale=1.0, reads=[kt("ssqq"), K("eps6")], writes=[kt("rq")])
                        S.op("act", "activation", out=cl("rq"), in_=cl("rq"), func=AF.Exp, scale=-0.5,
                             reads=[kt("rq")], writes=[kt("rq")])
                        S.op("dve", "tensor_scalar", out=cl("rq"), in0=cl("rq"), scalar1=128.0 ** -0.5, scalar2=None,
                             op0=ALU.mult, reads=[kt("rq")], writes=[kt("rq")])
                        S.op("dve", "scalar_tensor_tensor", out=cl("rq2"), in0=cl("rq"), scalar=1.0 / 128.0, in1=cl("rq"),
                             op0=ALU.mult, op1=ALU.mult, reads=[kt("rq")], writes=[kt("rq2")])
                        if GDN_LV < 4:
                            continue
                        S.op("dve", "tensor_scalar", out=T_(G1), in0=MU, scalar1=gt["g"][:, hs], scalar2=None, op0=ALU.mult,
                             reads=[K("cst"), kg("g")], writes=[kt("G1")])
                        mm(bB, 0, 128, T_(G1), MS, [kt("G1"), K("cst")], start=True, stop=False)
                        mm(bB, 0, 128, ident, NEGL, [K("cst")], start=False, stop=True)
                        mm(bB, 128, 256, MS, T_(G1), [kt("G1"), K("cst")], start=True, stop=False)
                        mm(bB, 128, 256, ident, NEGUT, [K("cst")], start=False, stop=True)
                        S.op("act", "activation", out=T_(Dl), in_=bank[bB][:, 0:128], func=AF.Exp, reads=[BK[bB]],
                             writes=[kt("Dl")])
                        S.op("act", "activation", out=T_(DTu), in_=bank[bB][:, 128:256], func=AF.Exp, reads=[BK[bB]],
                             writes=[kt("DTu")])
                        if GDN_LV < 5:
                            continue
                        mm(bB, 256, 384, T_(KnT), T_(KnT), [kt("KnT")])
                        mm(bB, 384, 512, T_(KnT), QT, [kt("KnT"), kq])
                        N0, NT0 = Nn[0][p2], NTn[0][p2]
                        S.op("dve", "scalar_tensor_tensor", out=N0[:], in0=bank[bB][:, 256:384], scalar=gt["nbeta"][:, hs],
                             in1=T_(Dl), op0=ALU.mult, op1=ALU.mult, reads=[BK[bB], kg("nbeta"), kt("Dl")], writes=[kt("N0")])
                        S.op("dve", "tensor_tensor", out=T_(AT), in0=bank[bB][:, 384:512], in1=T_(DTu), op=ALU.mult,
                             reads=[BK[bB], kt("DTu")], writes=[kt("AT")])
                        tr(bC, 0, N0[:], [kt("N0")])
                        S.op("act", "copy", out=NT0[:], in_=bank[bC][:, 0:128], reads=[BK[bC]], writes=[kt("NT0")])
                        S.op("pool", "tensor_tensor", out=T_(PT), in0=NT0[:], in1=ident, op=ALU.add,
                             reads=[kt("NT0"), K("cst")], writes=[kt("PT")])
                        if GDN_LV < 6:
                            continue
                        for j in range(1, 7):
                            a, b = (j - 1) % 2, j % 2
                            Na, NTa, Nb, NTb = Nn[a][p2], NTn[a][p2], Nn[b][p2], NTn[b][p2]
                            mm(bC, 128, 256, NTa[:], Na[:], [kt(f"N{a}"), kt(f"NT{a}")])
                            if j < 6:
                                mm(bC, 256, 384, Na[:], NTa[:], [kt(f"N{a}"), kt(f"NT{a}")])
                            S.op("act", "copy", out=Nb[:], in_=bank[bC][:, 128:256], reads=[BK[bC]], writes=[kt(f"N{b}")])
                            if j < 6:
                                S.op("dve", "tensor_copy", out=NTb[:], in_=bank[bC][:, 256:384], reads=[BK[bC]],
                                     writes=[kt(f"NT{b}")])
                            mm(bC, 384, 512, Nb[:], T_(PT), [kt(f"N{b}"), kt("PT")])
                            S.op("dve", "tensor_tensor", out=T_(PT), in0=bank[bC][:, 384:512], in1=T_(PT), op=ALU.add,
                                 reads=[BK[bC], kt("PT")], writes=[kt("PT")])
                        if GDN_LV < 7:
                            continue
                        mm(bC, 0, 128, T_(Kbg), T_(PT), [kt("Kbg"), kt("PT")])
                        S.op("act", "mul", out=T_(nWT), in_=bank[bC][:, 0:128], mul=-1.0, reads=[BK[bC]], writes=[kt("nWT")])
                        if GDN_LV < 8:
                            continue
                        St, kS = Sst[h], K(f"S{h}")
                        mm(bC, 0, 128, T_(PT), T_(Vb), [kt("PT"), kt("Vb")], start=True, stop=False)
                        mm(bC, 0, 128, T_(nWT), St[:], [kt("nWT"), kS], start=False, stop=True)
                        S.op("act", "copy", out=T_(Vnew), in_=bank[bC][:, 0:128], reads=[BK[bC]], writes=[kt("Vnew")])
                        mm(bC, 128, 256, T_(Qc), St[:], [kt("Qc"), kS])
                        mm(bC, 256, 384, T_(AT), T_(Vnew), [kt("AT"), kt("Vnew")])
                        mm(bC, 384, 512, T_(Kend), T_(Vnew), [kt("Kend"), kt("Vnew")])
                        S.op("dve", "tensor_scalar", out=T_(o1), in0=bank[bC][:, 128:256], scalar1=gt["eg"][:, hs], scalar2=None, op0=ALU.mult,
                             reads=[BK[bC], kg("eg")], writes=[kt("o1")])
                        S.op("dve", "tensor_tensor", out=T_(o_), in0=bank[bC][:, 256:384], in1=T_(o1), op=ALU.add,
                             reads=[BK[bC], kt("o1")], writes=[kt("o")])
                        S.op("dve", "scalar_tensor_tensor", out=St[:], in0=St[:], scalar=gt["elast"][:, hs],
                             in1=bank[bC][:, 384:512], op0=ALU.mult, op1=ALU.add, reads=[kS, kg("elast"), BK[bC]], writes=[kS])
                        if GDN_LV < 9:
                            continue
                        S.op("act", "activation", out=T_(junk), in_=T_(o_), func=AF.Square,
                             reads=[kt("o")], writes=[kt("junk")])
                        S.op("dve", "reduce_sum", out=cl("ssqo"), in_=T_(junk), axis=AX.X, reads=[kt("junk")],
                             writes=[kt("ssqo")])
                        S.op("dve", "tensor_tensor", out=cl("v1"), in0=cl("ssqo"), in1=cl("rq2"), op=ALU.mult,
                             reads=[kt("ssqo"), kt("rq2")], writes=[kt("v1")])
                        S.op("act", "activation", out=cl("ssqq"), in_=cl("v1"), func=AF.Ln, bias=eps6[:, 0:1], scale=1.0,
                             reads=[kt("v1"), K("eps6")], writes=[kt("ssqq")])
                        S.op("act", "activation", out=cl("v1"), in_=cl("ssqq"), func=AF.Exp, scale=-0.5,
                             reads=[kt("ssqq")], writes=[kt("v1")])
                        S.op("dve", "tensor_tensor", out=cl("fac"), in0=cl("v1"), in1=cl("rq"), op=ALU.mult,
                             reads=[kt("v1"), kt("rq")], writes=[kt("fac")])
                        if GDN_LV < 11:
                            continue
                        S.op("dve", "scalar_tensor_tensor", out=T_(og), in0=T_(o_), scalar=cl("fac"), in1=normg[:],
                             op0=ALU.mult, op1=ALU.mult, reads=[kt("o"), kt("fac"), K("normg")], writes=[kt("og")])
                        S.op("dve", "tensor_tensor", out=T_(og), in0=T_(og), in1=zs[:, h * 128:(h + 1) * 128], op=ALU.mult,
                             reads=[kt("og"), K("zs")], writes=[kt("og")])
                        if GDN_LV < 11:
                            continue
                        tr(bA, 0, T_(og), [kt("og")])
                        S.op("act", "copy", out=ogT_sb[:, h, tsl], in_=bank[bA][:, 0:128], reads=[BK[bA]],
                             writes=[K("ogT")])
                    finally:
                        S.op = real_op
                    recs.append(rec)
                na, nb = len(recs[0]), len(recs[1])
                for q in range(max(na, nb)):
                    for rr in recs:
                        if q < len(rr):
                            eng_, method_, reads_, writes_, kw_ = rr[q]
                            S.op(eng_, method_, reads=reads_, writes=writes_, **kw_)

        S.dma("sp", K("ogT"), ogT[:, t0:t0 + 512].rearrange("(h p) t -> p h t", p=128), ogT_sb[:], reads=[K("ogT")])


def build_gdn(T):
    nc = bass.Bass("TRN2", target_bir_lowering=False)
    dt = lambda n, s, k="ExternalInput": nc.dram_tensor(n, s, F32, kind=k).ap()
    xT = dt("xT", [D, T])
    wq = dt("wq", [D, 512]); wk = dt("wk", [D, 512]); wv = dt("wv", [D, 512]); wz = dt("wz", [D, 512])
    wba = dt("wba", [D, 8]); convw = dt("convw", [128, 3, 4, 4]); hp = dt("hpar", [128, 8])
    normg = dt("normg", [128, 128]); gconst = dt("gconst", [6, 128, 128])
    ogT = dt("ogT", [512, T], "ExternalOutput")
    with contextlib.ExitStack() as es:
        C = Ctx(nc, es)
        gdn_phase(C, T, xT, wq, wk, wv, wz, wba, convw, hp, normg, gconst, ogT)
        C.S.finish()
        C.S.emit()
    return nc


def gdn_consts():
    i = np.arange(128)
    ident = np.eye(128, dtype=np.float32)
    MU = (i[:, None] <= i[None, :]).astype(np.float32)
    MS = (i[:, None] > i[None, :]).astype(np.float32)
    NEGL = np.where(i[:, None] > i[None, :], 0.0, -100.0).astype(np.float32)
    NEGUT = np.where(i[None, :] >= i[:, None], 0.0, -100.0).astype(np.float32)
    ONES = np.ones((128, 128), np.float32)
    return np.stack([ident, MU, MS, NEGL, NEGUT, ONES])


def gdn_host_layout(hh, w_in, conv_w, a_log, dt_bias, norm_g):
    QK = 1024
    c0 = 512 * hh
    wq = np.ascontiguousarray(w_in[:, c0:c0 + 512])
    wk = np.ascontiguousarray(w_in[:, QK + c0:QK + c0 + 512])
    wv = np.ascontiguousarray(w_in[:, 2 * QK + c0:2 * QK + c0 + 512])
    wz = np.ascontiguousarray(w_in[:, 3 * QK + c0:3 * QK + c0 + 512])
    wba = np.ascontiguousarray(np.concatenate([w_in[:, 4 * QK + 4 * hh:4 * QK + 4 * hh + 4],
                                               w_in[:, 4 * QK + 8 + 4 * hh:4 * QK + 8 + 4 * hh + 4]], axis=1))
    convw = np.zeros((128, 3, 4, 4), np.float32)
    for i in range(3):
        for h in range(4):
            convw[:, i, h, :] = conv_w[:, i * QK + c0 + h * 128:i * QK + c0 + (h + 1) * 128].T
    hp = np.tile(np.concatenate([a_log[4 * hh:4 * hh + 4], dt_bias[4 * hh:4 * hh + 4]])[None, :], (128, 1)).astype(np.float32)
    normg = np.tile(norm_g[None, :], (128, 1)).astype(np.float32)
    return dict(wq=wq, wk=wk, wv=wv, wz=wz, wba=wba, convw=convw, hpar=hp, normg=normg, gconst=gdn_consts())


GDN_NAMES = ("wq", "wk", "wv", "wz", "wba", "convw", "hpar", "normg")
S5_NAMES = ("w_in", "lamr", "lami", "ldt", "bpad_re", "bpad_im", "cpad_re", "cpad_im", "dsk")
GDN_SHAPES = dict(wq=[D, 512], wk=[D, 512], wv=[D, 512], wz=[D, 512], wba=[D, 8], convw=[128, 3, 4, 4],
                  hpar=[128, 8], normg=[128, 128])
S5_SHAPES = dict(w_in=[D, 512], lamr=[128, 16], lami=[128, 16], ldt=[128, 16], bpad_re=[16, 128, 128],
                 bpad_im=[16, 128, 128], cpad_re=[16, 128, 128], cpad_im=[16, 128, 128], dsk=[128, 4])


def build_fused(T):
    nc = bass.Bass("TRN2", target_bir_lowering=False)
    ext = lambda n, sh: nc.dram_tensor(n, sh, F32, kind="ExternalInput").ap()
    xT0 = ext("xT0", [D, T])
    xtok0 = ext("xtok0", [T, D])
    ident_d = ext("ident", [128, 128])
    gconst = ext("gconst", [6, 128, 128])
    iota = ext("iota", [128, S5_L])
    wr0 = ext("wr_dummy", [D, NEXP])
    br0 = ext("br_dummy", [1, NEXP])
    y = nc.dram_tensor("y", [T, D], F32, kind="ExternalOutput").ap()
    XT = nc.dram_tensor("XT_i", [D, T], F32).ap()
    FEAT = nc.dram_tensor("FEAT_i", [D, T], F32).ap()
    XA = nc.dram_tensor("XA_i", [T, D], F32).ap()
    XB = nc.dram_tensor("XB_i", [T, D], F32).ap()
    TH = T // 2
    tb = min(1024, TH)
    with contextlib.ExitStack() as es:
        S = Sched(nc, es)

        def phase(fn, last=False):
            with contextlib.ExitStack() as pes:
                C = Ctx(nc, pes, S)
                fn(C)
                S.barrier()
                S.emit()
            if not last:
                S.new_phase()

        for i in range(DEPTH):
            xT_src = xT0 if i == 0 else XT
            xres_src = xtok0 if i == 0 else (XA if i % 2 == 1 else XB)
            xout_dst = y if i == DEPTH - 1 else (XA if i % 2 == 0 else XB)
            lnp = ext(f"L{i}_lnp", [4, D])
            if i % 2 == 0:
                for hh in range(2):
                    d = {n: ext(f"L{i}_{hh}_{n}", GDN_SHAPES[n]) for n in GDN_NAMES}
                    phase(lambda C, d=d, hh=hh: gdn_phase(
                        C, T, xT_src, d["wq"], d["wk"], d["wv"], d["wz"], d["wba"], d["convw"], d["hpar"], d["normg"],
                        gconst, FEAT[hh * 512:(hh + 1) * 512, :], pfx=f"g{i}{hh}"))
                glu, moe, ne, nproj = False, False, 2, D
            else:
                for gh in range(2):
                    d = {n: ext(f"L{i}_{gh}_{n}", S5_SHAPES[n]) for n in S5_NAMES}
                    phase(lambda C, d=d, gh=gh: s5_phase(
                        C, T, xT_src, d["w_in"], d["lamr"], d["lami"], d["ldt"], d["bpad_re"], d["bpad_im"],
                        d["cpad_re"], d["cpad_im"], d["dsk"], iota, FEAT[gh * 512:(gh + 1) * 512, :], pfx=f"s{i}{gh}"))
                glu, moe, ne, nproj = True, True, NEXP, 2 * D
            wproj = ext(f"L{i}_wproj", [D, nproj])
            w1 = ext(f"L{i}_w1", [ne, D, FE]); w3 = ext(f"L{i}_w3", [ne, D, FE]); w2 = ext(f"L{i}_w2", [ne, FE, D])
            if moe:
                wr = ext(f"L{i}_wr", [D, NEXP]); br = ext(f"L{i}_br", [1, NEXP])
            else:
                wr, br = wr0, br0
            for hf in range(2):
                sl = slice(hf * TH, (hf + 1) * TH)
                phase(lambda C, sl=sl, hf=hf: tp_phase(
                    C, TH, glu, moe, FEAT[:, sl], wproj, xres_src[sl, :], lnp, w1, w3, w2, wr, br, ident_d,
                    xout_dst[sl, :], XT[:, sl], tb=tb, pfx=f"t{i}{hf}"), last=(i == DEPTH - 1 and hf == 1))
        print("n_sems", len(S.sem), "n_ops", S.n_ops)
        S.finish()
        S.emit()
    return nc


def fused_inputs(xb, P):
    f = lambda a: np.ascontiguousarray(np.asarray(a, dtype=np.float32))
    m = dict(xT0=np.ascontiguousarray(xb.T), xtok0=np.ascontiguousarray(xb), ident=np.eye(128, dtype=np.float32),
             gconst=gdn_consts(), iota=np.tile(np.arange(1, S5_L + 1, dtype=np.float32)[None, :], (128, 1)),
             wr_dummy=np.zeros((D, NEXP), np.float32), br_dummy=np.zeros((1, NEXP), np.float32))
    for i in range(DEPTH):
        j = i // 2
        m[f"L{i}_lnp"] = f(np.stack([P["ln_g"][i, 0], P["ln_b"][i, 0], P["ln_g"][i, 1], P["ln_b"][i, 1]]))
        if i % 2 == 0:
            for hh in range(2):
                lay = gdn_host_layout(hh, f(P["gdn_w_in"][j]), f(P["gdn_conv_w"][j]), f(P["gdn_a_log"][j]),
                                      f(P["gdn_dt_bias"][j]), f(P["gdn_norm_g"][j]))
                for n in GDN_NAMES:
                    m[f"L{i}_{hh}_{n}"] = lay[n]
            m[f"L{i}_wproj"] = f(P["gdn_w_out"][j])
            m[f"L{i}_w1"] = f(np.stack([P["ffn_w1"][j][:, :FE], P["ffn_w1"][j][:, FE:]]))
            m[f"L{i}_w3"] = f(np.stack([P["ffn_w3"][j][:, :FE], P["ffn_w3"][j][:, FE:]]))
            m[f"L{i}_w2"] = f(np.stack([P["ffn_w2"][j][:FE], P["ffn_w2"][j][FE:]]))
        else:
            for gh in range(2):
                lay = s5_host_layout(gh, f(P["s5_lam_re"][j]), f(P["s5_lam_im"][j]), f(P["s5_log_dt"][j]),
                                     f(P["s5_b_re"][j]), f(P["s5_b_im"][j]), f(P["s5_c_re"][j]), f(P["s5_c_im"][j]),
                                     f(P["s5_d"][j]), f(P["s5_w_in"][j]))
                for n in S5_NAMES:
                    m[f"L{i}_{gh}_{n}"] = lay[n]
            m[f"L{i}_wproj"] = f(P["s5_w_glu"][j])
            m[f"L{i}_w1"] = f(P["moe_w1"][j]); m[f"L{i}_w3"] = f(P["moe_w3"][j]); m[f"L{i}_w2"] = f(P["moe_w2"][j])
            m[f"L{i}_wr"] = f(P["moe_w_router"][j]); m[f"L{i}_br"] = f(P["moe_b_router"][j]).reshape(1, NEXP)
    return m


SEQ = 8192
BATCH = 4
_PROGS = {}


def kernel(**P):
    x = np.ascontiguousarray(np.asarray(P["x"], dtype=np.float32))
    T = x.shape[1]
    if T not in _PROGS:
        _PROGS[T] = build_fused(T)
    nc = _PROGS[T]
    shared = None
    in_maps = []
    for c in range(8):
        b = c % BATCH
        m = fused_inputs(x[b], P) if shared is None else dict(shared)
        if shared is None:
            shared = m
        else:
            m["xT0"] = np.ascontiguousarray(x[b].T)
            m["xtok0"] = x[b]
        in_maps.append(m)
    res = run_bass_kernel_spmd(nc, in_maps, core_ids=list(range(8))).results
    return np.stack([res[b]["y"] for b in range(BATCH)]).astype(np.float32)
```
